# Optimizing a Trainium2 kernel written in Bass

```python
import math
import jax
import jax.numpy as jnp
from jax import lax
import numpy as np

D_MODEL = 1024
BATCH = 4
SEQ = 4096
DEPTH = 2

HEAD_DIM = 64
Q_BLOCK = 128
SB_HEADS = 8
NSA_HEADS = 8
NSA_KV_GROUPS = 2
CMP_BLOCK = 32
CMP_STRIDE = 16
CMP_HIDDEN = 128
SLC_BLOCK = 64
SLC_TOPN = 16
WINDOW = 512
FORCE_SCORE = 1e6
SB_W = SB_HEADS * HEAD_DIM
NSA_QW = NSA_HEADS * HEAD_DIM
NSA_KVW = NSA_KV_GROUPS * HEAD_DIM
ATTN_IN = 3 * SB_W + NSA_QW + 6 * NSA_KVW + 3 * NSA_HEADS
ATTN_OUT = SB_W + NSA_QW
SSM_EXPAND = 2
SSM_INNER = SSM_EXPAND * D_MODEL
SSM_HEAD_DIM = 64
SSM_HEADS = SSM_INNER // SSM_HEAD_DIM
SSM_GROUPS = 4
SSM_STATE = 128
SSM_CONV = 4
SSM_CHUNK = 256
SSM_CONV_DIM = SSM_INNER + 2 * SSM_GROUPS * SSM_STATE
SSM_IN = SSM_INNER + SSM_CONV_DIM + SSM_HEADS
N_EXPERTS = 32
TOP_K = 4
D_EXPERT = D_MODEL
SWIGLU_LIMIT = 7.0
SWIGLU_ALPHA = 1.702
MOE_BLOCK = 256
EPS = 1e-6

kernel_name = 'hybrid_sb_nsa_ssd_moe_block'


def rms_norm(x, gain):
    xf = x.astype(jnp.float32)
    y = xf * lax.rsqrt(jnp.mean(xf * xf, axis=-1, keepdims=True) + EPS)
    return (y * gain.astype(jnp.float32)).astype(x.dtype)


def alibi_slopes(n):
    return jnp.asarray(2.0 ** (-8.0 * np.arange(1, n + 1) / n), jnp.float32)


def masked_softmax(s, mask):
    s = jnp.where(mask, s, -jnp.inf)
    m = jnp.max(s, axis=-1, keepdims=True)
    m = jnp.where(jnp.isfinite(m), m, 0.0)
    p = jnp.exp(s - m)
    return p / jnp.maximum(jnp.sum(p, axis=-1, keepdims=True), 1e-30)


def stick_breaking_attention(q, k, v):
    b, s, h, dh = q.shape
    scale = dh ** -0.5
    outs = []
    for i in range(s // Q_BLOCK):
        q0, end = i * Q_BLOCK, (i + 1) * Q_BLOCK
        logits = jnp.einsum('bqhd,bkhd->bhqk', q[:, q0:end], k[:, :end]).astype(jnp.float32) * scale
        causal = jnp.arange(end)[None, :] < jnp.arange(q0, end)[:, None]
        log_beta = jax.nn.log_sigmoid(logits)
        log_keep = jnp.where(causal, jax.nn.log_sigmoid(-logits), 0.0)
        acc = lax.cumsum(log_keep, axis=3, reverse=True) - log_keep
        w = jnp.where(causal, jnp.exp(log_beta + acc), 0.0)
        outs.append(jnp.einsum('bhqk,bkhd->bqhd', w, v[:, :end]))
    return jnp.concatenate(outs, axis=1)


def compress_blocks(kv, pe, w1, w2):
    b, s, g, dh = kv.shape
    n_cmp = (s - CMP_BLOCK) // CMP_STRIDE + 1
    idx = np.arange(n_cmp)[:, None] * CMP_STRIDE + np.arange(CMP_BLOCK)[None, :]
    blocks = kv[:, idx] + pe[:, None, :]
    flat = jnp.swapaxes(blocks, 2, 3).reshape(b, n_cmp, g, CMP_BLOCK * dh)
    return jax.nn.silu(flat @ w1) @ w2


def nsa_attention(q, kc, vc, ks, vs, kw, vw, gates, q_norm, k_norm, pe_k, pe_v, w1_k, w2_k, w1_v, w2_v):
    b, s, h, dh = q.shape
    g = NSA_KV_GROUPS
    r = h // g
    scale = dh ** -0.5
    slopes = alibi_slopes(h).reshape(g, r)
    qg = (rms_norm(q, q_norm).astype(jnp.float32) * scale).reshape(b, s, g, r, dh)
    tpos = jnp.arange(s)

    k_cmp = rms_norm(compress_blocks(kc, pe_k, w1_k, w2_k), k_norm[0])
    v_cmp = compress_blocks(vc, pe_v, w1_v, w2_v)
    n_cmp = k_cmp.shape[1]
    cmp_start = np.arange(n_cmp) * CMP_STRIDE
    cmp_end = jnp.asarray(cmp_start + CMP_BLOCK - 1)
    mask_c = cmp_end[None, :] <= tpos[:, None]
    dist_c = (tpos[:, None] - cmp_end[None, :]).astype(jnp.float32)
    sc = jnp.einsum('bsgrd,bngd->bgrsn', qg, k_cmp) - slopes[:, :, None, None] * dist_c
    p_c = masked_softmax(sc, mask_c)
    o_c = jnp.einsum('bgrsn,bngd->bsgrd', p_c, v_cmp)

    n_slc = s // SLC_BLOCK
    n_sel = min(SLC_TOPN, n_slc)
    slc_start = np.arange(n_slc) * SLC_BLOCK
    overlap = ((cmp_start[:, None] < slc_start[None, :] + SLC_BLOCK)
               & (cmp_start[:, None] + CMP_BLOCK > slc_start[None, :])).astype(np.float32)
    p_slc = jnp.einsum('bgsn,nj->bgsj', jnp.sum(p_c, axis=2), jnp.asarray(overlap))
    jblk = jnp.arange(n_slc)
    valid = jblk[None, :] * SLC_BLOCK <= tpos[:, None]
    cur = tpos[:, None] // SLC_BLOCK
    forced = (jblk[None, :] == 0) | (jblk[None, :] == cur) | (jblk[None, :] == cur - 1)
    score = jnp.where(valid, jnp.where(forced, FORCE_SCORE, p_slc), -FORCE_SCORE)
    _, sel = lax.top_k(score, n_sel)

    k_sb = jnp.transpose(rms_norm(ks, k_norm[1]).reshape(b, n_slc, SLC_BLOCK, g, dh), (0, 3, 1, 2, 4))
    v_sb = jnp.transpose(vs.reshape(b, n_slc, SLC_BLOCK, g, dh), (0, 3, 1, 2, 4))
    nc = s // Q_BLOCK
    bi = jnp.arange(b)[:, None, None, None]
    gi = jnp.arange(g)[None, :, None, None]
    offs = jnp.arange(SLC_BLOCK)

    def selected_block(args):
        qc, ic, tc = args
        kb = k_sb[bi, gi, ic]
        vb = v_sb[bi, gi, ic]
        pos = ic[..., None] * SLC_BLOCK + offs
        tq = tc[None, None, :, None, None]
        mask = (pos <= tq)[:, :, None]
        dist = (tq - pos).astype(jnp.float32)[:, :, None]
        ss = jnp.einsum('bqgrd,bgqnld->bgrqnl', qc, kb) - slopes[None, :, :, None, None, None] * dist
        shp = ss.shape
        p = masked_softmax(ss.reshape(shp[:4] + (-1,)), jnp.broadcast_to(mask, shp).reshape(shp[:4] + (-1,)))
        return jnp.einsum('bgrqnl,bgqnld->bqgrd', p.reshape(shp), vb)

    q_ch = jnp.swapaxes(qg.reshape(b, nc, Q_BLOCK, g, r, dh), 0, 1)
    i_ch = jnp.transpose(sel.reshape(b, g, nc, Q_BLOCK, n_sel), (2, 0, 1, 3, 4))
    t_ch = jnp.arange(s, dtype=jnp.int32).reshape(nc, Q_BLOCK)
    o_s = jnp.swapaxes(lax.map(selected_block, (q_ch, i_ch, t_ch)), 0, 1).reshape(b, s, g, r, dh)

    k_w = jnp.pad(rms_norm(kw, k_norm[2]), ((0, 0), (WINDOW, 0), (0, 0), (0, 0)))
    v_w = jnp.pad(vw, ((0, 0), (WINDOW, 0), (0, 0), (0, 0)))
    widx = np.arange(nc)[:, None] * Q_BLOCK + np.arange(Q_BLOCK + WINDOW)[None, :]
    spos = widx - WINDOW
    tq = np.arange(nc)[:, None] * Q_BLOCK + np.arange(Q_BLOCK)[None, :]
    mask_w = ((spos[:, None, :] <= tq[:, :, None]) & (spos[:, None, :] > tq[:, :, None] - WINDOW)
              & (spos[:, None, :] >= 0))
    dist_w = jnp.asarray((tq[:, :, None] - spos[:, None, :]).astype(np.float32))
    qb = qg.reshape(b, nc, Q_BLOCK, g, r, dh)
    sw = jnp.einsum('bnqgrd,bnkgd->bgrnqk', qb, k_w[:, widx]) - slopes[:, :, None, None, None] * dist_w
    p_w = masked_softmax(sw, jnp.asarray(mask_w))
    o_w = jnp.einsum('bgrnqk,bnkgd->bnqgrd', p_w, v_w[:, widx]).reshape(b, s, g, r, dh)

    gt = jax.nn.sigmoid(gates.astype(jnp.float32)).reshape(b, s, g, r, 3, 1)
    o = gt[..., 0, :] * o_c + gt[..., 1, :] * o_s + gt[..., 2, :] * o_w
    return o.reshape(b, s, h, dh)


def attention_mixer(u, w_in, w_out, q_norm, k_norm, pe_k, pe_v, w1_k, w2_k, w1_v, w2_v):
    b, s, _ = u.shape
    proj = u @ w_in
    splits = [int(v) for v in np.cumsum([SB_W, SB_W, SB_W, NSA_QW] + [NSA_KVW] * 6)]
    sb_q, sb_k, sb_v, nq, kc, vc, ks, vs, kw, vw, gates = jnp.split(proj, splits, axis=-1)
    hs = lambda a, n: a.reshape(b, s, n, HEAD_DIM)
    o_sb = stick_breaking_attention(hs(sb_q, SB_HEADS), hs(sb_k, SB_HEADS), hs(sb_v, SB_HEADS))
    g = NSA_KV_GROUPS
    o_nsa = nsa_attention(hs(nq, NSA_HEADS), hs(kc, g), hs(vc, g), hs(ks, g), hs(vs, g), hs(kw, g), hs(vw, g),
                          gates, q_norm, k_norm, pe_k, pe_v, w1_k, w2_k, w1_v, w2_v)
    o = jnp.concatenate([o_sb.reshape(b, s, SB_W), o_nsa.reshape(b, s, NSA_QW)], axis=-1).astype(u.dtype)
    return o @ w_out


def ssd_chunked(x, dt, a, bm, cm):
    b, s, h, p = x.shape
    g, n = bm.shape[-2], bm.shape[-1]
    r = h // g
    chunk = math.gcd(s, SSM_CHUNK)
    nc = s // chunk
    xd = (x.astype(jnp.float32) * dt[..., None]).reshape(b, nc, chunk, g, r, p)
    cs = jnp.cumsum((dt * a).reshape(b, nc, chunk, g, r), axis=2)
    bm = bm.astype(jnp.float32).reshape(b, nc, chunk, g, n)
    cm = cm.astype(jnp.float32).reshape(b, nc, chunk, g, n)
    causal = jnp.tril(jnp.ones((chunk, chunk), bool))[None, None, :, :, None, None]
    seg = jnp.where(causal, cs[:, :, :, None] - cs[:, :, None, :], -jnp.inf)
    cb = jnp.einsum('bclgn,bcsgn->bclsg', cm, bm)
    y_diag = jnp.einsum('bclsgr,bcsgrp->bclgrp', cb[..., None] * jnp.exp(seg), xd)
    states = jnp.einsum('bclgn,bclgrp->bcgrpn', bm, xd * jnp.exp(cs[:, :, -1:] - cs)[..., None])
    chunk_decay = jnp.exp(cs[:, :, -1])

    def carry_state(hstate, inp):
        st, dec = inp
        return hstate * dec[..., None, None] + st, hstate

    _, prev = lax.scan(carry_state, jnp.zeros((b, g, r, p, n), jnp.float32),
                       (jnp.moveaxis(states, 1, 0), jnp.moveaxis(chunk_decay, 1, 0)))
    prev = jnp.moveaxis(prev, 0, 1)
    y_off = jnp.einsum('bclgn,bcgrpn->bclgrp', cm, prev) * jnp.exp(cs)[..., None]
    return (y_diag + y_off).reshape(b, s, h, p)


def mamba2_mixer(u, w_in, conv_w, conv_b, dt_bias, a_log, d_skip, norm_g, w_out):
    b, s, _ = u.shape
    zxbcdt = u @ w_in
    z, xbc, dt = jnp.split(zxbcdt, [SSM_INNER, SSM_INNER + SSM_CONV_DIM], axis=-1)
    xbc = lax.conv_general_dilated(xbc, conv_w[:, None, :], window_strides=(1,), padding=[(SSM_CONV - 1, 0)],
                                   dimension_numbers=('NWC', 'WIO', 'NWC'), feature_group_count=SSM_CONV_DIM)
    xbc = jax.nn.silu(xbc + conv_b)
    xs, bm, cm = jnp.split(xbc, [SSM_INNER, SSM_INNER + SSM_GROUPS * SSM_STATE], axis=-1)
    xs = xs.reshape(b, s, SSM_HEADS, SSM_HEAD_DIM)
    bm = bm.reshape(b, s, SSM_GROUPS, SSM_STATE)
    cm = cm.reshape(b, s, SSM_GROUPS, SSM_STATE)
    dt = jax.nn.softplus(dt.astype(jnp.float32) + dt_bias.astype(jnp.float32))
    a = -jnp.exp(a_log.astype(jnp.float32))
    y = ssd_chunked(xs, dt, a, bm, cm) + xs.astype(jnp.float32) * d_skip.astype(jnp.float32)[:, None]
    y = y.reshape(b, s, SSM_INNER) * jax.nn.silu(z.astype(jnp.float32))
    y = rms_norm(y.reshape(b, s, SSM_GROUPS, SSM_INNER // SSM_GROUPS), jnp.ones((), jnp.float32))
    y = (y.reshape(b, s, SSM_INNER) * norm_g).astype(u.dtype)
    return y @ w_out


def moe_ffn(h, w_router, b_router, w_gu, b_gu, w_down, b_down):
    b, s, d = h.shape
    n_tok = b * s
    tok = h.reshape(n_tok, d)
    logits = (tok @ w_router + b_router).astype(jnp.float32)
    top_logit, top_e = lax.top_k(logits, TOP_K)
    top_w = jax.nn.softmax(top_logit, axis=-1)
    n_rows = n_tok * TOP_K
    flat_e = top_e.reshape(n_rows)
    flat_t = jnp.repeat(jnp.arange(n_tok, dtype=jnp.int32), TOP_K)
    flat_w = top_w.reshape(n_rows)
    order = jnp.argsort(flat_e)
    sorted_e = flat_e[order]
    counts = jnp.bincount(flat_e, length=N_EXPERTS)
    padded = (counts + MOE_BLOCK - 1) // MOE_BLOCK * MOE_BLOCK
    pad_end = jnp.cumsum(padded)
    pad_start = pad_end - padded
    grp_start = jnp.cumsum(counts) - counts
    slot = pad_start[sorted_e] + jnp.arange(n_rows) - grp_start[sorted_e]
    n_slots = (n_rows + MOE_BLOCK - 1) // MOE_BLOCK * MOE_BLOCK + N_EXPERTS * MOE_BLOCK
    slot_tok = jnp.full((n_slots,), n_tok, jnp.int32).at[slot].set(flat_t[order])
    slot_w = jnp.zeros((n_slots,), jnp.float32).at[slot].set(flat_w[order])
    n_blocks = n_slots // MOE_BLOCK
    blk_e = jnp.minimum(jnp.searchsorted(pad_end, jnp.arange(n_blocks) * MOE_BLOCK, side='right'), N_EXPERTS - 1)
    tok_pad = jnp.concatenate([tok, jnp.zeros((1, d), tok.dtype)], axis=0)
    xs = tok_pad[slot_tok].reshape(n_blocks, MOE_BLOCK, d)

    def expert_block(args):
        xb, e = args
        gu = xb @ w_gu[e] + b_gu[e]
        gate = jnp.minimum(gu[:, :D_EXPERT], SWIGLU_LIMIT)
        up = jnp.clip(gu[:, D_EXPERT:], -SWIGLU_LIMIT, SWIGLU_LIMIT)
        act = (up + 1.0) * gate * jax.nn.sigmoid(SWIGLU_ALPHA * gate)
        return act @ w_down[e] + b_down[e]

    ys = lax.map(expert_block, (xs, blk_e)).reshape(n_slots, d) * slot_w[:, None]
    out = jax.ops.segment_sum(ys, slot_tok, num_segments=n_tok + 1)[:n_tok]
    return out.reshape(b, s, d).astype(h.dtype)


def setup_inputs(seed: int = 0) -> dict:
    key = jax.random.key(seed)
    ks = iter(jax.random.split(key, 40))
    nrm = lambda shape, sc: jax.random.normal(next(ks), shape, jnp.float32) * sc
    D = D_MODEL
    ne, no = (DEPTH + 1) // 2, DEPTH // 2
    fk = CMP_BLOCK * HEAD_DIM
    dt0 = jnp.exp(jax.random.uniform(next(ks), (no, SSM_HEADS), jnp.float32, math.log(1e-3), math.log(1e-1)))
    return {
        'x': nrm((BATCH, SEQ, D), 1.0),
        'c': nrm((BATCH, D), 1.0),
        'ada_w': nrm((DEPTH, D, 6 * D), 0.5 * D ** -0.5),
        'ada_b': nrm((DEPTH, 6 * D), 0.05),
        'norm_mix': 1.0 + nrm((DEPTH, D), 0.05),
        'norm_ffn': 1.0 + nrm((DEPTH, D), 0.05),
        'attn_w_in': nrm((ne, D, ATTN_IN), D ** -0.5),
        'attn_w_out': nrm((ne, ATTN_OUT, D), ATTN_OUT ** -0.5),
        'nsa_q_norm': 1.0 + nrm((ne, HEAD_DIM), 0.05),
        'nsa_k_norm': 1.0 + nrm((ne, 3, HEAD_DIM), 0.05),
        'cmp_pe_k': nrm((ne, CMP_BLOCK, HEAD_DIM), 0.1),
        'cmp_pe_v': nrm((ne, CMP_BLOCK, HEAD_DIM), 0.1),
        'cmp_w1_k': nrm((ne, fk, CMP_HIDDEN), fk ** -0.5),
        'cmp_w2_k': nrm((ne, CMP_HIDDEN, HEAD_DIM), CMP_HIDDEN ** -0.5),
        'cmp_w1_v': nrm((ne, fk, CMP_HIDDEN), fk ** -0.5),
        'cmp_w2_v': nrm((ne, CMP_HIDDEN, HEAD_DIM), CMP_HIDDEN ** -0.5),
        'ssm_w_in': nrm((no, D, SSM_IN), D ** -0.5),
        'ssm_conv_w': nrm((no, SSM_CONV, SSM_CONV_DIM), SSM_CONV ** -0.5),
        'ssm_conv_b': nrm((no, SSM_CONV_DIM), 0.02),
        'ssm_dt_bias': dt0 + jnp.log(-jnp.expm1(-dt0)),
        'ssm_a_log': jnp.log(jax.random.uniform(next(ks), (no, SSM_HEADS), jnp.float32, 1.0, 16.0)),
        'ssm_d': 1.0 + nrm((no, SSM_HEADS), 0.05),
        'ssm_norm': 1.0 + nrm((no, SSM_INNER), 0.05),
        'ssm_w_out': nrm((no, SSM_INNER, D), SSM_INNER ** -0.5),
        'router_w': nrm((DEPTH, D, N_EXPERTS), D ** -0.5),
        'router_b': nrm((DEPTH, N_EXPERTS), 0.01),
        'moe_w_gu': nrm((DEPTH, N_EXPERTS, D, 2 * D_EXPERT), D ** -0.5),
        'moe_b_gu': nrm((DEPTH, N_EXPERTS, 2 * D_EXPERT), 0.02),
        'moe_w_down': nrm((DEPTH, N_EXPERTS, D_EXPERT, D), D_EXPERT ** -0.5),
        'moe_b_down': nrm((DEPTH, N_EXPERTS, D), 0.02),
    }


def reference(x, c, ada_w, ada_b, norm_mix, norm_ffn, attn_w_in, attn_w_out, nsa_q_norm, nsa_k_norm,
              cmp_pe_k, cmp_pe_v, cmp_w1_k, cmp_w2_k, cmp_w1_v, cmp_w2_v, ssm_w_in, ssm_conv_w, ssm_conv_b,
              ssm_dt_bias, ssm_a_log, ssm_d, ssm_norm, ssm_w_out, router_w, router_b, moe_w_gu, moe_b_gu,
              moe_w_down, moe_b_down):
    h = x
    cond = jax.nn.silu(c)
    for layer in range(DEPTH):
        mod = (cond @ ada_w[layer] + ada_b[layer])[:, None, :]
        sh1, sc1, g1, sh2, sc2, g2 = jnp.split(mod, 6, axis=-1)
        u = rms_norm(h, norm_mix[layer]) * (1.0 + sc1) + sh1
        i = layer // 2
        if layer % 2 == 0:
            mix = attention_mixer(u, attn_w_in[i], attn_w_out[i], nsa_q_norm[i], nsa_k_norm[i], cmp_pe_k[i],
                                  cmp_pe_v[i], cmp_w1_k[i], cmp_w2_k[i], cmp_w1_v[i], cmp_w2_v[i])
        else:
            mix = mamba2_mixer(u, ssm_w_in[i], ssm_conv_w[i], ssm_conv_b[i], ssm_dt_bias[i], ssm_a_log[i],
                               ssm_d[i], ssm_norm[i], ssm_w_out[i])
        h = h + g1 * mix
        u = rms_norm(h, norm_ffn[layer]) * (1.0 + sc2) + sh2
        h = h + g2 * moe_ffn(u, router_w[layer], router_b[layer], moe_w_gu[layer], moe_b_gu[layer],
                             moe_w_down[layer], moe_b_down[layer])
    return h
```

```python
import contextlib
import numpy as np
import ml_dtypes
import concourse.bass as bass
import concourse.mybir as mybir
from concourse.bass_utils import run_bass_kernel_spmd

F32 = mybir.dt.float32
BF16 = mybir.dt.bfloat16
AF = mybir.ActivationFunctionType
ALU = mybir.AluOpType
AX = mybir.AxisListType
NPBF = ml_dtypes.bfloat16

N_DMA_SEMS = 16
ARENA_W = 51 * 1024
EPS = 1e-6


class Buf:
    __slots__ = ("t", "last_w", "readers", "name")

    REG = []

    def __init__(self, t, name=""):
        self.t = t
        self.last_w = None
        self.readers = {}
        self.name = name
        Buf.REG.append(self)

    def __getitem__(self, idx):
        return self.t[idx]


class KB:
    ENG = ("pe", "act", "dve", "pool", "sp")

    def __init__(self, nc, stack):
        self.nc = nc
        self.stack = stack
        self.prog = {e: [] for e in self.ENG}
        self.cnt = {e: 0 for e in self.ENG}
        self.sems = {}
        self.epoch = 0
        Buf.REG = []
        for e in self.ENG:
            self.sems[(e, 0)] = stack.enter_context(nc.semaphore("s_" + e))
        self.dsem = []
        for i in range(N_DMA_SEMS):
            self.dsem.append(stack.enter_context(nc.semaphore("d_%d" % i)))
            self.sems[("d", i)] = self.dsem[i]
        self.dcnt = [0] * N_DMA_SEMS
        self.dnext = 0
        self.waited = {}
        self.arena = None
        self.apeak = 0

    def sb(self, name, shape, dt):
        if self.arena is None:
            self.arena = self.stack.enter_context(self.nc.sbuf_tensor("arena", [128, ARENA_W], F32))
            self.aoff = 0
        esz = 2 if dt == BF16 else 4
        nel = 1
        for x in shape[1:]:
            nel *= x
        n32 = (nel * esz + 3) // 4
        n32 = (n32 + 7) // 8 * 8
        assert self.aoff + n32 <= ARENA_W, "SBUF arena overflow at %s (%d + %d)" % (name, self.aoff, n32)
        v = self.arena[0:shape[0], self.aoff:self.aoff + n32]
        self.aoff += n32
        self.apeak = max(self.apeak, self.aoff)
        if dt != F32:
            v = v.bitcast(dt)
        v = v[:, 0:nel]
        if len(shape) == 3:
            v = v.rearrange("p (a b) -> p a b", a=shape[1])
        elif len(shape) == 4:
            v = v.rearrange("p (a b c) -> p a b c", a=shape[1], b=shape[2])
        return Buf(v, name)

    def mark(self):
        return self.aoff

    def release(self, m):
        self.fence()
        self.aoff = m

    def ps(self, name, shape, dt=F32):
        return Buf(self.stack.enter_context(self.nc.psum_tensor(name, list(shape), dt)), name)

    def dram(self, name, shape, dt, kind):
        return Buf(self.nc.dram_tensor(name, list(shape), dt, kind=kind).ap(), name)

    def _wait(self, eng, key, val):
        if key == ("pe", self.epoch) and eng == "pe":
            return
        if self.waited.get((eng, key), 0) >= val:
            return
        self.waited[(eng, key)] = val
        self.prog[eng].append(("w", key, val))

    def _deps(self, eng, reads, writes):
        for b in reads:
            if b.last_w is not None:
                self._wait(eng, *b.last_w)
        for b in writes:
            if b.last_w is not None:
                self._wait(eng, *b.last_w)
            for tok in b.readers.values():
                self._wait(eng, *tok)

    def _mark(self, tok, reads, writes):
        for b in reads:
            b.readers[tok[0]] = tok
        for b in writes:
            b.last_w = tok
            b.readers = {}

    def op(self, eng, fn, reads=(), writes=()):
        self._deps(eng, reads, writes)
        self.cnt[eng] += 1
        tok = ((eng, self.epoch), self.cnt[eng])
        self.prog[eng].append(("o", fn, self.epoch))
        self._mark(tok, reads, writes)
        return tok

    def dma(self, eng, out, in_, reads=(), writes=(), **kw):
        i = self.dnext
        self.dnext = (self.dnext + 1) % N_DMA_SEMS
        key = ("d", i)
        if self.dcnt[i] > 0:
            self._wait(eng, key, self.dcnt[i])
        self._deps(eng, reads, writes)
        self.dcnt[i] += 16
        tok = (key, self.dcnt[i])
        self.prog[eng].append(("d", out, in_, i, kw))
        self._mark(tok, reads, writes)
        return tok

    def cc(self, eng, fn, reads=(), writes=()):
        i = self.dnext
        self.dnext = (self.dnext + 1) % N_DMA_SEMS
        key = ("d", i)
        if self.dcnt[i] > 0:
            self._wait(eng, key, self.dcnt[i])
        self._deps(eng, reads, writes)
        self.dcnt[i] += 16
        tok = (key, self.dcnt[i])
        self.prog[eng].append(("c", fn, i))
        self._mark(tok, reads, writes)
        return tok

    def new_epoch(self):
        self.fence()
        self.epoch += 1
        for e in self.ENG:
            self.sems[(e, self.epoch)] = self.stack.enter_context(self.nc.semaphore("s_%s_%d" % (e, self.epoch)))
            self.cnt[e] = 0
        self.waited = {kk: v for kk, v in self.waited.items() if isinstance(kk[1], tuple) and kk[1][0] == "d"}
        for b in Buf.REG:
            b.last_w = None
            b.readers = {}

    def fence(self):
        for e in self.ENG:
            for o in self.ENG:
                if self.cnt[o] > 0:
                    self._wait(e, (o, self.epoch), self.cnt[o])
            for i in range(N_DMA_SEMS):
                if self.dcnt[i] > 0:
                    self._wait(e, ("d", i), self.dcnt[i])

    def emit(self, final_waits=()):
        nc = self.nc
        engmap = {"pe": "tensor", "act": "scalar", "dve": "vector", "pool": "gpsimd", "sp": "sync"}
        for tok in final_waits:
            self._wait("sp", tok[0], tok[1])
        with nc.Block() as block:
            for e in self.ENG:
                def body(h, items=self.prog[e], e=e):
                    for it in items:
                        if it[0] == "w":
                            h.wait_ge(self.sems[it[1]], it[2])
                        elif it[0] == "o":
                            it[1](h).then_inc(self.sems[(e, it[2])], 1)
                        elif it[0] == "c":
                            it[1](h).then_inc(self.dsem[it[2]], 16)
                        else:
                            _, out, in_, i, kw = it
                            if callable(out):
                                out = out(h)
                            if callable(in_):
                                in_ = in_(h)
                            h.dma_start(out=out, in_=in_, **kw).then_inc(self.dsem[i], 16)
                getattr(block, engmap[e])(body)


def mm(k, ob, o, lb, l, rb, r, start, stop):
    k.op("pe", lambda e: e.matmul(o, lhsT=l, rhs=r, start=start, stop=stop), reads=[lb, rb], writes=[ob])


def tr(k, ob, o, ib, i, idb, ident):
    k.op("pe", lambda e: e.transpose(o, i, ident), reads=[ib, idb], writes=[ob])


def make_ident(k, name, dt):
    idf = k.sb(name + "_f", [128, 128], F32)
    k.op("pool", lambda e: e.memset(idf[:], 1.0), writes=[idf])
    k.op("pool", lambda e: e.affine_select(out=idf[:], in_=idf[:], pattern=[[-1, 128]], compare_op=ALU.is_equal,
                                           fill=0.0, base=0, channel_multiplier=1), reads=[idf], writes=[idf])
    if dt == F32:
        return idf, None
    idb = k.sb(name + "_b", [128, 128], dt)
    k.op("pool", lambda e: e.tensor_copy(out=idb[:], in_=idf[:]), reads=[idf], writes=[idb])
    return idf, idb


def adaln(k, ccol_d, adaw_d, adab_d, nvec, dst, stage, pbanks, tag=""):
    ccol = k.sb("ada_c" + tag, [128, 8], F32)
    cond = k.sb("ada_cond" + tag, [128, 8], F32)
    cbc = k.sb("ada_cbc" + tag, [128, 8, 128], F32)
    k.dma("sp", ccol[:], ccol_d[:], reads=[ccol_d], writes=[ccol])
    k.op("act", lambda e: e.activation(out=cond[:], in_=ccol[:], func=AF.Silu), reads=[ccol], writes=[cond])
    for kc in range(8):
        k.op("dve", lambda e, kc=kc: e.tensor_copy(out=cbc[:, kc, :], in_=cond[:, kc:kc + 1].to_broadcast([128, 128])),
             reads=[cond], writes=[cbc])
    for v in range(nvec):
        st = stage[v % 2]
        k.dma("sp", st[:], adaw_d[v], reads=[adaw_d], writes=[st])
        k.dma("sp", dst[v][:], adab_d[v].partition_broadcast(128), reads=[adab_d], writes=[dst[v]])
        for half in range(2):
            pb = pbanks[half]
            for kc in range(8):
                mm(k, pb, pb[:], cbc, cbc[:, kc, :], st, st[:, kc, half * 512:(half + 1) * 512], kc == 0, kc == 7)
            k.op("dve", lambda e, v=v, half=half, pb=pb: e.tensor_tensor(
                out=dst[v][:, half * 512:(half + 1) * 512], in0=pb[:], in1=dst[v][:, half * 512:(half + 1) * 512],
                op=ALU.add), reads=[pb, dst[v]], writes=[dst[v]])


NTOK = 2048
NT = NTOK // 128
SWIGLU_LIMIT = 7.0
SWIGLU_ALPHA = 1.702


def bd_dram(k, F, tag, n_exp=32, with_io=True):
    d = {}
    if with_io:
        d["oin"] = k.dram("oin" + tag, [NTOK, F], BF16, "ExternalInput")
        d["hres"] = k.dram("hres" + tag, [NTOK, 1024], F32, "ExternalInput")
    d["wout"] = k.dram("wout" + tag, [128, F // 128, 1024], F32, "ExternalInput")
    d["ccol"] = k.dram("ccol" + tag, [128, 8], F32, "ExternalInput")
    d["adaw"] = k.dram("adaw" + tag, [4, 128, 8, 1024], F32, "ExternalInput")
    d["adab"] = k.dram("adab" + tag, [4, 1024], F32, "ExternalInput")
    d["gain"] = k.dram("gain" + tag, [1024], F32, "ExternalInput")
    d["rw"] = k.dram("rw" + tag, [128, 8, 32], F32, "ExternalInput")
    d["rb"] = k.dram("rb" + tag, [32], F32, "ExternalInput")
    d["wgu"] = k.dram("wgu" + tag, [n_exp, 8, 128, 8, 2, 128], F32, "ExternalInput")
    d["bgu"] = k.dram("bgu" + tag, [128, 32, 2, 8], F32, "ExternalInput")
    d["wd"] = k.dram("wd" + tag, [n_exp, 128, 8, 1024], F32, "ExternalInput")
    d["bd"] = k.dram("bd" + tag, [32, 1024], F32, "ExternalInput")
    return d


def bd_host_inputs(layer, b, P, w_out, tag):
    m = {}
    F = w_out.shape[0]
    m["wout" + tag] = np.ascontiguousarray(w_out.reshape(F // 128, 128, 1024).transpose(1, 0, 2))
    m["ccol" + tag] = np.ascontiguousarray(P["c"][b].reshape(8, 128).T)
    aw = P["ada_w"][layer]
    sel = [2, 3, 4, 5]
    m["adaw" + tag] = np.ascontiguousarray(
        np.stack([aw[:, v * 1024:(v + 1) * 1024].reshape(8, 128, 1024).transpose(1, 0, 2) for v in sel]))
    m["adab" + tag] = np.ascontiguousarray(np.stack([P["ada_b"][layer][v * 1024:(v + 1) * 1024] for v in sel]))
    m["gain" + tag] = np.ascontiguousarray(P["norm_ffn"][layer])
    m["rw" + tag] = np.ascontiguousarray(P["router_w"][layer].reshape(8, 128, 32).transpose(1, 0, 2))
    m["rb" + tag] = np.ascontiguousarray(P["router_b"][layer])
    return m


def bd_host_shared(layer, P, tag):
    m = {}
    wgu = P["moe_w_gu"][layer]
    m["wgu" + tag] = np.ascontiguousarray(wgu.reshape(32, 8, 128, 2, 8, 128).transpose(0, 4, 2, 1, 3, 5))
    bgu = P["moe_b_gu"][layer]
    m["bgu" + tag] = np.ascontiguousarray(bgu.reshape(32, 2, 8, 128).transpose(3, 0, 1, 2))
    wd = P["moe_w_down"][layer]
    m["wd" + tag] = np.ascontiguousarray(wd.reshape(32, 8, 128, 1024).transpose(0, 2, 1, 3))
    m["bd" + tag] = np.ascontiguousarray(P["moe_b_down"][layer])
    return m


def emit_bd(k, F, d, psum, pbf, out_d, hpre_d, ident, tag="", n_exp=32, dbg=None, oin_parts=None, hres_src=None,
            out_row0=0, ridx_d=None):
    FC = F // 128
    if oin_parts is None:
        oin_parts = [(slice(0, F), d["oin"], slice(0, F), 0)]
    if hres_src is None:
        hres_src = (d["hres"], 0)
    pg, pu, pd, pm = psum[0:2], psum[2:4], psum[4:6], psum[6:8]
    pgb, pmb = pbf[0:2], pbf[6:8]
    idf, idb = ident
    m0 = k.mark()
    uT = k.sb("uT" + tag, [128, 8, NTOK], BF16)
    gw = k.sb("gw" + tag, [128, NT, 32], F32)
    gwT = k.sb("gwT" + tag, [32, NTOK], F32)
    m_g2 = k.sb("m_g2" + tag, [128, 1024], F32)
    bgu = k.sb("bgu" + tag, [128, 32, 2, 8], F32)
    bgu1 = k.sb("bgu1" + tag, [128, 32, 8], F32)
    bd_sb = k.sb("bdsb" + tag, [32, 1024], F32)
    epsb = k.sb("epsb" + tag, [128, 1], F32)
    k.op("pool", lambda e: e.memset(epsb[:], EPS), writes=[epsb])
    ridx = None
    if ridx_d is not None:
        ridx = k.sb("ridx" + tag, [128, NT], mybir.dt.uint32)
        k.dma("sp", ridx[:], ridx_d[:], reads=[ridx_d], writes=[ridx])
    k.dma("sp", bgu[:], d["bgu"][:], reads=[d["bgu"]], writes=[bgu])
    k.dma("sp", bd_sb[:], d["bd"][:], reads=[d["bd"]], writes=[bd_sb])
    k.op("pool", lambda e: e.tensor_scalar(out=bgu1[:], in0=bgu[:, :, 1, :], scalar1=1.0, scalar2=None, op0=ALU.add),
         reads=[bgu], writes=[bgu1])

    m1 = k.mark()
    m_g1 = k.sb("m_g1" + tag, [128, 1024], F32)
    m_sh2 = k.sb("m_sh2" + tag, [128, 1024], F32)
    m_gm = k.sb("m_gm" + tag, [128, 1024], F32)
    gain_bc = k.sb("gainbc" + tag, [128, 1024], F32)
    stage = k.sb("adast" + tag, [128, 8, 1024], F32)
    woutb = k.sb("woutb" + tag, [128, FC, 1024], BF16)
    rwb = k.sb("rwb" + tag, [128, 8, 32], BF16)
    rb_bc = k.sb("rbbc" + tag, [128, 32], F32)
    o_tok = [k.sb("otok%d%s" % (i, tag), [128, F], BF16) for i in range(2)]
    oT = [k.sb("oT%d%s" % (i, tag), [128, FC, 128], BF16) for i in range(2)]
    hres_t = [k.sb("hrt%d%s" % (i, tag), [128, 1024], F32) for i in range(2)]
    tmp = [k.sb("tmp%d%s" % (i, tag), [128, 1024], F32) for i in range(2)]
    u_tok = [k.sb("utok%d%s" % (i, tag), [128, 1024], BF16) for i in range(2)]
    st = [k.sb("st%d%s" % (i, tag), [128, 64], F32) for i in range(2)]
    lg = [k.sb("lg%d%s" % (i, tag), [128, 4, 32], F32) for i in range(2)]

    k.dma("pool", woutb[:], d["wout"][:], reads=[d["wout"]], writes=[woutb])
    k.dma("pool", rwb[:], d["rw"][:], reads=[d["rw"]], writes=[rwb])
    k.dma("sp", rb_bc[:], d["rb"][:].partition_broadcast(128), reads=[d["rb"]], writes=[rb_bc])
    k.dma("sp", gain_bc[:], d["gain"][:].partition_broadcast(128), reads=[d["gain"]], writes=[gain_bc])
    adaln(k, d["ccol"], d["adaw"], d["adab"], 4, [m_g1, m_sh2, m_gm, m_g2], [stage, stage], pm, tag)
    k.op("dve", lambda e: e.scalar_tensor_tensor(out=m_gm[:], in0=m_gm[:], scalar=1.0, in1=gain_bc[:],
                                                 op0=ALU.add, op1=ALU.mult), reads=[m_gm, gain_bc], writes=[m_gm])

    for t in range(NT):
        ot, oTt, hr, tp, ut, s_, l_ = o_tok[t % 2], oT[t % 2], hres_t[t % 2], tmp[t % 2], u_tok[t % 2], st[t % 2], lg[t % 2]
        rows = slice(t * 128, (t + 1) * 128)
        if ridx is None:
            for (dcs, sbuf_, scs, r0) in oin_parts:
                k.dma("sp", ot[:, dcs], sbuf_[r0 + t * 128:r0 + (t + 1) * 128, scs], reads=[sbuf_], writes=[ot])
            k.dma("sp", hr[:], hres_src[0][hres_src[1] + t * 128:hres_src[1] + (t + 1) * 128, :], reads=[hres_src[0]], writes=[hr])
        else:
            for (dcs, sbuf_, scs, r0) in oin_parts:
                k.cc("pool", lambda e, ot=ot, dcs=dcs, sbuf_=sbuf_, t=t: e.indirect_dma_start(
                    out=ot[:, dcs], out_offset=None, in_=sbuf_[:, :],
                    in_offset=bass.IndirectOffsetOnAxis(ap=ridx[:, t:t + 1], axis=0)), reads=[sbuf_, ridx], writes=[ot])
            k.cc("pool", lambda e, hr=hr, t=t: e.indirect_dma_start(
                out=hr[:], out_offset=None, in_=hres_src[0][:, :],
                in_offset=bass.IndirectOffsetOnAxis(ap=ridx[:, t:t + 1], axis=0)), reads=[hres_src[0], ridx], writes=[hr])
        for g in range(FC // 8):
            pb, pbv = pg[g % 2], pgb[g % 2]
            for c in range(8):
                fc = g * 8 + c
                tr(k, pb, pbv[:, c * 128:(c + 1) * 128], ot, ot[:, fc * 128:(fc + 1) * 128], idb, idb[:])
            k.op("act", lambda e, g=g, pbv=pbv, oTt=oTt: e.activation(
                out=oTt[:, g * 8:(g + 1) * 8, :], in_=pbv.rearrange("p (a b) -> p a b", a=8), func=AF.Copy),
                reads=[pb], writes=[oTt])
        for half in range(2):
            hs = slice(half * 512, (half + 1) * 512)
            for fc in range(FC):
                mm(k, pd[half], pd[half][:], oTt, oTt[:, fc, :], woutb, woutb[:, fc, hs], fc == 0, fc == FC - 1)
            k.op("dve", lambda e, half=half, hs=hs, tp=tp: e.tensor_tensor(
                out=tp[:, hs], in0=pd[half][:], in1=m_g1[:, hs], op=ALU.mult), reads=[pd[half], m_g1], writes=[tp])
        k.op("pool", lambda e, hr=hr, tp=tp: e.tensor_tensor(out=hr[:], in0=tp[:], in1=hr[:], op=ALU.add),
             reads=[tp, hr], writes=[hr])
        k.dma("sp", hpre_d[rows, :], hr[:], reads=[hr], writes=[hpre_d])
        k.op("act", lambda e, hr=hr, tp=tp, s_=s_: e.activation(out=tp[:], in_=hr[:], func=AF.Square,
                                                                 accum_out=s_[:, 0:1]), reads=[hr], writes=[tp, s_])
        k.op("act", lambda e, s_=s_: e.activation(out=s_[:, 1:2], in_=s_[:, 0:1], func=AF.Sqrt, bias=epsb[:],
                                                   scale=1.0 / 1024.0), reads=[s_, epsb], writes=[s_])
        k.op("dve", lambda e, s_=s_: e.reciprocal(out=s_[:, 2:3], in_=s_[:, 1:2]), reads=[s_], writes=[s_])
        k.op("dve", lambda e, hr=hr, tp=tp, s_=s_: e.scalar_tensor_tensor(
            out=tp[:], in0=hr[:], scalar=s_[:, 2:3], in1=m_gm[:], op0=ALU.mult, op1=ALU.mult),
            reads=[hr, s_, m_gm], writes=[tp])
        k.op("pool", lambda e, tp=tp, ut=ut: e.tensor_tensor(out=ut[:], in0=tp[:], in1=m_sh2[:], op=ALU.add),
             reads=[tp, m_sh2], writes=[ut])
        for kc in range(8):
            tr(k, pm[0], pmb[0][:, kc * 128:(kc + 1) * 128], ut, ut[:, kc * 128:(kc + 1) * 128], idb, idb[:])
        k.op("act", lambda e, t=t: e.activation(out=uT[:, :, t * 128:(t + 1) * 128],
                                                in_=pmb[0].rearrange("p (a b) -> p a b", a=8), func=AF.Copy),
             reads=[pm[0]], writes=[uT])
        for kc in range(8):
            mm(k, pm[1], pm[1][:, 0:32], uT, uT[:, kc, t * 128:(t + 1) * 128], rwb, rwb[:, kc, :], kc == 0, kc == 7)
        k.op("dve", lambda e, l_=l_: e.tensor_tensor(out=l_[:, 0, :], in0=pm[1][:, 0:32], in1=rb_bc[:], op=ALU.add),
             reads=[pm[1], rb_bc], writes=[l_])
        k.op("dve", lambda e, l_=l_, s_=s_: e.max(out=s_[:, 8:16], in_=l_[:, 0, :]), reads=[l_], writes=[s_])
        k.op("dve", lambda e, l_=l_, s_=s_: e.tensor_scalar(out=l_[:, 1, :], in0=l_[:, 0, :], scalar1=s_[:, 11:12],
                                                            scalar2=None, op0=ALU.is_ge), reads=[l_, s_], writes=[l_])
        k.op("dve", lambda e, s_=s_: e.tensor_scalar(out=s_[:, 16:17], in0=s_[:, 8:9], scalar1=-1.0, scalar2=None,
                                                     op0=ALU.mult), reads=[s_], writes=[s_])
        k.op("act", lambda e, l_=l_, s_=s_: e.activation(out=l_[:, 2, :], in_=l_[:, 0, :], func=AF.Exp,
                                                          bias=s_[:, 16:17], scale=1.0), reads=[l_, s_], writes=[l_])
        k.op("dve", lambda e, l_=l_, s_=s_: e.scalar_tensor_tensor(
            out=l_[:, 3, :], in0=l_[:, 2, :], scalar=1.0, in1=l_[:, 1, :], op0=ALU.mult, op1=ALU.mult,
            accum_out=s_[:, 17:18]), reads=[l_], writes=[l_, s_])
        k.op("dve", lambda e, s_=s_: e.reciprocal(out=s_[:, 18:19], in_=s_[:, 17:18]), reads=[s_], writes=[s_])
        k.op("dve", lambda e, l_=l_, s_=s_, t=t: e.tensor_scalar(out=gw[:, t, :], in0=l_[:, 3, :], scalar1=s_[:, 18:19],
                                                                 scalar2=None, op0=ALU.mult), reads=[l_, s_], writes=[gw])
        tr(k, pm[1], pm[1][0:32, 128:256], gw, gw[:, t, :], idf, idf[:])
        k.op("act", lambda e, t=t: e.activation(out=gwT[:, t * 128:(t + 1) * 128], in_=pm[1][0:32, 128:256],
                                                func=AF.Copy), reads=[pm[1]], writes=[gwT])

    toks = []
    if dbg is not None:
        toks.append(k.dma("sp", dbg["uT"][:], uT[:], reads=[uT], writes=[dbg["uT"]]))
        toks.append(k.dma("sp", dbg["gw"][:], gw[:], reads=[gw], writes=[dbg["gw"]]))
        for i, mv in enumerate([m_g1, m_sh2, m_gm, m_g2]):
            toks.append(k.dma("sp", dbg["mods"][i], mv[:], reads=[mv], writes=[dbg["mods"]]))
    k.release(m1)
    acc = [k.sb("acc%d%s" % (t, tag), [128, 1024], F32) for t in range(NT)]
    m2 = k.mark()
    actT = [k.sb("actT%d%s" % (j, tag), [128, NTOK], BF16) for j in range(8)]
    NR = 4
    wring = [k.sb("wgur%d%s" % (i, tag), [128, 8, 2, 128], BF16) for i in range(NR)]
    ND = 3
    dring = [k.sb("wdr%d%s" % (i, tag), [128, 8, 512], BF16) for i in range(ND)]
    g_sb = k.sb("g_sb" + tag, [128, 512], F32)
    s_sb = k.sb("s_sb" + tag, [128, 512], F32)
    t1 = k.sb("t1" + tag, [128, 512], F32)
    t2 = k.sb("t2" + tag, [128, 512], F32)
    m_sb = k.sb("m_sb" + tag, [128, 512], F32)

    for t in range(NT):
        for half in range(2):
            hs = slice(half * 512, (half + 1) * 512)
            pb = pd[(2 * t + half) % 2]
            mm(k, pb, pb[:], gwT, gwT[:, t * 128:(t + 1) * 128], bd_sb, bd_sb[:, hs], True, True)
            k.op("act", lambda e, t=t, hs=hs, pb=pb: e.activation(out=acc[t][:, hs], in_=pb[:], func=AF.Copy),
                 reads=[pb], writes=[acc[t]])

    units = [(e, j) for e in range(n_exp) for j in range(8)]
    dunits = [(e, h) for e in range(n_exp) for h in range(2)]

    def load_unit(i):
        if i < len(units):
            e, j = units[i]
            k.dma("pool", wring[i % NR][:], d["wgu"][e, j], reads=[d["wgu"]], writes=[wring[i % NR]])

    def load_dunit(i):
        if i < len(dunits):
            e, h = dunits[i]
            k.dma("pool", dring[i % ND][:], d["wd"][e, :, :, h * 512:(h + 1) * 512], reads=[d["wd"]],
                  writes=[dring[i % ND]])

    for i in range(NR - 1):
        load_unit(i)
    for i in range(ND - 1):
        load_dunit(i)
    cnt = 0
    for e_ in range(n_exp):
        for j in range(8):
            ui = e_ * 8 + j
            load_unit(ui + NR - 1)
            w = wring[ui % NR]
            for T in range(4):
                x = cnt % 2
                cnt += 1
                ts_ = slice(T * 512, (T + 1) * 512)
                for kc in range(8):
                    mm(k, pg[x], pg[x][:], w, w[:, kc, 0, :], uT, uT[:, kc, ts_], kc == 0, kc == 7)
                for kc in range(8):
                    mm(k, pu[x], pu[x][:], w, w[:, kc, 1, :], uT, uT[:, kc, ts_], kc == 0, kc == 7)
                k.op("dve", lambda e, x=x, e_=e_, j=j: e.tensor_scalar(
                    out=g_sb[:], in0=pg[x][:], scalar1=bgu[:, e_, 0, j:j + 1], scalar2=SWIGLU_LIMIT,
                    op0=ALU.add, op1=ALU.min), reads=[pg[x], bgu], writes=[g_sb])
                k.op("act", lambda e: e.activation(out=s_sb[:], in_=g_sb[:], func=AF.Sigmoid, scale=SWIGLU_ALPHA),
                     reads=[g_sb], writes=[s_sb])
                k.op("act", lambda e, x=x, e_=e_, j=j: e.activation(
                    out=t1[:], in_=pu[x][:], func=AF.Identity, bias=bgu1[:, e_, j:j + 1], scale=1.0),
                    reads=[pu[x], bgu1], writes=[t1])
                k.op("pool", lambda e: e.tensor_scalar(out=t2[:], in0=t1[:], scalar1=SWIGLU_LIMIT + 1.0,
                                                       scalar2=1.0 - SWIGLU_LIMIT, op0=ALU.min, op1=ALU.max),
                     reads=[t1], writes=[t2])
                k.op("dve", lambda e: e.tensor_tensor(out=m_sb[:], in0=g_sb[:], in1=s_sb[:], op=ALU.mult),
                     reads=[g_sb, s_sb], writes=[m_sb])
                k.op("pool", lambda e, j=j, ts_=ts_: e.tensor_tensor(out=actT[j][:, ts_], in0=m_sb[:], in1=t2[:],
                                                                     op=ALU.mult),
                     reads=[m_sb, t2], writes=[actT[j]])
        for half in range(2):
            di = e_ * 2 + half
            load_dunit(di + ND - 1)
            wdv = dring[di % ND]
            hs = slice(half * 512, (half + 1) * 512)
            for t in range(NT):
                pb = pd[t % 2]
                for fc in range(8):
                    mm(k, pb, pb[:], actT[fc], actT[fc][:, t * 128:(t + 1) * 128], wdv, wdv[:, fc, :], fc == 0, fc == 7)
                k.op("dve", lambda e, t=t, hs=hs, pb=pb, e_=e_: e.scalar_tensor_tensor(
                    out=acc[t][:, hs], in0=pb[:], scalar=gw[:, t, e_:e_ + 1], in1=acc[t][:, hs],
                    op0=ALU.mult, op1=ALU.add), reads=[pb, gw, acc[t]], writes=[acc[t]])

    if dbg is not None:
        toks.append(k.dma("sp", dbg["acc0"][:], acc[0][:], reads=[acc[0]], writes=[dbg["acc0"]]))
        toks.append(k.dma("sp", dbg["actT0"][:], actT[0][:], reads=[actT[0]], writes=[dbg["actT0"]]))
    k.release(m2)
    hp = [k.sb("hp%d%s" % (i, tag), [128, 1024], F32) for i in range(2)]
    for t in range(NT):
        rows = slice(t * 128, (t + 1) * 128)
        h_ = hp[t % 2]
        k.dma("sp", h_[:], hpre_d[rows, :], reads=[hpre_d], writes=[h_])
        k.op("dve", lambda e, t=t: e.tensor_tensor(out=acc[t][:], in0=acc[t][:], in1=m_g2[:], op=ALU.mult),
             reads=[acc[t], m_g2], writes=[acc[t]])
        k.op("pool", lambda e, t=t, h_=h_: e.tensor_tensor(out=h_[:], in0=acc[t][:], in1=h_[:], op=ALU.add),
             reads=[acc[t], h_], writes=[h_])
        toks.append(k.dma("sp", out_d[out_row0 + t * 128:out_row0 + (t + 1) * 128, :], h_[:], reads=[h_], writes=[out_d]))
    k.release(m0)
    return toks


def make_psum(k):
    psum = [k.ps("ps%d" % i, [128, 512], F32) for i in range(8)]
    pbf = [p[:].bitcast(BF16) for p in psum]
    return psum, pbf


def build_bd(F, n_exp=32, debug=False):
    nc = bass.Bass("TRN2", target_bir_lowering=False)
    with contextlib.ExitStack() as stack:
        k = KB(nc, stack)
        psum, pbf = make_psum(k)
        d = bd_dram(k, F, "", n_exp)
        out_d = k.dram("out", [NTOK, 1024], F32, "ExternalOutput")
        hpre_d = k.dram("hpre_scr", [NTOK, 1024], F32, "ExternalOutput" if debug else "Internal")
        ident = make_ident(k, "id", BF16)
        dbg = None
        if debug:
            dbg = {"uT": k.dram("dbg_uT", [128, 8, NTOK], BF16, "ExternalOutput"),
                   "gw": k.dram("dbg_gw", [128, NT, 32], F32, "ExternalOutput"),
                   "mods": k.dram("dbg_mods", [4, 128, 1024], F32, "ExternalOutput"),
                   "acc0": k.dram("dbg_acc0", [128, 1024], F32, "ExternalOutput"),
                   "actT0": k.dram("dbg_actT0", [128, NTOK], BF16, "ExternalOutput")}
        toks = emit_bd(k, F, d, psum, pbf, out_d, hpre_d, ident, "", n_exp, dbg)
        k.emit(final_waits=toks)
        print("BD arena peak words", k.apeak, "instr", {e: len(v) for e, v in k.prog.items()})
    return nc


SEQ = 4096
NEG = -30000.0


def c_dram(k, tag="", with_io=True):
    d = {}
    if with_io:
        d["hin"] = k.dram("c_hin" + tag, [SEQ, 1024], F32, "ExternalInput")
    d["ccol"] = k.dram("c_ccol" + tag, [128, 8], F32, "ExternalInput")
    d["adaw"] = k.dram("c_adaw" + tag, [2, 128, 8, 1024], F32, "ExternalInput")
    d["adab"] = k.dram("c_adab" + tag, [2, 1024], F32, "ExternalInput")
    d["gain"] = k.dram("c_gain" + tag, [1024], F32, "ExternalInput")
    d["wz"] = k.dram("c_wz" + tag, [128, 8, 1024], F32, "ExternalInput")
    d["wx"] = k.dram("c_wx" + tag, [128, 8, 1024], F32, "ExternalInput")
    d["wbc"] = k.dram("c_wbc" + tag, [128, 8, 512], F32, "ExternalInput")
    d["wdt"] = k.dram("c_wdt" + tag, [128, 8, 16], F32, "ExternalInput")
    d["convw"] = k.dram("c_convw" + tag, [128, 12, 4], F32, "ExternalInput")
    d["convb"] = k.dram("c_convb" + tag, [128, 12], F32, "ExternalInput")
    d["dtb"] = k.dram("c_dtb" + tag, [16], F32, "ExternalInput")
    d["alog"] = k.dram("c_alog" + tag, [16], F32, "ExternalInput")
    d["dsk"] = k.dram("c_dsk" + tag, [16], F32, "ExternalInput")
    d["ng"] = k.dram("c_ng" + tag, [1024], F32, "ExternalInput")
    return d


def c_host_inputs(b, hh, P, tag=""):
    m = {}
    lay = lambda w: np.ascontiguousarray(w.reshape(8, 128, -1).transpose(1, 0, 2))
    m["c_ccol" + tag] = np.ascontiguousarray(P["c"][b].reshape(8, 128).T)
    aw = P["ada_w"][1]
    m["c_adaw" + tag] = np.ascontiguousarray(np.stack([lay(aw[:, v * 1024:(v + 1) * 1024]) for v in (0, 1)]))
    m["c_adab" + tag] = np.ascontiguousarray(np.stack([P["ada_b"][1][v * 1024:(v + 1) * 1024] for v in (0, 1)]))
    m["c_gain" + tag] = np.ascontiguousarray(P["norm_mix"][1])
    w = P["ssm_w_in"][0]
    m["c_wz" + tag] = lay(w[:, hh * 1024:(hh + 1) * 1024])
    xo = 2048
    m["c_wx" + tag] = lay(w[:, xo + hh * 1024:xo + (hh + 1) * 1024])
    bo, co = xo + 2048, xo + 2048 + 512
    g0 = 2 * hh
    bc_cols = np.concatenate([np.arange(bo + g0 * 128, bo + (g0 + 2) * 128), np.arange(co + g0 * 128, co + (g0 + 2) * 128)])
    m["c_wbc" + tag] = lay(w[:, bc_cols])
    dto = xo + 3072
    m["c_wdt" + tag] = lay(w[:, dto + 16 * hh:dto + 16 * (hh + 1)])
    ch = np.concatenate([np.arange(hh * 1024, (hh + 1) * 1024), bc_cols - xo])
    cw = P["ssm_conv_w"][0][:, ch]
    m["c_convw" + tag] = np.ascontiguousarray(cw.reshape(4, 12, 128).transpose(2, 1, 0))
    m["c_convb" + tag] = np.ascontiguousarray(P["ssm_conv_b"][0][ch].reshape(12, 128).T)
    hs = slice(16 * hh, 16 * (hh + 1))
    m["c_dtb" + tag] = np.ascontiguousarray(P["ssm_dt_bias"][0][hs])
    m["c_alog" + tag] = np.ascontiguousarray(P["ssm_a_log"][0][hs])
    m["c_dsk" + tag] = np.ascontiguousarray(P["ssm_d"][0][hs])
    m["c_ng" + tag] = np.ascontiguousarray(P["ssm_norm"][0][hh * 1024:(hh + 1) * 1024])
    return m


def emit_c(k, d, psum, pbf, yn_d, ident, tag="", n_tiles=8, dbg=None):
    idf, idb = ident
    m0 = k.mark()
    bcol = lambda ap, n: ap.unsqueeze(2).to_broadcast([128, ap.shape[1], n])
    tri = k.sb("c_tri", [128, 128], F32)
    ones = k.sb("c_ones", [128, 128], F32)
    negm = k.sb("c_negm", [128, 128], BF16)
    zer = k.sb("c_zer", [128, 128], F32)
    epsb = k.sb("c_epsb", [128, 1], F32)
    k.op("pool", lambda e: e.memset(ones[:], 1.0), writes=[ones])
    k.op("pool", lambda e: e.memset(zer[:], 0.0), writes=[zer])
    k.op("pool", lambda e: e.memset(epsb[:], EPS), writes=[epsb])
    k.op("pool", lambda e: e.affine_select(out=tri[:], in_=ones[:], pattern=[[1, 128]], compare_op=ALU.is_ge, fill=0.0,
                                           base=0, channel_multiplier=-1), reads=[ones], writes=[tri])
    k.op("pool", lambda e: e.affine_select(out=negm[:], in_=zer[:], pattern=[[1, 128]], compare_op=ALU.is_ge, fill=NEG,
                                           base=0, channel_multiplier=-1), reads=[zer], writes=[negm])
    convw = k.sb("c_convw", [128, 12, 4], F32)
    convb = k.sb("c_convb", [128, 12], F32)
    dtb = k.sb("c_dtb", [128, 16], F32)
    a_bc = k.sb("c_abc", [128, 16], F32)
    dsk = k.sb("c_dsk", [128, 16], F32)
    ng = k.sb("c_ng", [128, 1024], F32)
    m_sh = k.sb("c_msh", [128, 1024], F32)
    m_gm = k.sb("c_mgm", [128, 1024], F32)
    k.dma("sp", convw[:], d["convw"][:], reads=[d["convw"]], writes=[convw])
    k.dma("sp", convb[:], d["convb"][:], reads=[d["convb"]], writes=[convb])
    k.dma("sp", dtb[:], d["dtb"][:].partition_broadcast(128), reads=[d["dtb"]], writes=[dtb])
    k.dma("sp", a_bc[:], d["alog"][:].partition_broadcast(128), reads=[d["alog"]], writes=[a_bc])
    k.dma("sp", dsk[:], d["dsk"][:].partition_broadcast(128), reads=[d["dsk"]], writes=[dsk])
    k.dma("sp", ng[:], d["ng"][:].partition_broadcast(128), reads=[d["ng"]], writes=[ng])
    k.op("act", lambda e: e.activation(out=a_bc[:], in_=a_bc[:], func=AF.Exp), reads=[a_bc], writes=[a_bc])
    k.op("dve", lambda e: e.tensor_scalar(out=a_bc[:], in0=a_bc[:], scalar1=-1.0, scalar2=None, op0=ALU.mult),
         reads=[a_bc], writes=[a_bc])
    wz = k.sb("c_wz", [128, 8, 1024], BF16)
    wx = k.sb("c_wx", [128, 8, 1024], BF16)
    wbc = k.sb("c_wbc", [128, 8, 512], BF16)
    wdt = k.sb("c_wdt", [128, 8, 16], BF16)
    for wt, nm in ((wz, "wz"), (wx, "wx"), (wbc, "wbc"), (wdt, "wdt")):
        k.dma("pool", wt[:], d[nm][:], reads=[d[nm]], writes=[wt])
    m1 = k.mark()
    stage = k.sb("c_adast", [128, 8, 1024], F32)
    gain_bc = k.sb("c_gainbc", [128, 1024], F32)
    k.dma("sp", gain_bc[:], d["gain"][:].partition_broadcast(128), reads=[d["gain"]], writes=[gain_bc])
    adaln(k, d["ccol"], d["adaw"], d["adab"], 2, [m_sh, m_gm], [stage, stage], psum[6:8], "c" + tag)
    k.op("dve", lambda e: e.scalar_tensor_tensor(out=m_gm[:], in0=m_gm[:], scalar=1.0, in1=gain_bc[:],
                                                 op0=ALU.add, op1=ALU.mult), reads=[m_gm, gain_bc], writes=[m_gm])
    k.release(m1)

    hr = [k.sb("c_hr%d" % i, [128, 1024], F32) for i in range(2)]
    tp = [k.sb("c_tp%d" % i, [128, 1024], F32) for i in range(2)]
    ut = [k.sb("c_ut%d" % i, [128, 1024], BF16) for i in range(2)]
    st = [k.sb("c_st%d" % i, [128, 8], F32) for i in range(2)]
    uT = [k.sb("c_uT%d" % i, [128, 8, 512], BF16) for i in range(2)]
    xpre = k.sb("c_xpre", [128, 12, 515], F32)
    ctmp = [k.sb("c_ctmp%d" % i, [128, 512], F32) for i in range(2)]
    xsa = k.sb("c_xsa", [128, 8, 512], F32)
    bcT = k.sb("c_bcT", [128, 4, 512], BF16)
    zs = k.sb("c_zs", [128, 4, 1024], BF16)
    dt_t = k.sb("c_dt", [128, 4, 16], F32)
    sm = k.sb("c_sm", [128, 4, 16], F32)
    sm2 = k.sb("c_sm2", [128, 4, 16], F32)
    xs_tok = [k.sb("c_xst%d" % i, [128, 16, 64], F32) for i in range(2)]
    xd = k.sb("c_xd", [128, 16, 64], BF16)
    xdd = k.sb("c_xdd", [128, 16, 64], BF16)
    btok = k.sb("c_btok", [128, 2, 128], BF16)
    dtA = k.sb("c_dtA", [128, 16], F32)
    dtAb = k.sb("c_dtAb", [128, 16, 128], F32)
    cs = k.sb("c_cs", [128, 6, 16], F32)
    dec = [k.sb("c_dec%d" % i, [128, 4, 128], F32) for i in range(2)]
    MT = k.sb("c_MT", [128, 16, 128], BF16)
    cbT = k.sb("c_cbT", [128, 2, 128], F32)
    y1 = k.sb("c_y1", [128, 16, 64], F32)
    y2 = k.sb("c_y2", [128, 16, 64], F32)
    ynt = [k.sb("c_ynt%d" % i, [128, 1024], BF16) for i in range(2)]
    S = k.sb("c_S", [128, 2, 512], F32)
    Sb = k.sb("c_Sb", [128, 2, 512], BF16)
    k.op("pool", lambda e: e.memset(S[:], 0.0), writes=[S])
    k.op("pool", lambda e: e.memset(Sb[:], 0.0), writes=[Sb])
    k.op("pool", lambda e: e.memset(xpre[:, :, 0:3], 0.0), writes=[xpre])

    toks = []
    for T in range(n_tiles):
        uTt = uT[T % 2]
        for s in range(4):
            i2 = (T * 4 + s) % 2
            hr_, tp_, ut_, st_ = hr[i2], tp[i2], ut[i2], st[i2]
            rows = slice(T * 512 + s * 128, T * 512 + (s + 1) * 128)
            k.dma("sp", hr_[:], d["hin"][rows, :], reads=[d["hin"]], writes=[hr_])
            k.op("act", lambda e, hr_=hr_, tp_=tp_, st_=st_: e.activation(out=tp_[:], in_=hr_[:], func=AF.Square,
                                                                         accum_out=st_[:, 0:1]), reads=[hr_], writes=[tp_, st_])
            k.op("act", lambda e, st_=st_: e.activation(out=st_[:, 1:2], in_=st_[:, 0:1], func=AF.Sqrt, bias=epsb[:],
                                                        scale=1.0 / 1024.0), reads=[st_, epsb], writes=[st_])
            k.op("dve", lambda e, st_=st_: e.reciprocal(out=st_[:, 2:3], in_=st_[:, 1:2]), reads=[st_], writes=[st_])
            k.op("dve", lambda e, hr_=hr_, tp_=tp_, st_=st_: e.scalar_tensor_tensor(
                out=tp_[:], in0=hr_[:], scalar=st_[:, 2:3], in1=m_gm[:], op0=ALU.mult, op1=ALU.mult),
                reads=[hr_, st_, m_gm], writes=[tp_])
            k.op("pool", lambda e, tp_=tp_, ut_=ut_: e.tensor_tensor(out=ut_[:], in0=tp_[:], in1=m_sh[:], op=ALU.add),
                 reads=[tp_, m_sh], writes=[ut_])
            pb, pbv = psum[6 + s % 2], pbf[6 + s % 2]
            for kc in range(8):
                tr(k, pb, pbv[:, kc * 128:(kc + 1) * 128], ut_, ut_[:, kc * 128:(kc + 1) * 128], idb, idb[:])
            k.op("act", lambda e, s=s, pbv=pbv, uTt=uTt: e.activation(
                out=uTt[:, :, s * 128:(s + 1) * 128], in_=pbv.rearrange("p (a b) -> p a b", a=8), func=AF.Copy),
                reads=[pb], writes=[uTt])
        for s in range(4):
            for half in range(2):
                pb = psum[(2 * s + half) % 2]
                for kc in range(8):
                    mm(k, pb, pb[:], uTt, uTt[:, kc, s * 128:(s + 1) * 128], wz, wz[:, kc, half * 512:(half + 1) * 512],
                       kc == 0, kc == 7)
                k.op("act", lambda e, s=s, half=half, pb=pb: e.activation(
                    out=zs[:, s, half * 512:(half + 1) * 512], in_=pb[:], func=AF.Silu), reads=[pb], writes=[zs])
        for s in range(4):
            pb = psum[2 + s % 2]
            for kc in range(8):
                mm(k, pb, pb[:, 0:16], uTt, uTt[:, kc, s * 128:(s + 1) * 128], wdt, wdt[:, kc, :], kc == 0, kc == 7)
            k.op("dve", lambda e, s=s, pb=pb: e.tensor_tensor(out=sm[:, s, :], in0=pb[:, 0:16], in1=dtb[:], op=ALU.add),
                 reads=[pb, dtb], writes=[sm])
        k.op("act", lambda e: e.activation(out=sm2[:], in_=sm[:], func=AF.Abs), reads=[sm], writes=[sm2])
        k.op("act", lambda e: e.activation(out=sm2[:], in_=sm2[:], func=AF.Exp, scale=-1.0), reads=[sm2], writes=[sm2])
        k.op("act", lambda e: e.activation(out=sm2[:], in_=sm2[:], func=AF.Ln, bias=1.0, scale=1.0), reads=[sm2], writes=[sm2])
        k.op("dve", lambda e: e.scalar_tensor_tensor(out=dt_t[:], in0=sm[:], scalar=0.0, in1=sm2[:], op0=ALU.max,
                                                     op1=ALU.add), reads=[sm, sm2], writes=[dt_t])
        for c in range(12):
            pb = psum[4 + c % 2]
            wsrc, col = (wx, c * 128) if c < 8 else (wbc, (c - 8) * 128)
            for kc in range(8):
                mm(k, pb, pb[:], wsrc, wsrc[:, kc, col:col + 128], uTt, uTt[:, kc, :], kc == 0, kc == 7)
            k.op("act", lambda e, c=c, pb=pb: e.activation(out=xpre[:, c, 3:515], in_=pb[:], func=AF.Copy),
                 reads=[pb], writes=[xpre])
        for c in range(12):
            ct = ctmp[c % 2]
            eng = "dve" if c % 2 == 0 else "pool"
            k.op(eng, lambda e, c=c, ct=ct: e.tensor_scalar(out=ct[:], in0=xpre[:, c, 0:512], scalar1=convw[:, c, 0:1],
                                                            scalar2=None, op0=ALU.mult), reads=[xpre, convw], writes=[ct])
            for tap in range(1, 4):
                k.op("dve", lambda e, c=c, ct=ct, tap=tap: e.scalar_tensor_tensor(
                    out=ct[:], in0=xpre[:, c, tap:tap + 512], scalar=convw[:, c, tap:tap + 1], in1=ct[:],
                    op0=ALU.mult, op1=ALU.add), reads=[xpre, convw, ct], writes=[ct])
            if c < 8:
                k.op("act", lambda e, c=c, ct=ct: e.activation(out=xsa[:, c, :], in_=ct[:], func=AF.Silu,
                                                               bias=convb[:, c:c + 1], scale=1.0),
                     reads=[ct, convb], writes=[xsa])
            else:
                k.op("act", lambda e, c=c, ct=ct: e.activation(out=bcT[:, c - 8, :], in_=ct[:], func=AF.Silu,
                                                               bias=convb[:, c:c + 1], scale=1.0),
                     reads=[ct, convb], writes=[bcT])
        k.op("pool", lambda e: e.tensor_copy(out=xpre[:, :, 0:3], in_=xpre[:, :, 512:515]), reads=[xpre], writes=[xpre])

        for s in range(4):
            ci = T * 4 + s
            cols = slice(s * 128, (s + 1) * 128)
            xst = xs_tok[ci % 2]
            for hf in range(2):
                pb = psum[hf]
                for c4 in range(4):
                    c = hf * 4 + c4
                    tr(k, pb, pb[:, c4 * 128:(c4 + 1) * 128], xsa, xsa[:, c, cols], idf, idf[:])
                k.op("act", lambda e, hf=hf, pb=pb, xst=xst: e.activation(
                    out=xst[:, hf * 8:(hf + 1) * 8, :], in_=pb[:].rearrange("p (a b) -> p a b", a=8), func=AF.Copy),
                    reads=[pb], writes=[xst])
            pb, pbv = psum[2], pbf[2]
            for g in range(2):
                tr(k, pb, pbv[:, g * 128:(g + 1) * 128], bcT, bcT[:, g, cols], idb, idb[:])
            k.op("act", lambda e, pbv=pbv: e.activation(out=btok[:], in_=pbv[:, 0:256].rearrange("p (a b) -> p a b", a=2),
                                                        func=AF.Copy), reads=[pb], writes=[btok])
            k.op("dve", lambda e, s=s: e.tensor_tensor(out=dtA[:], in0=dt_t[:, s, :], in1=a_bc[:], op=ALU.mult),
                 reads=[dt_t, a_bc], writes=[dtA])
            k.op("pool", lambda e: e.tensor_copy(out=dtAb[:], in_=bcol(dtA[:], 128)), reads=[dtA], writes=[dtAb])
            pb = psum[3]
            mm(k, pb, pb[:, 0:16], tri, tri[:], dtA, dtA[:], True, True)
            mm(k, pb, pb[:, 16:32], ones, ones[:], dtA, dtA[:], True, True)
            k.op("act", lambda e, pb=pb: e.activation(out=cs[:, 0, :], in_=pb[:, 0:16], func=AF.Copy), reads=[pb], writes=[cs])
            k.op("dve", lambda e, pb=pb: e.tensor_scalar(out=cs[:, 1, :], in0=pb[:, 0:16], scalar1=-1.0, scalar2=None,
                                                         op0=ALU.mult), reads=[pb], writes=[cs])
            k.op("act", lambda e, pb=pb: e.activation(out=cs[:, 2, :], in_=pb[:, 0:16], func=AF.Exp), reads=[pb], writes=[cs])
            k.op("act", lambda e, pb=pb: e.activation(out=cs[:, 5, :], in_=pb[:, 16:32], func=AF.Exp), reads=[pb], writes=[cs])
            k.op("dve", lambda e, pb=pb: e.tensor_tensor(out=cs[:, 3, :], in0=pb[:, 16:32], in1=cs[:, 0, :], op=ALU.subtract),
                 reads=[pb, cs], writes=[cs])
            k.op("act", lambda e: e.activation(out=cs[:, 3, :], in_=cs[:, 3, :], func=AF.Exp), reads=[cs], writes=[cs])
            k.op("dve", lambda e, s=s: e.tensor_tensor(out=cs[:, 4, :], in0=cs[:, 3, :], in1=dt_t[:, s, :], op=ALU.mult),
                 reads=[cs, dt_t], writes=[cs])
            k.op("dve", lambda e, s=s, xst=xst: e.tensor_tensor(out=xd[:], in0=xst[:], in1=bcol(dt_t[:, s, :], 64), op=ALU.mult),
                 reads=[xst, dt_t], writes=[xd])
            k.op("pool", lambda e, xst=xst: e.tensor_tensor(out=xdd[:], in0=xst[:], in1=bcol(cs[:, 4, :], 64), op=ALU.mult),
                 reads=[xst, cs], writes=[xdd])
            pb = psum[2]
            for g in range(2):
                mm(k, pb, pb[:, 256 + g * 128:256 + (g + 1) * 128], bcT, bcT[:, g, cols], bcT, bcT[:, 2 + g, cols], True, True)
            k.op("act", lambda e, pb=pb: e.activation(out=cbT[:], in_=pb[:, 256:512].rearrange("p (a b) -> p a b", a=2),
                                                      func=AF.Copy), reads=[pb], writes=[cbT])
            for q in range(4):
                pb = psum[4 + q % 2]
                dq = dec[q % 2]
                for hh_ in range(4):
                    h = q * 4 + hh_
                    o = pb[:, hh_ * 128:(hh_ + 1) * 128]
                    mm(k, pb, o, dtAb, dtAb[:, h, :], tri, tri[:], True, False)
                    mm(k, pb, o, idb, idb[:], negm, negm[:], False, True)
                for hh_ in range(4):
                    h = q * 4 + hh_
                    k.op("act", lambda e, pb=pb, dq=dq, hh_=hh_, h=h: e.activation(
                        out=dq[:, hh_, :], in_=pb[:, hh_ * 128:(hh_ + 1) * 128], func=AF.Exp, bias=cs[:, 1, h:h + 1], scale=1.0),
                        reads=[pb, cs], writes=[dq])
                g = q // 2
                eng = "dve" if q % 2 == 0 else "pool"
                k.op(eng, lambda e, q=q, dq=dq, g=g: e.tensor_tensor(
                    out=MT[:, q * 4:(q + 1) * 4, :], in0=dq[:], in1=cbT[:, g:g + 1, :].to_broadcast([128, 4, 128]), op=ALU.mult),
                    reads=[dq, cbT], writes=[MT])
            for h in range(16):
                pb = psum[h // 8]
                mm(k, pb, pb[:, (h % 8) * 64:(h % 8 + 1) * 64], MT, MT[:, h, :], xd, xd[:, h, :], True, True)
            for g in range(2):
                pb = psum[6 + g]
                mm(k, pb, pb[:], bcT, bcT[:, 2 + g, cols], Sb, Sb[:, g, :], True, True)
            for g in range(2):
                hs = slice(g * 8, (g + 1) * 8)
                po, pdg = psum[6 + g], psum[g]
                k.op("dve", lambda e, hs=hs, po=po: e.tensor_tensor(
                    out=y1[:, hs, :], in0=po[:].rearrange("p (a b) -> p a b", a=8), in1=bcol(cs[:, 2, hs], 64), op=ALU.mult),
                    reads=[po, cs], writes=[y1])
                k.op("pool", lambda e, hs=hs, xst=xst: e.tensor_tensor(
                    out=y2[:, hs, :], in0=xst[:, hs, :], in1=bcol(dsk[:, hs], 64), op=ALU.mult), reads=[xst, dsk], writes=[y2])
                k.op("dve", lambda e, hs=hs, pdg=pdg: e.tensor_tensor(
                    out=y1[:, hs, :], in0=pdg[:].rearrange("p (a b) -> p a b", a=8), in1=y1[:, hs, :], op=ALU.add),
                    reads=[pdg, y1], writes=[y1])
                k.op("pool", lambda e, hs=hs: e.tensor_tensor(out=y1[:, hs, :], in0=y1[:, hs, :], in1=y2[:, hs, :], op=ALU.add),
                     reads=[y1, y2], writes=[y1])
            if dbg is not None and "yssd" in dbg:
                toks.append(k.dma("sp", dbg["yssd"][ci * 128:(ci + 1) * 128, :], y1[:].rearrange("p a b -> p (a b)"),
                                  reads=[y1], writes=[dbg["yssd"]]))
            y1f = y1[:].rearrange("p a b -> p (a b)")
            y2f = y2[:].rearrange("p a b -> p (a b)")
            k.op("dve", lambda e, s=s, y1f=y1f: e.tensor_tensor(out=y1f, in0=y1f, in1=zs[:, s, :], op=ALU.mult),
                 reads=[y1, zs], writes=[y1])
            st_ = st[ci % 2]
            yo = ynt[ci % 2]
            for g in range(2):
                gs = slice(g * 512, (g + 1) * 512)
                k.op("act", lambda e, gs=gs, g=g, st_=st_, y1f=y1f, y2f=y2f: e.activation(
                    out=y2f[:, gs], in_=y1f[:, gs], func=AF.Square, accum_out=st_[:, 4 + g:5 + g]), reads=[y1], writes=[y2, st_])
            k.op("act", lambda e, st_=st_: e.activation(out=st_[:, 6:8], in_=st_[:, 4:6], func=AF.Sqrt, bias=epsb[:],
                                                        scale=1.0 / 512.0), reads=[st_, epsb], writes=[st_])
            k.op("dve", lambda e, st_=st_: e.reciprocal(out=st_[:, 6:8], in_=st_[:, 6:8]), reads=[st_], writes=[st_])
            for g in range(2):
                gs = slice(g * 512, (g + 1) * 512)
                k.op("dve", lambda e, gs=gs, g=g, st_=st_, yo=yo, y1f=y1f: e.scalar_tensor_tensor(
                    out=yo[:, gs], in0=y1f[:, gs], scalar=st_[:, 6 + g:7 + g], in1=ng[:, gs], op0=ALU.mult, op1=ALU.mult),
                    reads=[y1, st_, ng], writes=[yo])
            toks.append(k.dma("sp", yn_d[ci * 128:(ci + 1) * 128, :], yo[:], reads=[yo], writes=[yn_d]))
            for g in range(2):
                pb = psum[2 + g]
                hs = slice(g * 8, (g + 1) * 8)
                mm(k, pb, pb[:], btok, btok[:, g, :], xdd, xdd[:, hs, :].rearrange("p a b -> p (a b)"), True, True)
                Sg = S[:, g, :].rearrange("p (a b) -> p a b", a=8)
                eng = "dve" if g == 0 else "pool"
                k.op(eng, lambda e, Sg=Sg, hs=hs: e.tensor_tensor(out=Sg, in0=Sg, in1=bcol(cs[:, 5, hs], 64), op=ALU.mult),
                     reads=[S, cs], writes=[S])
                k.op("dve", lambda e, g=g, pb=pb: e.tensor_tensor(out=S[:, g, :], in0=pb[:], in1=S[:, g, :], op=ALU.add),
                     reads=[pb, S], writes=[S])
            k.op("act", lambda e: e.activation(out=Sb[:], in_=S[:], func=AF.Copy), reads=[S], writes=[Sb])
    k.release(m0)
    return toks


def build_c(n_tiles=8, debug=False):
    nc = bass.Bass("TRN2", target_bir_lowering=False)
    with contextlib.ExitStack() as stack:
        k = KB(nc, stack)
        psum, pbf = make_psum(k)
        d = c_dram(k)
        yn_d = k.dram("yn", [SEQ, 1024], BF16, "ExternalOutput")
        ident = make_ident(k, "id", BF16)
        dbg = None
        if debug:
            dbg = {"yssd": k.dram("dbg_yssd", [SEQ, 1024], F32, "ExternalOutput")}
        toks = emit_c(k, d, psum, pbf, yn_d, ident, "", n_tiles, dbg)
        k.emit(final_waits=toks)
        print("C arena peak words", k.apeak, "instr", {e: len(v) for e, v in k.prog.items()})
    return nc


def a_dram(k, tag=""):
    d = {}
    def inp(name, shape, dt=F32):
        d[name] = k.dram("a_" + name + tag, shape, dt, "ExternalInput")
    inp("xin", [SEQ, 1024]); inp("ccol", [128, 8]); inp("adaw", [2, 128, 8, 1024]); inp("adab", [2, 1024])
    inp("gain", [1024]); inp("wsb", [128, 8, 768]); inp("wnq", [128, 8, 256]); inp("wkv", [128, 8, 384])
    inp("wg", [128, 8, 12]); inp("qn", [64, 1]); inp("kn", [64, 3]); inp("pek", [64, 32, 2]); inp("pev", [64, 32, 2])
    inp("w1k", [64, 32, 128]); inp("w1v", [64, 32, 128]); inp("w2k", [128, 64]); inp("w2v", [128, 64])
    inp("qaug", [4, 4, SEQ], BF16); inp("kaug", [4, SEQ], BF16); inp("caug", [4, 256], BF16)
    inp("cmask", [128, 2, SEQ], BF16); inp("ovl", [128, 2, 64], BF16); inp("E", [64, 32, 128], BF16)
    inp("m1", [32, 128, 64]); inp("a1", [32, 128, 64])
    return d


def a_host_consts(hh):
    m = {}
    tok = np.arange(SEQ)
    slopes = np.array([2.0 ** (-(4 * hh + r + 1)) for r in range(4)], np.float32)
    qa = np.zeros((4, 4, SEQ), np.float32)
    for r in range(4):
        qa[0, r] = -slopes[r] * (tok % 128)
        qa[1, r] = slopes[r]
        qa[2, r] = -slopes[r] * 128.0 * (tok // 128)
        qa[3, r] = slopes[r] * 128.0
    m["a_qaug"] = qa.astype(NPBF)
    ka = np.stack([np.ones(SEQ), tok % 128, np.ones(SEQ), tok // 128]).astype(np.float32)
    m["a_kaug"] = ka.astype(NPBF)
    n = np.arange(256)
    ce = 16 * n + 31
    ca = np.stack([np.ones(256), ce % 128, np.ones(256), ce // 128]).astype(np.float32)
    ca[:, 255] = 0
    m["a_caug"] = ca.astype(NPBF)
    cm = np.where((ce[:, None] <= tok[None, :]) & (n[:, None] < 255), 0.0, NEG).astype(np.float32)
    m["a_cmask"] = np.ascontiguousarray(cm.reshape(2, 128, SEQ).transpose(1, 0, 2)).astype(NPBF)
    cs_ = n * 16
    sl = np.arange(64) * 64
    ov = ((cs_[:, None] < sl[None, :] + 64) & (cs_[:, None] + 32 > sl[None, :]) & (n[:, None] < 255)).astype(np.float32)
    m["a_ovl"] = np.ascontiguousarray(ov.reshape(2, 128, 64).transpose(1, 0, 2)).astype(NPBF)
    E = np.zeros((64, 32, 128), np.float32)
    for s in range(32):
        for kk in range(128):
            E[2 * s + kk // 64, s, kk] = 1.0
    m["a_E"] = E.astype(NPBF)
    j = np.arange(64)
    t = tok.reshape(32, 128)
    cur = t // 64
    valid = j[None, None, :] * 64 <= t[:, :, None]
    forced = (j[None, None, :] == 0) | (j[None, None, :] == cur[:, :, None]) | (j[None, None, :] == cur[:, :, None] - 1)
    m["a_m1"] = (valid & ~forced).astype(np.float32)
    m["a_a1"] = np.where(valid, np.where(forced, 1e6 + j[None, None, :], 0.0), -1e6).astype(np.float32)
    return m


def a_host_inputs(b, hh, P):
    m = {}
    lay = lambda w: np.ascontiguousarray(w.reshape(8, 128, -1).transpose(1, 0, 2))
    m["a_xin"] = np.ascontiguousarray(P["x"][b])
    m["a_ccol"] = np.ascontiguousarray(P["c"][b].reshape(8, 128).T)
    aw = P["ada_w"][0]
    m["a_adaw"] = np.ascontiguousarray(np.stack([lay(aw[:, v * 1024:(v + 1) * 1024]) for v in (0, 1)]))
    m["a_adab"] = np.ascontiguousarray(np.stack([P["ada_b"][0][v * 1024:(v + 1) * 1024] for v in (0, 1)]))
    m["a_gain"] = np.ascontiguousarray(P["norm_mix"][0])
    w = P["attn_w_in"][0]
    hs = slice(hh * 256, (hh + 1) * 256)
    m["a_wsb"] = lay(np.concatenate([w[:, 0:512][:, hs], w[:, 512:1024][:, hs], w[:, 1024:1536][:, hs]], axis=1))
    m["a_wnq"] = lay(w[:, 1536:2048][:, hs])
    kv = [w[:, 2048 + i * 128 + hh * 64: 2048 + i * 128 + (hh + 1) * 64] for i in range(6)]
    m["a_wkv"] = lay(np.concatenate(kv, axis=1))
    m["a_wg"] = lay(w[:, 2816 + 12 * hh: 2816 + 12 * (hh + 1)])
    m["a_qn"] = np.ascontiguousarray(P["nsa_q_norm"][0].reshape(64, 1))
    m["a_kn"] = np.ascontiguousarray(P["nsa_k_norm"][0].T)
    m["a_pek"] = np.ascontiguousarray(np.repeat(P["cmp_pe_k"][0].T[:, :, None], 2, axis=2))
    m["a_pev"] = np.ascontiguousarray(np.repeat(P["cmp_pe_v"][0].T[:, :, None], 2, axis=2))
    m["a_w1k"] = np.ascontiguousarray(P["cmp_w1_k"][0].reshape(32, 64, 128).transpose(1, 0, 2))
    m["a_w1v"] = np.ascontiguousarray(P["cmp_w1_v"][0].reshape(32, 64, 128).transpose(1, 0, 2))
    m["a_w2k"] = np.ascontiguousarray(P["cmp_w2_k"][0])
    m["a_w2v"] = np.ascontiguousarray(P["cmp_w2_v"][0])
    m.update(a_host_consts(hh))
    return m


def emit_a(k, d, psum, pbf, pall, o_d, ident, tag="", n_qt=32, dbg=None):
    idf, idb = ident
    m0 = k.mark()
    toks_dbg = []
    NTL = SEQ // 128
    sbqT = k.sb("a_sbqT", [128, 2, SEQ], BF16)
    sbkT = k.sb("a_sbkT", [128, 2, SEQ], BF16)
    sbv = k.sb("a_sbv", [128, NTL, 256], BF16)
    nqT = k.sb("a_nqT", [68, 4, SEQ], BF16)
    ksT = k.sb("a_ksT", [68, SEQ], BF16)
    kwT = k.sb("a_kwT", [68, SEQ], BF16)
    kcT = k.sb("a_kcT", [64, SEQ], BF16)
    vcT = k.sb("a_vcT", [64, SEQ], BF16)
    vsA = k.sb("a_vsA", [128, NTL, 65], BF16)
    vwA = k.sb("a_vwA", [128, NTL, 65], BF16)
    gts = k.sb("a_gts", [128, NTL, 12], F32)
    kcmpT = k.sb("a_kcmpT", [68, 256], BF16)
    vcmpA = k.sb("a_vcmpA", [128, 2, 129], BF16)
    ones = k.sb("a_ones", [128, 128], F32)
    epsb = k.sb("a_epsb", [128, 1], F32)
    qn8 = k.sb("a_qn8", [64, 1], F32)
    kn = k.sb("a_kn", [64, 3], F32)
    k.op("pool", lambda e: e.memset(ones[:], 1.0), writes=[ones])
    k.op("pool", lambda e: e.memset(epsb[:], EPS), writes=[epsb])
    k.op("pool", lambda e: e.memset(vsA[:], 1.0), writes=[vsA])
    k.op("pool", lambda e: e.memset(vwA[:], 1.0), writes=[vwA])
    k.op("pool", lambda e: e.memset(vcmpA[:], 1.0), writes=[vcmpA])
    k.dma("sp", qn8[:], d["qn"][:], reads=[d["qn"]], writes=[qn8])
    k.dma("sp", kn[:], d["kn"][:], reads=[d["kn"]], writes=[kn])
    k.op("dve", lambda e: e.tensor_scalar(out=qn8[:], in0=qn8[:], scalar1=0.125, scalar2=None, op0=ALU.mult),
         reads=[qn8], writes=[qn8])
    k.dma("sp", nqT[64:68, :, :], d["qaug"][:], reads=[d["qaug"]], writes=[nqT])
    k.dma("sp", ksT[64:68, :], d["kaug"][:], reads=[d["kaug"]], writes=[ksT])
    k.dma("sp", kwT[64:68, :], d["kaug"][:], reads=[d["kaug"]], writes=[kwT])
    k.dma("sp", kcmpT[64:68, :], d["caug"][:], reads=[d["caug"]], writes=[kcmpT])
    k.dma("sp", vcmpA[:, :, 65:129], d["ovl"][:], reads=[d["ovl"]], writes=[vcmpA])

    m1 = k.mark()
    m_sh = k.sb("a_msh", [128, 1024], F32)
    m_gm = k.sb("a_mgm", [128, 1024], F32)
    qf = [k.sb("a_qf%d" % i, [64, 512], F32) for i in range(2)]
    sq = [k.sb("a_sq%d" % i, [64, 512], F32) for i in range(2)]
    rs = [k.sb("a_rs%d" % i, [64, 512], F32) for i in range(2)]
    m2 = k.mark()
    stage = k.sb("a_adast", [128, 8, 1024], F32)
    gain_bc = k.sb("a_gainbc", [128, 1024], F32)
    k.dma("sp", gain_bc[:], d["gain"][:].partition_broadcast(128), reads=[d["gain"]], writes=[gain_bc])
    adaln(k, d["ccol"], d["adaw"], d["adab"], 2, [m_sh, m_gm], [stage, stage], psum[6:8], "a" + tag)
    k.op("dve", lambda e: e.scalar_tensor_tensor(out=m_gm[:], in0=m_gm[:], scalar=1.0, in1=gain_bc[:],
                                                 op0=ALU.add, op1=ALU.mult), reads=[m_gm, gain_bc], writes=[m_gm])
    k.release(m2)
    wsb = k.sb("a_wsb", [128, 8, 768], BF16)
    wnq = k.sb("a_wnq", [128, 8, 256], BF16)
    wkv = k.sb("a_wkv", [128, 8, 384], BF16)
    wg = k.sb("a_wg", [128, 8, 12], BF16)
    for wt, nm in ((wsb, "wsb"), (wnq, "wnq"), (wkv, "wkv"), (wg, "wg")):
        k.dma("pool", wt[:], d[nm][:], reads=[d[nm]], writes=[wt])
    hr = [k.sb("a_hr%d" % i, [128, 1024], F32) for i in range(2)]
    tp = [k.sb("a_tp%d" % i, [128, 1024], F32) for i in range(2)]
    ut = [k.sb("a_ut%d" % i, [128, 1024], BF16) for i in range(2)]
    st = [k.sb("a_st%d" % i, [128, 8], F32) for i in range(2)]
    uT = [k.sb("a_uT%d" % i, [128, 8, 512], BF16) for i in range(2)]
    n64 = [0]

    def norm64(pb, src, gain_ap, gain_buf, out_buf, out_ap, ncols):
        i = n64[0] % 2
        n64[0] += 1
        q_, s_, r_ = qf[i], sq[i], rs[i]
        k.op("act", lambda e: e.activation(out=q_[:, 0:ncols], in_=src, func=AF.Copy), reads=[pb], writes=[q_])
        k.op("act", lambda e: e.activation(out=s_[:, 0:ncols], in_=src, func=AF.Square), reads=[pb], writes=[s_])
        p2 = psum[5]
        mm(k, p2, p2[0:64, 0:ncols], ones, ones[0:64, 0:64], s_, s_[:, 0:ncols], True, True)
        k.op("act", lambda e: e.activation(out=r_[:, 0:ncols], in_=p2[0:64, 0:ncols], func=AF.Sqrt, bias=epsb[0:64, :],
                                           scale=1.0 / 64.0), reads=[p2, epsb], writes=[r_])
        k.op("dve", lambda e: e.reciprocal(out=r_[:, 0:ncols], in_=r_[:, 0:ncols]), reads=[r_], writes=[r_])
        k.op("dve", lambda e: e.scalar_tensor_tensor(out=out_ap, in0=q_[:, 0:ncols], scalar=gain_ap, in1=r_[:, 0:ncols],
                                                     op0=ALU.mult, op1=ALU.mult), reads=[q_, r_, gain_buf], writes=[out_buf])

    n_t1 = (n_qt * 128 + 511) // 512
    for T in range(n_t1):
        uTt = uT[T % 2]
        tcols = slice(T * 512, (T + 1) * 512)
        for s in range(4):
            i2 = (T * 4 + s) % 2
            hr_, tp_, ut_, st_ = hr[i2], tp[i2], ut[i2], st[i2]
            rows = slice(T * 512 + s * 128, T * 512 + (s + 1) * 128)
            k.dma("sp", hr_[:], d["xin"][rows, :], reads=[d["xin"]], writes=[hr_])
            k.op("act", lambda e, hr_=hr_, tp_=tp_, st_=st_: e.activation(out=tp_[:], in_=hr_[:], func=AF.Square,
                                                                         accum_out=st_[:, 0:1]), reads=[hr_], writes=[tp_, st_])
            k.op("act", lambda e, st_=st_: e.activation(out=st_[:, 1:2], in_=st_[:, 0:1], func=AF.Sqrt, bias=epsb[:],
                                                        scale=1.0 / 1024.0), reads=[st_, epsb], writes=[st_])
            k.op("dve", lambda e, st_=st_: e.reciprocal(out=st_[:, 2:3], in_=st_[:, 1:2]), reads=[st_], writes=[st_])
            k.op("dve", lambda e, hr_=hr_, tp_=tp_, st_=st_: e.scalar_tensor_tensor(
                out=tp_[:], in0=hr_[:], scalar=st_[:, 2:3], in1=m_gm[:], op0=ALU.mult, op1=ALU.mult),
                reads=[hr_, st_, m_gm], writes=[tp_])
            k.op("pool", lambda e, tp_=tp_, ut_=ut_: e.tensor_tensor(out=ut_[:], in0=tp_[:], in1=m_sh[:], op=ALU.add),
                 reads=[tp_, m_sh], writes=[ut_])
            pb, pbv = psum[6 + s % 2], pbf[6 + s % 2]
            for kc in range(8):
                tr(k, pb, pbv[:, kc * 128:(kc + 1) * 128], ut_, ut_[:, kc * 128:(kc + 1) * 128], idb, idb[:])
            k.op("act", lambda e, s=s, pbv=pbv, uTt=uTt: e.activation(
                out=uTt[:, :, s * 128:(s + 1) * 128], in_=pbv.rearrange("p (a b) -> p a b", a=8), func=AF.Copy),
                reads=[pb], writes=[uTt])
        for c in range(4):
            pb = psum[c % 2]
            for kc in range(8):
                mm(k, pb, pb[:], wsb, wsb[:, kc, c * 128:(c + 1) * 128], uTt, uTt[:, kc, :], kc == 0, kc == 7)
            if c < 2:
                k.op("act", lambda e, c=c, pb=pb, tcols=tcols: e.activation(out=sbqT[:, c, tcols], in_=pb[:], func=AF.Copy, scale=0.125),
                     reads=[pb], writes=[sbqT])
            else:
                k.op("act", lambda e, c=c, pb=pb, tcols=tcols: e.activation(out=sbkT[:, c - 2, tcols], in_=pb[:], func=AF.Copy),
                     reads=[pb], writes=[sbkT])
        for s in range(4):
            tl = T * 4 + s
            scol = slice(s * 128, (s + 1) * 128)
            pb = psum[2 + s % 2]
            for kc in range(8):
                mm(k, pb, pb[:, 0:256], uTt, uTt[:, kc, scol], wsb, wsb[:, kc, 512:768], kc == 0, kc == 7)
            k.op("dve", lambda e, tl=tl, pb=pb: e.tensor_copy(out=sbv[:, tl, :], in_=pb[:, 0:256]), reads=[pb], writes=[sbv])
            for kc in range(8):
                mm(k, pb, pb[:, 256:320], uTt, uTt[:, kc, scol], wkv, wkv[:, kc, 192:256], kc == 0, kc == 7)
            for kc in range(8):
                mm(k, pb, pb[:, 320:384], uTt, uTt[:, kc, scol], wkv, wkv[:, kc, 320:384], kc == 0, kc == 7)
            for kc in range(8):
                mm(k, pb, pb[:, 384:396], uTt, uTt[:, kc, scol], wg, wg[:, kc, :], kc == 0, kc == 7)
            k.op("dve", lambda e, tl=tl, pb=pb: e.tensor_copy(out=vsA[:, tl, 0:64], in_=pb[:, 256:320]), reads=[pb], writes=[vsA])
            k.op("dve", lambda e, tl=tl, pb=pb: e.tensor_copy(out=vwA[:, tl, 0:64], in_=pb[:, 320:384]), reads=[pb], writes=[vwA])
            k.op("act", lambda e, tl=tl, pb=pb: e.activation(out=gts[:, tl, :], in_=pb[:, 384:396], func=AF.Sigmoid),
                 reads=[pb], writes=[gts])
        def proj64(wt, col):
            pb = psum[4]
            for kc in range(8):
                mm(k, pb, pb[0:64, :], wt, wt[:, kc, col:col + 64], uTt, uTt[:, kc, :], kc == 0, kc == 7)
            return pb
        for h in range(4):
            pb = proj64(wnq, h * 64)
            norm64(pb, pb[0:64, :], qn8[:, 0:1], qn8, nqT, nqT[0:64, h, tcols], 512)
        pb = proj64(wkv, 128)
        norm64(pb, pb[0:64, :], kn[:, 1:2], kn, ksT, ksT[0:64, tcols], 512)
        pb = proj64(wkv, 256)
        norm64(pb, pb[0:64, :], kn[:, 2:3], kn, kwT, kwT[0:64, tcols], 512)
        pb = proj64(wkv, 0)
        k.op("act", lambda e, pb=pb, tcols=tcols: e.activation(out=kcT[:, tcols], in_=pb[0:64, :], func=AF.Copy), reads=[pb], writes=[kcT])
        pb = proj64(wkv, 64)
        k.op("act", lambda e, pb=pb, tcols=tcols: e.activation(out=vcT[:, tcols], in_=pb[0:64, :], func=AF.Copy), reads=[pb], writes=[vcT])

    k.release(m2)
    if True:
        w1 = k.sb("a_w1", [64, 32, 128], BF16)
        w2 = k.sb("a_w2", [128, 64], BF16)
        pe = k.sb("a_pe", [64, 32, 2], BF16)
        hb = k.sb("a_hb", [128, 2], F32)
        hsT = k.sb("a_hsT", [128, 256], BF16)
        for is_k in (True, False):
            sfx = "k" if is_k else "v"
            src = kcT if is_k else vcT
            k.dma("pool", w1[:], d["w1" + sfx][:], reads=[d["w1" + sfx]], writes=[w1])
            k.dma("pool", w2[:], d["w2" + sfx][:], reads=[d["w2" + sfx]], writes=[w2])
            k.dma("pool", pe[:], d["pe" + sfx][:], reads=[d["pe" + sfx]], writes=[pe])
            pb, pb2 = psum[0], psum[1]
            for l in range(32):
                mm(k, pb, pb[:, 0:255], w1, w1[:, l, :], src, src[0:64, l:l + 16 * 254 + 1:16], l == 0, l == 31)
            for l in range(32):
                mm(k, pb2, pb2[:, 0:2], w1, w1[:, l, :], pe, pe[:, l, :], l == 0, l == 31)
            k.op("act", lambda e, pb2=pb2: e.activation(out=hb[:], in_=pb2[:, 0:2], func=AF.Copy), reads=[pb2], writes=[hb])
            k.op("pool", lambda e: e.memset(hsT[:], 0.0), writes=[hsT])
            k.op("act", lambda e, pb=pb: e.activation(out=hsT[:, 0:255], in_=pb[:, 0:255], func=AF.Silu, bias=hb[:, 0:1],
                                                      scale=1.0), reads=[pb, hb], writes=[hsT])
            if is_k:
                pb3 = psum[4]
                mm(k, pb3, pb3[0:64, 0:256], w2, w2[:], hsT, hsT[:], True, True)
                norm64(pb3, pb3[0:64, 0:256], kn[:, 0:1], kn, kcmpT, kcmpT[0:64, :], 256)
            else:
                for c in range(2):
                    pb3 = psum[4]
                    mm(k, pb3, pb3[:, 0:64], hsT, hsT[:, c * 128:(c + 1) * 128], w2, w2[:], True, True)
                    k.op("act", lambda e, c=c, pb3=pb3: e.activation(out=vcmpA[:, c, 0:64], in_=pb3[:, 0:64], func=AF.Copy),
                         reads=[pb3], writes=[vcmpA])
    if dbg is not None:
        for nm, buf in (("sbqT", sbqT), ("sbkT", sbkT), ("sbv", sbv), ("nqT", nqT), ("ksT", ksT), ("kwT", kwT), ("kcT", kcT),
                        ("vsA", vsA), ("gts", gts), ("kcmpT", kcmpT), ("vcmpA", vcmpA)):
            if nm in dbg:
                toks_dbg.append(k.dma("sp", dbg[nm][:], buf[:], reads=[buf], writes=[dbg[nm]]))
    k.release(m1)

    cmask = k.sb("a_cmask", [128, 2, SEQ], BF16)
    Et = k.sb("a_E", [64, 32, 128], BF16)
    k.dma("sp", cmask[:], d["cmask"][:], reads=[d["cmask"]], writes=[cmask])
    k.dma("sp", Et[:], d["E"][:], reads=[d["E"]], writes=[Et])
    zer = k.sb("a_zer", [128, 128], F32)
    cneg = k.sb("a_cneg", [128, 128], BF16)
    sneg = k.sb("a_sneg", [128, 128], BF16)
    wneg = k.sb("a_wneg", [128, 128], BF16)
    triS = k.sb("a_triS", [128, 128], BF16)
    onesb = k.sb("a_onesb", [128, 128], BF16)
    k.op("pool", lambda e: e.memset(zer[:], 0.0), writes=[zer])
    k.op("pool", lambda e: e.tensor_copy(out=onesb[:], in_=ones[:]), reads=[ones], writes=[onesb])
    k.op("pool", lambda e: e.affine_select(out=cneg[:], in_=zer[:], pattern=[[1, 128]], compare_op=ALU.is_ge, fill=NEG,
                                           base=0, channel_multiplier=-1), reads=[zer], writes=[cneg])
    k.op("pool", lambda e: e.affine_select(out=sneg[:], in_=zer[:], pattern=[[1, 128]], compare_op=ALU.is_gt, fill=NEG,
                                           base=0, channel_multiplier=-1), reads=[zer], writes=[sneg])
    k.op("pool", lambda e: e.affine_select(out=wneg[:], in_=zer[:], pattern=[[-1, 128]], compare_op=ALU.is_gt, fill=NEG,
                                           base=0, channel_multiplier=1), reads=[zer], writes=[wneg])
    k.op("pool", lambda e: e.affine_select(out=triS[:], in_=ones[:], pattern=[[-1, 128]], compare_op=ALU.is_gt, fill=0.0,
                                           base=0, channel_multiplier=1), reads=[ones], writes=[triS])
    b4 = lambda ap: ap.unsqueeze(1).to_broadcast([ap.shape[0], 4, ap.shape[1]])
    zb = k.sb("a_zb", [128, 512], BF16)
    k.op("pool", lambda e: e.memset(zb[:], 0.0), writes=[zb])

    def zinit(bank, ncols):
        mm(k, bank, bank[:, 0:ncols], zb, zb[:, 0:128], zb, zb[:, 0:ncols], True, False)

    PT = [k.sb("a_PT%d" % i, [128, 512], BF16) for i in range(2)]
    oc = k.sb("a_oc", [128, 4, 65], F32)
    os_ = k.sb("a_os", [128, 4, 65], F32)
    ow = k.sb("a_ow", [128, 4, 65], F32)
    rd = k.sb("a_rd", [128, 16], F32)
    dn = k.sb("a_dn", [128, 4, 3], F32)
    psl = k.sb("a_psl", [128, 64], F32)
    sc = k.sb("a_sc", [128, 64], F32)
    sc2 = k.sb("a_sc2", [128, 64], F32)
    v8 = k.sb("a_v8", [128, 16], F32)
    nsel = k.sb("a_nsel", [128, 64], BF16)
    nselT = k.sb("a_nselT", [64, 128], BF16)
    m1t = [k.sb("a_m1t%d" % i, [128, 64], F32) for i in range(2)]
    a1t = [k.sb("a_a1t%d" % i, [128, 64], F32) for i in range(2)]
    mg = k.sb("a_mg", [128, 4, 64], F32)
    mg2 = k.sb("a_mg2", [128, 4, 64], F32)
    otile = [k.sb("a_ot%d" % i, [128, 512], BF16) for i in range(2)]
    Esb = k.sb("a_Esb", [128, 512], F32)
    SP = k.sb("a_SP", [128, 512], F32)
    SPb = k.sb("a_SPb", [128, 512], BF16)
    Rs = k.sb("a_Rs", [128, 512], F32)
    Rsb = k.sb("a_Rsb", [128, 512], BF16)
    T1 = k.sb("a_T1", [128, 512], F32)
    Wb = [k.sb("a_W%d" % i, [128, 512], BF16) for i in range(2)]
    npt = [0]

    def exp_pt(ST):
        p = PT[npt[0] % 2]
        npt[0] += 1
        k.op("act", lambda e: e.activation(out=p[:], in_=ST[:], func=AF.Exp), reads=[ST], writes=[p])
        return p

    toks = toks_dbg
    for t in range(n_qt):
        qs = slice(t * 128, (t + 1) * 128)
        ot = otile[t % 2]
        ncmp = 1 if t < 16 else 2
        k.dma("sp", m1t[t % 2][:], d["m1"][t], reads=[d["m1"]], writes=[m1t[t % 2]])
        k.dma("sp", a1t[t % 2][:], d["a1"][t], reads=[d["a1"]], writes=[a1t[t % 2]])
        pts = []
        for c in range(ncmp):
            ST = psum[c]
            full = 16 * (128 * c + 127) + 31 <= 128 * t
            mm(k, ST, ST[:], kcmpT, kcmpT[0:68, c * 128:(c + 1) * 128], nqT, nqT[0:68, :, qs], True, full)
            if not full:
                mm(k, ST, ST[:], idb, idb[:], cmask, cmask[:, c:c + 1, qs].to_broadcast([128, 4, 128]), False, True)
            pts.append(exp_pt(ST))
        outA, outB = psum[4], psum[5]
        zinit(outA, 260)
        zinit(outB, 256)
        first = False
        for h in range(4):
            for c in range(ncmp):
                p = pts[c]
                k.op("pe", lambda e, h=h, c=c, p=p, first=first: e.matmul(
                    outA[:, h * 65:(h + 1) * 65], lhsT=p[:, h * 128:(h + 1) * 128], rhs=vcmpA[:, c, 0:65],
                    start=first, stop=(h == 3 and c == ncmp - 1), skip_group_check=True), reads=[p, vcmpA], writes=[outA])
                first = False
        first = False
        for h in range(4):
            for c in range(ncmp):
                p = pts[c]
                k.op("pe", lambda e, h=h, c=c, p=p, first=first: e.matmul(
                    outB[:, h * 64:(h + 1) * 64], lhsT=p[:, h * 128:(h + 1) * 128], rhs=vcmpA[:, c, 65:129],
                    start=first, stop=(h == 3 and c == ncmp - 1), skip_group_check=True), reads=[p, vcmpA], writes=[outB])
                first = False
        k.op("act", lambda e: e.activation(out=oc[:], in_=outA[:, 0:260].rearrange("p (a b) -> p a b", a=4), func=AF.Copy),
             reads=[outA], writes=[oc])
        k.op("dve", lambda e: e.tensor_scalar(out=rd[:, 0:4], in0=oc[:, :, 64], scalar1=1e-30, scalar2=None, op0=ALU.max),
             reads=[oc], writes=[rd])
        k.op("dve", lambda e: e.reciprocal(out=rd[:, 4:8], in_=rd[:, 0:4]), reads=[rd], writes=[rd])
        k.op("dve", lambda e: e.tensor_scalar(out=psl[:], in0=outB[:, 0:64], scalar1=rd[:, 4:5], scalar2=None, op0=ALU.mult),
             reads=[outB, rd], writes=[psl])
        for h in range(1, 4):
            k.op("dve", lambda e, h=h: e.scalar_tensor_tensor(out=psl[:], in0=outB[:, h * 64:(h + 1) * 64], scalar=rd[:, 4 + h:5 + h],
                                                              in1=psl[:], op0=ALU.mult, op1=ALU.add), reads=[outB, rd, psl], writes=[psl])
        k.op("dve", lambda e, t=t: e.tensor_tensor(out=sc[:], in0=psl[:], in1=m1t[t % 2][:], op=ALU.mult),
             reads=[psl, m1t[t % 2]], writes=[sc])
        k.op("dve", lambda e, t=t: e.tensor_tensor(out=sc[:], in0=sc[:], in1=a1t[t % 2][:], op=ALU.add),
             reads=[sc, a1t[t % 2]], writes=[sc])
        k.op("dve", lambda e: e.max(out=v8[:, 0:8], in_=sc[:]), reads=[sc], writes=[v8])
        k.op("dve", lambda e: e.match_replace(out=sc2[:], in_to_replace=v8[:, 0:8], in_values=sc[:], imm_value=-3e6),
             reads=[sc, v8], writes=[sc2])
        k.op("dve", lambda e: e.max(out=v8[:, 8:16], in_=sc2[:]), reads=[sc2], writes=[v8])
        k.op("dve", lambda e: e.tensor_scalar(out=sc2[:], in0=sc[:], scalar1=v8[:, 15:16], scalar2=1.0, op0=ALU.is_ge,
                                              op1=ALU.subtract), reads=[sc, v8], writes=[sc2])
        k.op("dve", lambda e: e.tensor_scalar(out=nsel[:], in0=sc2[:], scalar1=-NEG, scalar2=None, op0=ALU.mult),
             reads=[sc2], writes=[nsel])
        if dbg is not None and "nsel" in dbg:
            toks.append(k.dma("sp", dbg["nsel"][qs, :], nsel[:], reads=[nsel], writes=[dbg["nsel"]]))
        pb, pbv = psum[2], pbf[2]
        tr(k, pb, pbv[0:64, 0:128], nsel, nsel[:], idb, idb[:])
        k.op("act", lambda e, pbv=pbv: e.activation(out=nselT[:], in_=pbv[0:64, 0:128], func=AF.Copy), reads=[pb], writes=[nselT])
        outS, outW = psum[6], psum[7]
        zinit(outS, 260)
        zinit(outW, 260)
        for s in range(t + 1):
            ST = psum[s % 2]
            ks_ = slice(s * 128, (s + 1) * 128)
            mm(k, ST, ST[:], ksT, ksT[0:68, ks_], nqT, nqT[0:68, :, qs], True, False)
            mm(k, ST, ST[:], Et, Et[:, s, :], nselT, b4(nselT[:]), False, s != t)
            if s == t:
                mm(k, ST, ST[:], idb, idb[:], cneg, b4(cneg[:]), False, True)
            p = exp_pt(ST)
            for h in range(4):
                k.op("pe", lambda e, h=h, s=s, p=p: e.matmul(
                    outS[:, h * 65:(h + 1) * 65], lhsT=p[:, h * 128:(h + 1) * 128], rhs=vsA[:, s, :],
                    start=False, stop=(s == t and h == 3), skip_group_check=True), reads=[p, vsA], writes=[outS])
        s0 = max(0, t - 4)
        for s in range(s0, t + 1):
            ST = psum[s % 2]
            ks_ = slice(s * 128, (s + 1) * 128)
            lo, hi = (s == t - 4), (s == t)
            mm(k, ST, ST[:], kwT, kwT[0:68, ks_], nqT, nqT[0:68, :, qs], True, not (lo or hi))
            if lo:
                mm(k, ST, ST[:], idb, idb[:], wneg, b4(wneg[:]), False, True)
            if hi:
                mm(k, ST, ST[:], idb, idb[:], cneg, b4(cneg[:]), False, True)
            p = exp_pt(ST)
            for h in range(4):
                k.op("pe", lambda e, h=h, s=s, p=p: e.matmul(
                    outW[:, h * 65:(h + 1) * 65], lhsT=p[:, h * 128:(h + 1) * 128], rhs=vwA[:, s, :],
                    start=False, stop=(s == t and h == 3), skip_group_check=True), reads=[p, vwA], writes=[outW])
        k.op("act", lambda e: e.activation(out=os_[:], in_=outS[:, 0:260].rearrange("p (a b) -> p a b", a=4), func=AF.Copy),
             reads=[outS], writes=[os_])
        k.op("act", lambda e: e.activation(out=ow[:], in_=outW[:, 0:260].rearrange("p (a b) -> p a b", a=4), func=AF.Copy),
             reads=[outW], writes=[ow])
        for bi, src in enumerate((oc, os_, ow)):
            k.op("dve", lambda e, bi=bi, src=src: e.tensor_scalar(out=dn[:, :, bi], in0=src[:, :, 64], scalar1=1e-30, scalar2=None,
                                                                  op0=ALU.max), reads=[src], writes=[dn])
        k.op("dve", lambda e: e.reciprocal(out=dn[:], in_=dn[:]), reads=[dn], writes=[dn])
        k.op("dve", lambda e, t=t: e.tensor_tensor(out=dn[:], in0=dn[:], in1=gts[:, t, :].rearrange("p (a b) -> p a b", a=4),
                                                   op=ALU.mult), reads=[dn, gts], writes=[dn])
        bc64 = lambda ap: ap.to_broadcast([128, 4, 64])
        k.op("dve", lambda e: e.tensor_tensor(out=mg[:], in0=oc[:, :, 0:64], in1=bc64(dn[:, :, 0:1]), op=ALU.mult),
             reads=[oc, dn], writes=[mg])
        k.op("pool", lambda e: e.tensor_tensor(out=mg2[:], in0=os_[:, :, 0:64], in1=bc64(dn[:, :, 1:2]), op=ALU.mult),
             reads=[os_, dn], writes=[mg2])
        k.op("dve", lambda e: e.tensor_tensor(out=mg[:], in0=mg[:], in1=mg2[:], op=ALU.add), reads=[mg, mg2], writes=[mg])
        k.op("pool", lambda e: e.tensor_tensor(out=mg2[:], in0=ow[:, :, 0:64], in1=bc64(dn[:, :, 2:3]), op=ALU.mult),
             reads=[ow, dn], writes=[mg2])
        k.op("dve", lambda e, ot=ot: e.tensor_tensor(out=ot[:, 256:512].rearrange("p (a b) -> p a b", a=4), in0=mg[:], in1=mg2[:],
                                                     op=ALU.add), reads=[mg, mg2], writes=[ot])
        hord = [0, 2, 1, 3]
        outSB = psum[6]
        zinit(outSB, 256)
        for s in range(t, -1, -1):
            pi = (t - s) % 2
            X0, X1 = psum[2 * pi], psum[2 * pi + 1]
            Xv = pall[:, 2 * pi * 512:(2 * pi + 2) * 512].rearrange("p (a b) -> p a b", a=2)[:, :, 0:256]
            ks_ = slice(s * 128, (s + 1) * 128)
            for hp, X in ((0, X0), (1, X1)):
                if s == t:
                    mm(k, X, X[:, 0:256], idb, idb[:], sneg, sneg[:].unsqueeze(1).to_broadcast([128, 2, 128]), True, False)
                for ci in range(2):
                    mm(k, X, X[:, ci * 128:(ci + 1) * 128], sbkT, sbkT[hp * 64:(hp + 1) * 64, ci, ks_],
                       sbqT, sbqT[hp * 64:(hp + 1) * 64, ci, qs], s != t, True)
            e3 = lambda ap: ap.rearrange("p (a b) -> p a b", a=2)
            k.op("act", lambda e, Xv=Xv: e.activation(out=e3(Esb[:]), in_=Xv, func=AF.Exp), reads=[X0, X1], writes=[Esb])
            k.op("act", lambda e: e.activation(out=SP[:], in_=Esb[:], func=AF.Ln, bias=1.0, scale=1.0), reads=[Esb], writes=[SP])
            k.op("pool", lambda e: e.tensor_copy(out=SPb[:], in_=SP[:]), reads=[SP], writes=[SPb])
            acc = psum[4 + pi]
            mm(k, acc, acc[:], triS, triS[:], SPb, SPb[:], True, s == t)
            if s != t:
                mm(k, acc, acc[:], onesb, onesb[:], Rsb, Rsb[:], False, True)
            k.op("dve", lambda e, Xv=Xv: e.tensor_tensor(out=e3(T1[:]), in0=Xv, in1=e3(SP[:]), op=ALU.subtract),
                 reads=[X0, X1, SP], writes=[T1])
            k.op("dve", lambda e, acc=acc: e.tensor_tensor(out=T1[:], in0=T1[:], in1=acc[:], op=ALU.subtract),
                 reads=[T1, acc], writes=[T1])
            W = Wb[pi]
            k.op("act", lambda e, W=W: e.activation(out=W[:], in_=T1[:], func=AF.Exp), reads=[T1], writes=[W])
            for h in range(4):
                pos = hord.index(h)
                k.op("pe", lambda e, h=h, s=s, W=W, pos=pos: e.matmul(
                    outSB[:, h * 64:(h + 1) * 64], lhsT=W[:, pos * 128:(pos + 1) * 128], rhs=sbv[:, s, h * 64:(h + 1) * 64],
                    start=False, stop=(s == 0 and h == 3), skip_group_check=True), reads=[W, sbv], writes=[outSB])
            if s > 0:
                if s == t:
                    k.op("pool", lambda e: e.tensor_copy(out=Rs[:], in_=SP[:]), reads=[SP], writes=[Rs])
                else:
                    k.op("pool", lambda e: e.tensor_tensor(out=Rs[:], in0=Rs[:], in1=SP[:], op=ALU.add), reads=[Rs, SP], writes=[Rs])
                k.op("pool", lambda e: e.tensor_copy(out=Rsb[:], in_=Rs[:]), reads=[Rs], writes=[Rsb])
        k.op("act", lambda e, ot=ot: e.activation(out=ot[:, 0:256], in_=outSB[:, 0:256], func=AF.Copy), reads=[outSB], writes=[ot])
        toks.append(k.dma("sp", o_d[qs, :], ot[:], reads=[ot], writes=[o_d]))
    k.release(m0)
    return toks


def make_psum_all(k):
    pall = k.stack.enter_context(k.nc.psum_tensor("psall", [128, 4096], F32))
    psum = [Buf(pall[:, i * 512:(i + 1) * 512], "ps%d" % i) for i in range(8)]
    pbf = [p[:].bitcast(BF16) for p in psum]
    return psum, pbf, pall


def build_a(n_qt=32, debug=False):
    nc = bass.Bass("TRN2", target_bir_lowering=False)
    with contextlib.ExitStack() as stack:
        k = KB(nc, stack)
        psum, pbf, pall = make_psum_all(k)
        d = a_dram(k)
        o_d = k.dram("o", [SEQ, 512], BF16, "ExternalOutput")
        ident = make_ident(k, "id", BF16)
        dbg = None
        if debug:
            dbg = {"nsel": k.dram("dbg_nsel", [SEQ, 64], BF16, "ExternalOutput"),
                   "sbqT": k.dram("dbg_sbqT", [128, 2, SEQ], BF16, "ExternalOutput"),
                   "sbkT": k.dram("dbg_sbkT", [128, 2, SEQ], BF16, "ExternalOutput"),
                   "sbv": k.dram("dbg_sbv", [128, 32, 256], BF16, "ExternalOutput"),
                   "nqT": k.dram("dbg_nqT", [68, 4, SEQ], BF16, "ExternalOutput"),
                   "ksT": k.dram("dbg_ksT", [68, SEQ], BF16, "ExternalOutput"),
                   "kwT": k.dram("dbg_kwT", [68, SEQ], BF16, "ExternalOutput"),
                   "kcT": k.dram("dbg_kcT", [64, SEQ], BF16, "ExternalOutput"),
                   "vsA": k.dram("dbg_vsA", [128, 32, 65], BF16, "ExternalOutput"),
                   "gts": k.dram("dbg_gts", [128, 32, 12], F32, "ExternalOutput"),
                   "kcmpT": k.dram("dbg_kcmpT", [68, 256], BF16, "ExternalOutput"),
                   "vcmpA": k.dram("dbg_vcmpA", [128, 2, 129], BF16, "ExternalOutput")}
        toks = emit_a(k, d, psum, pbf, pall, o_d, ident, "", n_qt, dbg)
        k.emit(final_waits=toks)
        print("A arena peak words", k.apeak, "instr", {e: len(v) for e, v in k.prog.items()})
    return nc


CORES = list(range(8))


def _run(nc, in_maps):
    return run_bass_kernel_spmd(nc, in_maps, core_ids=CORES).results


def kernel_unfused(**inputs):
    P = {k_: np.ascontiguousarray(np.asarray(v, dtype=np.float32)) for k_, v in inputs.items()}
    ncA = build_a(32)
    resA = _run(ncA, [a_host_inputs(c // 2, c % 2, P) for c in CORES])
    o_full = []
    for b in range(4):
        o0, o1 = np.asarray(resA[2 * b]["o"]), np.asarray(resA[2 * b + 1]["o"])
        o_full.append(np.concatenate([o0[:, :256], o1[:, :256], o0[:, 256:], o1[:, 256:]], axis=1))
    del resA
    ncB = build_bd(1024)
    shared = bd_host_shared(0, P, "")
    maps = []
    for c in CORES:
        b, hh = c // 2, c % 2
        m = dict(shared)
        m.update(bd_host_inputs(0, b, P, P["attn_w_out"][0], ""))
        m["oin"] = np.ascontiguousarray(o_full[b][hh * 2048:(hh + 1) * 2048])
        m["hres"] = np.ascontiguousarray(P["x"][b, hh * 2048:(hh + 1) * 2048])
        maps.append(m)
    resB = _run(ncB, maps)
    h0 = [np.concatenate([np.asarray(resB[2 * b]["out"]), np.asarray(resB[2 * b + 1]["out"])], axis=0) for b in range(4)]
    del resB, maps, shared
    ncC = build_c(8)
    maps = []
    for c in CORES:
        b, hh = c // 2, c % 2
        m = c_host_inputs(b, hh, P)
        m["c_hin"] = h0[b]
        maps.append(m)
    resC = _run(ncC, maps)
    yn_full = [np.concatenate([np.asarray(resC[2 * b]["yn"]), np.asarray(resC[2 * b + 1]["yn"])], axis=1) for b in range(4)]
    del resC
    ncD = build_bd(2048)
    shared = bd_host_shared(1, P, "")
    maps = []
    for c in CORES:
        b, hh = c // 2, c % 2
        m = dict(shared)
        m.update(bd_host_inputs(1, b, P, P["ssm_w_out"][0], ""))
        m["oin"] = np.ascontiguousarray(yn_full[b][hh * 2048:(hh + 1) * 2048])
        m["hres"] = np.ascontiguousarray(h0[b][hh * 2048:(hh + 1) * 2048])
        maps.append(m)
    resD = _run(ncD, maps)
    out = np.stack([np.concatenate([np.asarray(resD[2 * b]["out"]), np.asarray(resD[2 * b + 1]["out"])], axis=0)
                    for b in range(4)])
    return out.astype(np.float32)


def build_fused():
    nc = bass.Bass("TRN2", target_bir_lowering=False)
    with contextlib.ExitStack() as stack:
        k = KB(nc, stack)
        psum, pbf, pall = make_psum_all(k)
        dA = [a_dram(k, "_0"), a_dram(k, "_1")]
        dB = [bd_dram(k, 1024, "_l0", 32, False), bd_dram(k, 2048, "_l1", 32, False)]
        dC = [c_dram(k, "_0", False), c_dram(k, "_1", False)]
        out_d = k.dram("out", [NTOK, 1024], F32, "ExternalOutput")
        ridx_d = k.dram("ridx", [128, NT], mybir.dt.uint32, "ExternalInput")
        o_scr = [k.dram("o_scr%d" % i, [SEQ, 512], BF16, "Internal") for i in range(2)]
        yn_scr = [k.dram("yn_scr%d" % i, [SEQ, 1024], BF16, "Internal") for i in range(2)]
        h0_scr = k.dram("h0_scr", [SEQ, 1024], F32, "Internal")
        hpre_scr = k.dram("hpre_scr", [NTOK, 1024], F32, "Internal")
        scr = {"u": k.dram("u_scr", [NTOK, 1024], BF16, "Internal"),
               "y": k.dram("y_scr", [32 * CAP2, 1024], BF16, "Internal")}
        ident = make_ident(k, "id", BF16)
        for hh in range(2):
            emit_a(k, dA[hh], psum, pbf, pall, o_scr[hh], ident, "_%d" % hh, 32, None)
            k.new_epoch()
        for th in range(2):
            r0 = th * NTOK
            parts = [(slice(0, 256), o_scr[0], slice(0, 256), r0), (slice(256, 512), o_scr[1], slice(0, 256), r0),
                     (slice(512, 768), o_scr[0], slice(256, 512), r0), (slice(768, 1024), o_scr[1], slice(256, 512), r0)]
            emit_bd_sp2(k, 1024, dB[0], psum, pbf, h0_scr, hpre_scr, ident, scr, "_l0", 32, None, parts, (dA[0]["xin"], r0), r0)
            k.new_epoch()
        for hh in range(2):
            dC[hh]["hin"] = h0_scr
            emit_c(k, dC[hh], psum, pbf, yn_scr[hh], ident, "_%d" % hh, 8, None)
            k.new_epoch()
        parts = [(slice(0, 1024), yn_scr[0], slice(0, 1024), 0), (slice(1024, 2048), yn_scr[1], slice(0, 1024), 0)]
        toks = emit_bd_sp2(k, 2048, dB[1], psum, pbf, out_d, hpre_scr, ident, scr, "_l1", 32, None, parts, (h0_scr, 0), 0, ridx_d)
        k.emit(final_waits=toks)
        print("FUSED arena peak words", k.apeak, "instr", {e: len(v) for e, v in k.prog.items()})
    return nc


def fused_host_inputs(b, P):
    m = {}
    for hh in range(2):
        for kk, v in a_host_inputs(b, hh, P).items():
            m[kk + "_%d" % hh] = v
        for kk, v in c_host_inputs(b, hh, P).items():
            m[kk + "_%d" % hh] = v
    m.update(bd_host_inputs(0, b, P, P["attn_w_out"][0], "_l0"))
    m.update(bd_host_inputs(1, b, P, P["ssm_w_out"][0], "_l1"))
    return m


def kernel(**inputs):
    P = {k_: np.ascontiguousarray(np.asarray(v, dtype=np.float32)) for k_, v in inputs.items()}
    nc = build_fused()
    shared = {}
    shared.update(bd_host_shared(0, P, "_l0"))
    shared.update(bd_host_shared(1, P, "_l1"))
    per_b = []
    for b in range(4):
        m = dict(shared)
        m.update(fused_host_inputs(b, P))
        per_b.append(m)
    maps = []
    for c in CORES:
        m = dict(per_b[c % 4])
        th = c // 4
        m["ridx"] = np.ascontiguousarray((th * NTOK + np.arange(NTOK).reshape(NT, 128).T).astype(np.uint32))
        maps.append(m)
    res = run_bass_kernel_spmd(nc, maps, core_ids=CORES).results
    return np.stack([np.concatenate([np.asarray(res[b]["out"]), np.asarray(res[b + 4]["out"])], axis=0)
                     for b in range(4)]).astype(np.float32)


CAP = 384
NST = CAP // 128


def emit_bd_sparse(k, F, d, psum, pbf, out_d, hpre_d, ident, tag="", n_exp=32, dbg=None, oin_parts=None, hres_src=None,
                   out_row0=0):
    FC = F // 128
    if oin_parts is None:
        oin_parts = [(slice(0, F), d["oin"], slice(0, F), 0)]
    if hres_src is None:
        hres_src = (d["hres"], 0)
    pg, pu, pd, pm = psum[0:2], psum[2:4], psum[4:6], psum[6:8]
    pgb, pmb = pbf[0:2], pbf[6:8]
    idf, idb = ident
    m0 = k.mark()
    utok = [k.sb("utok%d%s" % (t, tag), [128, 1024], BF16) for t in range(NT)]
    gw = k.sb("gw" + tag, [128, NT, 32], F32)
    maskf = k.sb("maskf" + tag, [128, NT, 32], F32)
    maskb = k.sb("maskb" + tag, [128, NT, 32], BF16)
    pos_all = k.sb("pos" + tag, [128, NT, 32], F32)
    m_g2 = k.sb("m_g2" + tag, [128, 1024], F32)
    bgu = k.sb("bgu" + tag, [128, 32, 2, 8], F32)
    bgu1 = k.sb("bgu1" + tag, [128, 32, 8], F32)
    epsb = k.sb("epsb" + tag, [128, 1], F32)
    k.op("pool", lambda e: e.memset(epsb[:], EPS), writes=[epsb])
    k.dma("sp", bgu[:], d["bgu"][:], reads=[d["bgu"]], writes=[bgu])
    k.op("pool", lambda e: e.tensor_scalar(out=bgu1[:], in0=bgu[:, :, 1, :], scalar1=1.0, scalar2=None, op0=ALU.add),
         reads=[bgu], writes=[bgu1])

    m1 = k.mark()
    m_g1 = k.sb("m_g1" + tag, [128, 1024], F32)
    m_sh2 = k.sb("m_sh2" + tag, [128, 1024], F32)
    m_gm = k.sb("m_gm" + tag, [128, 1024], F32)
    gain_bc = k.sb("gainbc" + tag, [128, 1024], F32)
    stage = k.sb("adast" + tag, [128, 8, 1024], F32)
    woutb = k.sb("woutb" + tag, [128, FC, 1024], BF16)
    rwb = k.sb("rwb" + tag, [128, 8, 32], BF16)
    rb_bc = k.sb("rbbc" + tag, [128, 32], F32)
    o_tok = [k.sb("otok%d%s" % (i, tag), [128, F], BF16) for i in range(2)]
    oT = [k.sb("oT%d%s" % (i, tag), [128, FC, 128], BF16) for i in range(2)]
    hres_t = [k.sb("hrt%d%s" % (i, tag), [128, 1024], F32) for i in range(2)]
    tmp = [k.sb("tmp%d%s" % (i, tag), [128, 1024], F32) for i in range(2)]
    uTt = [k.sb("uTt%d%s" % (i, tag), [128, 8, 128], BF16) for i in range(2)]
    st = [k.sb("st%d%s" % (i, tag), [128, 64], F32) for i in range(2)]
    lg = [k.sb("lg%d%s" % (i, tag), [128, 4, 32], F32) for i in range(2)]
    triU = k.sb("triU" + tag, [128, 128], BF16)
    onesb = k.sb("onesb" + tag, [128, 128], BF16)
    onesf = k.sb("onesf" + tag, [128, 128], F32)
    k.op("pool", lambda e: e.memset(onesf[:], 1.0), writes=[onesf])
    k.op("pool", lambda e: e.tensor_copy(out=onesb[:], in_=onesf[:]), reads=[onesf], writes=[onesb])
    k.op("pool", lambda e: e.affine_select(out=triU[:], in_=onesf[:], pattern=[[1, 128]], compare_op=ALU.is_gt, fill=0.0,
                                           base=0, channel_multiplier=-1), reads=[onesf], writes=[triU])

    k.dma("pool", woutb[:], d["wout"][:], reads=[d["wout"]], writes=[woutb])
    k.dma("pool", rwb[:], d["rw"][:], reads=[d["rw"]], writes=[rwb])
    k.dma("sp", rb_bc[:], d["rb"][:].partition_broadcast(128), reads=[d["rb"]], writes=[rb_bc])
    k.dma("sp", gain_bc[:], d["gain"][:].partition_broadcast(128), reads=[d["gain"]], writes=[gain_bc])
    adaln(k, d["ccol"], d["adaw"], d["adab"], 4, [m_g1, m_sh2, m_gm, m_g2], [stage, stage], pm, tag)
    k.op("dve", lambda e: e.scalar_tensor_tensor(out=m_gm[:], in0=m_gm[:], scalar=1.0, in1=gain_bc[:],
                                                 op0=ALU.add, op1=ALU.mult), reads=[m_gm, gain_bc], writes=[m_gm])

    for t in range(NT):
        ot, oTt, hr, tp, ut, s_, l_, uT_ = o_tok[t % 2], oT[t % 2], hres_t[t % 2], tmp[t % 2], utok[t], st[t % 2], lg[t % 2], uTt[t % 2]
        rows = slice(t * 128, (t + 1) * 128)
        for (dcs, sbuf_, scs, r0) in oin_parts:
            k.dma("sp", ot[:, dcs], sbuf_[r0 + t * 128:r0 + (t + 1) * 128, scs], reads=[sbuf_], writes=[ot])
        k.dma("sp", hr[:], hres_src[0][hres_src[1] + t * 128:hres_src[1] + (t + 1) * 128, :], reads=[hres_src[0]], writes=[hr])
        for g in range(FC // 8):
            pb, pbv = pg[g % 2], pgb[g % 2]
            for c in range(8):
                fc = g * 8 + c
                tr(k, pb, pbv[:, c * 128:(c + 1) * 128], ot, ot[:, fc * 128:(fc + 1) * 128], idb, idb[:])
            k.op("act", lambda e, g=g, pbv=pbv, oTt=oTt: e.activation(
                out=oTt[:, g * 8:(g + 1) * 8, :], in_=pbv.rearrange("p (a b) -> p a b", a=8), func=AF.Copy),
                reads=[pb], writes=[oTt])
        for half in range(2):
            hs = slice(half * 512, (half + 1) * 512)
            for fc in range(FC):
                mm(k, pd[half], pd[half][:], oTt, oTt[:, fc, :], woutb, woutb[:, fc, hs], fc == 0, fc == FC - 1)
            k.op("dve", lambda e, half=half, hs=hs, tp=tp: e.tensor_tensor(
                out=tp[:, hs], in0=pd[half][:], in1=m_g1[:, hs], op=ALU.mult), reads=[pd[half], m_g1], writes=[tp])
        k.op("pool", lambda e, hr=hr, tp=tp: e.tensor_tensor(out=hr[:], in0=tp[:], in1=hr[:], op=ALU.add),
             reads=[tp, hr], writes=[hr])
        k.dma("sp", hpre_d[rows, :], hr[:], reads=[hr], writes=[hpre_d])
        k.op("act", lambda e, hr=hr, tp=tp, s_=s_: e.activation(out=tp[:], in_=hr[:], func=AF.Square,
                                                                 accum_out=s_[:, 0:1]), reads=[hr], writes=[tp, s_])
        k.op("act", lambda e, s_=s_: e.activation(out=s_[:, 1:2], in_=s_[:, 0:1], func=AF.Sqrt, bias=epsb[:],
                                                   scale=1.0 / 1024.0), reads=[s_, epsb], writes=[s_])
        k.op("dve", lambda e, s_=s_: e.reciprocal(out=s_[:, 2:3], in_=s_[:, 1:2]), reads=[s_], writes=[s_])
        k.op("dve", lambda e, hr=hr, tp=tp, s_=s_: e.scalar_tensor_tensor(
            out=tp[:], in0=hr[:], scalar=s_[:, 2:3], in1=m_gm[:], op0=ALU.mult, op1=ALU.mult),
            reads=[hr, s_, m_gm], writes=[tp])
        k.op("pool", lambda e, tp=tp, ut=ut: e.tensor_tensor(out=ut[:], in0=tp[:], in1=m_sh2[:], op=ALU.add),
             reads=[tp, m_sh2], writes=[ut])
        for kc in range(8):
            tr(k, pm[0], pmb[0][:, kc * 128:(kc + 1) * 128], ut, ut[:, kc * 128:(kc + 1) * 128], idb, idb[:])
        k.op("act", lambda e, uT_=uT_: e.activation(out=uT_[:], in_=pmb[0].rearrange("p (a b) -> p a b", a=8), func=AF.Copy),
             reads=[pm[0]], writes=[uT_])
        for kc in range(8):
            mm(k, pm[1], pm[1][:, 0:32], uT_, uT_[:, kc, :], rwb, rwb[:, kc, :], kc == 0, kc == 7)
        k.op("dve", lambda e, l_=l_: e.tensor_tensor(out=l_[:, 0, :], in0=pm[1][:, 0:32], in1=rb_bc[:], op=ALU.add),
             reads=[pm[1], rb_bc], writes=[l_])
        k.op("dve", lambda e, l_=l_, s_=s_: e.max(out=s_[:, 8:16], in_=l_[:, 0, :]), reads=[l_], writes=[s_])
        k.op("dve", lambda e, l_=l_, s_=s_, t=t: e.tensor_scalar(out=maskf[:, t, :], in0=l_[:, 0, :], scalar1=s_[:, 11:12],
                                                                 scalar2=None, op0=ALU.is_ge), reads=[l_, s_], writes=[maskf])
        k.op("pool", lambda e, t=t: e.tensor_copy(out=maskb[:, t, :], in_=maskf[:, t, :]), reads=[maskf], writes=[maskb])
        k.op("dve", lambda e, s_=s_: e.tensor_scalar(out=s_[:, 16:17], in0=s_[:, 8:9], scalar1=-1.0, scalar2=None,
                                                     op0=ALU.mult), reads=[s_], writes=[s_])
        k.op("act", lambda e, l_=l_, s_=s_: e.activation(out=l_[:, 2, :], in_=l_[:, 0, :], func=AF.Exp,
                                                          bias=s_[:, 16:17], scale=1.0), reads=[l_, s_], writes=[l_])
        k.op("dve", lambda e, l_=l_, s_=s_, t=t: e.scalar_tensor_tensor(
            out=l_[:, 3, :], in0=l_[:, 2, :], scalar=1.0, in1=maskf[:, t, :], op0=ALU.mult, op1=ALU.mult,
            accum_out=s_[:, 17:18]), reads=[l_, maskf], writes=[l_, s_])
        k.op("dve", lambda e, s_=s_: e.reciprocal(out=s_[:, 18:19], in_=s_[:, 17:18]), reads=[s_], writes=[s_])
        k.op("dve", lambda e, l_=l_, s_=s_, t=t: e.tensor_scalar(out=gw[:, t, :], in0=l_[:, 3, :], scalar1=s_[:, 18:19],
                                                                 scalar2=None, op0=ALU.mult), reads=[l_, s_], writes=[gw])
    for t in range(NT):
        pb = pm[t % 2]
        mm(k, pb, pb[:, 0:32], triU, triU[:], maskb, maskb[:, t, :], True, t == 0)
        for t2_ in range(t):
            mm(k, pb, pb[:, 0:32], onesb, onesb[:], maskb, maskb[:, t2_, :], False, t2_ == t - 1)
        k.op("act", lambda e, t=t, pb=pb: e.activation(out=pos_all[:, t, :], in_=pb[:, 0:32], func=AF.Copy),
             reads=[pb], writes=[pos_all])

    toks = []
    if dbg is not None:
        toks.append(k.dma("sp", dbg["gw"][:], gw[:], reads=[gw], writes=[dbg["gw"]]))
        toks.append(k.dma("sp", dbg["pos"][:], pos_all[:], reads=[pos_all], writes=[dbg["pos"]]))
    k.release(m1)
    acc = [k.sb("acc%d%s" % (t, tag), [128, 1024], F32) for t in range(NT)]
    for t in range(NT):
        k.op("pool", lambda e, t=t: e.memset(acc[t][:], 0.0), writes=[acc[t]])
    m2 = k.mark()
    iota_i = k.sb("iota_i" + tag, [128, CAP], mybir.dt.int32)
    iota_f = k.sb("iota_f" + tag, [128, CAP], F32)
    k.op("pool", lambda e: e.iota(iota_i[:], pattern=[[1, CAP]], base=0, channel_multiplier=0), writes=[iota_i])
    k.op("pool", lambda e: e.tensor_copy(out=iota_f[:], in_=iota_i[:]), reads=[iota_i], writes=[iota_f])
    Pm = [k.sb("Pm%d%s" % (t, tag), [128, CAP], BF16) for t in range(NT)]
    PmW = [k.sb("PmW%d%s" % (i, tag), [128, CAP], BF16) for i in range(2)]
    PmT = [k.sb("PmT%d%s" % (s_, tag), [128, NTOK], BF16) for s_ in range(NST)]
    XgT = k.sb("XgT" + tag, [128, 8, CAP], BF16)
    actT = [k.sb("actT%d%s" % (j, tag), [128, CAP], BF16) for j in range(8)]
    Y = [k.sb("Y%d%s" % (s_, tag), [128, 1024], BF16) for s_ in range(NST)]
    bdb = [k.sb("bdb%d%s" % (i, tag), [128, 1024], F32) for i in range(2)]
    NR = 3
    wring = [k.sb("wgur%d%s" % (i, tag), [128, 8, 2, 128], BF16) for i in range(NR)]
    ND = 2
    dring = [k.sb("wdr%d%s" % (i, tag), [128, 8, 512], BF16) for i in range(ND)]
    g_sb = k.sb("g_sb" + tag, [128, CAP], F32)
    s_sb = k.sb("s_sb" + tag, [128, CAP], F32)
    t1 = k.sb("t1" + tag, [128, CAP], F32)
    t2 = k.sb("t2" + tag, [128, CAP], F32)
    m_sb = k.sb("m_sb" + tag, [128, CAP], F32)

    units = [(e, j) for e in range(n_exp) for j in range(8)]
    dunits = [(e, h) for e in range(n_exp) for h in range(2)]

    def load_unit(i):
        if i < len(units):
            e, j = units[i]
            k.dma("pool", wring[i % NR][:], d["wgu"][e, j], reads=[d["wgu"]], writes=[wring[i % NR]])

    def load_dunit(i):
        if i < len(dunits):
            e, h = dunits[i]
            k.dma("pool", dring[i % ND][:], d["wd"][e, :, :, h * 512:(h + 1) * 512], reads=[d["wd"]],
                  writes=[dring[i % ND]])

    def build_P(e_):
        for t in range(NT):
            k.op("dve", lambda e, t=t, e_=e_: e.tensor_scalar(
                out=Pm[t][:], in0=iota_f[:], scalar1=pos_all[:, t, e_:e_ + 1], scalar2=maskf[:, t, e_:e_ + 1],
                op0=ALU.is_equal, op1=ALU.mult), reads=[iota_f, pos_all, maskf], writes=[Pm[t]])

    def build_PT(e_):
        for t in range(NT):
            pw = PmW[t % 2]
            k.op("pool", lambda e, t=t, e_=e_, pw=pw: e.tensor_scalar(
                out=pw[:], in0=Pm[t][:], scalar1=gw[:, t, e_:e_ + 1], scalar2=None, op0=ALU.mult),
                reads=[Pm[t], gw], writes=[pw])
            pb, pbv = psum[6 + t % 2], pbf[6 + t % 2]
            for s_ in range(NST):
                tr(k, pb, pbv[:, s_ * 128:(s_ + 1) * 128], pw, pw[:, s_ * 128:(s_ + 1) * 128], idb, idb[:])
            for s_ in range(NST):
                k.op("act", lambda e, t=t, s_=s_, pbv=pbv: e.activation(
                    out=PmT[s_][:, t * 128:(t + 1) * 128], in_=pbv[:, s_ * 128:(s_ + 1) * 128], func=AF.Copy),
                    reads=[pb], writes=[PmT[s_]])

    for i in range(NR - 1):
        load_unit(i)
    for i in range(ND - 1):
        load_dunit(i)
    build_P(0)
    cnt = 0
    for e_ in range(n_exp):
        bdt = bdb[e_ % 2]
        k.dma("sp", bdt[:], d["bd"][e_].partition_broadcast(128), reads=[d["bd"]], writes=[bdt])
        for grp in range(2):
            for kk in range(4):
                kc = grp * 4 + kk
                pb = psum[kk]
                for t in range(NT):
                    mm(k, pb, pb[:, 0:CAP], utok[t], utok[t][:, kc * 128:(kc + 1) * 128], Pm[t], Pm[t][:], t == 0, t == NT - 1)
            for kk in range(4):
                kc = grp * 4 + kk
                pb = psum[kk]
                k.op("act", lambda e, kc=kc, pb=pb: e.activation(out=XgT[:, kc, :], in_=pb[:, 0:CAP], func=AF.Copy),
                     reads=[pb], writes=[XgT])
        build_PT(e_)
        for j in range(8):
            ui = e_ * 8 + j
            load_unit(ui + NR - 1)
            w = wring[ui % NR]
            x = cnt % 2
            cnt += 1
            pgx, pux = psum[4 + 2 * x], psum[5 + 2 * x]
            for kc in range(8):
                mm(k, pgx, pgx[:, 0:CAP], w, w[:, kc, 0, :], XgT, XgT[:, kc, :], kc == 0, kc == 7)
            for kc in range(8):
                mm(k, pux, pux[:, 0:CAP], w, w[:, kc, 1, :], XgT, XgT[:, kc, :], kc == 0, kc == 7)
            k.op("dve", lambda e, pgx=pgx, e_=e_, j=j: e.tensor_scalar(
                out=g_sb[:], in0=pgx[:, 0:CAP], scalar1=bgu[:, e_, 0, j:j + 1], scalar2=SWIGLU_LIMIT,
                op0=ALU.add, op1=ALU.min), reads=[pgx, bgu], writes=[g_sb])
            k.op("act", lambda e: e.activation(out=s_sb[:], in_=g_sb[:], func=AF.Sigmoid, scale=SWIGLU_ALPHA),
                 reads=[g_sb], writes=[s_sb])
            k.op("act", lambda e, pux=pux, e_=e_, j=j: e.activation(
                out=t1[:], in_=pux[:, 0:CAP], func=AF.Identity, bias=bgu1[:, e_, j:j + 1], scale=1.0),
                reads=[pux, bgu1], writes=[t1])
            k.op("pool", lambda e: e.tensor_scalar(out=t2[:], in0=t1[:], scalar1=SWIGLU_LIMIT + 1.0,
                                                   scalar2=1.0 - SWIGLU_LIMIT, op0=ALU.min, op1=ALU.max),
                 reads=[t1], writes=[t2])
            k.op("dve", lambda e: e.tensor_tensor(out=m_sb[:], in0=g_sb[:], in1=s_sb[:], op=ALU.mult),
                 reads=[g_sb, s_sb], writes=[m_sb])
            k.op("pool", lambda e, j=j: e.tensor_tensor(out=actT[j][:], in0=m_sb[:], in1=t2[:], op=ALU.mult),
                 reads=[m_sb, t2], writes=[actT[j]])
        for half in range(2):
            di = e_ * 2 + half
            load_dunit(di + ND - 1)
            wdv = dring[di % ND]
            hs = slice(half * 512, (half + 1) * 512)
            for s_ in range(NST):
                pb = psum[s_ % 2]
                for fc in range(8):
                    mm(k, pb, pb[:], actT[fc], actT[fc][:, s_ * 128:(s_ + 1) * 128], wdv, wdv[:, fc, :], fc == 0, fc == 7)
                k.op("dve", lambda e, s_=s_, hs=hs, pb=pb, bdt=bdt: e.tensor_tensor(
                    out=Y[s_][:, hs], in0=pb[:], in1=bdt[:, hs], op=ALU.add), reads=[pb, bdt], writes=[Y[s_]])
        if e_ + 1 < n_exp:
            build_P(e_ + 1)
        for t in range(NT):
            for half in range(2):
                hs = slice(half * 512, (half + 1) * 512)
                pb = psum[2 + (2 * t + half) % 2]
                for s_ in range(NST):
                    mm(k, pb, pb[:], PmT[s_], PmT[s_][:, t * 128:(t + 1) * 128], Y[s_], Y[s_][:, hs], s_ == 0, s_ == NST - 1)
                k.op("dve", lambda e, t=t, hs=hs, pb=pb: e.tensor_tensor(
                    out=acc[t][:, hs], in0=pb[:], in1=acc[t][:, hs], op=ALU.add), reads=[pb, acc[t]], writes=[acc[t]])

    k.release(m2)
    hp = [k.sb("hp%d%s" % (i, tag), [128, 1024], F32) for i in range(2)]
    for t in range(NT):
        rows = slice(t * 128, (t + 1) * 128)
        h_ = hp[t % 2]
        k.dma("sp", h_[:], hpre_d[rows, :], reads=[hpre_d], writes=[h_])
        k.op("dve", lambda e, t=t: e.tensor_tensor(out=acc[t][:], in0=acc[t][:], in1=m_g2[:], op=ALU.mult),
             reads=[acc[t], m_g2], writes=[acc[t]])
        k.op("pool", lambda e, t=t, h_=h_: e.tensor_tensor(out=h_[:], in0=acc[t][:], in1=h_[:], op=ALU.add),
             reads=[acc[t], h_], writes=[h_])
        toks.append(k.dma("sp", out_d[out_row0 + t * 128:out_row0 + (t + 1) * 128, :], h_[:], reads=[h_], writes=[out_d]))
    k.release(m0)
    return toks


def build_bd_sparse(F, n_exp=32, debug=False):
    nc = bass.Bass("TRN2", target_bir_lowering=False)
    with contextlib.ExitStack() as stack:
        k = KB(nc, stack)
        psum, pbf, pall = make_psum_all(k)
        d = bd_dram(k, F, "", n_exp)
        out_d = k.dram("out", [NTOK, 1024], F32, "ExternalOutput")
        hpre_d = k.dram("hpre_scr", [NTOK, 1024], F32, "ExternalOutput" if debug else "Internal")
        ident = make_ident(k, "id", BF16)
        dbg = None
        if debug:
            dbg = {"gw": k.dram("dbg_gw", [128, NT, 32], F32, "ExternalOutput"),
                   "pos": k.dram("dbg_pos", [128, NT, 32], F32, "ExternalOutput")}
        toks = emit_bd_sparse(k, F, d, psum, pbf, out_d, hpre_d, ident, "", n_exp, dbg)
        k.emit(final_waits=toks)
        print("BDS arena peak words", k.apeak, "instr", {e: len(v) for e, v in k.prog.items()})
    return nc


CAP2 = 1024
NST2 = CAP2 // 128
U32 = mybir.dt.uint32
I32 = mybir.dt.int32


def emit_bd_sp2(k, F, d, psum, pbf, out_d, hpre_d, ident, scr, tag="", n_exp=32, dbg=None, oin_parts=None, hres_src=None,
                out_row0=0, ridx_d=None):
    FC = F // 128
    if oin_parts is None:
        oin_parts = [(slice(0, F), d["oin"], slice(0, F), 0)]
    if hres_src is None:
        hres_src = (d["hres"], 0)
    pg, pu, pd, pm = psum[0:2], psum[2:4], psum[4:6], psum[6:8]
    pgb, pmb = pbf[0:2], pbf[6:8]
    idf, idb = ident
    u_scr, y_scr = scr["u"], scr["y"]
    m0 = k.mark()
    gw = k.sb("gw" + tag, [128, NT, 32], F32)
    maskf = k.sb("maskf" + tag, [128, NT, 32], F32)
    maskb = k.sb("maskb" + tag, [128, NT, 32], BF16)
    pos_all = k.sb("pos" + tag, [128, NT, 32], F32)
    lgs = k.sb("lgs" + tag, [128, NT, 32], F32)
    tv = k.sb("tv" + tag, [128, NT, 8], F32)
    rt = k.sb("rt" + tag, [128, NT, 16], F32)
    rt_u = k.sb("rtu" + tag, [128, NT, 4], U32)
    m_g2 = k.sb("m_g2" + tag, [128, 1024], F32)
    bgu = k.sb("bgu" + tag, [128, 32, 2, 8], F32)
    bgu1 = k.sb("bgu1" + tag, [128, 32, 8], F32)
    epsb = k.sb("epsb" + tag, [128, 1], F32)
    k.op("pool", lambda e: e.memset(epsb[:], EPS), writes=[epsb])
    ridx = None
    if ridx_d is not None:
        ridx = k.sb("ridx" + tag, [128, NT], U32)
        k.dma("sp", ridx[:], ridx_d[:], reads=[ridx_d], writes=[ridx])
    k.dma("sp", bgu[:], d["bgu"][:], reads=[d["bgu"]], writes=[bgu])
    k.op("pool", lambda e: e.tensor_scalar(out=bgu1[:], in0=bgu[:, :, 1, :], scalar1=1.0, scalar2=None, op0=ALU.add),
         reads=[bgu], writes=[bgu1])

    m1 = k.mark()
    m_g1 = k.sb("m_g1" + tag, [128, 1024], F32)
    m_sh2 = k.sb("m_sh2" + tag, [128, 1024], F32)
    m_gm = k.sb("m_gm" + tag, [128, 1024], F32)
    gain_bc = k.sb("gainbc" + tag, [128, 1024], F32)
    stage = k.sb("adast" + tag, [128, 8, 1024], F32)
    woutb = k.sb("woutb" + tag, [128, FC, 1024], BF16)
    rwb = k.sb("rwb" + tag, [128, 8, 32], BF16)
    rb_bc = k.sb("rbbc" + tag, [128, 32], F32)
    o_tok = [k.sb("otok%d%s" % (i, tag), [128, F], BF16) for i in range(2)]
    oT = [k.sb("oT%d%s" % (i, tag), [128, FC, 128], BF16) for i in range(2)]
    hres_t = [k.sb("hrt%d%s" % (i, tag), [128, 1024], F32) for i in range(2)]
    tmp = [k.sb("tmp%d%s" % (i, tag), [128, 1024], F32) for i in range(2)]
    utk = [k.sb("utk%d%s" % (i, tag), [128, 1024], BF16) for i in range(2)]
    uTt = [k.sb("uTt%d%s" % (i, tag), [128, 8, 128], BF16) for i in range(2)]
    st = [k.sb("st%d%s" % (i, tag), [128, 64], F32) for i in range(2)]
    lg = [k.sb("lg%d%s" % (i, tag), [128, 4, 32], F32) for i in range(2)]
    triU = k.sb("triU" + tag, [128, 128], BF16)
    onesb = k.sb("onesb" + tag, [128, 128], BF16)
    onesf = k.sb("onesf" + tag, [128, 128], F32)
    ecap_i = k.sb("ecapi" + tag, [128, 32], I32)
    ecap = k.sb("ecap" + tag, [128, 32], F32)
    k.op("pool", lambda e: e.memset(onesf[:], 1.0), writes=[onesf])
    k.op("pool", lambda e: e.tensor_copy(out=onesb[:], in_=onesf[:]), reads=[onesf], writes=[onesb])
    k.op("pool", lambda e: e.affine_select(out=triU[:], in_=onesf[:], pattern=[[1, 128]], compare_op=ALU.is_gt, fill=0.0,
                                           base=0, channel_multiplier=-1), reads=[onesf], writes=[triU])
    k.op("pool", lambda e: e.iota(ecap_i[:], pattern=[[CAP2, 32]], base=0, channel_multiplier=0), writes=[ecap_i])
    k.op("pool", lambda e: e.tensor_copy(out=ecap[:], in_=ecap_i[:]), reads=[ecap_i], writes=[ecap])

    k.dma("pool", woutb[:], d["wout"][:], reads=[d["wout"]], writes=[woutb])
    k.dma("pool", rwb[:], d["rw"][:], reads=[d["rw"]], writes=[rwb])
    k.dma("sp", rb_bc[:], d["rb"][:].partition_broadcast(128), reads=[d["rb"]], writes=[rb_bc])
    k.dma("sp", gain_bc[:], d["gain"][:].partition_broadcast(128), reads=[d["gain"]], writes=[gain_bc])
    adaln(k, d["ccol"], d["adaw"], d["adab"], 4, [m_g1, m_sh2, m_gm, m_g2], [stage, stage], pm, tag)
    k.op("dve", lambda e: e.scalar_tensor_tensor(out=m_gm[:], in0=m_gm[:], scalar=1.0, in1=gain_bc[:],
                                                 op0=ALU.add, op1=ALU.mult), reads=[m_gm, gain_bc], writes=[m_gm])

    for t in range(NT):
        ot, oTt, hr, tp, ut, s_, l_, uT_ = o_tok[t % 2], oT[t % 2], hres_t[t % 2], tmp[t % 2], utk[t % 2], st[t % 2], lg[t % 2], uTt[t % 2]
        rows = slice(t * 128, (t + 1) * 128)
        if ridx is None:
            for (dcs, sbuf_, scs, r0) in oin_parts:
                k.dma("sp", ot[:, dcs], sbuf_[r0 + t * 128:r0 + (t + 1) * 128, scs], reads=[sbuf_], writes=[ot])
            k.dma("sp", hr[:], hres_src[0][hres_src[1] + t * 128:hres_src[1] + (t + 1) * 128, :], reads=[hres_src[0]], writes=[hr])
        else:
            for (dcs, sbuf_, scs, r0) in oin_parts:
                k.cc("pool", lambda e, ot=ot, dcs=dcs, sbuf_=sbuf_, t=t: e.indirect_dma_start(
                    out=ot[:, dcs], out_offset=None, in_=sbuf_[:, :],
                    in_offset=bass.IndirectOffsetOnAxis(ap=ridx[:, t:t + 1], axis=0)), reads=[sbuf_, ridx], writes=[ot])
            k.cc("pool", lambda e, hr=hr, t=t: e.indirect_dma_start(
                out=hr[:], out_offset=None, in_=hres_src[0][:, :],
                in_offset=bass.IndirectOffsetOnAxis(ap=ridx[:, t:t + 1], axis=0)), reads=[hres_src[0], ridx], writes=[hr])
        for g in range(FC // 8):
            pb, pbv = pg[g % 2], pgb[g % 2]
            for c in range(8):
                fc = g * 8 + c
                tr(k, pb, pbv[:, c * 128:(c + 1) * 128], ot, ot[:, fc * 128:(fc + 1) * 128], idb, idb[:])
            k.op("act", lambda e, g=g, pbv=pbv, oTt=oTt: e.activation(
                out=oTt[:, g * 8:(g + 1) * 8, :], in_=pbv.rearrange("p (a b) -> p a b", a=8), func=AF.Copy),
                reads=[pb], writes=[oTt])
        for half in range(2):
            hs = slice(half * 512, (half + 1) * 512)
            for fc in range(FC):
                mm(k, pd[half], pd[half][:], oTt, oTt[:, fc, :], woutb, woutb[:, fc, hs], fc == 0, fc == FC - 1)
            k.op("dve", lambda e, half=half, hs=hs, tp=tp: e.tensor_tensor(
                out=tp[:, hs], in0=pd[half][:], in1=m_g1[:, hs], op=ALU.mult), reads=[pd[half], m_g1], writes=[tp])
        k.op("pool", lambda e, hr=hr, tp=tp: e.tensor_tensor(out=hr[:], in0=tp[:], in1=hr[:], op=ALU.add),
             reads=[tp, hr], writes=[hr])
        k.dma("sp", hpre_d[rows, :], hr[:], reads=[hr], writes=[hpre_d])
        k.op("act", lambda e, hr=hr, tp=tp, s_=s_: e.activation(out=tp[:], in_=hr[:], func=AF.Square,
                                                                 accum_out=s_[:, 0:1]), reads=[hr], writes=[tp, s_])
        k.op("act", lambda e, s_=s_: e.activation(out=s_[:, 1:2], in_=s_[:, 0:1], func=AF.Sqrt, bias=epsb[:],
                                                   scale=1.0 / 1024.0), reads=[s_, epsb], writes=[s_])
        k.op("dve", lambda e, s_=s_: e.reciprocal(out=s_[:, 2:3], in_=s_[:, 1:2]), reads=[s_], writes=[s_])
        k.op("dve", lambda e, hr=hr, tp=tp, s_=s_: e.scalar_tensor_tensor(
            out=tp[:], in0=hr[:], scalar=s_[:, 2:3], in1=m_gm[:], op0=ALU.mult, op1=ALU.mult),
            reads=[hr, s_, m_gm], writes=[tp])
        k.op("pool", lambda e, tp=tp, ut=ut: e.tensor_tensor(out=ut[:], in0=tp[:], in1=m_sh2[:], op=ALU.add),
             reads=[tp, m_sh2], writes=[ut])
        k.dma("sp", u_scr[rows, :], ut[:], reads=[ut], writes=[u_scr])
        for kc in range(8):
            tr(k, pm[0], pmb[0][:, kc * 128:(kc + 1) * 128], ut, ut[:, kc * 128:(kc + 1) * 128], idb, idb[:])
        k.op("act", lambda e, uT_=uT_: e.activation(out=uT_[:], in_=pmb[0].rearrange("p (a b) -> p a b", a=8), func=AF.Copy),
             reads=[pm[0]], writes=[uT_])
        for kc in range(8):
            mm(k, pm[1], pm[1][:, 0:32], uT_, uT_[:, kc, :], rwb, rwb[:, kc, :], kc == 0, kc == 7)
        k.op("dve", lambda e, t=t: e.tensor_tensor(out=lgs[:, t, :], in0=pm[1][:, 0:32], in1=rb_bc[:], op=ALU.add),
             reads=[pm[1], rb_bc], writes=[lgs])
        k.op("dve", lambda e, t=t: e.max(out=tv[:, t, :], in_=lgs[:, t, :]), reads=[lgs], writes=[tv])
        k.op("dve", lambda e, t=t: e.tensor_scalar(out=maskf[:, t, :], in0=lgs[:, t, :], scalar1=tv[:, t, 3:4],
                                                   scalar2=None, op0=ALU.is_ge), reads=[lgs, tv], writes=[maskf])
        k.op("pool", lambda e, t=t: e.tensor_copy(out=maskb[:, t, :], in_=maskf[:, t, :]), reads=[maskf], writes=[maskb])
        k.op("dve", lambda e, s_=s_, t=t: e.tensor_scalar(out=s_[:, 16:17], in0=tv[:, t, 0:1], scalar1=-1.0, scalar2=None,
                                                          op0=ALU.mult), reads=[tv], writes=[s_])
        k.op("act", lambda e, l_=l_, s_=s_, t=t: e.activation(out=l_[:, 2, :], in_=lgs[:, t, :], func=AF.Exp,
                                                               bias=s_[:, 16:17], scale=1.0), reads=[lgs, s_], writes=[l_])
        k.op("dve", lambda e, l_=l_, s_=s_, t=t: e.scalar_tensor_tensor(
            out=l_[:, 3, :], in0=l_[:, 2, :], scalar=1.0, in1=maskf[:, t, :], op0=ALU.mult, op1=ALU.mult,
            accum_out=s_[:, 17:18]), reads=[l_, maskf], writes=[l_, s_])
        k.op("dve", lambda e, s_=s_: e.reciprocal(out=s_[:, 18:19], in_=s_[:, 17:18]), reads=[s_], writes=[s_])
        k.op("dve", lambda e, l_=l_, s_=s_, t=t: e.tensor_scalar(out=gw[:, t, :], in0=l_[:, 3, :], scalar1=s_[:, 18:19],
                                                                 scalar2=None, op0=ALU.mult), reads=[l_, s_], writes=[gw])
    for t in range(NT):
        pb = pm[t % 2]
        mm(k, pb, pb[:, 0:32], triU, triU[:], maskb, maskb[:, t, :], True, t == 0)
        for t2_ in range(t):
            mm(k, pb, pb[:, 0:32], onesb, onesb[:], maskb, maskb[:, t2_, :], False, t2_ == t - 1)
        k.op("act", lambda e, t=t, pb=pb: e.activation(out=pos_all[:, t, :], in_=pb[:, 0:32], func=AF.Copy),
             reads=[pb], writes=[pos_all])
    ohs = lg[0]
    for t in range(NT):
        for kk in range(4):
            k.op("dve", lambda e, t=t, kk=kk: e.tensor_scalar(out=ohs[:, 0, :], in0=lgs[:, t, :], scalar1=tv[:, t, kk:kk + 1],
                                                              scalar2=None, op0=ALU.is_equal), reads=[lgs, tv], writes=[ohs])
            for col, src in ((0, pos_all[:, t, :]), (4, ecap[:]), (8, gw[:, t, :])):
                k.op("dve", lambda e, t=t, kk=kk, col=col, src=src: e.scalar_tensor_tensor(
                    out=ohs[:, 1, :], in0=ohs[:, 0, :], scalar=1.0, in1=src, op0=ALU.mult, op1=ALU.mult,
                    accum_out=rt[:, t, col + kk:col + kk + 1]), reads=[ohs, pos_all, ecap, gw], writes=[ohs, rt])
    k.op("dve", lambda e: e.tensor_scalar(out=rt[:, :, 12:16], in0=rt[:, :, 0:4], scalar1=float(CAP2) - 0.5, scalar2=None,
                                          op0=ALU.is_lt), reads=[rt], writes=[rt])
    k.op("dve", lambda e: e.tensor_tensor(out=rt[:, :, 8:12], in0=rt[:, :, 8:12], in1=rt[:, :, 12:16], op=ALU.mult),
         reads=[rt], writes=[rt])
    k.op("dve", lambda e: e.tensor_scalar(out=rt[:, :, 12:16], in0=rt[:, :, 0:4], scalar1=float(CAP2 - 1), scalar2=None,
                                          op0=ALU.min), reads=[rt], writes=[rt])
    k.op("dve", lambda e: e.tensor_tensor(out=rt[:, :, 12:16], in0=rt[:, :, 12:16], in1=rt[:, :, 4:8], op=ALU.add),
         reads=[rt], writes=[rt])
    k.op("dve", lambda e: e.tensor_copy(out=rt_u[:], in_=rt[:, :, 12:16]), reads=[rt], writes=[rt_u])

    toks = []
    if dbg is not None:
        toks.append(k.dma("sp", dbg["gw"][:], gw[:], reads=[gw], writes=[dbg["gw"]]))
        toks.append(k.dma("sp", dbg["pos"][:], pos_all[:], reads=[pos_all], writes=[dbg["pos"]]))
        toks.append(k.dma("sp", dbg["rt"][:], rt[:], reads=[rt], writes=[dbg["rt"]]))
    k.release(m1)
    m2 = k.mark()
    iota_i = k.sb("iota_i" + tag, [128, CAP2], I32)
    iota_f = k.sb("iota_f" + tag, [128, CAP2], F32)
    tokc_i = k.sb("tokci" + tag, [128, NT, 2], I32)
    tokc = k.sb("tokc" + tag, [128, NT, 2], BF16)
    k.op("pool", lambda e: e.iota(iota_i[:], pattern=[[1, CAP2]], base=0, channel_multiplier=0), writes=[iota_i])
    k.op("pool", lambda e: e.tensor_copy(out=iota_f[:], in_=iota_i[:]), reads=[iota_i], writes=[iota_f])
    k.op("pool", lambda e: e.iota(tokc_i[:, :, 0], pattern=[[0, NT]], base=0, channel_multiplier=1), writes=[tokc_i])
    k.op("pool", lambda e: e.iota(tokc_i[:, :, 1], pattern=[[1, NT]], base=0, channel_multiplier=0), writes=[tokc_i])
    k.op("pool", lambda e: e.tensor_copy(out=tokc[:], in_=tokc_i[:]), reads=[tokc_i], writes=[tokc])
    Pm = [k.sb("Pm%d%s" % (t, tag), [128, CAP2], BF16) for t in range(NT)]
    sidx_f = k.sb("sidxf" + tag, [128, NST2, 2], F32)
    sidx = [k.sb("sidx%d%s" % (i, tag), [128, NST2], U32) for i in range(2)]
    sidx_t = k.sb("sidxt" + tag, [128, NST2], F32)
    Xg = [k.sb("Xg%d%s" % (i, tag), [128, 1024], BF16) for i in range(2)]
    XgT = k.sb("XgT" + tag, [128, 8, CAP2], BF16)
    actT = [k.sb("actT%d%s" % (j, tag), [128, CAP2], BF16) for j in range(8)]
    Ysb = k.sb("Ysb" + tag, [128, NST2, 1024], BF16)
    bdb = [k.sb("bdb%d%s" % (i, tag), [128, 1024], F32) for i in range(2)]
    NR = 3
    wring = [k.sb("wgur%d%s" % (i, tag), [128, 8, 2, 128], BF16) for i in range(NR)]
    ND = 2
    dring = [k.sb("wdr%d%s" % (i, tag), [128, 8, 512], BF16) for i in range(ND)]
    g_sb = k.sb("g_sb" + tag, [128, 512], F32)
    s_sb = k.sb("s_sb" + tag, [128, 512], F32)
    t1 = k.sb("t1" + tag, [128, 512], F32)
    t2 = k.sb("t2" + tag, [128, 512], F32)
    m_sb = k.sb("m_sb" + tag, [128, 512], F32)

    units = [(e, j) for e in range(n_exp) for j in range(8)]
    dunits = [(e, h) for e in range(n_exp) for h in range(2)]

    NSG = 3
    stgu = [k.sb("stgu%d%s" % (i, tag), [128, 8, 2, 128], F32) for i in range(NSG)]
    stgd = [k.sb("stgd%d%s" % (i, tag), [128, 4, 512], F32) for i in range(2)]

    def fetch_unit(i):
        if i < len(units):
            e, j = units[i]
            k.dma("sp", stgu[i % NSG][:], d["wgu"][e, j], reads=[d["wgu"]], writes=[stgu[i % NSG]])

    def cast_unit(i):
        if i < len(units):
            k.op("pool", lambda e, i=i: e.tensor_copy(out=wring[i % NR][:], in_=stgu[i % NSG][:]),
                 reads=[stgu[i % NSG]], writes=[wring[i % NR]])

    def fetch_dunit(i):
        if i < len(dunits):
            e, h = dunits[i]
            for q in range(2):
                k.dma("sp", stgd[q][:], d["wd"][e, :, q * 4:(q + 1) * 4, h * 512:(h + 1) * 512], reads=[d["wd"]],
                      writes=[stgd[q]])

    def cast_dunit(i):
        if i < len(dunits):
            for q in range(2):
                k.op("act", lambda e, i=i, q=q: e.activation(out=dring[i % ND][:, q * 4:(q + 1) * 4, :], in_=stgd[q][:],
                                                              func=AF.Copy), reads=[stgd[q]], writes=[dring[i % ND]])

    def build_P(e_):
        for t in range(NT):
            k.op("dve", lambda e, t=t, e_=e_: e.tensor_scalar(
                out=Pm[t][:], in0=iota_f[:], scalar1=pos_all[:, t, e_:e_ + 1], scalar2=maskf[:, t, e_:e_ + 1],
                op0=ALU.is_equal, op1=ALU.mult), reads=[iota_f, pos_all, maskf], writes=[Pm[t]])

    for i in range(NSG):
        fetch_unit(i)
    for i in range(2):
        cast_unit(i)
    build_P(0)
    cnt = 0
    for e_ in range(n_exp):
        bdt = bdb[e_ % 2]
        si = sidx[e_ % 2]
        k.dma("sp", bdt[:], d["bd"][e_].partition_broadcast(128), reads=[d["bd"]], writes=[bdt])
        pb = psum[7]
        for s_ in range(NST2):
            for t in range(NT):
                mm(k, pb, pb[:, 2 * s_:2 * s_ + 2], Pm[t], Pm[t][:, s_ * 128:(s_ + 1) * 128], tokc, tokc[:, t, :], t == 0, t == NT - 1)
        k.op("act", lambda e, pb=pb: e.activation(out=sidx_f[:], in_=pb[:, 0:2 * NST2].rearrange("p (a b) -> p a b", a=NST2),
                                                  func=AF.Copy), reads=[pb], writes=[sidx_f])
        k.op("dve", lambda e: e.scalar_tensor_tensor(out=sidx_t[:], in0=sidx_f[:, :, 1], scalar=128.0, in1=sidx_f[:, :, 0],
                                                     op0=ALU.mult, op1=ALU.add), reads=[sidx_f], writes=[sidx_t])
        k.op("dve", lambda e, si=si: e.tensor_copy(out=si[:], in_=sidx_t[:]), reads=[sidx_t], writes=[si])
        for s_ in range(NST2):
            xg = Xg[s_ % 2]
            k.cc("pool", lambda e, xg=xg, si=si, s_=s_: e.indirect_dma_start(
                out=xg[:], out_offset=None, in_=u_scr[:, :],
                in_offset=bass.IndirectOffsetOnAxis(ap=si[:, s_:s_ + 1], axis=0)), reads=[u_scr, si], writes=[xg])
            pbt, pbv = psum[6], pbf[6]
            for kc in range(8):
                tr(k, pbt, pbv[:, kc * 128:(kc + 1) * 128], xg, xg[:, kc * 128:(kc + 1) * 128], idb, idb[:])
            k.op("act", lambda e, s_=s_, pbv=pbv: e.activation(
                out=XgT[:, :, s_ * 128:(s_ + 1) * 128], in_=pbv.rearrange("p (a b) -> p a b", a=8), func=AF.Copy),
                reads=[pbt], writes=[XgT])
        if e_ + 1 < n_exp:
            build_P(e_ + 1)
        for j in range(8):
            ui = e_ * 8 + j
            w = wring[ui % NR]
            if j == 0:
                fetch_dunit(e_ * 2)
            if j == 4:
                cast_dunit(e_ * 2)
                fetch_dunit(e_ * 2 + 1)
            for T in range(CAP2 // 512):
                x = cnt % 2
                cnt += 1
                ts_ = slice(T * 512, (T + 1) * 512)
                for kc in range(8):
                    mm(k, pg[x], pg[x][:], w, w[:, kc, 0, :], XgT, XgT[:, kc, ts_], kc == 0, kc == 7)
                for kc in range(8):
                    mm(k, pu[x], pu[x][:], w, w[:, kc, 1, :], XgT, XgT[:, kc, ts_], kc == 0, kc == 7)
                k.op("dve", lambda e, x=x, e_=e_, j=j: e.tensor_scalar(
                    out=g_sb[:], in0=pg[x][:], scalar1=bgu[:, e_, 0, j:j + 1], scalar2=SWIGLU_LIMIT,
                    op0=ALU.add, op1=ALU.min), reads=[pg[x], bgu], writes=[g_sb])
                k.op("act", lambda e: e.activation(out=s_sb[:], in_=g_sb[:], func=AF.Sigmoid, scale=SWIGLU_ALPHA),
                     reads=[g_sb], writes=[s_sb])
                k.op("act", lambda e, x=x, e_=e_, j=j: e.activation(
                    out=t1[:], in_=pu[x][:], func=AF.Identity, bias=bgu1[:, e_, j:j + 1], scale=1.0),
                    reads=[pu[x], bgu1], writes=[t1])
                k.op("pool", lambda e: e.tensor_scalar(out=t2[:], in0=t1[:], scalar1=SWIGLU_LIMIT + 1.0,
                                                       scalar2=1.0 - SWIGLU_LIMIT, op0=ALU.min, op1=ALU.max),
                     reads=[t1], writes=[t2])
                k.op("dve", lambda e: e.tensor_tensor(out=m_sb[:], in0=g_sb[:], in1=s_sb[:], op=ALU.mult),
                     reads=[g_sb, s_sb], writes=[m_sb])
                k.op("pool", lambda e, j=j, ts_=ts_: e.tensor_tensor(out=actT[j][:, ts_], in0=m_sb[:], in1=t2[:], op=ALU.mult),
                     reads=[m_sb, t2], writes=[actT[j]])
            cast_unit(ui + 2)
            fetch_unit(ui + NSG)
        cast_dunit(e_ * 2 + 1)
        for half in range(2):
            di = e_ * 2 + half
            wdv = dring[di % ND]
            hs = slice(half * 512, (half + 1) * 512)
            for s_ in range(NST2):
                pb = pd[s_ % 2]
                for fc in range(8):
                    mm(k, pb, pb[:], actT[fc], actT[fc][:, s_ * 128:(s_ + 1) * 128], wdv, wdv[:, fc, :], fc == 0, fc == 7)
                k.op("dve", lambda e, s_=s_, hs=hs, pb=pb, bdt=bdt: e.tensor_tensor(
                    out=Ysb[:, s_, hs], in0=pb[:], in1=bdt[:, hs], op=ALU.add), reads=[pb, bdt], writes=[Ysb])
        k.dma("sp", y_scr[e_ * CAP2:(e_ + 1) * CAP2, :].rearrange("(a p) c -> p a c", p=128), Ysb[:], reads=[Ysb], writes=[y_scr])

    k.release(m2)
    hp = [k.sb("hp%d%s" % (i, tag), [128, 1024], F32) for i in range(2)]
    yk = [k.sb("yk%d%s" % (i, tag), [128, 1024], BF16) for i in range(4)]
    ac = [k.sb("ac%d%s" % (i, tag), [128, 1024], F32) for i in range(2)]
    for t in range(NT):
        rows = slice(t * 128, (t + 1) * 128)
        h_, a_ = hp[t % 2], ac[t % 2]
        k.dma("sp", h_[:], hpre_d[rows, :], reads=[hpre_d], writes=[h_])
        for kk in range(4):
            k.cc("pool", lambda e, kk=kk, t=t: e.indirect_dma_start(
                out=yk[kk][:], out_offset=None, in_=y_scr[:, :],
                in_offset=bass.IndirectOffsetOnAxis(ap=rt_u[:, t, kk:kk + 1], axis=0)), reads=[y_scr, rt_u], writes=[yk[kk]])
        k.op("dve", lambda e, t=t, a_=a_: e.tensor_scalar(out=a_[:], in0=yk[0][:], scalar1=rt[:, t, 8:9], scalar2=None,
                                                          op0=ALU.mult), reads=[yk[0], rt], writes=[a_])
        for kk in range(1, 4):
            k.op("dve", lambda e, t=t, kk=kk, a_=a_: e.scalar_tensor_tensor(
                out=a_[:], in0=yk[kk][:], scalar=rt[:, t, 8 + kk:9 + kk], in1=a_[:], op0=ALU.mult, op1=ALU.add),
                reads=[yk[kk], rt, a_], writes=[a_])
        k.op("pool", lambda e, a_=a_: e.tensor_tensor(out=a_[:], in0=a_[:], in1=m_g2[:], op=ALU.mult),
             reads=[a_, m_g2], writes=[a_])
        k.op("pool", lambda e, a_=a_, h_=h_: e.tensor_tensor(out=h_[:], in0=a_[:], in1=h_[:], op=ALU.add),
             reads=[a_, h_], writes=[h_])
        toks.append(k.dma("sp", out_d[out_row0 + t * 128:out_row0 + (t + 1) * 128, :], h_[:], reads=[h_], writes=[out_d]))
    k.release(m0)
    return toks


def build_bd_sp2(F, n_exp=32, debug=False):
    nc = bass.Bass("TRN2", target_bir_lowering=False)
    with contextlib.ExitStack() as stack:
        k = KB(nc, stack)
        psum, pbf, pall = make_psum_all(k)
        d = bd_dram(k, F, "", n_exp)
        out_d = k.dram("out", [NTOK, 1024], F32, "ExternalOutput")
        hpre_d = k.dram("hpre_scr", [NTOK, 1024], F32, "ExternalOutput" if debug else "Internal")
        scr = {"u": k.dram("u_scr", [NTOK, 1024], BF16, "Internal"),
               "y": k.dram("y_scr", [32 * CAP2, 1024], BF16, "ExternalOutput" if debug else "Internal")}
        ident = make_ident(k, "id", BF16)
        dbg = None
        if debug:
            dbg = {"gw": k.dram("dbg_gw", [128, NT, 32], F32, "ExternalOutput"),
                   "pos": k.dram("dbg_pos", [128, NT, 32], F32, "ExternalOutput"),
                   "rt": k.dram("dbg_rt", [128, NT, 16], F32, "ExternalOutput")}
        toks = emit_bd_sp2(k, F, d, psum, pbf, out_d, hpre_d, ident, scr, "", n_exp, dbg)
        k.emit(final_waits=toks)
        print("BDS2 arena peak words", k.apeak, "instr", {e: len(v) for e, v in k.prog.items()})
    return nc
```

```python
import contextlib
import numpy as np
import ml_dtypes
import concourse.bass as bass
import concourse.mybir as mybir
from concourse.bass_utils import run_bass_kernel_spmd

F32 = mybir.dt.float32
BF16 = mybir.dt.bfloat16
AF = mybir.ActivationFunctionType
ALU = mybir.AluOpType
AX = mybir.AxisListType
NPBF = ml_dtypes.bfloat16

N_DMA_SEMS = 16
ARENA_W = 51 * 1024
EPS = 1e-6


class Buf:
    __slots__ = ("t", "last_w", "readers", "name")

    REG = []

    def __init__(self, t, name=""):
        self.t = t
        self.last_w = None
        self.readers = {}
        self.name = name
        Buf.REG.append(self)

    def __getitem__(self, idx):
        return self.t[idx]


class KB:
    ENG = ("pe", "act", "dve", "pool", "sp")

    def __init__(self, nc, stack):
        self.nc = nc
        self.stack = stack
        self.prog = {e: [] for e in self.ENG}
        self.cnt = {e: 0 for e in self.ENG}
        self.sems = {}
        self.epoch = 0
        Buf.REG = []
        for e in self.ENG:
            self.sems[(e, 0)] = stack.enter_context(nc.semaphore("s_" + e))
        self.dsem = []
        for i in range(N_DMA_SEMS):
            self.dsem.append(stack.enter_context(nc.semaphore("d_%d" % i)))
            self.sems[("d", i)] = self.dsem[i]
        self.dcnt = [0] * N_DMA_SEMS
        self.dnext = 0
        self.waited = {}
        self.arena = None
        self.apeak = 0

    def sb(self, name, shape, dt):
        if self.arena is None:
            self.arena = self.stack.enter_context(self.nc.sbuf_tensor("arena", [128, ARENA_W], F32))
            self.aoff = 0
        esz = 2 if dt == BF16 else 4
        nel = 1
        for x in shape[1:]:
            nel *= x
        n32 = (nel * esz + 3) // 4
        n32 = (n32 + 7) // 8 * 8
        assert self.aoff + n32 <= ARENA_W, "SBUF arena overflow at %s (%d + %d)" % (name, self.aoff, n32)
        v = self.arena[0:shape[0], self.aoff:self.aoff + n32]
        self.aoff += n32
        self.apeak = max(self.apeak, self.aoff)
        if dt != F32:
            v = v.bitcast(dt)
        v = v[:, 0:nel]
        if len(shape) == 3:
            v = v.rearrange("p (a b) -> p a b", a=shape[1])
        elif len(shape) == 4:
            v = v.rearrange("p (a b c) -> p a b c", a=shape[1], b=shape[2])
        return Buf(v, name)

    def mark(self):
        return self.aoff

    def release(self, m):
        self.fence()
        self.aoff = m

    def ps(self, name, shape, dt=F32):
        return Buf(self.stack.enter_context(self.nc.psum_tensor(name, list(shape), dt)), name)

    def dram(self, name, shape, dt, kind):
        return Buf(self.nc.dram_tensor(name, list(shape), dt, kind=kind).ap(), name)

    def _wait(self, eng, key, val):
        if key == ("pe", self.epoch) and eng == "pe":
            return
        if self.waited.get((eng, key), 0) >= val:
            return
        self.waited[(eng, key)] = val
        self.prog[eng].append(("w", key, val))

    def _deps(self, eng, reads, writes):
        for b in reads:
            if b.last_w is not None:
                self._wait(eng, *b.last_w)
        for b in writes:
            if b.last_w is not None:
                self._wait(eng, *b.last_w)
            for tok in b.readers.values():
                self._wait(eng, *tok)

    def _mark(self, tok, reads, writes):
        for b in reads:
            b.readers[tok[0]] = tok
        for b in writes:
            b.last_w = tok
            b.readers = {}

    def op(self, eng, fn, reads=(), writes=()):
        self._deps(eng, reads, writes)
        self.cnt[eng] += 1
        tok = ((eng, self.epoch), self.cnt[eng])
        self.prog[eng].append(("o", fn, self.epoch))
        self._mark(tok, reads, writes)
        return tok

    def dma(self, eng, out, in_, reads=(), writes=(), **kw):
        i = self.dnext
        self.dnext = (self.dnext + 1) % N_DMA_SEMS
        key = ("d", i)
        if self.dcnt[i] > 0:
            self._wait(eng, key, self.dcnt[i])
        self._deps(eng, reads, writes)
        self.dcnt[i] += 16
        tok = (key, self.dcnt[i])
        self.prog[eng].append(("d", out, in_, i, kw))
        self._mark(tok, reads, writes)
        return tok

    def cc(self, eng, fn, reads=(), writes=()):
        i = self.dnext
        self.dnext = (self.dnext + 1) % N_DMA_SEMS
        key = ("d", i)
        if self.dcnt[i] > 0:
            self._wait(eng, key, self.dcnt[i])
        self._deps(eng, reads, writes)
        self.dcnt[i] += 16
        tok = (key, self.dcnt[i])
        self.prog[eng].append(("c", fn, i))
        self._mark(tok, reads, writes)
        return tok

    def new_epoch(self):
        self.fence()
        self.epoch += 1
        for e in self.ENG:
            self.sems[(e, self.epoch)] = self.stack.enter_context(self.nc.semaphore("s_%s_%d" % (e, self.epoch)))
            self.cnt[e] = 0
        self.waited = {kk: v for kk, v in self.waited.items() if isinstance(kk[1], tuple) and kk[1][0] == "d"}
        for b in Buf.REG:
            b.last_w = None
            b.readers = {}

    def fence(self):
        for e in self.ENG:
            for o in self.ENG:
                if self.cnt[o] > 0:
                    self._wait(e, (o, self.epoch), self.cnt[o])
            for i in range(N_DMA_SEMS):
                if self.dcnt[i] > 0:
                    self._wait(e, ("d", i), self.dcnt[i])

    def emit(self, final_waits=()):
        nc = self.nc
        engmap = {"pe": "tensor", "act": "scalar", "dve": "vector", "pool": "gpsimd", "sp": "sync"}
        for tok in final_waits:
            self._wait("sp", tok[0], tok[1])
        with nc.Block() as block:
            for e in self.ENG:
                def body(h, items=self.prog[e], e=e):
                    for it in items:
                        if it[0] == "w":
                            h.wait_ge(self.sems[it[1]], it[2])
                        elif it[0] == "o":
                            it[1](h).then_inc(self.sems[(e, it[2])], 1)
                        elif it[0] == "c":
                            it[1](h).then_inc(self.dsem[it[2]], 16)
                        else:
                            _, out, in_, i, kw = it
                            if callable(out):
                                out = out(h)
                            if callable(in_):
                                in_ = in_(h)
                            h.dma_start(out=out, in_=in_, **kw).then_inc(self.dsem[i], 16)
                getattr(block, engmap[e])(body)


def mm(k, ob, o, lb, l, rb, r, start, stop):
    k.op("pe", lambda e: e.matmul(o, lhsT=l, rhs=r, start=start, stop=stop), reads=[lb, rb], writes=[ob])


def tr(k, ob, o, ib, i, idb, ident):
    k.op("pe", lambda e: e.transpose(o, i, ident), reads=[ib, idb], writes=[ob])


def make_ident(k, name, dt):
    idf = k.sb(name + "_f", [128, 128], F32)
    k.op("pool", lambda e: e.memset(idf[:], 1.0), writes=[idf])
    k.op("pool", lambda e: e.affine_select(out=idf[:], in_=idf[:], pattern=[[-1, 128]], compare_op=ALU.is_equal,
                                           fill=0.0, base=0, channel_multiplier=1), reads=[idf], writes=[idf])
    if dt == F32:
        return idf, None
    idb = k.sb(name + "_b", [128, 128], dt)
    k.op("pool", lambda e: e.tensor_copy(out=idb[:], in_=idf[:]), reads=[idf], writes=[idb])
    return idf, idb


def adaln(k, ccol_d, adaw_d, adab_d, nvec, dst, stage, pbanks, tag=""):
    ccol = k.sb("ada_c" + tag, [128, 8], F32)
    cond = k.sb("ada_cond" + tag, [128, 8], F32)
    cbc = k.sb("ada_cbc" + tag, [128, 8, 128], F32)
    k.dma("sp", ccol[:], ccol_d[:], reads=[ccol_d], writes=[ccol])
    k.op("act", lambda e: e.activation(out=cond[:], in_=ccol[:], func=AF.Silu), reads=[ccol], writes=[cond])
    for kc in range(8):
        k.op("dve", lambda e, kc=kc: e.tensor_copy(out=cbc[:, kc, :], in_=cond[:, kc:kc + 1].to_broadcast([128, 128])),
             reads=[cond], writes=[cbc])
    for v in range(nvec):
        st = stage[v % 2]
        k.dma("sp", st[:], adaw_d[v], reads=[adaw_d], writes=[st])
        k.dma("sp", dst[v][:], adab_d[v].partition_broadcast(128), reads=[adab_d], writes=[dst[v]])
        for half in range(2):
            pb = pbanks[half]
            for kc in range(8):
                mm(k, pb, pb[:], cbc, cbc[:, kc, :], st, st[:, kc, half * 512:(half + 1) * 512], kc == 0, kc == 7)
            k.op("dve", lambda e, v=v, half=half, pb=pb: e.tensor_tensor(
                out=dst[v][:, half * 512:(half + 1) * 512], in0=pb[:], in1=dst[v][:, half * 512:(half + 1) * 512],
                op=ALU.add), reads=[pb, dst[v]], writes=[dst[v]])


NTOK = 2048
NT = NTOK // 128
SWIGLU_LIMIT = 7.0
SWIGLU_ALPHA = 1.702


def bd_dram(k, F, tag, n_exp=32, with_io=True):
    d = {}
    if with_io:
        d["oin"] = k.dram("oin" + tag, [NTOK, F], BF16, "ExternalInput")
        d["hres"] = k.dram("hres" + tag, [NTOK, 1024], F32, "ExternalInput")
    d["wout"] = k.dram("wout" + tag, [128, F // 128, 1024], F32, "ExternalInput")
    d["ccol"] = k.dram("ccol" + tag, [128, 8], F32, "ExternalInput")
    d["adaw"] = k.dram("adaw" + tag, [4, 128, 8, 1024], F32, "ExternalInput")
    d["adab"] = k.dram("adab" + tag, [4, 1024], F32, "ExternalInput")
    d["gain"] = k.dram("gain" + tag, [1024], F32, "ExternalInput")
    d["rw"] = k.dram("rw" + tag, [128, 8, 32], F32, "ExternalInput")
    d["rb"] = k.dram("rb" + tag, [32], F32, "ExternalInput")
    d["wgu"] = k.dram("wgu" + tag, [n_exp, 8, 128, 8, 2, 128], F32, "ExternalInput")
    d["bgu"] = k.dram("bgu" + tag, [128, 32, 2, 8], F32, "ExternalInput")
    d["wd"] = k.dram("wd" + tag, [n_exp, 128, 8, 1024], F32, "ExternalInput")
    d["bd"] = k.dram("bd" + tag, [32, 1024], F32, "ExternalInput")
    return d


def bd_host_inputs(layer, b, P, w_out, tag):
    m = {}
    F = w_out.shape[0]
    m["wout" + tag] = np.ascontiguousarray(w_out.reshape(F // 128, 128, 1024).transpose(1, 0, 2))
    m["ccol" + tag] = np.ascontiguousarray(P["c"][b].reshape(8, 128).T)
    aw = P["ada_w"][layer]
    sel = [2, 3, 4, 5]
    m["adaw" + tag] = np.ascontiguousarray(
        np.stack([aw[:, v * 1024:(v + 1) * 1024].reshape(8, 128, 1024).transpose(1, 0, 2) for v in sel]))
    m["adab" + tag] = np.ascontiguousarray(np.stack([P["ada_b"][layer][v * 1024:(v + 1) * 1024] for v in sel]))
    m["gain" + tag] = np.ascontiguousarray(P["norm_ffn"][layer])
    m["rw" + tag] = np.ascontiguousarray(P["router_w"][layer].reshape(8, 128, 32).transpose(1, 0, 2))
    m["rb" + tag] = np.ascontiguousarray(P["router_b"][layer])
    return m


def bd_host_shared(layer, P, tag):
    m = {}
    wgu = P["moe_w_gu"][layer]
    m["wgu" + tag] = np.ascontiguousarray(wgu.reshape(32, 8, 128, 2, 8, 128).transpose(0, 4, 2, 1, 3, 5))
    bgu = P["moe_b_gu"][layer]
    m["bgu" + tag] = np.ascontiguousarray(bgu.reshape(32, 2, 8, 128).transpose(3, 0, 1, 2))
    wd = P["moe_w_down"][layer]
    m["wd" + tag] = np.ascontiguousarray(wd.reshape(32, 8, 128, 1024).transpose(0, 2, 1, 3))
    m["bd" + tag] = np.ascontiguousarray(P["moe_b_down"][layer])
    return m


def emit_bd(k, F, d, psum, pbf, out_d, hpre_d, ident, tag="", n_exp=32, dbg=None, oin_parts=None, hres_src=None,
            out_row0=0, ridx_d=None):
    FC = F // 128
    if oin_parts is None:
        oin_parts = [(slice(0, F), d["oin"], slice(0, F), 0)]
    if hres_src is None:
        hres_src = (d["hres"], 0)
    pg, pu, pd, pm = psum[0:2], psum[2:4], psum[4:6], psum[6:8]
    pgb, pmb = pbf[0:2], pbf[6:8]
    idf, idb = ident
    m0 = k.mark()
    uT = k.sb("uT" + tag, [128, 8, NTOK], BF16)
    gw = k.sb("gw" + tag, [128, NT, 32], F32)
    gwT = k.sb("gwT" + tag, [32, NTOK], F32)
    m_g2 = k.sb("m_g2" + tag, [128, 1024], F32)
    bgu = k.sb("bgu" + tag, [128, 32, 2, 8], F32)
    bgu1 = k.sb("bgu1" + tag, [128, 32, 8], F32)
    bd_sb = k.sb("bdsb" + tag, [32, 1024], F32)
    epsb = k.sb("epsb" + tag, [128, 1], F32)
    k.op("pool", lambda e: e.memset(epsb[:], EPS), writes=[epsb])
    ridx = None
    if ridx_d is not None:
        ridx = k.sb("ridx" + tag, [128, NT], mybir.dt.uint32)
        k.dma("sp", ridx[:], ridx_d[:], reads=[ridx_d], writes=[ridx])
    k.dma("sp", bgu[:], d["bgu"][:], reads=[d["bgu"]], writes=[bgu])
    k.dma("sp", bd_sb[:], d["bd"][:], reads=[d["bd"]], writes=[bd_sb])
    k.op("pool", lambda e: e.tensor_scalar(out=bgu1[:], in0=bgu[:, :, 1, :], scalar1=1.0, scalar2=None, op0=ALU.add),
         reads=[bgu], writes=[bgu1])

    m1 = k.mark()
    m_g1 = k.sb("m_g1" + tag, [128, 1024], F32)
    m_sh2 = k.sb("m_sh2" + tag, [128, 1024], F32)
    m_gm = k.sb("m_gm" + tag, [128, 1024], F32)
    gain_bc = k.sb("gainbc" + tag, [128, 1024], F32)
    stage = k.sb("adast" + tag, [128, 8, 1024], F32)
    woutb = k.sb("woutb" + tag, [128, FC, 1024], BF16)
    rwb = k.sb("rwb" + tag, [128, 8, 32], BF16)
    rb_bc = k.sb("rbbc" + tag, [128, 32], F32)
    o_tok = [k.sb("otok%d%s" % (i, tag), [128, F], BF16) for i in range(2)]
    oT = [k.sb("oT%d%s" % (i, tag), [128, FC, 128], BF16) for i in range(2)]
    hres_t = [k.sb("hrt%d%s" % (i, tag), [128, 1024], F32) for i in range(2)]
    tmp = [k.sb("tmp%d%s" % (i, tag), [128, 1024], F32) for i in range(2)]
    u_tok = [k.sb("utok%d%s" % (i, tag), [128, 1024], BF16) for i in range(2)]
    st = [k.sb("st%d%s" % (i, tag), [128, 64], F32) for i in range(2)]
    lg = [k.sb("lg%d%s" % (i, tag), [128, 4, 32], F32) for i in range(2)]

    k.dma("pool", woutb[:], d["wout"][:], reads=[d["wout"]], writes=[woutb])
    k.dma("pool", rwb[:], d["rw"][:], reads=[d["rw"]], writes=[rwb])
    k.dma("sp", rb_bc[:], d["rb"][:].partition_broadcast(128), reads=[d["rb"]], writes=[rb_bc])
    k.dma("sp", gain_bc[:], d["gain"][:].partition_broadcast(128), reads=[d["gain"]], writes=[gain_bc])
    adaln(k, d["ccol"], d["adaw"], d["adab"], 4, [m_g1, m_sh2, m_gm, m_g2], [stage, stage], pm, tag)
    k.op("dve", lambda e: e.scalar_tensor_tensor(out=m_gm[:], in0=m_gm[:], scalar=1.0, in1=gain_bc[:],
                                                 op0=ALU.add, op1=ALU.mult), reads=[m_gm, gain_bc], writes=[m_gm])

    for t in range(NT):
        ot, oTt, hr, tp, ut, s_, l_ = o_tok[t % 2], oT[t % 2], hres_t[t % 2], tmp[t % 2], u_tok[t % 2], st[t % 2], lg[t % 2]
        rows = slice(t * 128, (t + 1) * 128)
        if ridx is None:
            for (dcs, sbuf_, scs, r0) in oin_parts:
                k.dma("sp", ot[:, dcs], sbuf_[r0 + t * 128:r0 + (t + 1) * 128, scs], reads=[sbuf_], writes=[ot])
            k.dma("sp", hr[:], hres_src[0][hres_src[1] + t * 128:hres_src[1] + (t + 1) * 128, :], reads=[hres_src[0]], writes=[hr])
        else:
            for (dcs, sbuf_, scs, r0) in oin_parts:
                k.cc("pool", lambda e, ot=ot, dcs=dcs, sbuf_=sbuf_, t=t: e.indirect_dma_start(
                    out=ot[:, dcs], out_offset=None, in_=sbuf_[:, :],
                    in_offset=bass.IndirectOffsetOnAxis(ap=ridx[:, t:t + 1], axis=0)), reads=[sbuf_, ridx], writes=[ot])
            k.cc("pool", lambda e, hr=hr, t=t: e.indirect_dma_start(
                out=hr[:], out_offset=None, in_=hres_src[0][:, :],
                in_offset=bass.IndirectOffsetOnAxis(ap=ridx[:, t:t + 1], axis=0)), reads=[hres_src[0], ridx], writes=[hr])
        for g in range(FC // 8):
            pb, pbv = pg[g % 2], pgb[g % 2]
            for c in range(8):
                fc = g * 8 + c
                tr(k, pb, pbv[:, c * 128:(c + 1) * 128], ot, ot[:, fc * 128:(fc + 1) * 128], idb, idb[:])
            k.op("act", lambda e, g=g, pbv=pbv, oTt=oTt: e.activation(
                out=oTt[:, g * 8:(g + 1) * 8, :], in_=pbv.rearrange("p (a b) -> p a b", a=8), func=AF.Copy),
                reads=[pb], writes=[oTt])
        for half in range(2):
            hs = slice(half * 512, (half + 1) * 512)
            for fc in range(FC):
                mm(k, pd[half], pd[half][:], oTt, oTt[:, fc, :], woutb, woutb[:, fc, hs], fc == 0, fc == FC - 1)
            k.op("dve", lambda e, half=half, hs=hs, tp=tp: e.tensor_tensor(
                out=tp[:, hs], in0=pd[half][:], in1=m_g1[:, hs], op=ALU.mult), reads=[pd[half], m_g1], writes=[tp])
        k.op("pool", lambda e, hr=hr, tp=tp: e.tensor_tensor(out=hr[:], in0=tp[:], in1=hr[:], op=ALU.add),
             reads=[tp, hr], writes=[hr])
        k.dma("sp", hpre_d[rows, :], hr[:], reads=[hr], writes=[hpre_d])
        k.op("act", lambda e, hr=hr, tp=tp, s_=s_: e.activation(out=tp[:], in_=hr[:], func=AF.Square,
                                                                 accum_out=s_[:, 0:1]), reads=[hr], writes=[tp, s_])
        k.op("act", lambda e, s_=s_: e.activation(out=s_[:, 1:2], in_=s_[:, 0:1], func=AF.Sqrt, bias=epsb[:],
                                                   scale=1.0 / 1024.0), reads=[s_, epsb], writes=[s_])
        k.op("dve", lambda e, s_=s_: e.reciprocal(out=s_[:, 2:3], in_=s_[:, 1:2]), reads=[s_], writes=[s_])
        k.op("dve", lambda e, hr=hr, tp=tp, s_=s_: e.scalar_tensor_tensor(
            out=tp[:], in0=hr[:], scalar=s_[:, 2:3], in1=m_gm[:], op0=ALU.mult, op1=ALU.mult),
            reads=[hr, s_, m_gm], writes=[tp])
        k.op("pool", lambda e, tp=tp, ut=ut: e.tensor_tensor(out=ut[:], in0=tp[:], in1=m_sh2[:], op=ALU.add),
             reads=[tp, m_sh2], writes=[ut])
        for kc in range(8):
            tr(k, pm[0], pmb[0][:, kc * 128:(kc + 1) * 128], ut, ut[:, kc * 128:(kc + 1) * 128], idb, idb[:])
        k.op("act", lambda e, t=t: e.activation(out=uT[:, :, t * 128:(t + 1) * 128],
                                                in_=pmb[0].rearrange("p (a b) -> p a b", a=8), func=AF.Copy),
             reads=[pm[0]], writes=[uT])
        for kc in range(8):
            mm(k, pm[1], pm[1][:, 0:32], uT, uT[:, kc, t * 128:(t + 1) * 128], rwb, rwb[:, kc, :], kc == 0, kc == 7)
        k.op("dve", lambda e, l_=l_: e.tensor_tensor(out=l_[:, 0, :], in0=pm[1][:, 0:32], in1=rb_bc[:], op=ALU.add),
             reads=[pm[1], rb_bc], writes=[l_])
        k.op("dve", lambda e, l_=l_, s_=s_: e.max(out=s_[:, 8:16], in_=l_[:, 0, :]), reads=[l_], writes=[s_])
        k.op("dve", lambda e, l_=l_, s_=s_: e.tensor_scalar(out=l_[:, 1, :], in0=l_[:, 0, :], scalar1=s_[:, 11:12],
                                                            scalar2=None, op0=ALU.is_ge), reads=[l_, s_], writes=[l_])
        k.op("dve", lambda e, s_=s_: e.tensor_scalar(out=s_[:, 16:17], in0=s_[:, 8:9], scalar1=-1.0, scalar2=None,
                                                     op0=ALU.mult), reads=[s_], writes=[s_])
        k.op("act", lambda e, l_=l_, s_=s_: e.activation(out=l_[:, 2, :], in_=l_[:, 0, :], func=AF.Exp,
                                                          bias=s_[:, 16:17], scale=1.0), reads=[l_, s_], writes=[l_])
        k.op("dve", lambda e, l_=l_, s_=s_: e.scalar_tensor_tensor(
            out=l_[:, 3, :], in0=l_[:, 2, :], scalar=1.0, in1=l_[:, 1, :], op0=ALU.mult, op1=ALU.mult,
            accum_out=s_[:, 17:18]), reads=[l_], writes=[l_, s_])
        k.op("dve", lambda e, s_=s_: e.reciprocal(out=s_[:, 18:19], in_=s_[:, 17:18]), reads=[s_], writes=[s_])
        k.op("dve", lambda e, l_=l_, s_=s_, t=t: e.tensor_scalar(out=gw[:, t, :], in0=l_[:, 3, :], scalar1=s_[:, 18:19],
                                                                 scalar2=None, op0=ALU.mult), reads=[l_, s_], writes=[gw])
        tr(k, pm[1], pm[1][0:32, 128:256], gw, gw[:, t, :], idf, idf[:])
        k.op("act", lambda e, t=t: e.activation(out=gwT[:, t * 128:(t + 1) * 128], in_=pm[1][0:32, 128:256],
                                                func=AF.Copy), reads=[pm[1]], writes=[gwT])

    toks = []
    if dbg is not None:
        toks.append(k.dma("sp", dbg["uT"][:], uT[:], reads=[uT], writes=[dbg["uT"]]))
        toks.append(k.dma("sp", dbg["gw"][:], gw[:], reads=[gw], writes=[dbg["gw"]]))
        for i, mv in enumerate([m_g1, m_sh2, m_gm, m_g2]):
            toks.append(k.dma("sp", dbg["mods"][i], mv[:], reads=[mv], writes=[dbg["mods"]]))
    k.release(m1)
    acc = [k.sb("acc%d%s" % (t, tag), [128, 1024], F32) for t in range(NT)]
    m2 = k.mark()
    actT = [k.sb("actT%d%s" % (j, tag), [128, NTOK], BF16) for j in range(8)]
    NR = 4
    wring = [k.sb("wgur%d%s" % (i, tag), [128, 8, 2, 128], BF16) for i in range(NR)]
    ND = 3
    dring = [k.sb("wdr%d%s" % (i, tag), [128, 8, 512], BF16) for i in range(ND)]
    g_sb = k.sb("g_sb" + tag, [128, 512], F32)
    s_sb = k.sb("s_sb" + tag, [128, 512], F32)
    t1 = k.sb("t1" + tag, [128, 512], F32)
    t2 = k.sb("t2" + tag, [128, 512], F32)
    m_sb = k.sb("m_sb" + tag, [128, 512], F32)

    for t in range(NT):
        for half in range(2):
            hs = slice(half * 512, (half + 1) * 512)
            pb = pd[(2 * t + half) % 2]
            mm(k, pb, pb[:], gwT, gwT[:, t * 128:(t + 1) * 128], bd_sb, bd_sb[:, hs], True, True)
            k.op("act", lambda e, t=t, hs=hs, pb=pb: e.activation(out=acc[t][:, hs], in_=pb[:], func=AF.Copy),
                 reads=[pb], writes=[acc[t]])

    units = [(e, j) for e in range(n_exp) for j in range(8)]
    dunits = [(e, h) for e in range(n_exp) for h in range(2)]

    def load_unit(i):
        if i < len(units):
            e, j = units[i]
            k.dma("pool", wring[i % NR][:], d["wgu"][e, j], reads=[d["wgu"]], writes=[wring[i % NR]])

    def load_dunit(i):
        if i < len(dunits):
            e, h = dunits[i]
            k.dma("pool", dring[i % ND][:], d["wd"][e, :, :, h * 512:(h + 1) * 512], reads=[d["wd"]],
                  writes=[dring[i % ND]])

    for i in range(NR - 1):
        load_unit(i)
    for i in range(ND - 1):
        load_dunit(i)
    cnt = 0
    for e_ in range(n_exp):
        for j in range(8):
            ui = e_ * 8 + j
            load_unit(ui + NR - 1)
            w = wring[ui % NR]
            for T in range(4):
                x = cnt % 2
                cnt += 1
                ts_ = slice(T * 512, (T + 1) * 512)
                for kc in range(8):
                    mm(k, pg[x], pg[x][:], w, w[:, kc, 0, :], uT, uT[:, kc, ts_], kc == 0, kc == 7)
                for kc in range(8):
                    mm(k, pu[x], pu[x][:], w, w[:, kc, 1, :], uT, uT[:, kc, ts_], kc == 0, kc == 7)
                k.op("dve", lambda e, x=x, e_=e_, j=j: e.tensor_scalar(
                    out=g_sb[:], in0=pg[x][:], scalar1=bgu[:, e_, 0, j:j + 1], scalar2=SWIGLU_LIMIT,
                    op0=ALU.add, op1=ALU.min), reads=[pg[x], bgu], writes=[g_sb])
                k.op("act", lambda e: e.activation(out=s_sb[:], in_=g_sb[:], func=AF.Sigmoid, scale=SWIGLU_ALPHA),
                     reads=[g_sb], writes=[s_sb])
                k.op("act", lambda e, x=x, e_=e_, j=j: e.activation(
                    out=t1[:], in_=pu[x][:], func=AF.Identity, bias=bgu1[:, e_, j:j + 1], scale=1.0),
                    reads=[pu[x], bgu1], writes=[t1])
                k.op("pool", lambda e: e.tensor_scalar(out=t2[:], in0=t1[:], scalar1=SWIGLU_LIMIT + 1.0,
                                                       scalar2=1.0 - SWIGLU_LIMIT, op0=ALU.min, op1=ALU.max),
                     reads=[t1], writes=[t2])
                k.op("dve", lambda e: e.tensor_tensor(out=m_sb[:], in0=g_sb[:], in1=s_sb[:], op=ALU.mult),
                     reads=[g_sb, s_sb], writes=[m_sb])
                k.op("pool", lambda e, j=j, ts_=ts_: e.tensor_tensor(out=actT[j][:, ts_], in0=m_sb[:], in1=t2[:],
                                                                     op=ALU.mult),
                     reads=[m_sb, t2], writes=[actT[j]])
        for half in range(2):
            di = e_ * 2 + half
            load_dunit(di + ND - 1)
            wdv = dring[di % ND]
            hs = slice(half * 512, (half + 1) * 512)
            for t in range(NT):
                pb = pd[t % 2]
                for fc in range(8):
                    mm(k, pb, pb[:], actT[fc], actT[fc][:, t * 128:(t + 1) * 128], wdv, wdv[:, fc, :], fc == 0, fc == 7)
                k.op("dve", lambda e, t=t, hs=hs, pb=pb, e_=e_: e.scalar_tensor_tensor(
                    out=acc[t][:, hs], in0=pb[:], scalar=gw[:, t, e_:e_ + 1], in1=acc[t][:, hs],
                    op0=ALU.mult, op1=ALU.add), reads=[pb, gw, acc[t]], writes=[acc[t]])

    if dbg is not None:
        toks.append(k.dma("sp", dbg["acc0"][:], acc[0][:], reads=[acc[0]], writes=[dbg["acc0"]]))
        toks.append(k.dma("sp", dbg["actT0"][:], actT[0][:], reads=[actT[0]], writes=[dbg["actT0"]]))
    k.release(m2)
    hp = [k.sb("hp%d%s" % (i, tag), [128, 1024], F32) for i in range(2)]
    for t in range(NT):
        rows = slice(t * 128, (t + 1) * 128)
        h_ = hp[t % 2]
        k.dma("sp", h_[:], hpre_d[rows, :], reads=[hpre_d], writes=[h_])
        k.op("dve", lambda e, t=t: e.tensor_tensor(out=acc[t][:], in0=acc[t][:], in1=m_g2[:], op=ALU.mult),
             reads=[acc[t], m_g2], writes=[acc[t]])
        k.op("pool", lambda e, t=t, h_=h_: e.tensor_tensor(out=h_[:], in0=acc[t][:], in1=h_[:], op=ALU.add),
             reads=[acc[t], h_], writes=[h_])
        toks.append(k.dma("sp", out_d[out_row0 + t * 128:out_row0 + (t + 1) * 128, :], h_[:], reads=[h_], writes=[out_d]))
    k.release(m0)
    return toks


def make_psum(k):
    psum = [k.ps("ps%d" % i, [128, 512], F32) for i in range(8)]
    pbf = [p[:].bitcast(BF16) for p in psum]
    return psum, pbf


def build_bd(F, n_exp=32, debug=False):
    nc = bass.Bass("TRN2", target_bir_lowering=False)
    with contextlib.ExitStack() as stack:
        k = KB(nc, stack)
        psum, pbf = make_psum(k)
        d = bd_dram(k, F, "", n_exp)
        out_d = k.dram("out", [NTOK, 1024], F32, "ExternalOutput")
        hpre_d = k.dram("hpre_scr", [NTOK, 1024], F32, "ExternalOutput" if debug else "Internal")
        ident = make_ident(k, "id", BF16)
        dbg = None
        if debug:
            dbg = {"uT": k.dram("dbg_uT", [128, 8, NTOK], BF16, "ExternalOutput"),
                   "gw": k.dram("dbg_gw", [128, NT, 32], F32, "ExternalOutput"),
                   "mods": k.dram("dbg_mods", [4, 128, 1024], F32, "ExternalOutput"),
                   "acc0": k.dram("dbg_acc0", [128, 1024], F32, "ExternalOutput"),
                   "actT0": k.dram("dbg_actT0", [128, NTOK], BF16, "ExternalOutput")}
        toks = emit_bd(k, F, d, psum, pbf, out_d, hpre_d, ident, "", n_exp, dbg)
        k.emit(final_waits=toks)
        print("BD arena peak words", k.apeak, "instr", {e: len(v) for e, v in k.prog.items()})
    return nc


SEQ = 4096
NEG = -30000.0


def c_dram(k, tag="", with_io=True):
    d = {}
    if with_io:
        d["hin"] = k.dram("c_hin" + tag, [SEQ, 1024], F32, "ExternalInput")
    d["ccol"] = k.dram("c_ccol" + tag, [128, 8], F32, "ExternalInput")
    d["adaw"] = k.dram("c_adaw" + tag, [2, 128, 8, 1024], F32, "ExternalInput")
    d["adab"] = k.dram("c_adab" + tag, [2, 1024], F32, "ExternalInput")
    d["gain"] = k.dram("c_gain" + tag, [1024], F32, "ExternalInput")
    d["wz"] = k.dram("c_wz" + tag, [128, 8, 1024], F32, "ExternalInput")
    d["wx"] = k.dram("c_wx" + tag, [128, 8, 1024], F32, "ExternalInput")
    d["wbc"] = k.dram("c_wbc" + tag, [128, 8, 512], F32, "ExternalInput")
    d["wdt"] = k.dram("c_wdt" + tag, [128, 8, 16], F32, "ExternalInput")
    d["convw"] = k.dram("c_convw" + tag, [128, 12, 4], F32, "ExternalInput")
    d["convb"] = k.dram("c_convb" + tag, [128, 12], F32, "ExternalInput")
    d["dtb"] = k.dram("c_dtb" + tag, [16], F32, "ExternalInput")
    d["alog"] = k.dram("c_alog" + tag, [16], F32, "ExternalInput")
    d["dsk"] = k.dram("c_dsk" + tag, [16], F32, "ExternalInput")
    d["ng"] = k.dram("c_ng" + tag, [1024], F32, "ExternalInput")
    return d


def c_host_inputs(b, hh, P, tag=""):
    m = {}
    lay = lambda w: np.ascontiguousarray(w.reshape(8, 128, -1).transpose(1, 0, 2))
    m["c_ccol" + tag] = np.ascontiguousarray(P["c"][b].reshape(8, 128).T)
    aw = P["ada_w"][1]
    m["c_adaw" + tag] = np.ascontiguousarray(np.stack([lay(aw[:, v * 1024:(v + 1) * 1024]) for v in (0, 1)]))
    m["c_adab" + tag] = np.ascontiguousarray(np.stack([P["ada_b"][1][v * 1024:(v + 1) * 1024] for v in (0, 1)]))
    m["c_gain" + tag] = np.ascontiguousarray(P["norm_mix"][1])
    w = P["ssm_w_in"][0]
    m["c_wz" + tag] = lay(w[:, hh * 1024:(hh + 1) * 1024])
    xo = 2048
    m["c_wx" + tag] = lay(w[:, xo + hh * 1024:xo + (hh + 1) * 1024])
    bo, co = xo + 2048, xo + 2048 + 512
    g0 = 2 * hh
    bc_cols = np.concatenate([np.arange(bo + g0 * 128, bo + (g0 + 2) * 128), np.arange(co + g0 * 128, co + (g0 + 2) * 128)])
    m["c_wbc" + tag] = lay(w[:, bc_cols])
    dto = xo + 3072
    m["c_wdt" + tag] = lay(w[:, dto + 16 * hh:dto + 16 * (hh + 1)])
    ch = np.concatenate([np.arange(hh * 1024, (hh + 1) * 1024), bc_cols - xo])
    cw = P["ssm_conv_w"][0][:, ch]
    m["c_convw" + tag] = np.ascontiguousarray(cw.reshape(4, 12, 128).transpose(2, 1, 0))
    m["c_convb" + tag] = np.ascontiguousarray(P["ssm_conv_b"][0][ch].reshape(12, 128).T)
    hs = slice(16 * hh, 16 * (hh + 1))
    m["c_dtb" + tag] = np.ascontiguousarray(P["ssm_dt_bias"][0][hs])
    m["c_alog" + tag] = np.ascontiguousarray(P["ssm_a_log"][0][hs])
    m["c_dsk" + tag] = np.ascontiguousarray(P["ssm_d"][0][hs])
    m["c_ng" + tag] = np.ascontiguousarray(P["ssm_norm"][0][hh * 1024:(hh + 1) * 1024])
    return m


def emit_c(k, d, psum, pbf, yn_d, ident, tag="", n_tiles=8, dbg=None):
    idf, idb = ident
    m0 = k.mark()
    bcol = lambda ap, n: ap.unsqueeze(2).to_broadcast([128, ap.shape[1], n])
    tri = k.sb("c_tri", [128, 128], F32)
    ones = k.sb("c_ones", [128, 128], F32)
    negm = k.sb("c_negm", [128, 128], BF16)
    zer = k.sb("c_zer", [128, 128], F32)
    epsb = k.sb("c_epsb", [128, 1], F32)
    k.op("pool", lambda e: e.memset(ones[:], 1.0), writes=[ones])
    k.op("pool", lambda e: e.memset(zer[:], 0.0), writes=[zer])
    k.op("pool", lambda e: e.memset(epsb[:], EPS), writes=[epsb])
    k.op("pool", lambda e: e.affine_select(out=tri[:], in_=ones[:], pattern=[[1, 128]], compare_op=ALU.is_ge, fill=0.0,
                                           base=0, channel_multiplier=-1), reads=[ones], writes=[tri])
    k.op("pool", lambda e: e.affine_select(out=negm[:], in_=zer[:], pattern=[[1, 128]], compare_op=ALU.is_ge, fill=NEG,
                                           base=0, channel_multiplier=-1), reads=[zer], writes=[negm])
    convw = k.sb("c_convw", [128, 12, 4], F32)
    convb = k.sb("c_convb", [128, 12], F32)
    dtb = k.sb("c_dtb", [128, 16], F32)
    a_bc = k.sb("c_abc", [128, 16], F32)
    dsk = k.sb("c_dsk", [128, 16], F32)
    ng = k.sb("c_ng", [128, 1024], F32)
    m_sh = k.sb("c_msh", [128, 1024], F32)
    m_gm = k.sb("c_mgm", [128, 1024], F32)
    k.dma("sp", convw[:], d["convw"][:], reads=[d["convw"]], writes=[convw])
    k.dma("sp", convb[:], d["convb"][:], reads=[d["convb"]], writes=[convb])
    k.dma("sp", dtb[:], d["dtb"][:].partition_broadcast(128), reads=[d["dtb"]], writes=[dtb])
    k.dma("sp", a_bc[:], d["alog"][:].partition_broadcast(128), reads=[d["alog"]], writes=[a_bc])
    k.dma("sp", dsk[:], d["dsk"][:].partition_broadcast(128), reads=[d["dsk"]], writes=[dsk])
    k.dma("sp", ng[:], d["ng"][:].partition_broadcast(128), reads=[d["ng"]], writes=[ng])
    k.op("act", lambda e: e.activation(out=a_bc[:], in_=a_bc[:], func=AF.Exp), reads=[a_bc], writes=[a_bc])
    k.op("dve", lambda e: e.tensor_scalar(out=a_bc[:], in0=a_bc[:], scalar1=-1.0, scalar2=None, op0=ALU.mult),
         reads=[a_bc], writes=[a_bc])
    wz = k.sb("c_wz", [128, 8, 1024], BF16)
    wx = k.sb("c_wx", [128, 8, 1024], BF16)
    wbc = k.sb("c_wbc", [128, 8, 512], BF16)
    wdt = k.sb("c_wdt", [128, 8, 16], BF16)
    for wt, nm in ((wz, "wz"), (wx, "wx"), (wbc, "wbc"), (wdt, "wdt")):
        k.dma("pool", wt[:], d[nm][:], reads=[d[nm]], writes=[wt])
    m1 = k.mark()
    stage = k.sb("c_adast", [128, 8, 1024], F32)
    gain_bc = k.sb("c_gainbc", [128, 1024], F32)
    k.dma("sp", gain_bc[:], d["gain"][:].partition_broadcast(128), reads=[d["gain"]], writes=[gain_bc])
    adaln(k, d["ccol"], d["adaw"], d["adab"], 2, [m_sh, m_gm], [stage, stage], psum[6:8], "c" + tag)
    k.op("dve", lambda e: e.scalar_tensor_tensor(out=m_gm[:], in0=m_gm[:], scalar=1.0, in1=gain_bc[:],
                                                 op0=ALU.add, op1=ALU.mult), reads=[m_gm, gain_bc], writes=[m_gm])
    k.release(m1)

    hr = [k.sb("c_hr%d" % i, [128, 1024], F32) for i in range(2)]
    tp = [k.sb("c_tp%d" % i, [128, 1024], F32) for i in range(2)]
    ut = [k.sb("c_ut%d" % i, [128, 1024], BF16) for i in range(2)]
    st = [k.sb("c_st%d" % i, [128, 8], F32) for i in range(2)]
    uT = [k.sb("c_uT%d" % i, [128, 8, 512], BF16) for i in range(2)]
    xpre = k.sb("c_xpre", [128, 12, 515], F32)
    ctmp = [k.sb("c_ctmp%d" % i, [128, 512], F32) for i in range(2)]
    xsa = k.sb("c_xsa", [128, 8, 512], F32)
    bcT = k.sb("c_bcT", [128, 4, 512], BF16)
    zs = k.sb("c_zs", [128, 4, 1024], BF16)
    dt_t = k.sb("c_dt", [128, 4, 16], F32)
    sm = k.sb("c_sm", [128, 4, 16], F32)
    sm2 = k.sb("c_sm2", [128, 4, 16], F32)
    xs_tok = [k.sb("c_xst%d" % i, [128, 16, 64], F32) for i in range(2)]
    xd = k.sb("c_xd", [128, 16, 64], BF16)
    xdd = k.sb("c_xdd", [128, 16, 64], BF16)
    btok = k.sb("c_btok", [128, 2, 128], BF16)
    dtA = k.sb("c_dtA", [128, 16], F32)
    dtAb = k.sb("c_dtAb", [128, 16, 128], F32)
    cs = k.sb("c_cs", [128, 6, 16], F32)
    dec = [k.sb("c_dec%d" % i, [128, 4, 128], F32) for i in range(2)]
    MT = k.sb("c_MT", [128, 16, 128], BF16)
    cbT = k.sb("c_cbT", [128, 2, 128], F32)
    y1 = k.sb("c_y1", [128, 16, 64], F32)
    y2 = k.sb("c_y2", [128, 16, 64], F32)
    ynt = [k.sb("c_ynt%d" % i, [128, 1024], BF16) for i in range(2)]
    S = k.sb("c_S", [128, 2, 512], F32)
    Sb = k.sb("c_Sb", [128, 2, 512], BF16)
    k.op("pool", lambda e: e.memset(S[:], 0.0), writes=[S])
    k.op("pool", lambda e: e.memset(Sb[:], 0.0), writes=[Sb])
    k.op("pool", lambda e: e.memset(xpre[:, :, 0:3], 0.0), writes=[xpre])

    toks = []
    for T in range(n_tiles):
        uTt = uT[T % 2]
        for s in range(4):
            i2 = (T * 4 + s) % 2
            hr_, tp_, ut_, st_ = hr[i2], tp[i2], ut[i2], st[i2]
            rows = slice(T * 512 + s * 128, T * 512 + (s + 1) * 128)
            k.dma("sp", hr_[:], d["hin"][rows, :], reads=[d["hin"]], writes=[hr_])
            k.op("act", lambda e, hr_=hr_, tp_=tp_, st_=st_: e.activation(out=tp_[:], in_=hr_[:], func=AF.Square,
                                                                         accum_out=st_[:, 0:1]), reads=[hr_], writes=[tp_, st_])
            k.op("act", lambda e, st_=st_: e.activation(out=st_[:, 1:2], in_=st_[:, 0:1], func=AF.Sqrt, bias=epsb[:],
                                                        scale=1.0 / 1024.0), reads=[st_, epsb], writes=[st_])
            k.op("dve", lambda e, st_=st_: e.reciprocal(out=st_[:, 2:3], in_=st_[:, 1:2]), reads=[st_], writes=[st_])
            k.op("dve", lambda e, hr_=hr_, tp_=tp_, st_=st_: e.scalar_tensor_tensor(
                out=tp_[:], in0=hr_[:], scalar=st_[:, 2:3], in1=m_gm[:], op0=ALU.mult, op1=ALU.mult),
                reads=[hr_, st_, m_gm], writes=[tp_])
            k.op("pool", lambda e, tp_=tp_, ut_=ut_: e.tensor_tensor(out=ut_[:], in0=tp_[:], in1=m_sh[:], op=ALU.add),
                 reads=[tp_, m_sh], writes=[ut_])
            pb, pbv = psum[6 + s % 2], pbf[6 + s % 2]
            for kc in range(8):
                tr(k, pb, pbv[:, kc * 128:(kc + 1) * 128], ut_, ut_[:, kc * 128:(kc + 1) * 128], idb, idb[:])
            k.op("act", lambda e, s=s, pbv=pbv, uTt=uTt: e.activation(
                out=uTt[:, :, s * 128:(s + 1) * 128], in_=pbv.rearrange("p (a b) -> p a b", a=8), func=AF.Copy),
                reads=[pb], writes=[uTt])
        for s in range(4):
            for half in range(2):
                pb = psum[(2 * s + half) % 2]
                for kc in range(8):
                    mm(k, pb, pb[:], uTt, uTt[:, kc, s * 128:(s + 1) * 128], wz, wz[:, kc, half * 512:(half + 1) * 512],
                       kc == 0, kc == 7)
                k.op("act", lambda e, s=s, half=half, pb=pb: e.activation(
                    out=zs[:, s, half * 512:(half + 1) * 512], in_=pb[:], func=AF.Silu), reads=[pb], writes=[zs])
        for s in range(4):
            pb = psum[2 + s % 2]
            for kc in range(8):
                mm(k, pb, pb[:, 0:16], uTt, uTt[:, kc, s * 128:(s + 1) * 128], wdt, wdt[:, kc, :], kc == 0, kc == 7)
            k.op("dve", lambda e, s=s, pb=pb: e.tensor_tensor(out=sm[:, s, :], in0=pb[:, 0:16], in1=dtb[:], op=ALU.add),
                 reads=[pb, dtb], writes=[sm])
        k.op("act", lambda e: e.activation(out=sm2[:], in_=sm[:], func=AF.Abs), reads=[sm], writes=[sm2])
        k.op("act", lambda e: e.activation(out=sm2[:], in_=sm2[:], func=AF.Exp, scale=-1.0), reads=[sm2], writes=[sm2])
        k.op("act", lambda e: e.activation(out=sm2[:], in_=sm2[:], func=AF.Ln, bias=1.0, scale=1.0), reads=[sm2], writes=[sm2])
        k.op("dve", lambda e: e.scalar_tensor_tensor(out=dt_t[:], in0=sm[:], scalar=0.0, in1=sm2[:], op0=ALU.max,
                                                     op1=ALU.add), reads=[sm, sm2], writes=[dt_t])
        for c in range(12):
            pb = psum[4 + c % 2]
            wsrc, col = (wx, c * 128) if c < 8 else (wbc, (c - 8) * 128)
            for kc in range(8):
                mm(k, pb, pb[:], wsrc, wsrc[:, kc, col:col + 128], uTt, uTt[:, kc, :], kc == 0, kc == 7)
            k.op("act", lambda e, c=c, pb=pb: e.activation(out=xpre[:, c, 3:515], in_=pb[:], func=AF.Copy),
                 reads=[pb], writes=[xpre])
        for c in range(12):
            ct = ctmp[c % 2]
            eng = "dve" if c % 2 == 0 else "pool"
            k.op(eng, lambda e, c=c, ct=ct: e.tensor_scalar(out=ct[:], in0=xpre[:, c, 0:512], scalar1=convw[:, c, 0:1],
                                                            scalar2=None, op0=ALU.mult), reads=[xpre, convw], writes=[ct])
            for tap in range(1, 4):
                k.op("dve", lambda e, c=c, ct=ct, tap=tap: e.scalar_tensor_tensor(
                    out=ct[:], in0=xpre[:, c, tap:tap + 512], scalar=convw[:, c, tap:tap + 1], in1=ct[:],
                    op0=ALU.mult, op1=ALU.add), reads=[xpre, convw, ct], writes=[ct])
            if c < 8:
                k.op("act", lambda e, c=c, ct=ct: e.activation(out=xsa[:, c, :], in_=ct[:], func=AF.Silu,
                                                               bias=convb[:, c:c + 1], scale=1.0),
                     reads=[ct, convb], writes=[xsa])
            else:
                k.op("act", lambda e, c=c, ct=ct: e.activation(out=bcT[:, c - 8, :], in_=ct[:], func=AF.Silu,
                                                               bias=convb[:, c:c + 1], scale=1.0),
                     reads=[ct, convb], writes=[bcT])
        k.op("pool", lambda e: e.tensor_copy(out=xpre[:, :, 0:3], in_=xpre[:, :, 512:515]), reads=[xpre], writes=[xpre])

        for s in range(4):
            ci = T * 4 + s
            cols = slice(s * 128, (s + 1) * 128)
            xst = xs_tok[ci % 2]
            for hf in range(2):
                pb = psum[hf]
                for c4 in range(4):
                    c = hf * 4 + c4
                    tr(k, pb, pb[:, c4 * 128:(c4 + 1) * 128], xsa, xsa[:, c, cols], idf, idf[:])
                k.op("act", lambda e, hf=hf, pb=pb, xst=xst: e.activation(
                    out=xst[:, hf * 8:(hf + 1) * 8, :], in_=pb[:].rearrange("p (a b) -> p a b", a=8), func=AF.Copy),
                    reads=[pb], writes=[xst])
            pb, pbv = psum[2], pbf[2]
            for g in range(2):
                tr(k, pb, pbv[:, g * 128:(g + 1) * 128], bcT, bcT[:, g, cols], idb, idb[:])
            k.op("act", lambda e, pbv=pbv: e.activation(out=btok[:], in_=pbv[:, 0:256].rearrange("p (a b) -> p a b", a=2),
                                                        func=AF.Copy), reads=[pb], writes=[btok])
            k.op("dve", lambda e, s=s: e.tensor_tensor(out=dtA[:], in0=dt_t[:, s, :], in1=a_bc[:], op=ALU.mult),
                 reads=[dt_t, a_bc], writes=[dtA])
            k.op("pool", lambda e: e.tensor_copy(out=dtAb[:], in_=bcol(dtA[:], 128)), reads=[dtA], writes=[dtAb])
            pb = psum[3]
            mm(k, pb, pb[:, 0:16], tri, tri[:], dtA, dtA[:], True, True)
            mm(k, pb, pb[:, 16:32], ones, ones[:], dtA, dtA[:], True, True)
            k.op("act", lambda e, pb=pb: e.activation(out=cs[:, 0, :], in_=pb[:, 0:16], func=AF.Copy), reads=[pb], writes=[cs])
            k.op("dve", lambda e, pb=pb: e.tensor_scalar(out=cs[:, 1, :], in0=pb[:, 0:16], scalar1=-1.0, scalar2=None,
                                                         op0=ALU.mult), reads=[pb], writes=[cs])
            k.op("act", lambda e, pb=pb: e.activation(out=cs[:, 2, :], in_=pb[:, 0:16], func=AF.Exp), reads=[pb], writes=[cs])
            k.op("act", lambda e, pb=pb: e.activation(out=cs[:, 5, :], in_=pb[:, 16:32], func=AF.Exp), reads=[pb], writes=[cs])
            k.op("dve", lambda e, pb=pb: e.tensor_tensor(out=cs[:, 3, :], in0=pb[:, 16:32], in1=cs[:, 0, :], op=ALU.subtract),
                 reads=[pb, cs], writes=[cs])
            k.op("act", lambda e: e.activation(out=cs[:, 3, :], in_=cs[:, 3, :], func=AF.Exp), reads=[cs], writes=[cs])
            k.op("dve", lambda e, s=s: e.tensor_tensor(out=cs[:, 4, :], in0=cs[:, 3, :], in1=dt_t[:, s, :], op=ALU.mult),
                 reads=[cs, dt_t], writes=[cs])
            k.op("dve", lambda e, s=s, xst=xst: e.tensor_tensor(out=xd[:], in0=xst[:], in1=bcol(dt_t[:, s, :], 64), op=ALU.mult),
                 reads=[xst, dt_t], writes=[xd])
            k.op("pool", lambda e, xst=xst: e.tensor_tensor(out=xdd[:], in0=xst[:], in1=bcol(cs[:, 4, :], 64), op=ALU.mult),
                 reads=[xst, cs], writes=[xdd])
            pb = psum[2]
            for g in range(2):
                mm(k, pb, pb[:, 256 + g * 128:256 + (g + 1) * 128], bcT, bcT[:, g, cols], bcT, bcT[:, 2 + g, cols], True, True)
            k.op("act", lambda e, pb=pb: e.activation(out=cbT[:], in_=pb[:, 256:512].rearrange("p (a b) -> p a b", a=2),
                                                      func=AF.Copy), reads=[pb], writes=[cbT])
            for q in range(4):
                pb = psum[4 + q % 2]
                dq = dec[q % 2]
                for hh_ in range(4):
                    h = q * 4 + hh_
                    o = pb[:, hh_ * 128:(hh_ + 1) * 128]
                    mm(k, pb, o, dtAb, dtAb[:, h, :], tri, tri[:], True, False)
                    mm(k, pb, o, idb, idb[:], negm, negm[:], False, True)
                for hh_ in range(4):
                    h = q * 4 + hh_
                    k.op("act", lambda e, pb=pb, dq=dq, hh_=hh_, h=h: e.activation(
                        out=dq[:, hh_, :], in_=pb[:, hh_ * 128:(hh_ + 1) * 128], func=AF.Exp, bias=cs[:, 1, h:h + 1], scale=1.0),
                        reads=[pb, cs], writes=[dq])
                g = q // 2
                eng = "dve" if q % 2 == 0 else "pool"
                k.op(eng, lambda e, q=q, dq=dq, g=g: e.tensor_tensor(
                    out=MT[:, q * 4:(q + 1) * 4, :], in0=dq[:], in1=cbT[:, g:g + 1, :].to_broadcast([128, 4, 128]), op=ALU.mult),
                    reads=[dq, cbT], writes=[MT])
            for h in range(16):
                pb = psum[h // 8]
                mm(k, pb, pb[:, (h % 8) * 64:(h % 8 + 1) * 64], MT, MT[:, h, :], xd, xd[:, h, :], True, True)
            for g in range(2):
                pb = psum[6 + g]
                mm(k, pb, pb[:], bcT, bcT[:, 2 + g, cols], Sb, Sb[:, g, :], True, True)
            for g in range(2):
                hs = slice(g * 8, (g + 1) * 8)
                po, pdg = psum[6 + g], psum[g]
                k.op("dve", lambda e, hs=hs, po=po: e.tensor_tensor(
                    out=y1[:, hs, :], in0=po[:].rearrange("p (a b) -> p a b", a=8), in1=bcol(cs[:, 2, hs], 64), op=ALU.mult),
                    reads=[po, cs], writes=[y1])
                k.op("pool", lambda e, hs=hs, xst=xst: e.tensor_tensor(
                    out=y2[:, hs, :], in0=xst[:, hs, :], in1=bcol(dsk[:, hs], 64), op=ALU.mult), reads=[xst, dsk], writes=[y2])
                k.op("dve", lambda e, hs=hs, pdg=pdg: e.tensor_tensor(
                    out=y1[:, hs, :], in0=pdg[:].rearrange("p (a b) -> p a b", a=8), in1=y1[:, hs, :], op=ALU.add),
                    reads=[pdg, y1], writes=[y1])
                k.op("pool", lambda e, hs=hs: e.tensor_tensor(out=y1[:, hs, :], in0=y1[:, hs, :], in1=y2[:, hs, :], op=ALU.add),
                     reads=[y1, y2], writes=[y1])
            if dbg is not None and "yssd" in dbg:
                toks.append(k.dma("sp", dbg["yssd"][ci * 128:(ci + 1) * 128, :], y1[:].rearrange("p a b -> p (a b)"),
                                  reads=[y1], writes=[dbg["yssd"]]))
            y1f = y1[:].rearrange("p a b -> p (a b)")
            y2f = y2[:].rearrange("p a b -> p (a b)")
            k.op("dve", lambda e, s=s, y1f=y1f: e.tensor_tensor(out=y1f, in0=y1f, in1=zs[:, s, :], op=ALU.mult),
                 reads=[y1, zs], writes=[y1])
            st_ = st[ci % 2]
            yo = ynt[ci % 2]
            for g in range(2):
                gs = slice(g * 512, (g + 1) * 512)
                k.op("act", lambda e, gs=gs, g=g, st_=st_, y1f=y1f, y2f=y2f: e.activation(
                    out=y2f[:, gs], in_=y1f[:, gs], func=AF.Square, accum_out=st_[:, 4 + g:5 + g]), reads=[y1], writes=[y2, st_])
            k.op("act", lambda e, st_=st_: e.activation(out=st_[:, 6:8], in_=st_[:, 4:6], func=AF.Sqrt, bias=epsb[:],
                                                        scale=1.0 / 512.0), reads=[st_, epsb], writes=[st_])
            k.op("dve", lambda e, st_=st_: e.reciprocal(out=st_[:, 6:8], in_=st_[:, 6:8]), reads=[st_], writes=[st_])
            for g in range(2):
                gs = slice(g * 512, (g + 1) * 512)
                k.op("dve", lambda e, gs=gs, g=g, st_=st_, yo=yo, y1f=y1f: e.scalar_tensor_tensor(
                    out=yo[:, gs], in0=y1f[:, gs], scalar=st_[:, 6 + g:7 + g], in1=ng[:, gs], op0=ALU.mult, op1=ALU.mult),
                    reads=[y1, st_, ng], writes=[yo])
            toks.append(k.dma("sp", yn_d[ci * 128:(ci + 1) * 128, :], yo[:], reads=[yo], writes=[yn_d]))
            for g in range(2):
                pb = psum[2 + g]
                hs = slice(g * 8, (g + 1) * 8)
                mm(k, pb, pb[:], btok, btok[:, g, :], xdd, xdd[:, hs, :].rearrange("p a b -> p (a b)"), True, True)
                Sg = S[:, g, :].rearrange("p (a b) -> p a b", a=8)
                eng = "dve" if g == 0 else "pool"
                k.op(eng, lambda e, Sg=Sg, hs=hs: e.tensor_tensor(out=Sg, in0=Sg, in1=bcol(cs[:, 5, hs], 64), op=ALU.mult),
                     reads=[S, cs], writes=[S])
                k.op("dve", lambda e, g=g, pb=pb: e.tensor_tensor(out=S[:, g, :], in0=pb[:], in1=S[:, g, :], op=ALU.add),
                     reads=[pb, S], writes=[S])
            k.op("act", lambda e: e.activation(out=Sb[:], in_=S[:], func=AF.Copy), reads=[S], writes=[Sb])
    k.release(m0)
    return toks


def build_c(n_tiles=8, debug=False):
    nc = bass.Bass("TRN2", target_bir_lowering=False)
    with contextlib.ExitStack() as stack:
        k = KB(nc, stack)
        psum, pbf = make_psum(k)
        d = c_dram(k)
        yn_d = k.dram("yn", [SEQ, 1024], BF16, "ExternalOutput")
        ident = make_ident(k, "id", BF16)
        dbg = None
        if debug:
            dbg = {"yssd": k.dram("dbg_yssd", [SEQ, 1024], F32, "ExternalOutput")}
        toks = emit_c(k, d, psum, pbf, yn_d, ident, "", n_tiles, dbg)
        k.emit(final_waits=toks)
        print("C arena peak words", k.apeak, "instr", {e: len(v) for e, v in k.prog.items()})
    return nc


def a_dram(k, tag=""):
    d = {}
    def inp(name, shape, dt=F32):
        d[name] = k.dram("a_" + name + tag, shape, dt, "ExternalInput")
    inp("xin", [SEQ, 1024]); inp("ccol", [128, 8]); inp("adaw", [2, 128, 8, 1024]); inp("adab", [2, 1024])
    inp("gain", [1024]); inp("wsb", [128, 8, 768]); inp("wnq", [128, 8, 256]); inp("wkv", [128, 8, 384])
    inp("wg", [128, 8, 12]); inp("qn", [64, 1]); inp("kn", [64, 3]); inp("pek", [64, 32, 2]); inp("pev", [64, 32, 2])
    inp("w1k", [64, 32, 128]); inp("w1v", [64, 32, 128]); inp("w2k", [128, 64]); inp("w2v", [128, 64])
    inp("qaug", [4, 4, SEQ], BF16); inp("kaug", [4, SEQ], BF16); inp("caug", [4, 256], BF16)
    inp("cmask", [128, 2, SEQ], BF16); inp("ovl", [128, 2, 64], BF16); inp("E", [64, 32, 128], BF16)
    inp("m1", [32, 128, 64]); inp("a1", [32, 128, 64])
    return d


def a_host_consts(hh):
    m = {}
    tok = np.arange(SEQ)
    slopes = np.array([2.0 ** (-(4 * hh + r + 1)) for r in range(4)], np.float32)
    qa = np.zeros((4, 4, SEQ), np.float32)
    for r in range(4):
        qa[0, r] = -slopes[r] * (tok % 128)
        qa[1, r] = slopes[r]
        qa[2, r] = -slopes[r] * 128.0 * (tok // 128)
        qa[3, r] = slopes[r] * 128.0
    m["a_qaug"] = qa.astype(NPBF)
    ka = np.stack([np.ones(SEQ), tok % 128, np.ones(SEQ), tok // 128]).astype(np.float32)
    m["a_kaug"] = ka.astype(NPBF)
    n = np.arange(256)
    ce = 16 * n + 31
    ca = np.stack([np.ones(256), ce % 128, np.ones(256), ce // 128]).astype(np.float32)
    ca[:, 255] = 0
    m["a_caug"] = ca.astype(NPBF)
    cm = np.where((ce[:, None] <= tok[None, :]) & (n[:, None] < 255), 0.0, NEG).astype(np.float32)
    m["a_cmask"] = np.ascontiguousarray(cm.reshape(2, 128, SEQ).transpose(1, 0, 2)).astype(NPBF)
    cs_ = n * 16
    sl = np.arange(64) * 64
    ov = ((cs_[:, None] < sl[None, :] + 64) & (cs_[:, None] + 32 > sl[None, :]) & (n[:, None] < 255)).astype(np.float32)
    m["a_ovl"] = np.ascontiguousarray(ov.reshape(2, 128, 64).transpose(1, 0, 2)).astype(NPBF)
    E = np.zeros((64, 32, 128), np.float32)
    for s in range(32):
        for kk in range(128):
            E[2 * s + kk // 64, s, kk] = 1.0
    m["a_E"] = E.astype(NPBF)
    j = np.arange(64)
    t = tok.reshape(32, 128)
    cur = t // 64
    valid = j[None, None, :] * 64 <= t[:, :, None]
    forced = (j[None, None, :] == 0) | (j[None, None, :] == cur[:, :, None]) | (j[None, None, :] == cur[:, :, None] - 1)
    m["a_m1"] = (valid & ~forced).astype(np.float32)
    m["a_a1"] = np.where(valid, np.where(forced, 1e6 + j[None, None, :], 0.0), -1e6).astype(np.float32)
    return m


def a_host_inputs(b, hh, P):
    m = {}
    lay = lambda w: np.ascontiguousarray(w.reshape(8, 128, -1).transpose(1, 0, 2))
    m["a_xin"] = np.ascontiguousarray(P["x"][b])
    m["a_ccol"] = np.ascontiguousarray(P["c"][b].reshape(8, 128).T)
    aw = P["ada_w"][0]
    m["a_adaw"] = np.ascontiguousarray(np.stack([lay(aw[:, v * 1024:(v + 1) * 1024]) for v in (0, 1)]))
    m["a_adab"] = np.ascontiguousarray(np.stack([P["ada_b"][0][v * 1024:(v + 1) * 1024] for v in (0, 1)]))
    m["a_gain"] = np.ascontiguousarray(P["norm_mix"][0])
    w = P["attn_w_in"][0]
    hs = slice(hh * 256, (hh + 1) * 256)
    m["a_wsb"] = lay(np.concatenate([w[:, 0:512][:, hs], w[:, 512:1024][:, hs], w[:, 1024:1536][:, hs]], axis=1))
    m["a_wnq"] = lay(w[:, 1536:2048][:, hs])
    kv = [w[:, 2048 + i * 128 + hh * 64: 2048 + i * 128 + (hh + 1) * 64] for i in range(6)]
    m["a_wkv"] = lay(np.concatenate(kv, axis=1))
    m["a_wg"] = lay(w[:, 2816 + 12 * hh: 2816 + 12 * (hh + 1)])
    m["a_qn"] = np.ascontiguousarray(P["nsa_q_norm"][0].reshape(64, 1))
    m["a_kn"] = np.ascontiguousarray(P["nsa_k_norm"][0].T)
    m["a_pek"] = np.ascontiguousarray(np.repeat(P["cmp_pe_k"][0].T[:, :, None], 2, axis=2))
    m["a_pev"] = np.ascontiguousarray(np.repeat(P["cmp_pe_v"][0].T[:, :, None], 2, axis=2))
    m["a_w1k"] = np.ascontiguousarray(P["cmp_w1_k"][0].reshape(32, 64, 128).transpose(1, 0, 2))
    m["a_w1v"] = np.ascontiguousarray(P["cmp_w1_v"][0].reshape(32, 64, 128).transpose(1, 0, 2))
    m["a_w2k"] = np.ascontiguousarray(P["cmp_w2_k"][0])
    m["a_w2v"] = np.ascontiguousarray(P["cmp_w2_v"][0])
    m.update(a_host_consts(hh))
    return m


def emit_a(k, d, psum, pbf, pall, o_d, ident, tag="", n_qt=32, dbg=None):
    idf, idb = ident
    m0 = k.mark()
    toks_dbg = []
    NTL = SEQ // 128
    sbqT = k.sb("a_sbqT", [128, 2, SEQ], BF16)
    sbkT = k.sb("a_sbkT", [128, 2, SEQ], BF16)
    sbv = k.sb("a_sbv", [128, NTL, 256], BF16)
    nqT = k.sb("a_nqT", [68, 4, SEQ], BF16)
    ksT = k.sb("a_ksT", [68, SEQ], BF16)
    kwT = k.sb("a_kwT", [68, SEQ], BF16)
    kcT = k.sb("a_kcT", [64, SEQ], BF16)
    vcT = k.sb("a_vcT", [64, SEQ], BF16)
    vsA = k.sb("a_vsA", [128, NTL, 65], BF16)
    vwA = k.sb("a_vwA", [128, NTL, 65], BF16)
    gts = k.sb("a_gts", [128, NTL, 12], F32)
    kcmpT = k.sb("a_kcmpT", [68, 256], BF16)
    vcmpA = k.sb("a_vcmpA", [128, 2, 129], BF16)
    ones = k.sb("a_ones", [128, 128], F32)
    epsb = k.sb("a_epsb", [128, 1], F32)
    qn8 = k.sb("a_qn8", [64, 1], F32)
    kn = k.sb("a_kn", [64, 3], F32)
    k.op("pool", lambda e: e.memset(ones[:], 1.0), writes=[ones])
    k.op("pool", lambda e: e.memset(epsb[:], EPS), writes=[epsb])
    k.op("pool", lambda e: e.memset(vsA[:], 1.0), writes=[vsA])
    k.op("pool", lambda e: e.memset(vwA[:], 1.0), writes=[vwA])
    k.op("pool", lambda e: e.memset(vcmpA[:], 1.0), writes=[vcmpA])
    k.dma("sp", qn8[:], d["qn"][:], reads=[d["qn"]], writes=[qn8])
    k.dma("sp", kn[:], d["kn"][:], reads=[d["kn"]], writes=[kn])
    k.op("dve", lambda e: e.tensor_scalar(out=qn8[:], in0=qn8[:], scalar1=0.125, scalar2=None, op0=ALU.mult),
         reads=[qn8], writes=[qn8])
    k.dma("sp", nqT[64:68, :, :], d["qaug"][:], reads=[d["qaug"]], writes=[nqT])
    k.dma("sp", ksT[64:68, :], d["kaug"][:], reads=[d["kaug"]], writes=[ksT])
    k.dma("sp", kwT[64:68, :], d["kaug"][:], reads=[d["kaug"]], writes=[kwT])
    k.dma("sp", kcmpT[64:68, :], d["caug"][:], reads=[d["caug"]], writes=[kcmpT])
    k.dma("sp", vcmpA[:, :, 65:129], d["ovl"][:], reads=[d["ovl"]], writes=[vcmpA])

    m1 = k.mark()
    m_sh = k.sb("a_msh", [128, 1024], F32)
    m_gm = k.sb("a_mgm", [128, 1024], F32)
    qf = [k.sb("a_qf%d" % i, [64, 512], F32) for i in range(2)]
    sq = [k.sb("a_sq%d" % i, [64, 512], F32) for i in range(2)]
    rs = [k.sb("a_rs%d" % i, [64, 512], F32) for i in range(2)]
    m2 = k.mark()
    stage = k.sb("a_adast", [128, 8, 1024], F32)
    gain_bc = k.sb("a_gainbc", [128, 1024], F32)
    k.dma("sp", gain_bc[:], d["gain"][:].partition_broadcast(128), reads=[d["gain"]], writes=[gain_bc])
    adaln(k, d["ccol"], d["adaw"], d["adab"], 2, [m_sh, m_gm], [stage, stage], psum[6:8], "a" + tag)
    k.op("dve", lambda e: e.scalar_tensor_tensor(out=m_gm[:], in0=m_gm[:], scalar=1.0, in1=gain_bc[:],
                                                 op0=ALU.add, op1=ALU.mult), reads=[m_gm, gain_bc], writes=[m_gm])
    k.release(m2)
    wsb = k.sb("a_wsb", [128, 8, 768], BF16)
    wnq = k.sb("a_wnq", [128, 8, 256], BF16)
    wkv = k.sb("a_wkv", [128, 8, 384], BF16)
    wg = k.sb("a_wg", [128, 8, 12], BF16)
    for wt, nm in ((wsb, "wsb"), (wnq, "wnq"), (wkv, "wkv"), (wg, "wg")):
        k.dma("pool", wt[:], d[nm][:], reads=[d[nm]], writes=[wt])
    hr = [k.sb("a_hr%d" % i, [128, 1024], F32) for i in range(2)]
    tp = [k.sb("a_tp%d" % i, [128, 1024], F32) for i in range(2)]
    ut = [k.sb("a_ut%d" % i, [128, 1024], BF16) for i in range(2)]
    st = [k.sb("a_st%d" % i, [128, 8], F32) for i in range(2)]
    uT = [k.sb("a_uT%d" % i, [128, 8, 512], BF16) for i in range(2)]
    n64 = [0]

    def norm64(pb, src, gain_ap, gain_buf, out_buf, out_ap, ncols):
        i = n64[0] % 2
        n64[0] += 1
        q_, s_, r_ = qf[i], sq[i], rs[i]
        k.op("act", lambda e: e.activation(out=q_[:, 0:ncols], in_=src, func=AF.Copy), reads=[pb], writes=[q_])
        k.op("act", lambda e: e.activation(out=s_[:, 0:ncols], in_=src, func=AF.Square), reads=[pb], writes=[s_])
        p2 = psum[5]
        mm(k, p2, p2[0:64, 0:ncols], ones, ones[0:64, 0:64], s_, s_[:, 0:ncols], True, True)
        k.op("act", lambda e: e.activation(out=r_[:, 0:ncols], in_=p2[0:64, 0:ncols], func=AF.Sqrt, bias=epsb[0:64, :],
                                           scale=1.0 / 64.0), reads=[p2, epsb], writes=[r_])
        k.op("dve", lambda e: e.reciprocal(out=r_[:, 0:ncols], in_=r_[:, 0:ncols]), reads=[r_], writes=[r_])
        k.op("dve", lambda e: e.scalar_tensor_tensor(out=out_ap, in0=q_[:, 0:ncols], scalar=gain_ap, in1=r_[:, 0:ncols],
                                                     op0=ALU.mult, op1=ALU.mult), reads=[q_, r_, gain_buf], writes=[out_buf])

    n_t1 = (n_qt * 128 + 511) // 512
    for T in range(n_t1):
        uTt = uT[T % 2]
        tcols = slice(T * 512, (T + 1) * 512)
        for s in range(4):
            i2 = (T * 4 + s) % 2
            hr_, tp_, ut_, st_ = hr[i2], tp[i2], ut[i2], st[i2]
            rows = slice(T * 512 + s * 128, T * 512 + (s + 1) * 128)
            k.dma("sp", hr_[:], d["xin"][rows, :], reads=[d["xin"]], writes=[hr_])
            k.op("act", lambda e, hr_=hr_, tp_=tp_, st_=st_: e.activation(out=tp_[:], in_=hr_[:], func=AF.Square,
                                                                         accum_out=st_[:, 0:1]), reads=[hr_], writes=[tp_, st_])
            k.op("act", lambda e, st_=st_: e.activation(out=st_[:, 1:2], in_=st_[:, 0:1], func=AF.Sqrt, bias=epsb[:],
                                                        scale=1.0 / 1024.0), reads=[st_, epsb], writes=[st_])
            k.op("dve", lambda e, st_=st_: e.reciprocal(out=st_[:, 2:3], in_=st_[:, 1:2]), reads=[st_], writes=[st_])
            k.op("dve", lambda e, hr_=hr_, tp_=tp_, st_=st_: e.scalar_tensor_tensor(
                out=tp_[:], in0=hr_[:], scalar=st_[:, 2:3], in1=m_gm[:], op0=ALU.mult, op1=ALU.mult),
                reads=[hr_, st_, m_gm], writes=[tp_])
            k.op("pool", lambda e, tp_=tp_, ut_=ut_: e.tensor_tensor(out=ut_[:], in0=tp_[:], in1=m_sh[:], op=ALU.add),
                 reads=[tp_, m_sh], writes=[ut_])
            pb, pbv = psum[6 + s % 2], pbf[6 + s % 2]
            for kc in range(8):
                tr(k, pb, pbv[:, kc * 128:(kc + 1) * 128], ut_, ut_[:, kc * 128:(kc + 1) * 128], idb, idb[:])
            k.op("act", lambda e, s=s, pbv=pbv, uTt=uTt: e.activation(
                out=uTt[:, :, s * 128:(s + 1) * 128], in_=pbv.rearrange("p (a b) -> p a b", a=8), func=AF.Copy),
                reads=[pb], writes=[uTt])
        for c in range(4):
            pb = psum[c % 2]
            for kc in range(8):
                mm(k, pb, pb[:], wsb, wsb[:, kc, c * 128:(c + 1) * 128], uTt, uTt[:, kc, :], kc == 0, kc == 7)
            if c < 2:
                k.op("act", lambda e, c=c, pb=pb, tcols=tcols: e.activation(out=sbqT[:, c, tcols], in_=pb[:], func=AF.Copy, scale=0.125),
                     reads=[pb], writes=[sbqT])
            else:
                k.op("act", lambda e, c=c, pb=pb, tcols=tcols: e.activation(out=sbkT[:, c - 2, tcols], in_=pb[:], func=AF.Copy),
                     reads=[pb], writes=[sbkT])
        for s in range(4):
            tl = T * 4 + s
            scol = slice(s * 128, (s + 1) * 128)
            pb = psum[2 + s % 2]
            for kc in range(8):
                mm(k, pb, pb[:, 0:256], uTt, uTt[:, kc, scol], wsb, wsb[:, kc, 512:768], kc == 0, kc == 7)
            k.op("dve", lambda e, tl=tl, pb=pb: e.tensor_copy(out=sbv[:, tl, :], in_=pb[:, 0:256]), reads=[pb], writes=[sbv])
            for kc in range(8):
                mm(k, pb, pb[:, 256:320], uTt, uTt[:, kc, scol], wkv, wkv[:, kc, 192:256], kc == 0, kc == 7)
            for kc in range(8):
                mm(k, pb, pb[:, 320:384], uTt, uTt[:, kc, scol], wkv, wkv[:, kc, 320:384], kc == 0, kc == 7)
            for kc in range(8):
                mm(k, pb, pb[:, 384:396], uTt, uTt[:, kc, scol], wg, wg[:, kc, :], kc == 0, kc == 7)
            k.op("dve", lambda e, tl=tl, pb=pb: e.tensor_copy(out=vsA[:, tl, 0:64], in_=pb[:, 256:320]), reads=[pb], writes=[vsA])
            k.op("dve", lambda e, tl=tl, pb=pb: e.tensor_copy(out=vwA[:, tl, 0:64], in_=pb[:, 320:384]), reads=[pb], writes=[vwA])
            k.op("act", lambda e, tl=tl, pb=pb: e.activation(out=gts[:, tl, :], in_=pb[:, 384:396], func=AF.Sigmoid),
                 reads=[pb], writes=[gts])
        def proj64(wt, col):
            pb = psum[4]
            for kc in range(8):
                mm(k, pb, pb[0:64, :], wt, wt[:, kc, col:col + 64], uTt, uTt[:, kc, :], kc == 0, kc == 7)
            return pb
        for h in range(4):
            pb = proj64(wnq, h * 64)
            norm64(pb, pb[0:64, :], qn8[:, 0:1], qn8, nqT, nqT[0:64, h, tcols], 512)
        pb = proj64(wkv, 128)
        norm64(pb, pb[0:64, :], kn[:, 1:2], kn, ksT, ksT[0:64, tcols], 512)
        pb = proj64(wkv, 256)
        norm64(pb, pb[0:64, :], kn[:, 2:3], kn, kwT, kwT[0:64, tcols], 512)
        pb = proj64(wkv, 0)
        k.op("act", lambda e, pb=pb, tcols=tcols: e.activation(out=kcT[:, tcols], in_=pb[0:64, :], func=AF.Copy), reads=[pb], writes=[kcT])
        pb = proj64(wkv, 64)
        k.op("act", lambda e, pb=pb, tcols=tcols: e.activation(out=vcT[:, tcols], in_=pb[0:64, :], func=AF.Copy), reads=[pb], writes=[vcT])

    k.release(m2)
    if True:
        w1 = k.sb("a_w1", [64, 32, 128], BF16)
        w2 = k.sb("a_w2", [128, 64], BF16)
        pe = k.sb("a_pe", [64, 32, 2], BF16)
        hb = k.sb("a_hb", [128, 2], F32)
        hsT = k.sb("a_hsT", [128, 256], BF16)
        for is_k in (True, False):
            sfx = "k" if is_k else "v"
            src = kcT if is_k else vcT
            k.dma("pool", w1[:], d["w1" + sfx][:], reads=[d["w1" + sfx]], writes=[w1])
            k.dma("pool", w2[:], d["w2" + sfx][:], reads=[d["w2" + sfx]], writes=[w2])
            k.dma("pool", pe[:], d["pe" + sfx][:], reads=[d["pe" + sfx]], writes=[pe])
            pb, pb2 = psum[0], psum[1]
            for l in range(32):
                mm(k, pb, pb[:, 0:255], w1, w1[:, l, :], src, src[0:64, l:l + 16 * 254 + 1:16], l == 0, l == 31)
            for l in range(32):
                mm(k, pb2, pb2[:, 0:2], w1, w1[:, l, :], pe, pe[:, l, :], l == 0, l == 31)
            k.op("act", lambda e, pb2=pb2: e.activation(out=hb[:], in_=pb2[:, 0:2], func=AF.Copy), reads=[pb2], writes=[hb])
            k.op("pool", lambda e: e.memset(hsT[:], 0.0), writes=[hsT])
            k.op("act", lambda e, pb=pb: e.activation(out=hsT[:, 0:255], in_=pb[:, 0:255], func=AF.Silu, bias=hb[:, 0:1],
                                                      scale=1.0), reads=[pb, hb], writes=[hsT])
            if is_k:
                pb3 = psum[4]
                mm(k, pb3, pb3[0:64, 0:256], w2, w2[:], hsT, hsT[:], True, True)
                norm64(pb3, pb3[0:64, 0:256], kn[:, 0:1], kn, kcmpT, kcmpT[0:64, :], 256)
            else:
                for c in range(2):
                    pb3 = psum[4]
                    mm(k, pb3, pb3[:, 0:64], hsT, hsT[:, c * 128:(c + 1) * 128], w2, w2[:], True, True)
                    k.op("act", lambda e, c=c, pb3=pb3: e.activation(out=vcmpA[:, c, 0:64], in_=pb3[:, 0:64], func=AF.Copy),
                         reads=[pb3], writes=[vcmpA])
    if dbg is not None:
        for nm, buf in (("sbqT", sbqT), ("sbkT", sbkT), ("sbv", sbv), ("nqT", nqT), ("ksT", ksT), ("kwT", kwT), ("kcT", kcT),
                        ("vsA", vsA), ("gts", gts), ("kcmpT", kcmpT), ("vcmpA", vcmpA)):
            if nm in dbg:
                toks_dbg.append(k.dma("sp", dbg[nm][:], buf[:], reads=[buf], writes=[dbg[nm]]))
    k.release(m1)

    cmask = k.sb("a_cmask", [128, 2, SEQ], BF16)
    Et = k.sb("a_E", [64, 32, 128], BF16)
    k.dma("sp", cmask[:], d["cmask"][:], reads=[d["cmask"]], writes=[cmask])
    k.dma("sp", Et[:], d["E"][:], reads=[d["E"]], writes=[Et])
    zer = k.sb("a_zer", [128, 128], F32)
    cneg = k.sb("a_cneg", [128, 128], BF16)
    sneg = k.sb("a_sneg", [128, 128], BF16)
    wneg = k.sb("a_wneg", [128, 128], BF16)
    triS = k.sb("a_triS", [128, 128], BF16)
    onesb = k.sb("a_onesb", [128, 128], BF16)
    k.op("pool", lambda e: e.memset(zer[:], 0.0), writes=[zer])
    k.op("pool", lambda e: e.tensor_copy(out=onesb[:], in_=ones[:]), reads=[ones], writes=[onesb])
    k.op("pool", lambda e: e.affine_select(out=cneg[:], in_=zer[:], pattern=[[1, 128]], compare_op=ALU.is_ge, fill=NEG,
                                           base=0, channel_multiplier=-1), reads=[zer], writes=[cneg])
    k.op("pool", lambda e: e.affine_select(out=sneg[:], in_=zer[:], pattern=[[1, 128]], compare_op=ALU.is_gt, fill=NEG,
                                           base=0, channel_multiplier=-1), reads=[zer], writes=[sneg])
    k.op("pool", lambda e: e.affine_select(out=wneg[:], in_=zer[:], pattern=[[-1, 128]], compare_op=ALU.is_gt, fill=NEG,
                                           base=0, channel_multiplier=1), reads=[zer], writes=[wneg])
    k.op("pool", lambda e: e.affine_select(out=triS[:], in_=ones[:], pattern=[[-1, 128]], compare_op=ALU.is_gt, fill=0.0,
                                           base=0, channel_multiplier=1), reads=[ones], writes=[triS])
    b4 = lambda ap: ap.unsqueeze(1).to_broadcast([ap.shape[0], 4, ap.shape[1]])
    zb = k.sb("a_zb", [128, 512], BF16)
    k.op("pool", lambda e: e.memset(zb[:], 0.0), writes=[zb])

    def zinit(bank, ncols):
        mm(k, bank, bank[:, 0:ncols], zb, zb[:, 0:128], zb, zb[:, 0:ncols], True, False)

    PT = [k.sb("a_PT%d" % i, [128, 512], BF16) for i in range(4)]
    oc = k.sb("a_oc", [128, 4, 65], F32)
    os_ = k.sb("a_os", [128, 4, 65], F32)
    ow = k.sb("a_ow", [128, 4, 65], F32)
    rd = k.sb("a_rd", [128, 16], F32)
    dn = k.sb("a_dn", [128, 4, 3], F32)
    psl = k.sb("a_psl", [128, 64], F32)
    sc = k.sb("a_sc", [128, 64], F32)
    sc2 = k.sb("a_sc2", [128, 64], F32)
    v8 = k.sb("a_v8", [128, 16], F32)
    nsel = k.sb("a_nsel", [128, 64], BF16)
    nselT = k.sb("a_nselT", [64, 128], BF16)
    m1t = [k.sb("a_m1t%d" % i, [128, 64], F32) for i in range(2)]
    a1t = [k.sb("a_a1t%d" % i, [128, 64], F32) for i in range(2)]
    mg = k.sb("a_mg", [128, 4, 64], F32)
    mg2 = k.sb("a_mg2", [128, 4, 64], F32)
    otile = [k.sb("a_ot%d" % i, [128, 512], BF16) for i in range(2)]
    Esb2 = [k.sb("a_Esb%d" % i, [128, 512], F32) for i in range(2)]
    SP2 = [k.sb("a_SP%d" % i, [128, 512], F32) for i in range(2)]
    SPb2 = [k.sb("a_SPb%d" % i, [128, 512], BF16) for i in range(2)]
    T12 = [k.sb("a_T1%d" % i, [128, 512], F32) for i in range(2)]
    Wb = [k.sb("a_W%d" % i, [128, 512], BF16) for i in range(2)]
    npt = [0]

    def exp_pt(ST):
        p = PT[npt[0] % 4]
        npt[0] += 1
        k.op("act", lambda e: e.activation(out=p[:], in_=ST[:], func=AF.Exp), reads=[ST], writes=[p])
        return p

    toks = toks_dbg
    for t in range(n_qt):
        qs = slice(t * 128, (t + 1) * 128)
        ot = otile[t % 2]
        ncmp = 1 if t < 16 else 2
        k.dma("sp", m1t[t % 2][:], d["m1"][t], reads=[d["m1"]], writes=[m1t[t % 2]])
        k.dma("sp", a1t[t % 2][:], d["a1"][t], reads=[d["a1"]], writes=[a1t[t % 2]])
        pts = []
        for c in range(ncmp):
            ST = psum[c]
            full = 16 * (128 * c + 127) + 31 <= 128 * t
            mm(k, ST, ST[:], kcmpT, kcmpT[0:68, c * 128:(c + 1) * 128], nqT, nqT[0:68, :, qs], True, full)
            if not full:
                mm(k, ST, ST[:], idb, idb[:], cmask, cmask[:, c:c + 1, qs].to_broadcast([128, 4, 128]), False, True)
            pts.append(exp_pt(ST))
        outA, outB = psum[4], psum[5]
        zinit(outA, 260)
        zinit(outB, 256)
        first = False
        for h in range(4):
            for c in range(ncmp):
                p = pts[c]
                k.op("pe", lambda e, h=h, c=c, p=p, first=first: e.matmul(
                    outA[:, h * 65:(h + 1) * 65], lhsT=p[:, h * 128:(h + 1) * 128], rhs=vcmpA[:, c, 0:65],
                    start=first, stop=(h == 3 and c == ncmp - 1), skip_group_check=True), reads=[p, vcmpA], writes=[outA])
                first = False
        first = False
        for h in range(4):
            for c in range(ncmp):
                p = pts[c]
                k.op("pe", lambda e, h=h, c=c, p=p, first=first: e.matmul(
                    outB[:, h * 64:(h + 1) * 64], lhsT=p[:, h * 128:(h + 1) * 128], rhs=vcmpA[:, c, 65:129],
                    start=first, stop=(h == 3 and c == ncmp - 1), skip_group_check=True), reads=[p, vcmpA], writes=[outB])
                first = False
        k.op("act", lambda e: e.activation(out=oc[:], in_=outA[:, 0:260].rearrange("p (a b) -> p a b", a=4), func=AF.Copy),
             reads=[outA], writes=[oc])
        k.op("dve", lambda e: e.tensor_scalar(out=rd[:, 0:4], in0=oc[:, :, 64], scalar1=1e-30, scalar2=None, op0=ALU.max),
             reads=[oc], writes=[rd])
        k.op("dve", lambda e: e.reciprocal(out=rd[:, 4:8], in_=rd[:, 0:4]), reads=[rd], writes=[rd])
        k.op("dve", lambda e: e.tensor_scalar(out=psl[:], in0=outB[:, 0:64], scalar1=rd[:, 4:5], scalar2=None, op0=ALU.mult),
             reads=[outB, rd], writes=[psl])
        for h in range(1, 4):
            k.op("dve", lambda e, h=h: e.scalar_tensor_tensor(out=psl[:], in0=outB[:, h * 64:(h + 1) * 64], scalar=rd[:, 4 + h:5 + h],
                                                              in1=psl[:], op0=ALU.mult, op1=ALU.add), reads=[outB, rd, psl], writes=[psl])
        k.op("dve", lambda e, t=t: e.tensor_tensor(out=sc[:], in0=psl[:], in1=m1t[t % 2][:], op=ALU.mult),
             reads=[psl, m1t[t % 2]], writes=[sc])
        k.op("dve", lambda e, t=t: e.tensor_tensor(out=sc[:], in0=sc[:], in1=a1t[t % 2][:], op=ALU.add),
             reads=[sc, a1t[t % 2]], writes=[sc])
        k.op("dve", lambda e: e.max(out=v8[:, 0:8], in_=sc[:]), reads=[sc], writes=[v8])
        k.op("dve", lambda e: e.match_replace(out=sc2[:], in_to_replace=v8[:, 0:8], in_values=sc[:], imm_value=-3e6),
             reads=[sc, v8], writes=[sc2])
        k.op("dve", lambda e: e.max(out=v8[:, 8:16], in_=sc2[:]), reads=[sc2], writes=[v8])
        k.op("dve", lambda e: e.tensor_scalar(out=sc2[:], in0=sc[:], scalar1=v8[:, 15:16], scalar2=1.0, op0=ALU.is_ge,
                                              op1=ALU.subtract), reads=[sc, v8], writes=[sc2])
        k.op("dve", lambda e: e.tensor_scalar(out=nsel[:], in0=sc2[:], scalar1=-NEG, scalar2=None, op0=ALU.mult),
             reads=[sc2], writes=[nsel])
        if dbg is not None and "nsel" in dbg:
            toks.append(k.dma("sp", dbg["nsel"][qs, :], nsel[:], reads=[nsel], writes=[dbg["nsel"]]))
        pb, pbv = psum[2], pbf[2]
        tr(k, pb, pbv[0:64, 0:128], nsel, nsel[:], idb, idb[:])
        k.op("act", lambda e, pbv=pbv: e.activation(out=nselT[:], in_=pbv[0:64, 0:128], func=AF.Copy), reads=[pb], writes=[nselT])
        outS, outW = psum[6], psum[7]
        zinit(outS, 260)
        zinit(outW, 260)
        for s in range(t + 1):
            ST = psum[s % 4]
            ks_ = slice(s * 128, (s + 1) * 128)
            mm(k, ST, ST[:], ksT, ksT[0:68, ks_], nqT, nqT[0:68, :, qs], True, False)
            mm(k, ST, ST[:], Et, Et[:, s, :], nselT, b4(nselT[:]), False, s != t)
            if s == t:
                mm(k, ST, ST[:], idb, idb[:], cneg, b4(cneg[:]), False, True)
            p = exp_pt(ST)
            for h in range(4):
                k.op("pe", lambda e, h=h, s=s, p=p: e.matmul(
                    outS[:, h * 65:(h + 1) * 65], lhsT=p[:, h * 128:(h + 1) * 128], rhs=vsA[:, s, :],
                    start=False, stop=(s == t and h == 3), skip_group_check=True), reads=[p, vsA], writes=[outS])
        s0 = max(0, t - 4)
        for s in range(s0, t + 1):
            ST = psum[s % 4]
            ks_ = slice(s * 128, (s + 1) * 128)
            lo, hi = (s == t - 4), (s == t)
            mm(k, ST, ST[:], kwT, kwT[0:68, ks_], nqT, nqT[0:68, :, qs], True, not (lo or hi))
            if lo:
                mm(k, ST, ST[:], idb, idb[:], wneg, b4(wneg[:]), False, True)
            if hi:
                mm(k, ST, ST[:], idb, idb[:], cneg, b4(cneg[:]), False, True)
            p = exp_pt(ST)
            for h in range(4):
                k.op("pe", lambda e, h=h, s=s, p=p: e.matmul(
                    outW[:, h * 65:(h + 1) * 65], lhsT=p[:, h * 128:(h + 1) * 128], rhs=vwA[:, s, :],
                    start=False, stop=(s == t and h == 3), skip_group_check=True), reads=[p, vwA], writes=[outW])
        k.op("act", lambda e: e.activation(out=os_[:], in_=outS[:, 0:260].rearrange("p (a b) -> p a b", a=4), func=AF.Copy),
             reads=[outS], writes=[os_])
        k.op("act", lambda e: e.activation(out=ow[:], in_=outW[:, 0:260].rearrange("p (a b) -> p a b", a=4), func=AF.Copy),
             reads=[outW], writes=[ow])
        for bi, src in enumerate((oc, os_, ow)):
            k.op("dve", lambda e, bi=bi, src=src: e.tensor_scalar(out=dn[:, :, bi], in0=src[:, :, 64], scalar1=1e-30, scalar2=None,
                                                                  op0=ALU.max), reads=[src], writes=[dn])
        k.op("dve", lambda e: e.reciprocal(out=dn[:], in_=dn[:]), reads=[dn], writes=[dn])
        k.op("dve", lambda e, t=t: e.tensor_tensor(out=dn[:], in0=dn[:], in1=gts[:, t, :].rearrange("p (a b) -> p a b", a=4),
                                                   op=ALU.mult), reads=[dn, gts], writes=[dn])
        bc64 = lambda ap: ap.to_broadcast([128, 4, 64])
        k.op("dve", lambda e: e.tensor_tensor(out=mg[:], in0=oc[:, :, 0:64], in1=bc64(dn[:, :, 0:1]), op=ALU.mult),
             reads=[oc, dn], writes=[mg])
        k.op("pool", lambda e: e.tensor_tensor(out=mg2[:], in0=os_[:, :, 0:64], in1=bc64(dn[:, :, 1:2]), op=ALU.mult),
             reads=[os_, dn], writes=[mg2])
        k.op("dve", lambda e: e.tensor_tensor(out=mg[:], in0=mg[:], in1=mg2[:], op=ALU.add), reads=[mg, mg2], writes=[mg])
        k.op("pool", lambda e: e.tensor_tensor(out=mg2[:], in0=ow[:, :, 0:64], in1=bc64(dn[:, :, 2:3]), op=ALU.mult),
             reads=[ow, dn], writes=[mg2])
        k.op("dve", lambda e, ot=ot: e.tensor_tensor(out=ot[:, 256:512].rearrange("p (a b) -> p a b", a=4), in0=mg[:], in1=mg2[:],
                                                     op=ALU.add), reads=[mg, mg2], writes=[ot])
        hord = [0, 2, 1, 3]
        outSB = psum[6]
        CS = psum[7]
        zinit(outSB, 256)
        if t > 0:
            zinit(CS, 512)
        for s in range(t, -1, -1):
            pi = (t - s) % 2
            X0, X1 = psum[2 * pi], psum[2 * pi + 1]
            Xv = pall[:, 2 * pi * 512:(2 * pi + 2) * 512].rearrange("p (a b) -> p a b", a=2)[:, :, 0:256]
            E_, SP_, SPb_, T1_ = Esb2[pi], SP2[pi], SPb2[pi], T12[pi]
            ks_ = slice(s * 128, (s + 1) * 128)
            for hp, X in ((0, X0), (1, X1)):
                if s == t:
                    mm(k, X, X[:, 0:256], idb, idb[:], sneg, sneg[:].unsqueeze(1).to_broadcast([128, 2, 128]), True, False)
                for ci in range(2):
                    mm(k, X, X[:, ci * 128:(ci + 1) * 128], sbkT, sbkT[hp * 64:(hp + 1) * 64, ci, ks_],
                       sbqT, sbqT[hp * 64:(hp + 1) * 64, ci, qs], s != t, True)
            e3 = lambda ap: ap.rearrange("p (a b) -> p a b", a=2)
            k.op("act", lambda e, Xv=Xv, E_=E_: e.activation(out=e3(E_[:]), in_=Xv, func=AF.Exp), reads=[X0, X1], writes=[E_])
            k.op("act", lambda e, E_=E_, SPb_=SPb_: e.activation(out=SPb_[:], in_=E_[:], func=AF.Ln, bias=1.0, scale=1.0),
                 reads=[E_], writes=[SPb_])
            k.op("act", lambda e, E_=E_, SP_=SP_: e.activation(out=SP_[:], in_=E_[:], func=AF.Ln, bias=1.0, scale=1.0),
                 reads=[E_], writes=[SP_])
            acc = psum[4 + pi]
            mm(k, acc, acc[:], triS, triS[:], SPb_, SPb_[:], True, True)
            k.op("dve", lambda e, Xv=Xv, T1_=T1_, SP_=SP_: e.tensor_tensor(out=e3(T1_[:]), in0=Xv, in1=e3(SP_[:]), op=ALU.subtract),
                 reads=[X0, X1, SP_], writes=[T1_])
            k.op("dve", lambda e, acc=acc, T1_=T1_: e.tensor_tensor(out=T1_[:], in0=T1_[:], in1=acc[:], op=ALU.subtract),
                 reads=[T1_, acc], writes=[T1_])
            if s != t:
                k.op("dve", lambda e, T1_=T1_: e.tensor_tensor(out=T1_[:], in0=T1_[:], in1=CS[:], op=ALU.subtract),
                     reads=[T1_, CS], writes=[T1_])
            W = Wb[pi]
            k.op("act", lambda e, W=W, T1_=T1_: e.activation(out=W[:], in_=T1_[:], func=AF.Exp), reads=[T1_], writes=[W])
            for h in range(4):
                pos = hord.index(h)
                k.op("pe", lambda e, h=h, s=s, W=W, pos=pos: e.matmul(
                    outSB[:, h * 64:(h + 1) * 64], lhsT=W[:, pos * 128:(pos + 1) * 128], rhs=sbv[:, s, h * 64:(h + 1) * 64],
                    start=False, stop=(s == 0 and h == 3), skip_group_check=True), reads=[W, sbv], writes=[outSB])
            if s > 0:
                k.op("pe", lambda e, SPb_=SPb_, s=s: e.matmul(CS[:], lhsT=onesb[:], rhs=SPb_[:], start=False, stop=(s == 1),
                                                               skip_group_check=True), reads=[onesb, SPb_], writes=[CS])
        k.op("act", lambda e, ot=ot: e.activation(out=ot[:, 0:256], in_=outSB[:, 0:256], func=AF.Copy), reads=[outSB], writes=[ot])
        toks.append(k.dma("sp", o_d[qs, :], ot[:], reads=[ot], writes=[o_d]))
    k.release(m0)
    return toks


def make_psum_all(k):
    pall = k.stack.enter_context(k.nc.psum_tensor("psall", [128, 4096], F32))
    psum = [Buf(pall[:, i * 512:(i + 1) * 512], "ps%d" % i) for i in range(8)]
    pbf = [p[:].bitcast(BF16) for p in psum]
    return psum, pbf, pall


def build_a(n_qt=32, debug=False):
    nc = bass.Bass("TRN2", target_bir_lowering=False)
    with contextlib.ExitStack() as stack:
        k = KB(nc, stack)
        psum, pbf, pall = make_psum_all(k)
        d = a_dram(k)
        o_d = k.dram("o", [SEQ, 512], BF16, "ExternalOutput")
        ident = make_ident(k, "id", BF16)
        dbg = None
        if debug:
            dbg = {"nsel": k.dram("dbg_nsel", [SEQ, 64], BF16, "ExternalOutput"),
                   "sbqT": k.dram("dbg_sbqT", [128, 2, SEQ], BF16, "ExternalOutput"),
                   "sbkT": k.dram("dbg_sbkT", [128, 2, SEQ], BF16, "ExternalOutput"),
                   "sbv": k.dram("dbg_sbv", [128, 32, 256], BF16, "ExternalOutput"),
                   "nqT": k.dram("dbg_nqT", [68, 4, SEQ], BF16, "ExternalOutput"),
                   "ksT": k.dram("dbg_ksT", [68, SEQ], BF16, "ExternalOutput"),
                   "kwT": k.dram("dbg_kwT", [68, SEQ], BF16, "ExternalOutput"),
                   "kcT": k.dram("dbg_kcT", [64, SEQ], BF16, "ExternalOutput"),
                   "vsA": k.dram("dbg_vsA", [128, 32, 65], BF16, "ExternalOutput"),
                   "gts": k.dram("dbg_gts", [128, 32, 12], F32, "ExternalOutput"),
                   "kcmpT": k.dram("dbg_kcmpT", [68, 256], BF16, "ExternalOutput"),
                   "vcmpA": k.dram("dbg_vcmpA", [128, 2, 129], BF16, "ExternalOutput")}
        toks = emit_a(k, d, psum, pbf, pall, o_d, ident, "", n_qt, dbg)
        k.emit(final_waits=toks)
        print("A arena peak words", k.apeak, "instr", {e: len(v) for e, v in k.prog.items()})
    return nc


CORES = list(range(8))


def _run(nc, in_maps):
    return run_bass_kernel_spmd(nc, in_maps, core_ids=CORES).results


def kernel_unfused(**inputs):
    P = {k_: np.ascontiguousarray(np.asarray(v, dtype=np.float32)) for k_, v in inputs.items()}
    ncA = build_a(32)
    resA = _run(ncA, [a_host_inputs(c // 2, c % 2, P) for c in CORES])
    o_full = []
    for b in range(4):
        o0, o1 = np.asarray(resA[2 * b]["o"]), np.asarray(resA[2 * b + 1]["o"])
        o_full.append(np.concatenate([o0[:, :256], o1[:, :256], o0[:, 256:], o1[:, 256:]], axis=1))
    del resA
    ncB = build_bd(1024)
    shared = bd_host_shared(0, P, "")
    maps = []
    for c in CORES:
        b, hh = c // 2, c % 2
        m = dict(shared)
        m.update(bd_host_inputs(0, b, P, P["attn_w_out"][0], ""))
        m["oin"] = np.ascontiguousarray(o_full[b][hh * 2048:(hh + 1) * 2048])
        m["hres"] = np.ascontiguousarray(P["x"][b, hh * 2048:(hh + 1) * 2048])
        maps.append(m)
    resB = _run(ncB, maps)
    h0 = [np.concatenate([np.asarray(resB[2 * b]["out"]), np.asarray(resB[2 * b + 1]["out"])], axis=0) for b in range(4)]
    del resB, maps, shared
    ncC = build_c(8)
    maps = []
    for c in CORES:
        b, hh = c // 2, c % 2
        m = c_host_inputs(b, hh, P)
        m["c_hin"] = h0[b]
        maps.append(m)
    resC = _run(ncC, maps)
    yn_full = [np.concatenate([np.asarray(resC[2 * b]["yn"]), np.asarray(resC[2 * b + 1]["yn"])], axis=1) for b in range(4)]
    del resC
    ncD = build_bd(2048)
    shared = bd_host_shared(1, P, "")
    maps = []
    for c in CORES:
        b, hh = c // 2, c % 2
        m = dict(shared)
        m.update(bd_host_inputs(1, b, P, P["ssm_w_out"][0], ""))
        m["oin"] = np.ascontiguousarray(yn_full[b][hh * 2048:(hh + 1) * 2048])
        m["hres"] = np.ascontiguousarray(h0[b][hh * 2048:(hh + 1) * 2048])
        maps.append(m)
    resD = _run(ncD, maps)
    out = np.stack([np.concatenate([np.asarray(resD[2 * b]["out"]), np.asarray(resD[2 * b + 1]["out"])], axis=0)
                    for b in range(4)])
    return out.astype(np.float32)


def build_fused():
    nc = bass.Bass("TRN2", target_bir_lowering=False)
    with contextlib.ExitStack() as stack:
        k = KB(nc, stack)
        psum, pbf, pall = make_psum_all(k)
        dA = [a_dram(k, "_0"), a_dram(k, "_1")]
        dB = [bd_dram(k, 1024, "_l0", 32, False), bd_dram(k, 2048, "_l1", 32, False)]
        dC = [c_dram(k, "_0", False), c_dram(k, "_1", False)]
        out_d = k.dram("out", [NTOK, 1024], F32, "ExternalOutput")
        ridx_d = k.dram("ridx", [128, NT], mybir.dt.uint32, "ExternalInput")
        o_scr = [k.dram("o_scr%d" % i, [SEQ, 512], BF16, "Internal") for i in range(2)]
        yn_scr = [k.dram("yn_scr%d" % i, [SEQ, 1024], BF16, "Internal") for i in range(2)]
        h0_scr = k.dram("h0_scr", [SEQ, 1024], F32, "Internal")
        hpre_scr = k.dram("hpre_scr", [NTOK, 1024], F32, "Internal")
        scr = {"u": k.dram("u_scr", [NTOK, 1024], BF16, "Internal"),
               "y": k.dram("y_scr", [32 * CAP2, 1024], BF16, "Internal")}
        ident = make_ident(k, "id", BF16)
        for hh in range(2):
            emit_a(k, dA[hh], psum, pbf, pall, o_scr[hh], ident, "_%d" % hh, 32, None)
            k.new_epoch()
        for th in range(2):
            r0 = th * NTOK
            parts = [(slice(0, 256), o_scr[0], slice(0, 256), r0), (slice(256, 512), o_scr[1], slice(0, 256), r0),
                     (slice(512, 768), o_scr[0], slice(256, 512), r0), (slice(768, 1024), o_scr[1], slice(256, 512), r0)]
            emit_bd_sp2(k, 1024, dB[0], psum, pbf, h0_scr, hpre_scr, ident, scr, "_l0", 32, None, parts, (dA[0]["xin"], r0), r0)
            k.new_epoch()
        for hh in range(2):
            dC[hh]["hin"] = h0_scr
            emit_c(k, dC[hh], psum, pbf, yn_scr[hh], ident, "_%d" % hh, 8, None)
            k.new_epoch()
        parts = [(slice(0, 1024), yn_scr[0], slice(0, 1024), 0), (slice(1024, 2048), yn_scr[1], slice(0, 1024), 0)]
        toks = emit_bd_sp2(k, 2048, dB[1], psum, pbf, out_d, hpre_scr, ident, scr, "_l1", 32, None, parts, (h0_scr, 0), 0, ridx_d)
        k.emit(final_waits=toks)
        print("FUSED arena peak words", k.apeak, "instr", {e: len(v) for e, v in k.prog.items()})
    return nc


def fused_host_inputs(b, P):
    m = {}
    for hh in range(2):
        for kk, v in a_host_inputs(b, hh, P).items():
            m[kk + "_%d" % hh] = v
        for kk, v in c_host_inputs(b, hh, P).items():
            m[kk + "_%d" % hh] = v
    m.update(bd_host_inputs(0, b, P, P["attn_w_out"][0], "_l0"))
    m.update(bd_host_inputs(1, b, P, P["ssm_w_out"][0], "_l1"))
    return m


def kernel(**inputs):
    P = {k_: np.ascontiguousarray(np.asarray(v, dtype=np.float32)) for k_, v in inputs.items()}
    nc = build_fused()
    shared = {}
    shared.update(bd_host_shared(0, P, "_l0"))
    shared.update(bd_host_shared(1, P, "_l1"))
    per_b = []
    for b in range(4):
        m = dict(shared)
        m.update(fused_host_inputs(b, P))
        per_b.append(m)
    maps = []
    for c in CORES:
        m = dict(per_b[c % 4])
        th = c // 4
        m["ridx"] = np.ascontiguousarray((th * NTOK + np.arange(NTOK).reshape(NT, 128).T).astype(np.uint32))
        maps.append(m)
    res = run_bass_kernel_spmd(nc, maps, core_ids=CORES).results
    return np.stack([np.concatenate([np.asarray(res[b]["out"]), np.asarray(res[b + 4]["out"])], axis=0)
                     for b in range(4)]).astype(np.float32)


CAP = 384
NST = CAP // 128


def emit_bd_sparse(k, F, d, psum, pbf, out_d, hpre_d, ident, tag="", n_exp=32, dbg=None, oin_parts=None, hres_src=None,
                   out_row0=0):
    FC = F // 128
    if oin_parts is None:
        oin_parts = [(slice(0, F), d["oin"], slice(0, F), 0)]
    if hres_src is None:
        hres_src = (d["hres"], 0)
    pg, pu, pd, pm = psum[0:2], psum[2:4], psum[4:6], psum[6:8]
    pgb, pmb = pbf[0:2], pbf[6:8]
    idf, idb = ident
    m0 = k.mark()
    utok = [k.sb("utok%d%s" % (t, tag), [128, 1024], BF16) for t in range(NT)]
    gw = k.sb("gw" + tag, [128, NT, 32], F32)
    maskf = k.sb("maskf" + tag, [128, NT, 32], F32)
    maskb = k.sb("maskb" + tag, [128, NT, 32], BF16)
    pos_all = k.sb("pos" + tag, [128, NT, 32], F32)
    m_g2 = k.sb("m_g2" + tag, [128, 1024], F32)
    bgu = k.sb("bgu" + tag, [128, 32, 2, 8], F32)
    bgu1 = k.sb("bgu1" + tag, [128, 32, 8], F32)
    epsb = k.sb("epsb" + tag, [128, 1], F32)
    k.op("pool", lambda e: e.memset(epsb[:], EPS), writes=[epsb])
    k.dma("sp", bgu[:], d["bgu"][:], reads=[d["bgu"]], writes=[bgu])
    k.op("pool", lambda e: e.tensor_scalar(out=bgu1[:], in0=bgu[:, :, 1, :], scalar1=1.0, scalar2=None, op0=ALU.add),
         reads=[bgu], writes=[bgu1])

    m1 = k.mark()
    m_g1 = k.sb("m_g1" + tag, [128, 1024], F32)
    m_sh2 = k.sb("m_sh2" + tag, [128, 1024], F32)
    m_gm = k.sb("m_gm" + tag, [128, 1024], F32)
    gain_bc = k.sb("gainbc" + tag, [128, 1024], F32)
    stage = k.sb("adast" + tag, [128, 8, 1024], F32)
    woutb = k.sb("woutb" + tag, [128, FC, 1024], BF16)
    rwb = k.sb("rwb" + tag, [128, 8, 32], BF16)
    rb_bc = k.sb("rbbc" + tag, [128, 32], F32)
    o_tok = [k.sb("otok%d%s" % (i, tag), [128, F], BF16) for i in range(2)]
    oT = [k.sb("oT%d%s" % (i, tag), [128, FC, 128], BF16) for i in range(2)]
    hres_t = [k.sb("hrt%d%s" % (i, tag), [128, 1024], F32) for i in range(2)]
    tmp = [k.sb("tmp%d%s" % (i, tag), [128, 1024], F32) for i in range(2)]
    uTt = [k.sb("uTt%d%s" % (i, tag), [128, 8, 128], BF16) for i in range(2)]
    st = [k.sb("st%d%s" % (i, tag), [128, 64], F32) for i in range(2)]
    lg = [k.sb("lg%d%s" % (i, tag), [128, 4, 32], F32) for i in range(2)]
    triU = k.sb("triU" + tag, [128, 128], BF16)
    onesb = k.sb("onesb" + tag, [128, 128], BF16)
    onesf = k.sb("onesf" + tag, [128, 128], F32)
    k.op("pool", lambda e: e.memset(onesf[:], 1.0), writes=[onesf])
    k.op("pool", lambda e: e.tensor_copy(out=onesb[:], in_=onesf[:]), reads=[onesf], writes=[onesb])
    k.op("pool", lambda e: e.affine_select(out=triU[:], in_=onesf[:], pattern=[[1, 128]], compare_op=ALU.is_gt, fill=0.0,
                                           base=0, channel_multiplier=-1), reads=[onesf], writes=[triU])

    k.dma("pool", woutb[:], d["wout"][:], reads=[d["wout"]], writes=[woutb])
    k.dma("pool", rwb[:], d["rw"][:], reads=[d["rw"]], writes=[rwb])
    k.dma("sp", rb_bc[:], d["rb"][:].partition_broadcast(128), reads=[d["rb"]], writes=[rb_bc])
    k.dma("sp", gain_bc[:], d["gain"][:].partition_broadcast(128), reads=[d["gain"]], writes=[gain_bc])
    adaln(k, d["ccol"], d["adaw"], d["adab"], 4, [m_g1, m_sh2, m_gm, m_g2], [stage, stage], pm, tag)
    k.op("dve", lambda e: e.scalar_tensor_tensor(out=m_gm[:], in0=m_gm[:], scalar=1.0, in1=gain_bc[:],
                                                 op0=ALU.add, op1=ALU.mult), reads=[m_gm, gain_bc], writes=[m_gm])

    for t in range(NT):
        ot, oTt, hr, tp, ut, s_, l_, uT_ = o_tok[t % 2], oT[t % 2], hres_t[t % 2], tmp[t % 2], utok[t], st[t % 2], lg[t % 2], uTt[t % 2]
        rows = slice(t * 128, (t + 1) * 128)
        for (dcs, sbuf_, scs, r0) in oin_parts:
            k.dma("sp", ot[:, dcs], sbuf_[r0 + t * 128:r0 + (t + 1) * 128, scs], reads=[sbuf_], writes=[ot])
        k.dma("sp", hr[:], hres_src[0][hres_src[1] + t * 128:hres_src[1] + (t + 1) * 128, :], reads=[hres_src[0]], writes=[hr])
        for g in range(FC // 8):
            pb, pbv = pg[g % 2], pgb[g % 2]
            for c in range(8):
                fc = g * 8 + c
                tr(k, pb, pbv[:, c * 128:(c + 1) * 128], ot, ot[:, fc * 128:(fc + 1) * 128], idb, idb[:])
            k.op("act", lambda e, g=g, pbv=pbv, oTt=oTt: e.activation(
                out=oTt[:, g * 8:(g + 1) * 8, :], in_=pbv.rearrange("p (a b) -> p a b", a=8), func=AF.Copy),
                reads=[pb], writes=[oTt])
        for half in range(2):
            hs = slice(half * 512, (half + 1) * 512)
            for fc in range(FC):
                mm(k, pd[half], pd[half][:], oTt, oTt[:, fc, :], woutb, woutb[:, fc, hs], fc == 0, fc == FC - 1)
            k.op("dve", lambda e, half=half, hs=hs, tp=tp: e.tensor_tensor(
                out=tp[:, hs], in0=pd[half][:], in1=m_g1[:, hs], op=ALU.mult), reads=[pd[half], m_g1], writes=[tp])
        k.op("pool", lambda e, hr=hr, tp=tp: e.tensor_tensor(out=hr[:], in0=tp[:], in1=hr[:], op=ALU.add),
             reads=[tp, hr], writes=[hr])
        k.dma("sp", hpre_d[rows, :], hr[:], reads=[hr], writes=[hpre_d])
        k.op("act", lambda e, hr=hr, tp=tp, s_=s_: e.activation(out=tp[:], in_=hr[:], func=AF.Square,
                                                                 accum_out=s_[:, 0:1]), reads=[hr], writes=[tp, s_])
        k.op("act", lambda e, s_=s_: e.activation(out=s_[:, 1:2], in_=s_[:, 0:1], func=AF.Sqrt, bias=epsb[:],
                                                   scale=1.0 / 1024.0), reads=[s_, epsb], writes=[s_])
        k.op("dve", lambda e, s_=s_: e.reciprocal(out=s_[:, 2:3], in_=s_[:, 1:2]), reads=[s_], writes=[s_])
        k.op("dve", lambda e, hr=hr, tp=tp, s_=s_: e.scalar_tensor_tensor(
            out=tp[:], in0=hr[:], scalar=s_[:, 2:3], in1=m_gm[:], op0=ALU.mult, op1=ALU.mult),
            reads=[hr, s_, m_gm], writes=[tp])
        k.op("pool", lambda e, tp=tp, ut=ut: e.tensor_tensor(out=ut[:], in0=tp[:], in1=m_sh2[:], op=ALU.add),
             reads=[tp, m_sh2], writes=[ut])
        for kc in range(8):
            tr(k, pm[0], pmb[0][:, kc * 128:(kc + 1) * 128], ut, ut[:, kc * 128:(kc + 1) * 128], idb, idb[:])
        k.op("act", lambda e, uT_=uT_: e.activation(out=uT_[:], in_=pmb[0].rearrange("p (a b) -> p a b", a=8), func=AF.Copy),
             reads=[pm[0]], writes=[uT_])
        for kc in range(8):
            mm(k, pm[1], pm[1][:, 0:32], uT_, uT_[:, kc, :], rwb, rwb[:, kc, :], kc == 0, kc == 7)
        k.op("dve", lambda e, l_=l_: e.tensor_tensor(out=l_[:, 0, :], in0=pm[1][:, 0:32], in1=rb_bc[:], op=ALU.add),
             reads=[pm[1], rb_bc], writes=[l_])
        k.op("dve", lambda e, l_=l_, s_=s_: e.max(out=s_[:, 8:16], in_=l_[:, 0, :]), reads=[l_], writes=[s_])
        k.op("dve", lambda e, l_=l_, s_=s_, t=t: e.tensor_scalar(out=maskf[:, t, :], in0=l_[:, 0, :], scalar1=s_[:, 11:12],
                                                                 scalar2=None, op0=ALU.is_ge), reads=[l_, s_], writes=[maskf])
        k.op("pool", lambda e, t=t: e.tensor_copy(out=maskb[:, t, :], in_=maskf[:, t, :]), reads=[maskf], writes=[maskb])
        k.op("dve", lambda e, s_=s_: e.tensor_scalar(out=s_[:, 16:17], in0=s_[:, 8:9], scalar1=-1.0, scalar2=None,
                                                     op0=ALU.mult), reads=[s_], writes=[s_])
        k.op("act", lambda e, l_=l_, s_=s_: e.activation(out=l_[:, 2, :], in_=l_[:, 0, :], func=AF.Exp,
                                                          bias=s_[:, 16:17], scale=1.0), reads=[l_, s_], writes=[l_])
        k.op("dve", lambda e, l_=l_, s_=s_, t=t: e.scalar_tensor_tensor(
            out=l_[:, 3, :], in0=l_[:, 2, :], scalar=1.0, in1=maskf[:, t, :], op0=ALU.mult, op1=ALU.mult,
            accum_out=s_[:, 17:18]), reads=[l_, maskf], writes=[l_, s_])
        k.op("dve", lambda e, s_=s_: e.reciprocal(out=s_[:, 18:19], in_=s_[:, 17:18]), reads=[s_], writes=[s_])
        k.op("dve", lambda e, l_=l_, s_=s_, t=t: e.tensor_scalar(out=gw[:, t, :], in0=l_[:, 3, :], scalar1=s_[:, 18:19],
                                                                 scalar2=None, op0=ALU.mult), reads=[l_, s_], writes=[gw])
    for t in range(NT):
        pb = pm[t % 2]
        mm(k, pb, pb[:, 0:32], triU, triU[:], maskb, maskb[:, t, :], True, t == 0)
        for t2_ in range(t):
            mm(k, pb, pb[:, 0:32], onesb, onesb[:], maskb, maskb[:, t2_, :], False, t2_ == t - 1)
        k.op("act", lambda e, t=t, pb=pb: e.activation(out=pos_all[:, t, :], in_=pb[:, 0:32], func=AF.Copy),
             reads=[pb], writes=[pos_all])

    toks = []
    if dbg is not None:
        toks.append(k.dma("sp", dbg["gw"][:], gw[:], reads=[gw], writes=[dbg["gw"]]))
        toks.append(k.dma("sp", dbg["pos"][:], pos_all[:], reads=[pos_all], writes=[dbg["pos"]]))
    k.release(m1)
    acc = [k.sb("acc%d%s" % (t, tag), [128, 1024], F32) for t in range(NT)]
    for t in range(NT):
        k.op("pool", lambda e, t=t: e.memset(acc[t][:], 0.0), writes=[acc[t]])
    m2 = k.mark()
    iota_i = k.sb("iota_i" + tag, [128, CAP], mybir.dt.int32)
    iota_f = k.sb("iota_f" + tag, [128, CAP], F32)
    k.op("pool", lambda e: e.iota(iota_i[:], pattern=[[1, CAP]], base=0, channel_multiplier=0), writes=[iota_i])
    k.op("pool", lambda e: e.tensor_copy(out=iota_f[:], in_=iota_i[:]), reads=[iota_i], writes=[iota_f])
    Pm = [k.sb("Pm%d%s" % (t, tag), [128, CAP], BF16) for t in range(NT)]
    PmW = [k.sb("PmW%d%s" % (i, tag), [128, CAP], BF16) for i in range(2)]
    PmT = [k.sb("PmT%d%s" % (s_, tag), [128, NTOK], BF16) for s_ in range(NST)]
    XgT = k.sb("XgT" + tag, [128, 8, CAP], BF16)
    actT = [k.sb("actT%d%s" % (j, tag), [128, CAP], BF16) for j in range(8)]
    Y = [k.sb("Y%d%s" % (s_, tag), [128, 1024], BF16) for s_ in range(NST)]
    bdb = [k.sb("bdb%d%s" % (i, tag), [128, 1024], F32) for i in range(2)]
    NR = 3
    wring = [k.sb("wgur%d%s" % (i, tag), [128, 8, 2, 128], BF16) for i in range(NR)]
    ND = 2
    dring = [k.sb("wdr%d%s" % (i, tag), [128, 8, 512], BF16) for i in range(ND)]
    g_sb = k.sb("g_sb" + tag, [128, CAP], F32)
    s_sb = k.sb("s_sb" + tag, [128, CAP], F32)
    t1 = k.sb("t1" + tag, [128, CAP], F32)
    t2 = k.sb("t2" + tag, [128, CAP], F32)
    m_sb = k.sb("m_sb" + tag, [128, CAP], F32)

    units = [(e, j) for e in range(n_exp) for j in range(8)]
    dunits = [(e, h) for e in range(n_exp) for h in range(2)]

    def load_unit(i):
        if i < len(units):
            e, j = units[i]
            k.dma("pool", wring[i % NR][:], d["wgu"][e, j], reads=[d["wgu"]], writes=[wring[i % NR]])

    def load_dunit(i):
        if i < len(dunits):
            e, h = dunits[i]
            k.dma("pool", dring[i % ND][:], d["wd"][e, :, :, h * 512:(h + 1) * 512], reads=[d["wd"]],
                  writes=[dring[i % ND]])

    def build_P(e_):
        for t in range(NT):
            k.op("dve", lambda e, t=t, e_=e_: e.tensor_scalar(
                out=Pm[t][:], in0=iota_f[:], scalar1=pos_all[:, t, e_:e_ + 1], scalar2=maskf[:, t, e_:e_ + 1],
                op0=ALU.is_equal, op1=ALU.mult), reads=[iota_f, pos_all, maskf], writes=[Pm[t]])

    def build_PT(e_):
        for t in range(NT):
            pw = PmW[t % 2]
            k.op("pool", lambda e, t=t, e_=e_, pw=pw: e.tensor_scalar(
                out=pw[:], in0=Pm[t][:], scalar1=gw[:, t, e_:e_ + 1], scalar2=None, op0=ALU.mult),
                reads=[Pm[t], gw], writes=[pw])
            pb, pbv = psum[6 + t % 2], pbf[6 + t % 2]
            for s_ in range(NST):
                tr(k, pb, pbv[:, s_ * 128:(s_ + 1) * 128], pw, pw[:, s_ * 128:(s_ + 1) * 128], idb, idb[:])
            for s_ in range(NST):
                k.op("act", lambda e, t=t, s_=s_, pbv=pbv: e.activation(
                    out=PmT[s_][:, t * 128:(t + 1) * 128], in_=pbv[:, s_ * 128:(s_ + 1) * 128], func=AF.Copy),
                    reads=[pb], writes=[PmT[s_]])

    for i in range(NR - 1):
        load_unit(i)
    for i in range(ND - 1):
        load_dunit(i)
    build_P(0)
    cnt = 0
    for e_ in range(n_exp):
        bdt = bdb[e_ % 2]
        k.dma("sp", bdt[:], d["bd"][e_].partition_broadcast(128), reads=[d["bd"]], writes=[bdt])
        for grp in range(2):
            for kk in range(4):
                kc = grp * 4 + kk
                pb = psum[kk]
                for t in range(NT):
                    mm(k, pb, pb[:, 0:CAP], utok[t], utok[t][:, kc * 128:(kc + 1) * 128], Pm[t], Pm[t][:], t == 0, t == NT - 1)
            for kk in range(4):
                kc = grp * 4 + kk
                pb = psum[kk]
                k.op("act", lambda e, kc=kc, pb=pb: e.activation(out=XgT[:, kc, :], in_=pb[:, 0:CAP], func=AF.Copy),
                     reads=[pb], writes=[XgT])
        build_PT(e_)
        for j in range(8):
            ui = e_ * 8 + j
            load_unit(ui + NR - 1)
            w = wring[ui % NR]
            x = cnt % 2
            cnt += 1
            pgx, pux = psum[4 + 2 * x], psum[5 + 2 * x]
            for kc in range(8):
                mm(k, pgx, pgx[:, 0:CAP], w, w[:, kc, 0, :], XgT, XgT[:, kc, :], kc == 0, kc == 7)
            for kc in range(8):
                mm(k, pux, pux[:, 0:CAP], w, w[:, kc, 1, :], XgT, XgT[:, kc, :], kc == 0, kc == 7)
            k.op("dve", lambda e, pgx=pgx, e_=e_, j=j: e.tensor_scalar(
                out=g_sb[:], in0=pgx[:, 0:CAP], scalar1=bgu[:, e_, 0, j:j + 1], scalar2=SWIGLU_LIMIT,
                op0=ALU.add, op1=ALU.min), reads=[pgx, bgu], writes=[g_sb])
            k.op("act", lambda e: e.activation(out=s_sb[:], in_=g_sb[:], func=AF.Sigmoid, scale=SWIGLU_ALPHA),
                 reads=[g_sb], writes=[s_sb])
            k.op("act", lambda e, pux=pux, e_=e_, j=j: e.activation(
                out=t1[:], in_=pux[:, 0:CAP], func=AF.Identity, bias=bgu1[:, e_, j:j + 1], scale=1.0),
                reads=[pux, bgu1], writes=[t1])
            k.op("pool", lambda e: e.tensor_scalar(out=t2[:], in0=t1[:], scalar1=SWIGLU_LIMIT + 1.0,
                                                   scalar2=1.0 - SWIGLU_LIMIT, op0=ALU.min, op1=ALU.max),
                 reads=[t1], writes=[t2])
            k.op("dve", lambda e: e.tensor_tensor(out=m_sb[:], in0=g_sb[:], in1=s_sb[:], op=ALU.mult),
                 reads=[g_sb, s_sb], writes=[m_sb])
            k.op("pool", lambda e, j=j: e.tensor_tensor(out=actT[j][:], in0=m_sb[:], in1=t2[:], op=ALU.mult),
                 reads=[m_sb, t2], writes=[actT[j]])
        for half in range(2):
            di = e_ * 2 + half
            load_dunit(di + ND - 1)
            wdv = dring[di % ND]
            hs = slice(half * 512, (half + 1) * 512)
            for s_ in range(NST):
                pb = psum[s_ % 2]
                for fc in range(8):
                    mm(k, pb, pb[:], actT[fc], actT[fc][:, s_ * 128:(s_ + 1) * 128], wdv, wdv[:, fc, :], fc == 0, fc == 7)
                k.op("dve", lambda e, s_=s_, hs=hs, pb=pb, bdt=bdt: e.tensor_tensor(
                    out=Y[s_][:, hs], in0=pb[:], in1=bdt[:, hs], op=ALU.add), reads=[pb, bdt], writes=[Y[s_]])
        if e_ + 1 < n_exp:
            build_P(e_ + 1)
        for t in range(NT):
            for half in range(2):
                hs = slice(half * 512, (half + 1) * 512)
                pb = psum[2 + (2 * t + half) % 2]
                for s_ in range(NST):
                    mm(k, pb, pb[:], PmT[s_], PmT[s_][:, t * 128:(t + 1) * 128], Y[s_], Y[s_][:, hs], s_ == 0, s_ == NST - 1)
                k.op("dve", lambda e, t=t, hs=hs, pb=pb: e.tensor_tensor(
                    out=acc[t][:, hs], in0=pb[:], in1=acc[t][:, hs], op=ALU.add), reads=[pb, acc[t]], writes=[acc[t]])

    k.release(m2)
    hp = [k.sb("hp%d%s" % (i, tag), [128, 1024], F32) for i in range(2)]
    for t in range(NT):
        rows = slice(t * 128, (t + 1) * 128)
        h_ = hp[t % 2]
        k.dma("sp", h_[:], hpre_d[rows, :], reads=[hpre_d], writes=[h_])
        k.op("dve", lambda e, t=t: e.tensor_tensor(out=acc[t][:], in0=acc[t][:], in1=m_g2[:], op=ALU.mult),
             reads=[acc[t], m_g2], writes=[acc[t]])
        k.op("pool", lambda e, t=t, h_=h_: e.tensor_tensor(out=h_[:], in0=acc[t][:], in1=h_[:], op=ALU.add),
             reads=[acc[t], h_], writes=[h_])
        toks.append(k.dma("sp", out_d[out_row0 + t * 128:out_row0 + (t + 1) * 128, :], h_[:], reads=[h_], writes=[out_d]))
    k.release(m0)
    return toks


def build_bd_sparse(F, n_exp=32, debug=False):
    nc = bass.Bass("TRN2", target_bir_lowering=False)
    with contextlib.ExitStack() as stack:
        k = KB(nc, stack)
        psum, pbf, pall = make_psum_all(k)
        d = bd_dram(k, F, "", n_exp)
        out_d = k.dram("out", [NTOK, 1024], F32, "ExternalOutput")
        hpre_d = k.dram("hpre_scr", [NTOK, 1024], F32, "ExternalOutput" if debug else "Internal")
        ident = make_ident(k, "id", BF16)
        dbg = None
        if debug:
            dbg = {"gw": k.dram("dbg_gw", [128, NT, 32], F32, "ExternalOutput"),
                   "pos": k.dram("dbg_pos", [128, NT, 32], F32, "ExternalOutput")}
        toks = emit_bd_sparse(k, F, d, psum, pbf, out_d, hpre_d, ident, "", n_exp, dbg)
        k.emit(final_waits=toks)
        print("BDS arena peak words", k.apeak, "instr", {e: len(v) for e, v in k.prog.items()})
    return nc


CAP2 = 1024
NST2 = CAP2 // 128
U32 = mybir.dt.uint32
I32 = mybir.dt.int32


def emit_bd_sp2(k, F, d, psum, pbf, out_d, hpre_d, ident, scr, tag="", n_exp=32, dbg=None, oin_parts=None, hres_src=None,
                out_row0=0, ridx_d=None):
    FC = F // 128
    if oin_parts is None:
        oin_parts = [(slice(0, F), d["oin"], slice(0, F), 0)]
    if hres_src is None:
        hres_src = (d["hres"], 0)
    pg, pu, pd, pm = psum[0:2], psum[2:4], psum[4:6], psum[6:8]
    pgb, pmb = pbf[0:2], pbf[6:8]
    idf, idb = ident
    u_scr, y_scr = scr["u"], scr["y"]
    m0 = k.mark()
    gw = k.sb("gw" + tag, [128, NT, 32], F32)
    maskf = k.sb("maskf" + tag, [128, NT, 32], F32)
    maskb = k.sb("maskb" + tag, [128, NT, 32], BF16)
    pos_all = k.sb("pos" + tag, [128, NT, 32], F32)
    lgs = k.sb("lgs" + tag, [128, NT, 32], F32)
    tv = k.sb("tv" + tag, [128, NT, 8], F32)
    rt = k.sb("rt" + tag, [128, NT, 16], F32)
    rt_u = k.sb("rtu" + tag, [128, NT, 4], U32)
    m_g2 = k.sb("m_g2" + tag, [128, 1024], F32)
    bgu = k.sb("bgu" + tag, [128, 32, 2, 8], F32)
    bgu1 = k.sb("bgu1" + tag, [128, 32, 8], F32)
    epsb = k.sb("epsb" + tag, [128, 1], F32)
    k.op("pool", lambda e: e.memset(epsb[:], EPS), writes=[epsb])
    ridx = None
    if ridx_d is not None:
        ridx = k.sb("ridx" + tag, [128, NT], U32)
        k.dma("sp", ridx[:], ridx_d[:], reads=[ridx_d], writes=[ridx])
    k.dma("sp", bgu[:], d["bgu"][:], reads=[d["bgu"]], writes=[bgu])
    k.op("pool", lambda e: e.tensor_scalar(out=bgu1[:], in0=bgu[:, :, 1, :], scalar1=1.0, scalar2=None, op0=ALU.add),
         reads=[bgu], writes=[bgu1])

    m1 = k.mark()
    m_g1 = k.sb("m_g1" + tag, [128, 1024], F32)
    m_sh2 = k.sb("m_sh2" + tag, [128, 1024], F32)
    m_gm = k.sb("m_gm" + tag, [128, 1024], F32)
    gain_bc = k.sb("gainbc" + tag, [128, 1024], F32)
    stage = k.sb("adast" + tag, [128, 8, 1024], F32)
    woutb = k.sb("woutb" + tag, [128, FC, 1024], BF16)
    rwb = k.sb("rwb" + tag, [128, 8, 32], BF16)
    rb_bc = k.sb("rbbc" + tag, [128, 32], F32)
    o_tok = [k.sb("otok%d%s" % (i, tag), [128, F], BF16) for i in range(2)]
    oT = [k.sb("oT%d%s" % (i, tag), [128, FC, 128], BF16) for i in range(2)]
    hres_t = [k.sb("hrt%d%s" % (i, tag), [128, 1024], F32) for i in range(2)]
    tmp = [k.sb("tmp%d%s" % (i, tag), [128, 1024], F32) for i in range(2)]
    utk = [k.sb("utk%d%s" % (i, tag), [128, 1024], BF16) for i in range(2)]
    uTt = [k.sb("uTt%d%s" % (i, tag), [128, 8, 128], BF16) for i in range(2)]
    st = [k.sb("st%d%s" % (i, tag), [128, 64], F32) for i in range(2)]
    lg = [k.sb("lg%d%s" % (i, tag), [128, 4, 32], F32) for i in range(2)]
    triU = k.sb("triU" + tag, [128, 128], BF16)
    onesb = k.sb("onesb" + tag, [128, 128], BF16)
    onesf = k.sb("onesf" + tag, [128, 128], F32)
    ecap_i = k.sb("ecapi" + tag, [128, 32], I32)
    ecap = k.sb("ecap" + tag, [128, 32], F32)
    k.op("pool", lambda e: e.memset(onesf[:], 1.0), writes=[onesf])
    k.op("pool", lambda e: e.tensor_copy(out=onesb[:], in_=onesf[:]), reads=[onesf], writes=[onesb])
    k.op("pool", lambda e: e.affine_select(out=triU[:], in_=onesf[:], pattern=[[1, 128]], compare_op=ALU.is_gt, fill=0.0,
                                           base=0, channel_multiplier=-1), reads=[onesf], writes=[triU])
    k.op("pool", lambda e: e.iota(ecap_i[:], pattern=[[CAP2, 32]], base=0, channel_multiplier=0), writes=[ecap_i])
    k.op("pool", lambda e: e.tensor_copy(out=ecap[:], in_=ecap_i[:]), reads=[ecap_i], writes=[ecap])

    k.dma("pool", woutb[:], d["wout"][:], reads=[d["wout"]], writes=[woutb])
    k.dma("pool", rwb[:], d["rw"][:], reads=[d["rw"]], writes=[rwb])
    k.dma("sp", rb_bc[:], d["rb"][:].partition_broadcast(128), reads=[d["rb"]], writes=[rb_bc])
    k.dma("sp", gain_bc[:], d["gain"][:].partition_broadcast(128), reads=[d["gain"]], writes=[gain_bc])
    adaln(k, d["ccol"], d["adaw"], d["adab"], 4, [m_g1, m_sh2, m_gm, m_g2], [stage, stage], pm, tag)
    k.op("dve", lambda e: e.scalar_tensor_tensor(out=m_gm[:], in0=m_gm[:], scalar=1.0, in1=gain_bc[:],
                                                 op0=ALU.add, op1=ALU.mult), reads=[m_gm, gain_bc], writes=[m_gm])

    for t in range(NT):
        ot, oTt, hr, tp, ut, s_, l_, uT_ = o_tok[t % 2], oT[t % 2], hres_t[t % 2], tmp[t % 2], utk[t % 2], st[t % 2], lg[t % 2], uTt[t % 2]
        rows = slice(t * 128, (t + 1) * 128)
        if ridx is None:
            for (dcs, sbuf_, scs, r0) in oin_parts:
                k.dma("sp", ot[:, dcs], sbuf_[r0 + t * 128:r0 + (t + 1) * 128, scs], reads=[sbuf_], writes=[ot])
            k.dma("sp", hr[:], hres_src[0][hres_src[1] + t * 128:hres_src[1] + (t + 1) * 128, :], reads=[hres_src[0]], writes=[hr])
        else:
            for (dcs, sbuf_, scs, r0) in oin_parts:
                k.cc("pool", lambda e, ot=ot, dcs=dcs, sbuf_=sbuf_, t=t: e.indirect_dma_start(
                    out=ot[:, dcs], out_offset=None, in_=sbuf_[:, :],
                    in_offset=bass.IndirectOffsetOnAxis(ap=ridx[:, t:t + 1], axis=0)), reads=[sbuf_, ridx], writes=[ot])
            k.cc("pool", lambda e, hr=hr, t=t: e.indirect_dma_start(
                out=hr[:], out_offset=None, in_=hres_src[0][:, :],
                in_offset=bass.IndirectOffsetOnAxis(ap=ridx[:, t:t + 1], axis=0)), reads=[hres_src[0], ridx], writes=[hr])
        for g in range(FC // 8):
            pb, pbv = pg[g % 2], pgb[g % 2]
            for c in range(8):
                fc = g * 8 + c
                tr(k, pb, pbv[:, c * 128:(c + 1) * 128], ot, ot[:, fc * 128:(fc + 1) * 128], idb, idb[:])
            k.op("act", lambda e, g=g, pbv=pbv, oTt=oTt: e.activation(
                out=oTt[:, g * 8:(g + 1) * 8, :], in_=pbv.rearrange("p (a b) -> p a b", a=8), func=AF.Copy),
                reads=[pb], writes=[oTt])
        for half in range(2):
            hs = slice(half * 512, (half + 1) * 512)
            for fc in range(FC):
                mm(k, pd[half], pd[half][:], oTt, oTt[:, fc, :], woutb, woutb[:, fc, hs], fc == 0, fc == FC - 1)
            k.op("dve", lambda e, half=half, hs=hs, tp=tp: e.tensor_tensor(
                out=tp[:, hs], in0=pd[half][:], in1=m_g1[:, hs], op=ALU.mult), reads=[pd[half], m_g1], writes=[tp])
        k.op("pool", lambda e, hr=hr, tp=tp: e.tensor_tensor(out=hr[:], in0=tp[:], in1=hr[:], op=ALU.add),
             reads=[tp, hr], writes=[hr])
        k.dma("sp", hpre_d[rows, :], hr[:], reads=[hr], writes=[hpre_d])
        k.op("act", lambda e, hr=hr, tp=tp, s_=s_: e.activation(out=tp[:], in_=hr[:], func=AF.Square,
                                                                 accum_out=s_[:, 0:1]), reads=[hr], writes=[tp, s_])
        k.op("act", lambda e, s_=s_: e.activation(out=s_[:, 1:2], in_=s_[:, 0:1], func=AF.Sqrt, bias=epsb[:],
                                                   scale=1.0 / 1024.0), reads=[s_, epsb], writes=[s_])
        k.op("dve", lambda e, s_=s_: e.reciprocal(out=s_[:, 2:3], in_=s_[:, 1:2]), reads=[s_], writes=[s_])
        k.op("dve", lambda e, hr=hr, tp=tp, s_=s_: e.scalar_tensor_tensor(
            out=tp[:], in0=hr[:], scalar=s_[:, 2:3], in1=m_gm[:], op0=ALU.mult, op1=ALU.mult),
            reads=[hr, s_, m_gm], writes=[tp])
        k.op("pool", lambda e, tp=tp, ut=ut: e.tensor_tensor(out=ut[:], in0=tp[:], in1=m_sh2[:], op=ALU.add),
             reads=[tp, m_sh2], writes=[ut])
        k.dma("sp", u_scr[rows, :], ut[:], reads=[ut], writes=[u_scr])
        for kc in range(8):
            tr(k, pm[0], pmb[0][:, kc * 128:(kc + 1) * 128], ut, ut[:, kc * 128:(kc + 1) * 128], idb, idb[:])
        k.op("act", lambda e, uT_=uT_: e.activation(out=uT_[:], in_=pmb[0].rearrange("p (a b) -> p a b", a=8), func=AF.Copy),
             reads=[pm[0]], writes=[uT_])
        for kc in range(8):
            mm(k, pm[1], pm[1][:, 0:32], uT_, uT_[:, kc, :], rwb, rwb[:, kc, :], kc == 0, kc == 7)
        k.op("dve", lambda e, t=t: e.tensor_tensor(out=lgs[:, t, :], in0=pm[1][:, 0:32], in1=rb_bc[:], op=ALU.add),
             reads=[pm[1], rb_bc], writes=[lgs])
        k.op("dve", lambda e, t=t: e.max(out=tv[:, t, :], in_=lgs[:, t, :]), reads=[lgs], writes=[tv])
        k.op("dve", lambda e, t=t: e.tensor_scalar(out=maskf[:, t, :], in0=lgs[:, t, :], scalar1=tv[:, t, 3:4],
                                                   scalar2=None, op0=ALU.is_ge), reads=[lgs, tv], writes=[maskf])
        k.op("pool", lambda e, t=t: e.tensor_copy(out=maskb[:, t, :], in_=maskf[:, t, :]), reads=[maskf], writes=[maskb])
        k.op("dve", lambda e, s_=s_, t=t: e.tensor_scalar(out=s_[:, 16:17], in0=tv[:, t, 0:1], scalar1=-1.0, scalar2=None,
                                                          op0=ALU.mult), reads=[tv], writes=[s_])
        k.op("act", lambda e, l_=l_, s_=s_, t=t: e.activation(out=l_[:, 2, :], in_=lgs[:, t, :], func=AF.Exp,
                                                               bias=s_[:, 16:17], scale=1.0), reads=[lgs, s_], writes=[l_])
        k.op("dve", lambda e, l_=l_, s_=s_, t=t: e.scalar_tensor_tensor(
            out=l_[:, 3, :], in0=l_[:, 2, :], scalar=1.0, in1=maskf[:, t, :], op0=ALU.mult, op1=ALU.mult,
            accum_out=s_[:, 17:18]), reads=[l_, maskf], writes=[l_, s_])
        k.op("dve", lambda e, s_=s_: e.reciprocal(out=s_[:, 18:19], in_=s_[:, 17:18]), reads=[s_], writes=[s_])
        k.op("dve", lambda e, l_=l_, s_=s_, t=t: e.tensor_scalar(out=gw[:, t, :], in0=l_[:, 3, :], scalar1=s_[:, 18:19],
                                                                 scalar2=None, op0=ALU.mult), reads=[l_, s_], writes=[gw])
    for t in range(NT):
        pb = pm[t % 2]
        mm(k, pb, pb[:, 0:32], triU, triU[:], maskb, maskb[:, t, :], True, t == 0)
        for t2_ in range(t):
            mm(k, pb, pb[:, 0:32], onesb, onesb[:], maskb, maskb[:, t2_, :], False, t2_ == t - 1)
        k.op("act", lambda e, t=t, pb=pb: e.activation(out=pos_all[:, t, :], in_=pb[:, 0:32], func=AF.Copy),
             reads=[pb], writes=[pos_all])
    ohs = lg[0]
    for t in range(NT):
        for kk in range(4):
            k.op("dve", lambda e, t=t, kk=kk: e.tensor_scalar(out=ohs[:, 0, :], in0=lgs[:, t, :], scalar1=tv[:, t, kk:kk + 1],
                                                              scalar2=None, op0=ALU.is_equal), reads=[lgs, tv], writes=[ohs])
            for col, src in ((0, pos_all[:, t, :]), (4, ecap[:]), (8, gw[:, t, :])):
                k.op("dve", lambda e, t=t, kk=kk, col=col, src=src: e.scalar_tensor_tensor(
                    out=ohs[:, 1, :], in0=ohs[:, 0, :], scalar=1.0, in1=src, op0=ALU.mult, op1=ALU.mult,
                    accum_out=rt[:, t, col + kk:col + kk + 1]), reads=[ohs, pos_all, ecap, gw], writes=[ohs, rt])
    k.op("dve", lambda e: e.tensor_scalar(out=rt[:, :, 12:16], in0=rt[:, :, 0:4], scalar1=float(CAP2) - 0.5, scalar2=None,
                                          op0=ALU.is_lt), reads=[rt], writes=[rt])
    k.op("dve", lambda e: e.tensor_tensor(out=rt[:, :, 8:12], in0=rt[:, :, 8:12], in1=rt[:, :, 12:16], op=ALU.mult),
         reads=[rt], writes=[rt])
    k.op("dve", lambda e: e.tensor_scalar(out=rt[:, :, 12:16], in0=rt[:, :, 0:4], scalar1=float(CAP2 - 1), scalar2=None,
                                          op0=ALU.min), reads=[rt], writes=[rt])
    k.op("dve", lambda e: e.tensor_tensor(out=rt[:, :, 12:16], in0=rt[:, :, 12:16], in1=rt[:, :, 4:8], op=ALU.add),
         reads=[rt], writes=[rt])
    k.op("dve", lambda e: e.tensor_copy(out=rt_u[:], in_=rt[:, :, 12:16]), reads=[rt], writes=[rt_u])

    toks = []
    if dbg is not None:
        toks.append(k.dma("sp", dbg["gw"][:], gw[:], reads=[gw], writes=[dbg["gw"]]))
        toks.append(k.dma("sp", dbg["pos"][:], pos_all[:], reads=[pos_all], writes=[dbg["pos"]]))
        toks.append(k.dma("sp", dbg["rt"][:], rt[:], reads=[rt], writes=[dbg["rt"]]))
    k.release(m1)
    m2 = k.mark()
    iota_i = k.sb("iota_i" + tag, [128, CAP2], I32)
    iota_f = k.sb("iota_f" + tag, [128, CAP2], F32)
    tokc_i = k.sb("tokci" + tag, [128, NT, 2], I32)
    tokc = k.sb("tokc" + tag, [128, NT, 2], BF16)
    k.op("pool", lambda e: e.iota(iota_i[:], pattern=[[1, CAP2]], base=0, channel_multiplier=0), writes=[iota_i])
    k.op("pool", lambda e: e.tensor_copy(out=iota_f[:], in_=iota_i[:]), reads=[iota_i], writes=[iota_f])
    k.op("pool", lambda e: e.iota(tokc_i[:, :, 0], pattern=[[0, NT]], base=0, channel_multiplier=1), writes=[tokc_i])
    k.op("pool", lambda e: e.iota(tokc_i[:, :, 1], pattern=[[1, NT]], base=0, channel_multiplier=0), writes=[tokc_i])
    k.op("pool", lambda e: e.tensor_copy(out=tokc[:], in_=tokc_i[:]), reads=[tokc_i], writes=[tokc])
    Pm = [k.sb("Pm%d%s" % (t, tag), [128, CAP2], BF16) for t in range(NT)]
    sidx_f = k.sb("sidxf" + tag, [128, NST2, 2], F32)
    sidx = [k.sb("sidx%d%s" % (i, tag), [128, NST2], U32) for i in range(2)]
    sidx_t = k.sb("sidxt" + tag, [128, NST2], F32)
    Xg = [k.sb("Xg%d%s" % (i, tag), [128, 1024], BF16) for i in range(2)]
    XgT = k.sb("XgT" + tag, [128, 8, CAP2], BF16)
    actT = [k.sb("actT%d%s" % (j, tag), [128, CAP2], BF16) for j in range(8)]
    Ysb = k.sb("Ysb" + tag, [128, NST2, 1024], BF16)
    bdb = [k.sb("bdb%d%s" % (i, tag), [128, 1024], F32) for i in range(2)]
    NR = 3
    wring = [k.sb("wgur%d%s" % (i, tag), [128, 8, 2, 128], BF16) for i in range(NR)]
    ND = 2
    dring = [k.sb("wdr%d%s" % (i, tag), [128, 8, 512], BF16) for i in range(ND)]
    g_sb = k.sb("g_sb" + tag, [128, 512], F32)
    s_sb = k.sb("s_sb" + tag, [128, 512], F32)
    t1 = k.sb("t1" + tag, [128, 512], F32)
    t2 = k.sb("t2" + tag, [128, 512], F32)
    m_sb = k.sb("m_sb" + tag, [128, 512], F32)

    units = [(e, j) for e in range(n_exp) for j in range(8)]
    dunits = [(e, h) for e in range(n_exp) for h in range(2)]

    def load_unit(i):
        if i < len(units):
            e, j = units[i]
            k.dma("pool", wring[i % NR][:], d["wgu"][e, j], reads=[d["wgu"]], writes=[wring[i % NR]])

    def load_dunit(i):
        if i < len(dunits):
            e, h = dunits[i]
            k.dma("pool", dring[i % ND][:], d["wd"][e, :, :, h * 512:(h + 1) * 512], reads=[d["wd"]],
                  writes=[dring[i % ND]])

    def build_P(e_):
        for t in range(NT):
            k.op("dve", lambda e, t=t, e_=e_: e.tensor_scalar(
                out=Pm[t][:], in0=iota_f[:], scalar1=pos_all[:, t, e_:e_ + 1], scalar2=maskf[:, t, e_:e_ + 1],
                op0=ALU.is_equal, op1=ALU.mult), reads=[iota_f, pos_all, maskf], writes=[Pm[t]])

    for i in range(NR - 1):
        load_unit(i)
    for i in range(ND - 1):
        load_dunit(i)
    build_P(0)
    cnt = 0
    for e_ in range(n_exp):
        bdt = bdb[e_ % 2]
        si = sidx[e_ % 2]
        k.dma("sp", bdt[:], d["bd"][e_].partition_broadcast(128), reads=[d["bd"]], writes=[bdt])
        pb = psum[7]
        for s_ in range(NST2):
            for t in range(NT):
                mm(k, pb, pb[:, 2 * s_:2 * s_ + 2], Pm[t], Pm[t][:, s_ * 128:(s_ + 1) * 128], tokc, tokc[:, t, :], t == 0, t == NT - 1)
        k.op("act", lambda e, pb=pb: e.activation(out=sidx_f[:], in_=pb[:, 0:2 * NST2].rearrange("p (a b) -> p a b", a=NST2),
                                                  func=AF.Copy), reads=[pb], writes=[sidx_f])
        k.op("dve", lambda e: e.scalar_tensor_tensor(out=sidx_t[:], in0=sidx_f[:, :, 1], scalar=128.0, in1=sidx_f[:, :, 0],
                                                     op0=ALU.mult, op1=ALU.add), reads=[sidx_f], writes=[sidx_t])
        k.op("dve", lambda e, si=si: e.tensor_copy(out=si[:], in_=sidx_t[:]), reads=[sidx_t], writes=[si])
        for s_ in range(NST2):
            xg = Xg[s_ % 2]
            k.cc("pool", lambda e, xg=xg, si=si, s_=s_: e.indirect_dma_start(
                out=xg[:], out_offset=None, in_=u_scr[:, :],
                in_offset=bass.IndirectOffsetOnAxis(ap=si[:, s_:s_ + 1], axis=0)), reads=[u_scr, si], writes=[xg])
            pbt, pbv = psum[6], pbf[6]
            for kc in range(8):
                tr(k, pbt, pbv[:, kc * 128:(kc + 1) * 128], xg, xg[:, kc * 128:(kc + 1) * 128], idb, idb[:])
            k.op("act", lambda e, s_=s_, pbv=pbv: e.activation(
                out=XgT[:, :, s_ * 128:(s_ + 1) * 128], in_=pbv.rearrange("p (a b) -> p a b", a=8), func=AF.Copy),
                reads=[pbt], writes=[XgT])
        if e_ + 1 < n_exp:
            build_P(e_ + 1)
        for j in range(8):
            ui = e_ * 8 + j
            load_unit(ui + NR - 1)
            w = wring[ui % NR]
            for T in range(CAP2 // 512):
                x = cnt % 2
                cnt += 1
                ts_ = slice(T * 512, (T + 1) * 512)
                for kc in range(8):
                    mm(k, pg[x], pg[x][:], w, w[:, kc, 0, :], XgT, XgT[:, kc, ts_], kc == 0, kc == 7)
                for kc in range(8):
                    mm(k, pu[x], pu[x][:], w, w[:, kc, 1, :], XgT, XgT[:, kc, ts_], kc == 0, kc == 7)
                k.op("dve", lambda e, x=x, e_=e_, j=j: e.tensor_scalar(
                    out=g_sb[:], in0=pg[x][:], scalar1=bgu[:, e_, 0, j:j + 1], scalar2=SWIGLU_LIMIT,
                    op0=ALU.add, op1=ALU.min), reads=[pg[x], bgu], writes=[g_sb])
                k.op("act", lambda e: e.activation(out=s_sb[:], in_=g_sb[:], func=AF.Sigmoid, scale=SWIGLU_ALPHA),
                     reads=[g_sb], writes=[s_sb])
                k.op("act", lambda e, x=x, e_=e_, j=j: e.activation(
                    out=t1[:], in_=pu[x][:], func=AF.Identity, bias=bgu1[:, e_, j:j + 1], scale=1.0),
                    reads=[pu[x], bgu1], writes=[t1])
                k.op("pool", lambda e: e.tensor_scalar(out=t2[:], in0=t1[:], scalar1=SWIGLU_LIMIT + 1.0,
                                                       scalar2=1.0 - SWIGLU_LIMIT, op0=ALU.min, op1=ALU.max),
                     reads=[t1], writes=[t2])
                k.op("dve", lambda e: e.tensor_tensor(out=m_sb[:], in0=g_sb[:], in1=s_sb[:], op=ALU.mult),
                     reads=[g_sb, s_sb], writes=[m_sb])
                k.op("pool", lambda e, j=j, ts_=ts_: e.tensor_tensor(out=actT[j][:, ts_], in0=m_sb[:], in1=t2[:], op=ALU.mult),
                     reads=[m_sb, t2], writes=[actT[j]])
        for half in range(2):
            di = e_ * 2 + half
            load_dunit(di + ND - 1)
            wdv = dring[di % ND]
            hs = slice(half * 512, (half + 1) * 512)
            for s_ in range(NST2):
                pb = pd[s_ % 2]
                for fc in range(8):
                    mm(k, pb, pb[:], actT[fc], actT[fc][:, s_ * 128:(s_ + 1) * 128], wdv, wdv[:, fc, :], fc == 0, fc == 7)
                k.op("dve", lambda e, s_=s_, hs=hs, pb=pb, bdt=bdt: e.tensor_tensor(
                    out=Ysb[:, s_, hs], in0=pb[:], in1=bdt[:, hs], op=ALU.add), reads=[pb, bdt], writes=[Ysb])
        k.dma("sp", y_scr[e_ * CAP2:(e_ + 1) * CAP2, :].rearrange("(a p) c -> p a c", p=128), Ysb[:], reads=[Ysb], writes=[y_scr])

    k.release(m2)
    hp = [k.sb("hp%d%s" % (i, tag), [128, 1024], F32) for i in range(2)]
    yk = [k.sb("yk%d%s" % (i, tag), [128, 1024], BF16) for i in range(4)]
    ac = [k.sb("ac%d%s" % (i, tag), [128, 1024], F32) for i in range(2)]
    for t in range(NT):
        rows = slice(t * 128, (t + 1) * 128)
        h_, a_ = hp[t % 2], ac[t % 2]
        k.dma("sp", h_[:], hpre_d[rows, :], reads=[hpre_d], writes=[h_])
        for kk in range(4):
            k.cc("pool", lambda e, kk=kk, t=t: e.indirect_dma_start(
                out=yk[kk][:], out_offset=None, in_=y_scr[:, :],
                in_offset=bass.IndirectOffsetOnAxis(ap=rt_u[:, t, kk:kk + 1], axis=0)), reads=[y_scr, rt_u], writes=[yk[kk]])
        k.op("dve", lambda e, t=t, a_=a_: e.tensor_scalar(out=a_[:], in0=yk[0][:], scalar1=rt[:, t, 8:9], scalar2=None,
                                                          op0=ALU.mult), reads=[yk[0], rt], writes=[a_])
        for kk in range(1, 4):
            k.op("dve", lambda e, t=t, kk=kk, a_=a_: e.scalar_tensor_tensor(
                out=a_[:], in0=yk[kk][:], scalar=rt[:, t, 8 + kk:9 + kk], in1=a_[:], op0=ALU.mult, op1=ALU.add),
                reads=[yk[kk], rt, a_], writes=[a_])
        k.op("pool", lambda e, a_=a_: e.tensor_tensor(out=a_[:], in0=a_[:], in1=m_g2[:], op=ALU.mult),
             reads=[a_, m_g2], writes=[a_])
        k.op("pool", lambda e, a_=a_, h_=h_: e.tensor_tensor(out=h_[:], in0=a_[:], in1=h_[:], op=ALU.add),
             reads=[a_, h_], writes=[h_])
        toks.append(k.dma("sp", out_d[out_row0 + t * 128:out_row0 + (t + 1) * 128, :], h_[:], reads=[h_], writes=[out_d]))
    k.release(m0)
    return toks


def build_bd_sp2(F, n_exp=32, debug=False):
    nc = bass.Bass("TRN2", target_bir_lowering=False)
    with contextlib.ExitStack() as stack:
        k = KB(nc, stack)
        psum, pbf, pall = make_psum_all(k)
        d = bd_dram(k, F, "", n_exp)
        out_d = k.dram("out", [NTOK, 1024], F32, "ExternalOutput")
        hpre_d = k.dram("hpre_scr", [NTOK, 1024], F32, "ExternalOutput" if debug else "Internal")
        scr = {"u": k.dram("u_scr", [NTOK, 1024], BF16, "Internal"),
               "y": k.dram("y_scr", [32 * CAP2, 1024], BF16, "ExternalOutput" if debug else "Internal")}
        ident = make_ident(k, "id", BF16)
        dbg = None
        if debug:
            dbg = {"gw": k.dram("dbg_gw", [128, NT, 32], F32, "ExternalOutput"),
                   "pos": k.dram("dbg_pos", [128, NT, 32], F32, "ExternalOutput"),
                   "rt": k.dram("dbg_rt", [128, NT, 16], F32, "ExternalOutput")}
        toks = emit_bd_sp2(k, F, d, psum, pbf, out_d, hpre_d, ident, scr, "", n_exp, dbg)
        k.emit(final_waits=toks)
        print("BDS2 arena peak words", k.apeak, "instr", {e: len(v) for e, v in k.prog.items()})
    return nc
```

```python
import contextlib
import numpy as np
import ml_dtypes
import concourse.bass as bass
import concourse.mybir as mybir
from concourse.bass_utils import run_bass_kernel_spmd

F32 = mybir.dt.float32
BF16 = mybir.dt.bfloat16
AF = mybir.ActivationFunctionType
ALU = mybir.AluOpType
AX = mybir.AxisListType
NPBF = ml_dtypes.bfloat16

N_DMA_SEMS = 16
ARENA_W = 51 * 1024
EPS = 1e-6


class Buf:
    __slots__ = ("t", "last_w", "readers", "name")

    REG = []

    def __init__(self, t, name=""):
        self.t = t
        self.last_w = None
        self.readers = {}
        self.name = name
        Buf.REG.append(self)

    def __getitem__(self, idx):
        return self.t[idx]


class KB:
    ENG = ("pe", "act", "dve", "pool", "sp")

    def __init__(self, nc, stack):
        self.nc = nc
        self.stack = stack
        self.prog = {e: [] for e in self.ENG}
        self.cnt = {e: 0 for e in self.ENG}
        self.sems = {}
        self.epoch = 0
        Buf.REG = []
        for e in self.ENG:
            self.sems[(e, 0)] = stack.enter_context(nc.semaphore("s_" + e))
        self.dsem = []
        for i in range(N_DMA_SEMS):
            self.dsem.append(stack.enter_context(nc.semaphore("d_%d" % i)))
            self.sems[("d", i)] = self.dsem[i]
        self.dcnt = [0] * N_DMA_SEMS
        self.dnext = 0
        self.waited = {}
        self.arena = None
        self.apeak = 0

    def sb(self, name, shape, dt):
        if self.arena is None:
            self.arena = self.stack.enter_context(self.nc.sbuf_tensor("arena", [128, ARENA_W], F32))
            self.aoff = 0
        esz = 2 if dt == BF16 else 4
        nel = 1
        for x in shape[1:]:
            nel *= x
        n32 = (nel * esz + 3) // 4
        n32 = (n32 + 7) // 8 * 8
        assert self.aoff + n32 <= ARENA_W, "SBUF arena overflow at %s (%d + %d)" % (name, self.aoff, n32)
        v = self.arena[0:shape[0], self.aoff:self.aoff + n32]
        self.aoff += n32
        self.apeak = max(self.apeak, self.aoff)
        if dt != F32:
            v = v.bitcast(dt)
        v = v[:, 0:nel]
        if len(shape) == 3:
            v = v.rearrange("p (a b) -> p a b", a=shape[1])
        elif len(shape) == 4:
            v = v.rearrange("p (a b c) -> p a b c", a=shape[1], b=shape[2])
        return Buf(v, name)

    def mark(self):
        return self.aoff

    def release(self, m):
        self.fence()
        self.aoff = m

    def ps(self, name, shape, dt=F32):
        return Buf(self.stack.enter_context(self.nc.psum_tensor(name, list(shape), dt)), name)

    def dram(self, name, shape, dt, kind):
        return Buf(self.nc.dram_tensor(name, list(shape), dt, kind=kind).ap(), name)

    def _wait(self, eng, key, val):
        if key == ("pe", self.epoch) and eng == "pe":
            return
        if self.waited.get((eng, key), 0) >= val:
            return
        self.waited[(eng, key)] = val
        self.prog[eng].append(("w", key, val))

    def _deps(self, eng, reads, writes):
        for b in reads:
            if b.last_w is not None:
                self._wait(eng, *b.last_w)
        for b in writes:
            if b.last_w is not None:
                self._wait(eng, *b.last_w)
            for tok in b.readers.values():
                self._wait(eng, *tok)

    def _mark(self, tok, reads, writes):
        for b in reads:
            b.readers[tok[0]] = tok
        for b in writes:
            b.last_w = tok
            b.readers = {}

    def op(self, eng, fn, reads=(), writes=()):
        self._deps(eng, reads, writes)
        self.cnt[eng] += 1
        tok = ((eng, self.epoch), self.cnt[eng])
        self.prog[eng].append(("o", fn, self.epoch))
        self._mark(tok, reads, writes)
        return tok

    def dma(self, eng, out, in_, reads=(), writes=(), **kw):
        i = self.dnext
        self.dnext = (self.dnext + 1) % N_DMA_SEMS
        key = ("d", i)
        if self.dcnt[i] > 0:
            self._wait(eng, key, self.dcnt[i])
        self._deps(eng, reads, writes)
        self.dcnt[i] += 16
        tok = (key, self.dcnt[i])
        self.prog[eng].append(("d", out, in_, i, kw))
        self._mark(tok, reads, writes)
        return tok

    def cc(self, eng, fn, reads=(), writes=()):
        i = self.dnext
        self.dnext = (self.dnext + 1) % N_DMA_SEMS
        key = ("d", i)
        if self.dcnt[i] > 0:
            self._wait(eng, key, self.dcnt[i])
        self._deps(eng, reads, writes)
        self.dcnt[i] += 16
        tok = (key, self.dcnt[i])
        self.prog[eng].append(("c", fn, i))
        self._mark(tok, reads, writes)
        return tok

    def new_epoch(self):
        self.fence()
        self.epoch += 1
        for e in self.ENG:
            self.sems[(e, self.epoch)] = self.stack.enter_context(self.nc.semaphore("s_%s_%d" % (e, self.epoch)))
            self.cnt[e] = 0
        self.waited = {kk: v for kk, v in self.waited.items() if isinstance(kk[1], tuple) and kk[1][0] == "d"}
        for b in Buf.REG:
            b.last_w = None
            b.readers = {}

    def fence(self):
        for e in self.ENG:
            for o in self.ENG:
                if self.cnt[o] > 0:
                    self._wait(e, (o, self.epoch), self.cnt[o])
            for i in range(N_DMA_SEMS):
                if self.dcnt[i] > 0:
                    self._wait(e, ("d", i), self.dcnt[i])

    def emit(self, final_waits=()):
        nc = self.nc
        engmap = {"pe": "tensor", "act": "scalar", "dve": "vector", "pool": "gpsimd", "sp": "sync"}
        for tok in final_waits:
            self._wait("sp", tok[0], tok[1])
        with nc.Block() as block:
            for e in self.ENG:
                def body(h, items=self.prog[e], e=e):
                    for it in items:
                        if it[0] == "w":
                            h.wait_ge(self.sems[it[1]], it[2])
                        elif it[0] == "o":
                            it[1](h).then_inc(self.sems[(e, it[2])], 1)
                        elif it[0] == "c":
                            it[1](h).then_inc(self.dsem[it[2]], 16)
                        else:
                            _, out, in_, i, kw = it
                            if callable(out):
                                out = out(h)
                            if callable(in_):
                                in_ = in_(h)
                            h.dma_start(out=out, in_=in_, **kw).then_inc(self.dsem[i], 16)
                getattr(block, engmap[e])(body)


def mm(k, ob, o, lb, l, rb, r, start, stop):
    k.op("pe", lambda e: e.matmul(o, lhsT=l, rhs=r, start=start, stop=stop), reads=[lb, rb], writes=[ob])


def tr(k, ob, o, ib, i, idb, ident):
    k.op("pe", lambda e: e.transpose(o, i, ident), reads=[ib, idb], writes=[ob])


def make_ident(k, name, dt):
    idf = k.sb(name + "_f", [128, 128], F32)
    k.op("pool", lambda e: e.memset(idf[:], 1.0), writes=[idf])
    k.op("pool", lambda e: e.affine_select(out=idf[:], in_=idf[:], pattern=[[-1, 128]], compare_op=ALU.is_equal,
                                           fill=0.0, base=0, channel_multiplier=1), reads=[idf], writes=[idf])
    if dt == F32:
        return idf, None
    idb = k.sb(name + "_b", [128, 128], dt)
    k.op("pool", lambda e: e.tensor_copy(out=idb[:], in_=idf[:]), reads=[idf], writes=[idb])
    return idf, idb


def adaln(k, ccol_d, adaw_d, adab_d, nvec, dst, stage, pbanks, tag=""):
    ccol = k.sb("ada_c" + tag, [128, 8], F32)
    cond = k.sb("ada_cond" + tag, [128, 8], F32)
    cbc = k.sb("ada_cbc" + tag, [128, 8, 128], F32)
    k.dma("sp", ccol[:], ccol_d[:], reads=[ccol_d], writes=[ccol])
    k.op("act", lambda e: e.activation(out=cond[:], in_=ccol[:], func=AF.Silu), reads=[ccol], writes=[cond])
    for kc in range(8):
        k.op("dve", lambda e, kc=kc: e.tensor_copy(out=cbc[:, kc, :], in_=cond[:, kc:kc + 1].to_broadcast([128, 128])),
             reads=[cond], writes=[cbc])
    for v in range(nvec):
        st = stage[v % 2]
        k.dma("sp", st[:], adaw_d[v], reads=[adaw_d], writes=[st])
        k.dma("sp", dst[v][:], adab_d[v].partition_broadcast(128), reads=[adab_d], writes=[dst[v]])
        for half in range(2):
            pb = pbanks[half]
            for kc in range(8):
                mm(k, pb, pb[:], cbc, cbc[:, kc, :], st, st[:, kc, half * 512:(half + 1) * 512], kc == 0, kc == 7)
            k.op("dve", lambda e, v=v, half=half, pb=pb: e.tensor_tensor(
                out=dst[v][:, half * 512:(half + 1) * 512], in0=pb[:], in1=dst[v][:, half * 512:(half + 1) * 512],
                op=ALU.add), reads=[pb, dst[v]], writes=[dst[v]])


NTOK = 2048
NT = NTOK // 128
SWIGLU_LIMIT = 7.0
SWIGLU_ALPHA = 1.702


def bd_dram(k, F, tag, n_exp=32, with_io=True):
    d = {}
    if with_io:
        d["oin"] = k.dram("oin" + tag, [NTOK, F], BF16, "ExternalInput")
        d["hres"] = k.dram("hres" + tag, [NTOK, 1024], F32, "ExternalInput")
    d["wout"] = k.dram("wout" + tag, [128, F // 128, 1024], F32, "ExternalInput")
    d["ccol"] = k.dram("ccol" + tag, [128, 8], F32, "ExternalInput")
    d["adaw"] = k.dram("adaw" + tag, [4, 128, 8, 1024], F32, "ExternalInput")
    d["adab"] = k.dram("adab" + tag, [4, 1024], F32, "ExternalInput")
    d["gain"] = k.dram("gain" + tag, [1024], F32, "ExternalInput")
    d["rw"] = k.dram("rw" + tag, [128, 8, 32], F32, "ExternalInput")
    d["rb"] = k.dram("rb" + tag, [32], F32, "ExternalInput")
    d["wgu"] = k.dram("wgu" + tag, [n_exp, 8, 128, 8, 2, 128], F32, "ExternalInput")
    d["bgu"] = k.dram("bgu" + tag, [128, 32, 2, 8], F32, "ExternalInput")
    d["wd"] = k.dram("wd" + tag, [n_exp, 128, 8, 1024], F32, "ExternalInput")
    d["bd"] = k.dram("bd" + tag, [32, 1024], F32, "ExternalInput")
    return d


def bd_host_inputs(layer, b, P, w_out, tag):
    m = {}
    F = w_out.shape[0]
    m["wout" + tag] = np.ascontiguousarray(w_out.reshape(F // 128, 128, 1024).transpose(1, 0, 2))
    m["ccol" + tag] = np.ascontiguousarray(P["c"][b].reshape(8, 128).T)
    aw = P["ada_w"][layer]
    sel = [2, 3, 4, 5]
    m["adaw" + tag] = np.ascontiguousarray(
        np.stack([aw[:, v * 1024:(v + 1) * 1024].reshape(8, 128, 1024).transpose(1, 0, 2) for v in sel]))
    m["adab" + tag] = np.ascontiguousarray(np.stack([P["ada_b"][layer][v * 1024:(v + 1) * 1024] for v in sel]))
    m["gain" + tag] = np.ascontiguousarray(P["norm_ffn"][layer])
    m["rw" + tag] = np.ascontiguousarray(P["router_w"][layer].reshape(8, 128, 32).transpose(1, 0, 2))
    m["rb" + tag] = np.ascontiguousarray(P["router_b"][layer])
    return m


def bd_host_shared(layer, P, tag):
    m = {}
    wgu = P["moe_w_gu"][layer]
    m["wgu" + tag] = np.ascontiguousarray(wgu.reshape(32, 8, 128, 2, 8, 128).transpose(0, 4, 2, 1, 3, 5))
    bgu = P["moe_b_gu"][layer]
    m["bgu" + tag] = np.ascontiguousarray(bgu.reshape(32, 2, 8, 128).transpose(3, 0, 1, 2))
    wd = P["moe_w_down"][layer]
    m["wd" + tag] = np.ascontiguousarray(wd.reshape(32, 8, 128, 1024).transpose(0, 2, 1, 3))
    m["bd" + tag] = np.ascontiguousarray(P["moe_b_down"][layer])
    return m


def emit_bd(k, F, d, psum, pbf, out_d, hpre_d, ident, tag="", n_exp=32, dbg=None, oin_parts=None, hres_src=None,
            out_row0=0, ridx_d=None):
    FC = F // 128
    if oin_parts is None:
        oin_parts = [(slice(0, F), d["oin"], slice(0, F), 0)]
    if hres_src is None:
        hres_src = (d["hres"], 0)
    pg, pu, pd, pm = psum[0:2], psum[2:4], psum[4:6], psum[6:8]
    pgb, pmb = pbf[0:2], pbf[6:8]
    idf, idb = ident
    m0 = k.mark()
    uT = k.sb("uT" + tag, [128, 8, NTOK], BF16)
    gw = k.sb("gw" + tag, [128, NT, 32], F32)
    gwT = k.sb("gwT" + tag, [32, NTOK], F32)
    m_g2 = k.sb("m_g2" + tag, [128, 1024], F32)
    bgu = k.sb("bgu" + tag, [128, 32, 2, 8], F32)
    bgu1 = k.sb("bgu1" + tag, [128, 32, 8], F32)
    bd_sb = k.sb("bdsb" + tag, [32, 1024], F32)
    epsb = k.sb("epsb" + tag, [128, 1], F32)
    k.op("pool", lambda e: e.memset(epsb[:], EPS), writes=[epsb])
    ridx = None
    if ridx_d is not None:
        ridx = k.sb("ridx" + tag, [128, NT], mybir.dt.uint32)
        k.dma("sp", ridx[:], ridx_d[:], reads=[ridx_d], writes=[ridx])
    k.dma("sp", bgu[:], d["bgu"][:], reads=[d["bgu"]], writes=[bgu])
    k.dma("sp", bd_sb[:], d["bd"][:], reads=[d["bd"]], writes=[bd_sb])
    k.op("pool", lambda e: e.tensor_scalar(out=bgu1[:], in0=bgu[:, :, 1, :], scalar1=1.0, scalar2=None, op0=ALU.add),
         reads=[bgu], writes=[bgu1])

    m1 = k.mark()
    m_g1 = k.sb("m_g1" + tag, [128, 1024], F32)
    m_sh2 = k.sb("m_sh2" + tag, [128, 1024], F32)
    m_gm = k.sb("m_gm" + tag, [128, 1024], F32)
    gain_bc = k.sb("gainbc" + tag, [128, 1024], F32)
    stage = k.sb("adast" + tag, [128, 8, 1024], F32)
    woutb = k.sb("woutb" + tag, [128, FC, 1024], BF16)
    rwb = k.sb("rwb" + tag, [128, 8, 32], BF16)
    rb_bc = k.sb("rbbc" + tag, [128, 32], F32)
    o_tok = [k.sb("otok%d%s" % (i, tag), [128, F], BF16) for i in range(2)]
    oT = [k.sb("oT%d%s" % (i, tag), [128, FC, 128], BF16) for i in range(2)]
    hres_t = [k.sb("hrt%d%s" % (i, tag), [128, 1024], F32) for i in range(2)]
    tmp = [k.sb("tmp%d%s" % (i, tag), [128, 1024], F32) for i in range(2)]
    u_tok = [k.sb("utok%d%s" % (i, tag), [128, 1024], BF16) for i in range(2)]
    st = [k.sb("st%d%s" % (i, tag), [128, 64], F32) for i in range(2)]
    lg = [k.sb("lg%d%s" % (i, tag), [128, 4, 32], F32) for i in range(2)]

    k.dma("pool", woutb[:], d["wout"][:], reads=[d["wout"]], writes=[woutb])
    k.dma("pool", rwb[:], d["rw"][:], reads=[d["rw"]], writes=[rwb])
    k.dma("sp", rb_bc[:], d["rb"][:].partition_broadcast(128), reads=[d["rb"]], writes=[rb_bc])
    k.dma("sp", gain_bc[:], d["gain"][:].partition_broadcast(128), reads=[d["gain"]], writes=[gain_bc])
    adaln(k, d["ccol"], d["adaw"], d["adab"], 4, [m_g1, m_sh2, m_gm, m_g2], [stage, stage], pm, tag)
    k.op("dve", lambda e: e.scalar_tensor_tensor(out=m_gm[:], in0=m_gm[:], scalar=1.0, in1=gain_bc[:],
                                                 op0=ALU.add, op1=ALU.mult), reads=[m_gm, gain_bc], writes=[m_gm])

    for t in range(NT):
        ot, oTt, hr, tp, ut, s_, l_ = o_tok[t % 2], oT[t % 2], hres_t[t % 2], tmp[t % 2], u_tok[t % 2], st[t % 2], lg[t % 2]
        rows = slice(t * 128, (t + 1) * 128)
        if ridx is None:
            for (dcs, sbuf_, scs, r0) in oin_parts:
                k.dma("sp", ot[:, dcs], sbuf_[r0 + t * 128:r0 + (t + 1) * 128, scs], reads=[sbuf_], writes=[ot])
            k.dma("sp", hr[:], hres_src[0][hres_src[1] + t * 128:hres_src[1] + (t + 1) * 128, :], reads=[hres_src[0]], writes=[hr])
        else:
            for (dcs, sbuf_, scs, r0) in oin_parts:
                k.cc("pool", lambda e, ot=ot, dcs=dcs, sbuf_=sbuf_, t=t: e.indirect_dma_start(
                    out=ot[:, dcs], out_offset=None, in_=sbuf_[:, :],
                    in_offset=bass.IndirectOffsetOnAxis(ap=ridx[:, t:t + 1], axis=0)), reads=[sbuf_, ridx], writes=[ot])
            k.cc("pool", lambda e, hr=hr, t=t: e.indirect_dma_start(
                out=hr[:], out_offset=None, in_=hres_src[0][:, :],
                in_offset=bass.IndirectOffsetOnAxis(ap=ridx[:, t:t + 1], axis=0)), reads=[hres_src[0], ridx], writes=[hr])
        for g in range(FC // 8):
            pb, pbv = pg[g % 2], pgb[g % 2]
            for c in range(8):
                fc = g * 8 + c
                tr(k, pb, pbv[:, c * 128:(c + 1) * 128], ot, ot[:, fc * 128:(fc + 1) * 128], idb, idb[:])
            k.op("act", lambda e, g=g, pbv=pbv, oTt=oTt: e.activation(
                out=oTt[:, g * 8:(g + 1) * 8, :], in_=pbv.rearrange("p (a b) -> p a b", a=8), func=AF.Copy),
                reads=[pb], writes=[oTt])
        for half in range(2):
            hs = slice(half * 512, (half + 1) * 512)
            for fc in range(FC):
                mm(k, pd[half], pd[half][:], oTt, oTt[:, fc, :], woutb, woutb[:, fc, hs], fc == 0, fc == FC - 1)
            k.op("dve", lambda e, half=half, hs=hs, tp=tp: e.tensor_tensor(
                out=tp[:, hs], in0=pd[half][:], in1=m_g1[:, hs], op=ALU.mult), reads=[pd[half], m_g1], writes=[tp])
        k.op("pool", lambda e, hr=hr, tp=tp: e.tensor_tensor(out=hr[:], in0=tp[:], in1=hr[:], op=ALU.add),
             reads=[tp, hr], writes=[hr])
        k.dma("sp", hpre_d[rows, :], hr[:], reads=[hr], writes=[hpre_d])
        k.op("act", lambda e, hr=hr, tp=tp, s_=s_: e.activation(out=tp[:], in_=hr[:], func=AF.Square,
                                                                 accum_out=s_[:, 0:1]), reads=[hr], writes=[tp, s_])
        k.op("act", lambda e, s_=s_: e.activation(out=s_[:, 1:2], in_=s_[:, 0:1], func=AF.Sqrt, bias=epsb[:],
                                                   scale=1.0 / 1024.0), reads=[s_, epsb], writes=[s_])
        k.op("dve", lambda e, s_=s_: e.reciprocal(out=s_[:, 2:3], in_=s_[:, 1:2]), reads=[s_], writes=[s_])
        k.op("dve", lambda e, hr=hr, tp=tp, s_=s_: e.scalar_tensor_tensor(
            out=tp[:], in0=hr[:], scalar=s_[:, 2:3], in1=m_gm[:], op0=ALU.mult, op1=ALU.mult),
            reads=[hr, s_, m_gm], writes=[tp])
        k.op("pool", lambda e, tp=tp, ut=ut: e.tensor_tensor(out=ut[:], in0=tp[:], in1=m_sh2[:], op=ALU.add),
             reads=[tp, m_sh2], writes=[ut])
        for kc in range(8):
            tr(k, pm[0], pmb[0][:, kc * 128:(kc + 1) * 128], ut, ut[:, kc * 128:(kc + 1) * 128], idb, idb[:])
        k.op("act", lambda e, t=t: e.activation(out=uT[:, :, t * 128:(t + 1) * 128],
                                                in_=pmb[0].rearrange("p (a b) -> p a b", a=8), func=AF.Copy),
             reads=[pm[0]], writes=[uT])
        for kc in range(8):
            mm(k, pm[1], pm[1][:, 0:32], uT, uT[:, kc, t * 128:(t + 1) * 128], rwb, rwb[:, kc, :], kc == 0, kc == 7)
        k.op("dve", lambda e, l_=l_: e.tensor_tensor(out=l_[:, 0, :], in0=pm[1][:, 0:32], in1=rb_bc[:], op=ALU.add),
             reads=[pm[1], rb_bc], writes=[l_])
        k.op("dve", lambda e, l_=l_, s_=s_: e.max(out=s_[:, 8:16], in_=l_[:, 0, :]), reads=[l_], writes=[s_])
        k.op("dve", lambda e, l_=l_, s_=s_: e.tensor_scalar(out=l_[:, 1, :], in0=l_[:, 0, :], scalar1=s_[:, 11:12],
                                                            scalar2=None, op0=ALU.is_ge), reads=[l_, s_], writes=[l_])
        k.op("dve", lambda e, s_=s_: e.tensor_scalar(out=s_[:, 16:17], in0=s_[:, 8:9], scalar1=-1.0, scalar2=None,
                                                     op0=ALU.mult), reads=[s_], writes=[s_])
        k.op("act", lambda e, l_=l_, s_=s_: e.activation(out=l_[:, 2, :], in_=l_[:, 0, :], func=AF.Exp,
                                                          bias=s_[:, 16:17], scale=1.0), reads=[l_, s_], writes=[l_])
        k.op("dve", lambda e, l_=l_, s_=s_: e.scalar_tensor_tensor(
            out=l_[:, 3, :], in0=l_[:, 2, :], scalar=1.0, in1=l_[:, 1, :], op0=ALU.mult, op1=ALU.mult,
            accum_out=s_[:, 17:18]), reads=[l_], writes=[l_, s_])
        k.op("dve", lambda e, s_=s_: e.reciprocal(out=s_[:, 18:19], in_=s_[:, 17:18]), reads=[s_], writes=[s_])
        k.op("dve", lambda e, l_=l_, s_=s_, t=t: e.tensor_scalar(out=gw[:, t, :], in0=l_[:, 3, :], scalar1=s_[:, 18:19],
                                                                 scalar2=None, op0=ALU.mult), reads=[l_, s_], writes=[gw])
        tr(k, pm[1], pm[1][0:32, 128:256], gw, gw[:, t, :], idf, idf[:])
        k.op("act", lambda e, t=t: e.activation(out=gwT[:, t * 128:(t + 1) * 128], in_=pm[1][0:32, 128:256],
                                                func=AF.Copy), reads=[pm[1]], writes=[gwT])

    toks = []
    if dbg is not None:
        toks.append(k.dma("sp", dbg["uT"][:], uT[:], reads=[uT], writes=[dbg["uT"]]))
        toks.append(k.dma("sp", dbg["gw"][:], gw[:], reads=[gw], writes=[dbg["gw"]]))
        for i, mv in enumerate([m_g1, m_sh2, m_gm, m_g2]):
            toks.append(k.dma("sp", dbg["mods"][i], mv[:], reads=[mv], writes=[dbg["mods"]]))
    k.release(m1)
    acc = [k.sb("acc%d%s" % (t, tag), [128, 1024], F32) for t in range(NT)]
    m2 = k.mark()
    actT = [k.sb("actT%d%s" % (j, tag), [128, NTOK], BF16) for j in range(8)]
    NR = 4
    wring = [k.sb("wgur%d%s" % (i, tag), [128, 8, 2, 128], BF16) for i in range(NR)]
    ND = 3
    dring = [k.sb("wdr%d%s" % (i, tag), [128, 8, 512], BF16) for i in range(ND)]
    g_sb = k.sb("g_sb" + tag, [128, 512], F32)
    s_sb = k.sb("s_sb" + tag, [128, 512], F32)
    t1 = k.sb("t1" + tag, [128, 512], F32)
    t2 = k.sb("t2" + tag, [128, 512], F32)
    m_sb = k.sb("m_sb" + tag, [128, 512], F32)

    for t in range(NT):
        for half in range(2):
            hs = slice(half * 512, (half + 1) * 512)
            pb = pd[(2 * t + half) % 2]
            mm(k, pb, pb[:], gwT, gwT[:, t * 128:(t + 1) * 128], bd_sb, bd_sb[:, hs], True, True)
            k.op("act", lambda e, t=t, hs=hs, pb=pb: e.activation(out=acc[t][:, hs], in_=pb[:], func=AF.Copy),
                 reads=[pb], writes=[acc[t]])

    units = [(e, j) for e in range(n_exp) for j in range(8)]
    dunits = [(e, h) for e in range(n_exp) for h in range(2)]

    def load_unit(i):
        if i < len(units):
            e, j = units[i]
            k.dma("pool", wring[i % NR][:], d["wgu"][e, j], reads=[d["wgu"]], writes=[wring[i % NR]])

    def load_dunit(i):
        if i < len(dunits):
            e, h = dunits[i]
            k.dma("pool", dring[i % ND][:], d["wd"][e, :, :, h * 512:(h + 1) * 512], reads=[d["wd"]],
                  writes=[dring[i % ND]])

    for i in range(NR - 1):
        load_unit(i)
    for i in range(ND - 1):
        load_dunit(i)
    cnt = 0
    for e_ in range(n_exp):
        for j in range(8):
            ui = e_ * 8 + j
            load_unit(ui + NR - 1)
            w = wring[ui % NR]
            for T in range(4):
                x = cnt % 2
                cnt += 1
                ts_ = slice(T * 512, (T + 1) * 512)
                for kc in range(8):
                    mm(k, pg[x], pg[x][:], w, w[:, kc, 0, :], uT, uT[:, kc, ts_], kc == 0, kc == 7)
                for kc in range(8):
                    mm(k, pu[x], pu[x][:], w, w[:, kc, 1, :], uT, uT[:, kc, ts_], kc == 0, kc == 7)
                k.op("dve", lambda e, x=x, e_=e_, j=j: e.tensor_scalar(
                    out=g_sb[:], in0=pg[x][:], scalar1=bgu[:, e_, 0, j:j + 1], scalar2=SWIGLU_LIMIT,
                    op0=ALU.add, op1=ALU.min), reads=[pg[x], bgu], writes=[g_sb])
                k.op("act", lambda e: e.activation(out=s_sb[:], in_=g_sb[:], func=AF.Sigmoid, scale=SWIGLU_ALPHA),
                     reads=[g_sb], writes=[s_sb])
                k.op("act", lambda e, x=x, e_=e_, j=j: e.activation(
                    out=t1[:], in_=pu[x][:], func=AF.Identity, bias=bgu1[:, e_, j:j + 1], scale=1.0),
                    reads=[pu[x], bgu1], writes=[t1])
                k.op("pool", lambda e: e.tensor_scalar(out=t2[:], in0=t1[:], scalar1=SWIGLU_LIMIT + 1.0,
                                                       scalar2=1.0 - SWIGLU_LIMIT, op0=ALU.min, op1=ALU.max),
                     reads=[t1], writes=[t2])
                k.op("dve", lambda e: e.tensor_tensor(out=m_sb[:], in0=g_sb[:], in1=s_sb[:], op=ALU.mult),
                     reads=[g_sb, s_sb], writes=[m_sb])
                k.op("pool", lambda e, j=j, ts_=ts_: e.tensor_tensor(out=actT[j][:, ts_], in0=m_sb[:], in1=t2[:],
                                                                     op=ALU.mult),
                     reads=[m_sb, t2], writes=[actT[j]])
        for half in range(2):
            di = e_ * 2 + half
            load_dunit(di + ND - 1)
            wdv = dring[di % ND]
            hs = slice(half * 512, (half + 1) * 512)
            for t in range(NT):
                pb = pd[t % 2]
                for fc in range(8):
                    mm(k, pb, pb[:], actT[fc], actT[fc][:, t * 128:(t + 1) * 128], wdv, wdv[:, fc, :], fc == 0, fc == 7)
                k.op("dve", lambda e, t=t, hs=hs, pb=pb, e_=e_: e.scalar_tensor_tensor(
                    out=acc[t][:, hs], in0=pb[:], scalar=gw[:, t, e_:e_ + 1], in1=acc[t][:, hs],
                    op0=ALU.mult, op1=ALU.add), reads=[pb, gw, acc[t]], writes=[acc[t]])

    if dbg is not None:
        toks.append(k.dma("sp", dbg["acc0"][:], acc[0][:], reads=[acc[0]], writes=[dbg["acc0"]]))
        toks.append(k.dma("sp", dbg["actT0"][:], actT[0][:], reads=[actT[0]], writes=[dbg["actT0"]]))
    k.release(m2)
    hp = [k.sb("hp%d%s" % (i, tag), [128, 1024], F32) for i in range(2)]
    for t in range(NT):
        rows = slice(t * 128, (t + 1) * 128)
        h_ = hp[t % 2]
        k.dma("sp", h_[:], hpre_d[rows, :], reads=[hpre_d], writes=[h_])
        k.op("dve", lambda e, t=t: e.tensor_tensor(out=acc[t][:], in0=acc[t][:], in1=m_g2[:], op=ALU.mult),
             reads=[acc[t], m_g2], writes=[acc[t]])
        k.op("pool", lambda e, t=t, h_=h_: e.tensor_tensor(out=h_[:], in0=acc[t][:], in1=h_[:], op=ALU.add),
             reads=[acc[t], h_], writes=[h_])
        toks.append(k.dma("sp", out_d[out_row0 + t * 128:out_row0 + (t + 1) * 128, :], h_[:], reads=[h_], writes=[out_d]))
    k.release(m0)
    return toks


def make_psum(k):
    psum = [k.ps("ps%d" % i, [128, 512], F32) for i in range(8)]
    pbf = [p[:].bitcast(BF16) for p in psum]
    return psum, pbf


def build_bd(F, n_exp=32, debug=False):
    nc = bass.Bass("TRN2", target_bir_lowering=False)
    with contextlib.ExitStack() as stack:
        k = KB(nc, stack)
        psum, pbf = make_psum(k)
        d = bd_dram(k, F, "", n_exp)
        out_d = k.dram("out", [NTOK, 1024], F32, "ExternalOutput")
        hpre_d = k.dram("hpre_scr", [NTOK, 1024], F32, "ExternalOutput" if debug else "Internal")
        ident = make_ident(k, "id", BF16)
        dbg = None
        if debug:
            dbg = {"uT": k.dram("dbg_uT", [128, 8, NTOK], BF16, "ExternalOutput"),
                   "gw": k.dram("dbg_gw", [128, NT, 32], F32, "ExternalOutput"),
                   "mods": k.dram("dbg_mods", [4, 128, 1024], F32, "ExternalOutput"),
                   "acc0": k.dram("dbg_acc0", [128, 1024], F32, "ExternalOutput"),
                   "actT0": k.dram("dbg_actT0", [128, NTOK], BF16, "ExternalOutput")}
        toks = emit_bd(k, F, d, psum, pbf, out_d, hpre_d, ident, "", n_exp, dbg)
        k.emit(final_waits=toks)
        print("BD arena peak words", k.apeak, "instr", {e: len(v) for e, v in k.prog.items()})
    return nc


SEQ = 4096
NEG = -30000.0


def c_dram(k, tag="", with_io=True):
    d = {}
    if with_io:
        d["hin"] = k.dram("c_hin" + tag, [SEQ, 1024], F32, "ExternalInput")
    d["ccol"] = k.dram("c_ccol" + tag, [128, 8], F32, "ExternalInput")
    d["adaw"] = k.dram("c_adaw" + tag, [2, 128, 8, 1024], F32, "ExternalInput")
    d["adab"] = k.dram("c_adab" + tag, [2, 1024], F32, "ExternalInput")
    d["gain"] = k.dram("c_gain" + tag, [1024], F32, "ExternalInput")
    d["wz"] = k.dram("c_wz" + tag, [128, 8, 1024], F32, "ExternalInput")
    d["wx"] = k.dram("c_wx" + tag, [128, 8, 1024], F32, "ExternalInput")
    d["wbc"] = k.dram("c_wbc" + tag, [128, 8, 512], F32, "ExternalInput")
    d["wdt"] = k.dram("c_wdt" + tag, [128, 8, 16], F32, "ExternalInput")
    d["convw"] = k.dram("c_convw" + tag, [128, 12, 4], F32, "ExternalInput")
    d["convb"] = k.dram("c_convb" + tag, [128, 12], F32, "ExternalInput")
    d["dtb"] = k.dram("c_dtb" + tag, [16], F32, "ExternalInput")
    d["alog"] = k.dram("c_alog" + tag, [16], F32, "ExternalInput")
    d["dsk"] = k.dram("c_dsk" + tag, [16], F32, "ExternalInput")
    d["ng"] = k.dram("c_ng" + tag, [1024], F32, "ExternalInput")
    return d


def c_host_inputs(b, hh, P, tag=""):
    m = {}
    lay = lambda w: np.ascontiguousarray(w.reshape(8, 128, -1).transpose(1, 0, 2))
    m["c_ccol" + tag] = np.ascontiguousarray(P["c"][b].reshape(8, 128).T)
    aw = P["ada_w"][1]
    m["c_adaw" + tag] = np.ascontiguousarray(np.stack([lay(aw[:, v * 1024:(v + 1) * 1024]) for v in (0, 1)]))
    m["c_adab" + tag] = np.ascontiguousarray(np.stack([P["ada_b"][1][v * 1024:(v + 1) * 1024] for v in (0, 1)]))
    m["c_gain" + tag] = np.ascontiguousarray(P["norm_mix"][1])
    w = P["ssm_w_in"][0]
    m["c_wz" + tag] = lay(w[:, hh * 1024:(hh + 1) * 1024])
    xo = 2048
    m["c_wx" + tag] = lay(w[:, xo + hh * 1024:xo + (hh + 1) * 1024])
    bo, co = xo + 2048, xo + 2048 + 512
    g0 = 2 * hh
    bc_cols = np.concatenate([np.arange(bo + g0 * 128, bo + (g0 + 2) * 128), np.arange(co + g0 * 128, co + (g0 + 2) * 128)])
    m["c_wbc" + tag] = lay(w[:, bc_cols])
    dto = xo + 3072
    m["c_wdt" + tag] = lay(w[:, dto + 16 * hh:dto + 16 * (hh + 1)])
    ch = np.concatenate([np.arange(hh * 1024, (hh + 1) * 1024), bc_cols - xo])
    cw = P["ssm_conv_w"][0][:, ch]
    m["c_convw" + tag] = np.ascontiguousarray(cw.reshape(4, 12, 128).transpose(2, 1, 0))
    m["c_convb" + tag] = np.ascontiguousarray(P["ssm_conv_b"][0][ch].reshape(12, 128).T)
    hs = slice(16 * hh, 16 * (hh + 1))
    m["c_dtb" + tag] = np.ascontiguousarray(P["ssm_dt_bias"][0][hs])
    m["c_alog" + tag] = np.ascontiguousarray(P["ssm_a_log"][0][hs])
    m["c_dsk" + tag] = np.ascontiguousarray(P["ssm_d"][0][hs])
    m["c_ng" + tag] = np.ascontiguousarray(P["ssm_norm"][0][hh * 1024:(hh + 1) * 1024])
    return m


def emit_c(k, d, psum, pbf, yn_d, ident, tag="", n_tiles=8, dbg=None):
    idf, idb = ident
    m0 = k.mark()
    bcol = lambda ap, n: ap.unsqueeze(2).to_broadcast([128, ap.shape[1], n])
    tri = k.sb("c_tri", [128, 128], F32)
    ones = k.sb("c_ones", [128, 128], F32)
    negm = k.sb("c_negm", [128, 128], BF16)
    zer = k.sb("c_zer", [128, 128], F32)
    epsb = k.sb("c_epsb", [128, 1], F32)
    k.op("pool", lambda e: e.memset(ones[:], 1.0), writes=[ones])
    k.op("pool", lambda e: e.memset(zer[:], 0.0), writes=[zer])
    k.op("pool", lambda e: e.memset(epsb[:], EPS), writes=[epsb])
    k.op("pool", lambda e: e.affine_select(out=tri[:], in_=ones[:], pattern=[[1, 128]], compare_op=ALU.is_ge, fill=0.0,
                                           base=0, channel_multiplier=-1), reads=[ones], writes=[tri])
    k.op("pool", lambda e: e.affine_select(out=negm[:], in_=zer[:], pattern=[[1, 128]], compare_op=ALU.is_ge, fill=NEG,
                                           base=0, channel_multiplier=-1), reads=[zer], writes=[negm])
    convw = k.sb("c_convw", [128, 12, 4], F32)
    convb = k.sb("c_convb", [128, 12], F32)
    dtb = k.sb("c_dtb", [128, 16], F32)
    a_bc = k.sb("c_abc", [128, 16], F32)
    dsk = k.sb("c_dsk", [128, 16], F32)
    ng = k.sb("c_ng", [128, 1024], F32)
    m_sh = k.sb("c_msh", [128, 1024], F32)
    m_gm = k.sb("c_mgm", [128, 1024], F32)
    k.dma("sp", convw[:], d["convw"][:], reads=[d["convw"]], writes=[convw])
    k.dma("sp", convb[:], d["convb"][:], reads=[d["convb"]], writes=[convb])
    k.dma("sp", dtb[:], d["dtb"][:].partition_broadcast(128), reads=[d["dtb"]], writes=[dtb])
    k.dma("sp", a_bc[:], d["alog"][:].partition_broadcast(128), reads=[d["alog"]], writes=[a_bc])
    k.dma("sp", dsk[:], d["dsk"][:].partition_broadcast(128), reads=[d["dsk"]], writes=[dsk])
    k.dma("sp", ng[:], d["ng"][:].partition_broadcast(128), reads=[d["ng"]], writes=[ng])
    k.op("act", lambda e: e.activation(out=a_bc[:], in_=a_bc[:], func=AF.Exp), reads=[a_bc], writes=[a_bc])
    k.op("dve", lambda e: e.tensor_scalar(out=a_bc[:], in0=a_bc[:], scalar1=-1.0, scalar2=None, op0=ALU.mult),
         reads=[a_bc], writes=[a_bc])
    wz = k.sb("c_wz", [128, 8, 1024], BF16)
    wx = k.sb("c_wx", [128, 8, 1024], BF16)
    wbc = k.sb("c_wbc", [128, 8, 512], BF16)
    wdt = k.sb("c_wdt", [128, 8, 16], BF16)
    for wt, nm in ((wz, "wz"), (wx, "wx"), (wbc, "wbc"), (wdt, "wdt")):
        k.dma("pool", wt[:], d[nm][:], reads=[d[nm]], writes=[wt])
    m1 = k.mark()
    stage = k.sb("c_adast", [128, 8, 1024], F32)
    gain_bc = k.sb("c_gainbc", [128, 1024], F32)
    k.dma("sp", gain_bc[:], d["gain"][:].partition_broadcast(128), reads=[d["gain"]], writes=[gain_bc])
    adaln(k, d["ccol"], d["adaw"], d["adab"], 2, [m_sh, m_gm], [stage, stage], psum[6:8], "c" + tag)
    k.op("dve", lambda e: e.scalar_tensor_tensor(out=m_gm[:], in0=m_gm[:], scalar=1.0, in1=gain_bc[:],
                                                 op0=ALU.add, op1=ALU.mult), reads=[m_gm, gain_bc], writes=[m_gm])
    k.release(m1)

    hr = [k.sb("c_hr%d" % i, [128, 1024], F32) for i in range(2)]
    tp = [k.sb("c_tp%d" % i, [128, 1024], F32) for i in range(2)]
    ut = [k.sb("c_ut%d" % i, [128, 1024], BF16) for i in range(2)]
    st = [k.sb("c_st%d" % i, [128, 8], F32) for i in range(2)]
    uT = [k.sb("c_uT%d" % i, [128, 8, 512], BF16) for i in range(2)]
    xpre = k.sb("c_xpre", [128, 12, 515], F32)
    ctmp = [k.sb("c_ctmp%d" % i, [128, 512], F32) for i in range(2)]
    xsa = k.sb("c_xsa", [128, 8, 512], F32)
    bcT = k.sb("c_bcT", [128, 4, 512], BF16)
    zs = k.sb("c_zs", [128, 4, 1024], BF16)
    dt_t = k.sb("c_dt", [128, 4, 16], F32)
    sm = k.sb("c_sm", [128, 4, 16], F32)
    sm2 = k.sb("c_sm2", [128, 4, 16], F32)
    xs_tok = [k.sb("c_xst%d" % i, [128, 16, 64], F32) for i in range(2)]
    xd = k.sb("c_xd", [128, 16, 64], BF16)
    xdd = k.sb("c_xdd", [128, 16, 64], BF16)
    btok = k.sb("c_btok", [128, 2, 128], BF16)
    dtA = k.sb("c_dtA", [128, 16], F32)
    dtAb = k.sb("c_dtAb", [128, 16, 128], F32)
    cs = k.sb("c_cs", [128, 6, 16], F32)
    dec = [k.sb("c_dec%d" % i, [128, 4, 128], F32) for i in range(2)]
    MT = k.sb("c_MT", [128, 16, 128], BF16)
    cbT = k.sb("c_cbT", [128, 2, 128], F32)
    y1 = k.sb("c_y1", [128, 16, 64], F32)
    y2 = k.sb("c_y2", [128, 16, 64], F32)
    ynt = [k.sb("c_ynt%d" % i, [128, 1024], BF16) for i in range(2)]
    S = k.sb("c_S", [128, 2, 512], F32)
    Sb = k.sb("c_Sb", [128, 2, 512], BF16)
    k.op("pool", lambda e: e.memset(S[:], 0.0), writes=[S])
    k.op("pool", lambda e: e.memset(Sb[:], 0.0), writes=[Sb])
    k.op("pool", lambda e: e.memset(xpre[:, :, 0:3], 0.0), writes=[xpre])

    toks = []
    for T in range(n_tiles):
        uTt = uT[T % 2]
        for s in range(4):
            i2 = (T * 4 + s) % 2
            hr_, tp_, ut_, st_ = hr[i2], tp[i2], ut[i2], st[i2]
            rows = slice(T * 512 + s * 128, T * 512 + (s + 1) * 128)
            k.dma("sp", hr_[:], d["hin"][rows, :], reads=[d["hin"]], writes=[hr_])
            k.op("act", lambda e, hr_=hr_, tp_=tp_, st_=st_: e.activation(out=tp_[:], in_=hr_[:], func=AF.Square,
                                                                         accum_out=st_[:, 0:1]), reads=[hr_], writes=[tp_, st_])
            k.op("act", lambda e, st_=st_: e.activation(out=st_[:, 1:2], in_=st_[:, 0:1], func=AF.Sqrt, bias=epsb[:],
                                                        scale=1.0 / 1024.0), reads=[st_, epsb], writes=[st_])
            k.op("dve", lambda e, st_=st_: e.reciprocal(out=st_[:, 2:3], in_=st_[:, 1:2]), reads=[st_], writes=[st_])
            k.op("dve", lambda e, hr_=hr_, tp_=tp_, st_=st_: e.scalar_tensor_tensor(
                out=tp_[:], in0=hr_[:], scalar=st_[:, 2:3], in1=m_gm[:], op0=ALU.mult, op1=ALU.mult),
                reads=[hr_, st_, m_gm], writes=[tp_])
            k.op("pool", lambda e, tp_=tp_, ut_=ut_: e.tensor_tensor(out=ut_[:], in0=tp_[:], in1=m_sh[:], op=ALU.add),
                 reads=[tp_, m_sh], writes=[ut_])
            pb, pbv = psum[6 + s % 2], pbf[6 + s % 2]
            for kc in range(8):
                tr(k, pb, pbv[:, kc * 128:(kc + 1) * 128], ut_, ut_[:, kc * 128:(kc + 1) * 128], idb, idb[:])
            k.op("act", lambda e, s=s, pbv=pbv, uTt=uTt: e.activation(
                out=uTt[:, :, s * 128:(s + 1) * 128], in_=pbv.rearrange("p (a b) -> p a b", a=8), func=AF.Copy),
                reads=[pb], writes=[uTt])
        for s in range(4):
            for half in range(2):
                pb = psum[(2 * s + half) % 2]
                for kc in range(8):
                    mm(k, pb, pb[:], uTt, uTt[:, kc, s * 128:(s + 1) * 128], wz, wz[:, kc, half * 512:(half + 1) * 512],
                       kc == 0, kc == 7)
                k.op("act", lambda e, s=s, half=half, pb=pb: e.activation(
                    out=zs[:, s, half * 512:(half + 1) * 512], in_=pb[:], func=AF.Silu), reads=[pb], writes=[zs])
        for s in range(4):
            pb = psum[2 + s % 2]
            for kc in range(8):
                mm(k, pb, pb[:, 0:16], uTt, uTt[:, kc, s * 128:(s + 1) * 128], wdt, wdt[:, kc, :], kc == 0, kc == 7)
            k.op("dve", lambda e, s=s, pb=pb: e.tensor_tensor(out=sm[:, s, :], in0=pb[:, 0:16], in1=dtb[:], op=ALU.add),
                 reads=[pb, dtb], writes=[sm])
        k.op("act", lambda e: e.activation(out=sm2[:], in_=sm[:], func=AF.Abs), reads=[sm], writes=[sm2])
        k.op("act", lambda e: e.activation(out=sm2[:], in_=sm2[:], func=AF.Exp, scale=-1.0), reads=[sm2], writes=[sm2])
        k.op("act", lambda e: e.activation(out=sm2[:], in_=sm2[:], func=AF.Ln, bias=1.0, scale=1.0), reads=[sm2], writes=[sm2])
        k.op("dve", lambda e: e.scalar_tensor_tensor(out=dt_t[:], in0=sm[:], scalar=0.0, in1=sm2[:], op0=ALU.max,
                                                     op1=ALU.add), reads=[sm, sm2], writes=[dt_t])
        for c in range(12):
            pb = psum[4 + c % 2]
            wsrc, col = (wx, c * 128) if c < 8 else (wbc, (c - 8) * 128)
            for kc in range(8):
                mm(k, pb, pb[:], wsrc, wsrc[:, kc, col:col + 128], uTt, uTt[:, kc, :], kc == 0, kc == 7)
            k.op("act", lambda e, c=c, pb=pb: e.activation(out=xpre[:, c, 3:515], in_=pb[:], func=AF.Copy),
                 reads=[pb], writes=[xpre])
        for c in range(12):
            ct = ctmp[c % 2]
            eng = "dve" if c % 2 == 0 else "pool"
            k.op(eng, lambda e, c=c, ct=ct: e.tensor_scalar(out=ct[:], in0=xpre[:, c, 0:512], scalar1=convw[:, c, 0:1],
                                                            scalar2=None, op0=ALU.mult), reads=[xpre, convw], writes=[ct])
            for tap in range(1, 4):
                k.op("dve", lambda e, c=c, ct=ct, tap=tap: e.scalar_tensor_tensor(
                    out=ct[:], in0=xpre[:, c, tap:tap + 512], scalar=convw[:, c, tap:tap + 1], in1=ct[:],
                    op0=ALU.mult, op1=ALU.add), reads=[xpre, convw, ct], writes=[ct])
            if c < 8:
                k.op("act", lambda e, c=c, ct=ct: e.activation(out=xsa[:, c, :], in_=ct[:], func=AF.Silu,
                                                               bias=convb[:, c:c + 1], scale=1.0),
                     reads=[ct, convb], writes=[xsa])
            else:
                k.op("act", lambda e, c=c, ct=ct: e.activation(out=bcT[:, c - 8, :], in_=ct[:], func=AF.Silu,
                                                               bias=convb[:, c:c + 1], scale=1.0),
                     reads=[ct, convb], writes=[bcT])
        k.op("pool", lambda e: e.tensor_copy(out=xpre[:, :, 0:3], in_=xpre[:, :, 512:515]), reads=[xpre], writes=[xpre])

        for s in range(4):
            ci = T * 4 + s
            cols = slice(s * 128, (s + 1) * 128)
            xst = xs_tok[ci % 2]
            for hf in range(2):
                pb = psum[hf]
                for c4 in range(4):
                    c = hf * 4 + c4
                    tr(k, pb, pb[:, c4 * 128:(c4 + 1) * 128], xsa, xsa[:, c, cols], idf, idf[:])
                k.op("act", lambda e, hf=hf, pb=pb, xst=xst: e.activation(
                    out=xst[:, hf * 8:(hf + 1) * 8, :], in_=pb[:].rearrange("p (a b) -> p a b", a=8), func=AF.Copy),
                    reads=[pb], writes=[xst])
            pb, pbv = psum[2], pbf[2]
            for g in range(2):
                tr(k, pb, pbv[:, g * 128:(g + 1) * 128], bcT, bcT[:, g, cols], idb, idb[:])
            k.op("act", lambda e, pbv=pbv: e.activation(out=btok[:], in_=pbv[:, 0:256].rearrange("p (a b) -> p a b", a=2),
                                                        func=AF.Copy), reads=[pb], writes=[btok])
            k.op("dve", lambda e, s=s: e.tensor_tensor(out=dtA[:], in0=dt_t[:, s, :], in1=a_bc[:], op=ALU.mult),
                 reads=[dt_t, a_bc], writes=[dtA])
            k.op("pool", lambda e: e.tensor_copy(out=dtAb[:], in_=bcol(dtA[:], 128)), reads=[dtA], writes=[dtAb])
            pb = psum[3]
            mm(k, pb, pb[:, 0:16], tri, tri[:], dtA, dtA[:], True, True)
            mm(k, pb, pb[:, 16:32], ones, ones[:], dtA, dtA[:], True, True)
            k.op("act", lambda e, pb=pb: e.activation(out=cs[:, 0, :], in_=pb[:, 0:16], func=AF.Copy), reads=[pb], writes=[cs])
            k.op("dve", lambda e, pb=pb: e.tensor_scalar(out=cs[:, 1, :], in0=pb[:, 0:16], scalar1=-1.0, scalar2=None,
                                                         op0=ALU.mult), reads=[pb], writes=[cs])
            k.op("act", lambda e, pb=pb: e.activation(out=cs[:, 2, :], in_=pb[:, 0:16], func=AF.Exp), reads=[pb], writes=[cs])
            k.op("act", lambda e, pb=pb: e.activation(out=cs[:, 5, :], in_=pb[:, 16:32], func=AF.Exp), reads=[pb], writes=[cs])
            k.op("dve", lambda e, pb=pb: e.tensor_tensor(out=cs[:, 3, :], in0=pb[:, 16:32], in1=cs[:, 0, :], op=ALU.subtract),
                 reads=[pb, cs], writes=[cs])
            k.op("act", lambda e: e.activation(out=cs[:, 3, :], in_=cs[:, 3, :], func=AF.Exp), reads=[cs], writes=[cs])
            k.op("dve", lambda e, s=s: e.tensor_tensor(out=cs[:, 4, :], in0=cs[:, 3, :], in1=dt_t[:, s, :], op=ALU.mult),
                 reads=[cs, dt_t], writes=[cs])
            k.op("dve", lambda e, s=s, xst=xst: e.tensor_tensor(out=xd[:], in0=xst[:], in1=bcol(dt_t[:, s, :], 64), op=ALU.mult),
                 reads=[xst, dt_t], writes=[xd])
            k.op("pool", lambda e, xst=xst: e.tensor_tensor(out=xdd[:], in0=xst[:], in1=bcol(cs[:, 4, :], 64), op=ALU.mult),
                 reads=[xst, cs], writes=[xdd])
            pb = psum[2]
            for g in range(2):
                mm(k, pb, pb[:, 256 + g * 128:256 + (g + 1) * 128], bcT, bcT[:, g, cols], bcT, bcT[:, 2 + g, cols], True, True)
            k.op("act", lambda e, pb=pb: e.activation(out=cbT[:], in_=pb[:, 256:512].rearrange("p (a b) -> p a b", a=2),
                                                      func=AF.Copy), reads=[pb], writes=[cbT])
            for q in range(4):
                pb = psum[4 + q % 2]
                dq = dec[q % 2]
                for hh_ in range(4):
                    h = q * 4 + hh_
                    o = pb[:, hh_ * 128:(hh_ + 1) * 128]
                    mm(k, pb, o, dtAb, dtAb[:, h, :], tri, tri[:], True, False)
                    mm(k, pb, o, idb, idb[:], negm, negm[:], False, True)
                for hh_ in range(4):
                    h = q * 4 + hh_
                    k.op("act", lambda e, pb=pb, dq=dq, hh_=hh_, h=h: e.activation(
                        out=dq[:, hh_, :], in_=pb[:, hh_ * 128:(hh_ + 1) * 128], func=AF.Exp, bias=cs[:, 1, h:h + 1], scale=1.0),
                        reads=[pb, cs], writes=[dq])
                g = q // 2
                eng = "dve" if q % 2 == 0 else "pool"
                k.op(eng, lambda e, q=q, dq=dq, g=g: e.tensor_tensor(
                    out=MT[:, q * 4:(q + 1) * 4, :], in0=dq[:], in1=cbT[:, g:g + 1, :].to_broadcast([128, 4, 128]), op=ALU.mult),
                    reads=[dq, cbT], writes=[MT])
            for h in range(16):
                pb = psum[h // 8]
                mm(k, pb, pb[:, (h % 8) * 64:(h % 8 + 1) * 64], MT, MT[:, h, :], xd, xd[:, h, :], True, True)
            for g in range(2):
                pb = psum[6 + g]
                mm(k, pb, pb[:], bcT, bcT[:, 2 + g, cols], Sb, Sb[:, g, :], True, True)
            for g in range(2):
                hs = slice(g * 8, (g + 1) * 8)
                po, pdg = psum[6 + g], psum[g]
                k.op("dve", lambda e, hs=hs, po=po: e.tensor_tensor(
                    out=y1[:, hs, :], in0=po[:].rearrange("p (a b) -> p a b", a=8), in1=bcol(cs[:, 2, hs], 64), op=ALU.mult),
                    reads=[po, cs], writes=[y1])
                k.op("pool", lambda e, hs=hs, xst=xst: e.tensor_tensor(
                    out=y2[:, hs, :], in0=xst[:, hs, :], in1=bcol(dsk[:, hs], 64), op=ALU.mult), reads=[xst, dsk], writes=[y2])
                k.op("dve", lambda e, hs=hs, pdg=pdg: e.tensor_tensor(
                    out=y1[:, hs, :], in0=pdg[:].rearrange("p (a b) -> p a b", a=8), in1=y1[:, hs, :], op=ALU.add),
                    reads=[pdg, y1], writes=[y1])
                k.op("pool", lambda e, hs=hs: e.tensor_tensor(out=y1[:, hs, :], in0=y1[:, hs, :], in1=y2[:, hs, :], op=ALU.add),
                     reads=[y1, y2], writes=[y1])
            if dbg is not None and "yssd" in dbg:
                toks.append(k.dma("sp", dbg["yssd"][ci * 128:(ci + 1) * 128, :], y1[:].rearrange("p a b -> p (a b)"),
                                  reads=[y1], writes=[dbg["yssd"]]))
            y1f = y1[:].rearrange("p a b -> p (a b)")
            y2f = y2[:].rearrange("p a b -> p (a b)")
            k.op("dve", lambda e, s=s, y1f=y1f: e.tensor_tensor(out=y1f, in0=y1f, in1=zs[:, s, :], op=ALU.mult),
                 reads=[y1, zs], writes=[y1])
            st_ = st[ci % 2]
            yo = ynt[ci % 2]
            for g in range(2):
                gs = slice(g * 512, (g + 1) * 512)
                k.op("act", lambda e, gs=gs, g=g, st_=st_, y1f=y1f, y2f=y2f: e.activation(
                    out=y2f[:, gs], in_=y1f[:, gs], func=AF.Square, accum_out=st_[:, 4 + g:5 + g]), reads=[y1], writes=[y2, st_])
            k.op("act", lambda e, st_=st_: e.activation(out=st_[:, 6:8], in_=st_[:, 4:6], func=AF.Sqrt, bias=epsb[:],
                                                        scale=1.0 / 512.0), reads=[st_, epsb], writes=[st_])
            k.op("dve", lambda e, st_=st_: e.reciprocal(out=st_[:, 6:8], in_=st_[:, 6:8]), reads=[st_], writes=[st_])
            for g in range(2):
                gs = slice(g * 512, (g + 1) * 512)
                k.op("dve", lambda e, gs=gs, g=g, st_=st_, yo=yo, y1f=y1f: e.scalar_tensor_tensor(
                    out=yo[:, gs], in0=y1f[:, gs], scalar=st_[:, 6 + g:7 + g], in1=ng[:, gs], op0=ALU.mult, op1=ALU.mult),
                    reads=[y1, st_, ng], writes=[yo])
            toks.append(k.dma("sp", yn_d[ci * 128:(ci + 1) * 128, :], yo[:], reads=[yo], writes=[yn_d]))
            for g in range(2):
                pb = psum[2 + g]
                hs = slice(g * 8, (g + 1) * 8)
                mm(k, pb, pb[:], btok, btok[:, g, :], xdd, xdd[:, hs, :].rearrange("p a b -> p (a b)"), True, True)
                Sg = S[:, g, :].rearrange("p (a b) -> p a b", a=8)
                eng = "dve" if g == 0 else "pool"
                k.op(eng, lambda e, Sg=Sg, hs=hs: e.tensor_tensor(out=Sg, in0=Sg, in1=bcol(cs[:, 5, hs], 64), op=ALU.mult),
                     reads=[S, cs], writes=[S])
                k.op("dve", lambda e, g=g, pb=pb: e.tensor_tensor(out=S[:, g, :], in0=pb[:], in1=S[:, g, :], op=ALU.add),
                     reads=[pb, S], writes=[S])
            k.op("act", lambda e: e.activation(out=Sb[:], in_=S[:], func=AF.Copy), reads=[S], writes=[Sb])
    k.release(m0)
    return toks


def build_c(n_tiles=8, debug=False):
    nc = bass.Bass("TRN2", target_bir_lowering=False)
    with contextlib.ExitStack() as stack:
        k = KB(nc, stack)
        psum, pbf = make_psum(k)
        d = c_dram(k)
        yn_d = k.dram("yn", [SEQ, 1024], BF16, "ExternalOutput")
        ident = make_ident(k, "id", BF16)
        dbg = None
        if debug:
            dbg = {"yssd": k.dram("dbg_yssd", [SEQ, 1024], F32, "ExternalOutput")}
        toks = emit_c(k, d, psum, pbf, yn_d, ident, "", n_tiles, dbg)
        k.emit(final_waits=toks)
        print("C arena peak words", k.apeak, "instr", {e: len(v) for e, v in k.prog.items()})
    return nc


def a_dram(k, tag=""):
    d = {}
    def inp(name, shape, dt=F32):
        d[name] = k.dram("a_" + name + tag, shape, dt, "ExternalInput")
    inp("xin", [SEQ, 1024]); inp("ccol", [128, 8]); inp("adaw", [2, 128, 8, 1024]); inp("adab", [2, 1024])
    inp("gain", [1024]); inp("wsb", [128, 8, 768]); inp("wnq", [128, 8, 256]); inp("wkv", [128, 8, 384])
    inp("wg", [128, 8, 12]); inp("qn", [64, 1]); inp("kn", [64, 3]); inp("pek", [64, 32, 2]); inp("pev", [64, 32, 2])
    inp("w1k", [64, 32, 128]); inp("w1v", [64, 32, 128]); inp("w2k", [128, 64]); inp("w2v", [128, 64])
    inp("qaug", [4, 4, SEQ], BF16); inp("kaug", [4, SEQ], BF16); inp("caug", [4, 256], BF16)
    inp("cmask", [128, 2, SEQ], BF16); inp("ovl", [128, 2, 64], BF16); inp("E", [64, 32, 128], BF16)
    inp("m1", [32, 128, 64]); inp("a1", [32, 128, 64])
    return d


def a_host_consts(hh):
    m = {}
    tok = np.arange(SEQ)
    slopes = np.array([2.0 ** (-(4 * hh + r + 1)) for r in range(4)], np.float32)
    qa = np.zeros((4, 4, SEQ), np.float32)
    for r in range(4):
        qa[0, r] = -slopes[r] * (tok % 128)
        qa[1, r] = slopes[r]
        qa[2, r] = -slopes[r] * 128.0 * (tok // 128)
        qa[3, r] = slopes[r] * 128.0
    m["a_qaug"] = qa.astype(NPBF)
    ka = np.stack([np.ones(SEQ), tok % 128, np.ones(SEQ), tok // 128]).astype(np.float32)
    m["a_kaug"] = ka.astype(NPBF)
    n = np.arange(256)
    ce = 16 * n + 31
    ca = np.stack([np.ones(256), ce % 128, np.ones(256), ce // 128]).astype(np.float32)
    ca[:, 255] = 0
    m["a_caug"] = ca.astype(NPBF)
    cm = np.where((ce[:, None] <= tok[None, :]) & (n[:, None] < 255), 0.0, NEG).astype(np.float32)
    m["a_cmask"] = np.ascontiguousarray(cm.reshape(2, 128, SEQ).transpose(1, 0, 2)).astype(NPBF)
    cs_ = n * 16
    sl = np.arange(64) * 64
    ov = ((cs_[:, None] < sl[None, :] + 64) & (cs_[:, None] + 32 > sl[None, :]) & (n[:, None] < 255)).astype(np.float32)
    m["a_ovl"] = np.ascontiguousarray(ov.reshape(2, 128, 64).transpose(1, 0, 2)).astype(NPBF)
    E = np.zeros((64, 32, 128), np.float32)
    for s in range(32):
        for kk in range(128):
            E[2 * s + kk // 64, s, kk] = 1.0
    m["a_E"] = E.astype(NPBF)
    j = np.arange(64)
    t = tok.reshape(32, 128)
    cur = t // 64
    valid = j[None, None, :] * 64 <= t[:, :, None]
    forced = (j[None, None, :] == 0) | (j[None, None, :] == cur[:, :, None]) | (j[None, None, :] == cur[:, :, None] - 1)
    m["a_m1"] = (valid & ~forced).astype(np.float32)
    m["a_a1"] = np.where(valid, np.where(forced, 1e6 + j[None, None, :], 0.0), -1e6).astype(np.float32)
    return m


def a_host_inputs(b, hh, P):
    m = {}
    lay = lambda w: np.ascontiguousarray(w.reshape(8, 128, -1).transpose(1, 0, 2))
    m["a_xin"] = np.ascontiguousarray(P["x"][b])
    m["a_ccol"] = np.ascontiguousarray(P["c"][b].reshape(8, 128).T)
    aw = P["ada_w"][0]
    m["a_adaw"] = np.ascontiguousarray(np.stack([lay(aw[:, v * 1024:(v + 1) * 1024]) for v in (0, 1)]))
    m["a_adab"] = np.ascontiguousarray(np.stack([P["ada_b"][0][v * 1024:(v + 1) * 1024] for v in (0, 1)]))
    m["a_gain"] = np.ascontiguousarray(P["norm_mix"][0])
    w = P["attn_w_in"][0]
    hs = slice(hh * 256, (hh + 1) * 256)
    m["a_wsb"] = lay(np.concatenate([w[:, 0:512][:, hs], w[:, 512:1024][:, hs], w[:, 1024:1536][:, hs]], axis=1))
    m["a_wnq"] = lay(w[:, 1536:2048][:, hs])
    kv = [w[:, 2048 + i * 128 + hh * 64: 2048 + i * 128 + (hh + 1) * 64] for i in range(6)]
    m["a_wkv"] = lay(np.concatenate(kv, axis=1))
    m["a_wg"] = lay(w[:, 2816 + 12 * hh: 2816 + 12 * (hh + 1)])
    m["a_qn"] = np.ascontiguousarray(P["nsa_q_norm"][0].reshape(64, 1))
    m["a_kn"] = np.ascontiguousarray(P["nsa_k_norm"][0].T)
    m["a_pek"] = np.ascontiguousarray(np.repeat(P["cmp_pe_k"][0].T[:, :, None], 2, axis=2))
    m["a_pev"] = np.ascontiguousarray(np.repeat(P["cmp_pe_v"][0].T[:, :, None], 2, axis=2))
    m["a_w1k"] = np.ascontiguousarray(P["cmp_w1_k"][0].reshape(32, 64, 128).transpose(1, 0, 2))
    m["a_w1v"] = np.ascontiguousarray(P["cmp_w1_v"][0].reshape(32, 64, 128).transpose(1, 0, 2))
    m["a_w2k"] = np.ascontiguousarray(P["cmp_w2_k"][0])
    m["a_w2v"] = np.ascontiguousarray(P["cmp_w2_v"][0])
    m.update(a_host_consts(hh))
    return m


def emit_a(k, d, psum, pbf, pall, o_d, ident, tag="", n_qt=32, dbg=None):
    idf, idb = ident
    m0 = k.mark()
    toks_dbg = []
    NTL = SEQ // 128
    sbqT = k.sb("a_sbqT", [128, 2, SEQ], BF16)
    sbkT = k.sb("a_sbkT", [128, 2, SEQ], BF16)
    sbv = k.sb("a_sbv", [128, NTL, 256], BF16)
    nqT = k.sb("a_nqT", [68, 4, SEQ], BF16)
    ksT = k.sb("a_ksT", [68, SEQ], BF16)
    kwT = k.sb("a_kwT", [68, SEQ], BF16)
    kcT = k.sb("a_kcT", [64, SEQ], BF16)
    vcT = k.sb("a_vcT", [64, SEQ], BF16)
    vsA = k.sb("a_vsA", [128, NTL, 65], BF16)
    vwA = k.sb("a_vwA", [128, NTL, 65], BF16)
    gts = k.sb("a_gts", [128, NTL, 12], F32)
    kcmpT = k.sb("a_kcmpT", [68, 256], BF16)
    vcmpA = k.sb("a_vcmpA", [128, 2, 129], BF16)
    ones = k.sb("a_ones", [128, 128], F32)
    epsb = k.sb("a_epsb", [128, 1], F32)
    qn8 = k.sb("a_qn8", [64, 1], F32)
    kn = k.sb("a_kn", [64, 3], F32)
    k.op("pool", lambda e: e.memset(ones[:], 1.0), writes=[ones])
    k.op("pool", lambda e: e.memset(epsb[:], EPS), writes=[epsb])
    k.op("pool", lambda e: e.memset(vsA[:], 1.0), writes=[vsA])
    k.op("pool", lambda e: e.memset(vwA[:], 1.0), writes=[vwA])
    k.op("pool", lambda e: e.memset(vcmpA[:], 1.0), writes=[vcmpA])
    k.dma("sp", qn8[:], d["qn"][:], reads=[d["qn"]], writes=[qn8])
    k.dma("sp", kn[:], d["kn"][:], reads=[d["kn"]], writes=[kn])
    k.op("dve", lambda e: e.tensor_scalar(out=qn8[:], in0=qn8[:], scalar1=0.125, scalar2=None, op0=ALU.mult),
         reads=[qn8], writes=[qn8])
    k.dma("sp", nqT[64:68, :, :], d["qaug"][:], reads=[d["qaug"]], writes=[nqT])
    k.dma("sp", ksT[64:68, :], d["kaug"][:], reads=[d["kaug"]], writes=[ksT])
    k.dma("sp", kwT[64:68, :], d["kaug"][:], reads=[d["kaug"]], writes=[kwT])
    k.dma("sp", kcmpT[64:68, :], d["caug"][:], reads=[d["caug"]], writes=[kcmpT])
    k.dma("sp", vcmpA[:, :, 65:129], d["ovl"][:], reads=[d["ovl"]], writes=[vcmpA])

    m1 = k.mark()
    m_sh = k.sb("a_msh", [128, 1024], F32)
    m_gm = k.sb("a_mgm", [128, 1024], F32)
    qf = [k.sb("a_qf%d" % i, [64, 512], F32) for i in range(2)]
    sq = [k.sb("a_sq%d" % i, [64, 512], F32) for i in range(2)]
    rs = [k.sb("a_rs%d" % i, [64, 512], F32) for i in range(2)]
    m2 = k.mark()
    stage = k.sb("a_adast", [128, 8, 1024], F32)
    gain_bc = k.sb("a_gainbc", [128, 1024], F32)
    k.dma("sp", gain_bc[:], d["gain"][:].partition_broadcast(128), reads=[d["gain"]], writes=[gain_bc])
    adaln(k, d["ccol"], d["adaw"], d["adab"], 2, [m_sh, m_gm], [stage, stage], psum[6:8], "a" + tag)
    k.op("dve", lambda e: e.scalar_tensor_tensor(out=m_gm[:], in0=m_gm[:], scalar=1.0, in1=gain_bc[:],
                                                 op0=ALU.add, op1=ALU.mult), reads=[m_gm, gain_bc], writes=[m_gm])
    k.release(m2)
    wsb = k.sb("a_wsb", [128, 8, 768], BF16)
    wnq = k.sb("a_wnq", [128, 8, 256], BF16)
    wkv = k.sb("a_wkv", [128, 8, 384], BF16)
    wg = k.sb("a_wg", [128, 8, 12], BF16)
    for wt, nm in ((wsb, "wsb"), (wnq, "wnq"), (wkv, "wkv"), (wg, "wg")):
        k.dma("pool", wt[:], d[nm][:], reads=[d[nm]], writes=[wt])
    hr = [k.sb("a_hr%d" % i, [128, 1024], F32) for i in range(2)]
    tp = [k.sb("a_tp%d" % i, [128, 1024], F32) for i in range(2)]
    ut = [k.sb("a_ut%d" % i, [128, 1024], BF16) for i in range(2)]
    st = [k.sb("a_st%d" % i, [128, 8], F32) for i in range(2)]
    uT = [k.sb("a_uT%d" % i, [128, 8, 512], BF16) for i in range(2)]
    n64 = [0]

    def norm64(pb, src, gain_ap, gain_buf, out_buf, out_ap, ncols):
        i = n64[0] % 2
        n64[0] += 1
        q_, s_, r_ = qf[i], sq[i], rs[i]
        k.op("act", lambda e: e.activation(out=q_[:, 0:ncols], in_=src, func=AF.Copy), reads=[pb], writes=[q_])
        k.op("act", lambda e: e.activation(out=s_[:, 0:ncols], in_=src, func=AF.Square), reads=[pb], writes=[s_])
        p2 = psum[5]
        mm(k, p2, p2[0:64, 0:ncols], ones, ones[0:64, 0:64], s_, s_[:, 0:ncols], True, True)
        k.op("act", lambda e: e.activation(out=r_[:, 0:ncols], in_=p2[0:64, 0:ncols], func=AF.Sqrt, bias=epsb[0:64, :],
                                           scale=1.0 / 64.0), reads=[p2, epsb], writes=[r_])
        k.op("dve", lambda e: e.reciprocal(out=r_[:, 0:ncols], in_=r_[:, 0:ncols]), reads=[r_], writes=[r_])
        k.op("dve", lambda e: e.scalar_tensor_tensor(out=out_ap, in0=q_[:, 0:ncols], scalar=gain_ap, in1=r_[:, 0:ncols],
                                                     op0=ALU.mult, op1=ALU.mult), reads=[q_, r_, gain_buf], writes=[out_buf])

    n_t1 = (n_qt * 128 + 511) // 512
    for T in range(n_t1):
        uTt = uT[T % 2]
        tcols = slice(T * 512, (T + 1) * 512)
        for s in range(4):
            i2 = (T * 4 + s) % 2
            hr_, tp_, ut_, st_ = hr[i2], tp[i2], ut[i2], st[i2]
            rows = slice(T * 512 + s * 128, T * 512 + (s + 1) * 128)
            k.dma("sp", hr_[:], d["xin"][rows, :], reads=[d["xin"]], writes=[hr_])
            k.op("act", lambda e, hr_=hr_, tp_=tp_, st_=st_: e.activation(out=tp_[:], in_=hr_[:], func=AF.Square,
                                                                         accum_out=st_[:, 0:1]), reads=[hr_], writes=[tp_, st_])
            k.op("act", lambda e, st_=st_: e.activation(out=st_[:, 1:2], in_=st_[:, 0:1], func=AF.Sqrt, bias=epsb[:],
                                                        scale=1.0 / 1024.0), reads=[st_, epsb], writes=[st_])
            k.op("dve", lambda e, st_=st_: e.reciprocal(out=st_[:, 2:3], in_=st_[:, 1:2]), reads=[st_], writes=[st_])
            k.op("dve", lambda e, hr_=hr_, tp_=tp_, st_=st_: e.scalar_tensor_tensor(
                out=tp_[:], in0=hr_[:], scalar=st_[:, 2:3], in1=m_gm[:], op0=ALU.mult, op1=ALU.mult),
                reads=[hr_, st_, m_gm], writes=[tp_])
            k.op("pool", lambda e, tp_=tp_, ut_=ut_: e.tensor_tensor(out=ut_[:], in0=tp_[:], in1=m_sh[:], op=ALU.add),
                 reads=[tp_, m_sh], writes=[ut_])
            pb, pbv = psum[6 + s % 2], pbf[6 + s % 2]
            for kc in range(8):
                tr(k, pb, pbv[:, kc * 128:(kc + 1) * 128], ut_, ut_[:, kc * 128:(kc + 1) * 128], idb, idb[:])
            k.op("act", lambda e, s=s, pbv=pbv, uTt=uTt: e.activation(
                out=uTt[:, :, s * 128:(s + 1) * 128], in_=pbv.rearrange("p (a b) -> p a b", a=8), func=AF.Copy),
                reads=[pb], writes=[uTt])
        for c in range(4):
            pb = psum[c % 2]
            for kc in range(8):
                mm(k, pb, pb[:], wsb, wsb[:, kc, c * 128:(c + 1) * 128], uTt, uTt[:, kc, :], kc == 0, kc == 7)
            if c < 2:
                k.op("act", lambda e, c=c, pb=pb, tcols=tcols: e.activation(out=sbqT[:, c, tcols], in_=pb[:], func=AF.Copy, scale=0.125),
                     reads=[pb], writes=[sbqT])
            else:
                k.op("act", lambda e, c=c, pb=pb, tcols=tcols: e.activation(out=sbkT[:, c - 2, tcols], in_=pb[:], func=AF.Copy),
                     reads=[pb], writes=[sbkT])
        for s in range(4):
            tl = T * 4 + s
            scol = slice(s * 128, (s + 1) * 128)
            pb = psum[2 + s % 2]
            for kc in range(8):
                mm(k, pb, pb[:, 0:256], uTt, uTt[:, kc, scol], wsb, wsb[:, kc, 512:768], kc == 0, kc == 7)
            k.op("dve", lambda e, tl=tl, pb=pb: e.tensor_copy(out=sbv[:, tl, :], in_=pb[:, 0:256]), reads=[pb], writes=[sbv])
            for kc in range(8):
                mm(k, pb, pb[:, 256:320], uTt, uTt[:, kc, scol], wkv, wkv[:, kc, 192:256], kc == 0, kc == 7)
            for kc in range(8):
                mm(k, pb, pb[:, 320:384], uTt, uTt[:, kc, scol], wkv, wkv[:, kc, 320:384], kc == 0, kc == 7)
            for kc in range(8):
                mm(k, pb, pb[:, 384:396], uTt, uTt[:, kc, scol], wg, wg[:, kc, :], kc == 0, kc == 7)
            k.op("dve", lambda e, tl=tl, pb=pb: e.tensor_copy(out=vsA[:, tl, 0:64], in_=pb[:, 256:320]), reads=[pb], writes=[vsA])
            k.op("dve", lambda e, tl=tl, pb=pb: e.tensor_copy(out=vwA[:, tl, 0:64], in_=pb[:, 320:384]), reads=[pb], writes=[vwA])
            k.op("act", lambda e, tl=tl, pb=pb: e.activation(out=gts[:, tl, :], in_=pb[:, 384:396], func=AF.Sigmoid),
                 reads=[pb], writes=[gts])
        def proj64(wt, col):
            pb = psum[4]
            for kc in range(8):
                mm(k, pb, pb[0:64, :], wt, wt[:, kc, col:col + 64], uTt, uTt[:, kc, :], kc == 0, kc == 7)
            return pb
        for h in range(4):
            pb = proj64(wnq, h * 64)
            norm64(pb, pb[0:64, :], qn8[:, 0:1], qn8, nqT, nqT[0:64, h, tcols], 512)
        pb = proj64(wkv, 128)
        norm64(pb, pb[0:64, :], kn[:, 1:2], kn, ksT, ksT[0:64, tcols], 512)
        pb = proj64(wkv, 256)
        norm64(pb, pb[0:64, :], kn[:, 2:3], kn, kwT, kwT[0:64, tcols], 512)
        pb = proj64(wkv, 0)
        k.op("act", lambda e, pb=pb, tcols=tcols: e.activation(out=kcT[:, tcols], in_=pb[0:64, :], func=AF.Copy), reads=[pb], writes=[kcT])
        pb = proj64(wkv, 64)
        k.op("act", lambda e, pb=pb, tcols=tcols: e.activation(out=vcT[:, tcols], in_=pb[0:64, :], func=AF.Copy), reads=[pb], writes=[vcT])

    k.release(m2)
    if True:
        w1 = k.sb("a_w1", [64, 32, 128], BF16)
        w2 = k.sb("a_w2", [128, 64], BF16)
        pe = k.sb("a_pe", [64, 32, 2], BF16)
        hb = k.sb("a_hb", [128, 2], F32)
        hsT = k.sb("a_hsT", [128, 256], BF16)
        for is_k in (True, False):
            sfx = "k" if is_k else "v"
            src = kcT if is_k else vcT
            k.dma("pool", w1[:], d["w1" + sfx][:], reads=[d["w1" + sfx]], writes=[w1])
            k.dma("pool", w2[:], d["w2" + sfx][:], reads=[d["w2" + sfx]], writes=[w2])
            k.dma("pool", pe[:], d["pe" + sfx][:], reads=[d["pe" + sfx]], writes=[pe])
            pb, pb2 = psum[0], psum[1]
            for l in range(32):
                mm(k, pb, pb[:, 0:255], w1, w1[:, l, :], src, src[0:64, l:l + 16 * 254 + 1:16], l == 0, l == 31)
            for l in range(32):
                mm(k, pb2, pb2[:, 0:2], w1, w1[:, l, :], pe, pe[:, l, :], l == 0, l == 31)
            k.op("act", lambda e, pb2=pb2: e.activation(out=hb[:], in_=pb2[:, 0:2], func=AF.Copy), reads=[pb2], writes=[hb])
            k.op("pool", lambda e: e.memset(hsT[:], 0.0), writes=[hsT])
            k.op("act", lambda e, pb=pb: e.activation(out=hsT[:, 0:255], in_=pb[:, 0:255], func=AF.Silu, bias=hb[:, 0:1],
                                                      scale=1.0), reads=[pb, hb], writes=[hsT])
            if is_k:
                pb3 = psum[4]
                mm(k, pb3, pb3[0:64, 0:256], w2, w2[:], hsT, hsT[:], True, True)
                norm64(pb3, pb3[0:64, 0:256], kn[:, 0:1], kn, kcmpT, kcmpT[0:64, :], 256)
            else:
                for c in range(2):
                    pb3 = psum[4]
                    mm(k, pb3, pb3[:, 0:64], hsT, hsT[:, c * 128:(c + 1) * 128], w2, w2[:], True, True)
                    k.op("act", lambda e, c=c, pb3=pb3: e.activation(out=vcmpA[:, c, 0:64], in_=pb3[:, 0:64], func=AF.Copy),
                         reads=[pb3], writes=[vcmpA])
    if dbg is not None:
        for nm, buf in (("sbqT", sbqT), ("sbkT", sbkT), ("sbv", sbv), ("nqT", nqT), ("ksT", ksT), ("kwT", kwT), ("kcT", kcT),
                        ("vsA", vsA), ("gts", gts), ("kcmpT", kcmpT), ("vcmpA", vcmpA)):
            if nm in dbg:
                toks_dbg.append(k.dma("sp", dbg[nm][:], buf[:], reads=[buf], writes=[dbg[nm]]))
    k.release(m1)

    cmask = k.sb("a_cmask", [128, 2, SEQ], BF16)
    Et = k.sb("a_E", [64, 32, 128], BF16)
    k.dma("sp", cmask[:], d["cmask"][:], reads=[d["cmask"]], writes=[cmask])
    k.dma("sp", Et[:], d["E"][:], reads=[d["E"]], writes=[Et])
    zer = k.sb("a_zer", [128, 128], F32)
    cneg = k.sb("a_cneg", [128, 128], BF16)
    sneg = k.sb("a_sneg", [128, 128], BF16)
    wneg = k.sb("a_wneg", [128, 128], BF16)
    triS = k.sb("a_triS", [128, 128], BF16)
    onesb = k.sb("a_onesb", [128, 128], BF16)
    k.op("pool", lambda e: e.memset(zer[:], 0.0), writes=[zer])
    k.op("pool", lambda e: e.tensor_copy(out=onesb[:], in_=ones[:]), reads=[ones], writes=[onesb])
    k.op("pool", lambda e: e.affine_select(out=cneg[:], in_=zer[:], pattern=[[1, 128]], compare_op=ALU.is_ge, fill=NEG,
                                           base=0, channel_multiplier=-1), reads=[zer], writes=[cneg])
    k.op("pool", lambda e: e.affine_select(out=sneg[:], in_=zer[:], pattern=[[1, 128]], compare_op=ALU.is_gt, fill=NEG,
                                           base=0, channel_multiplier=-1), reads=[zer], writes=[sneg])
    k.op("pool", lambda e: e.affine_select(out=wneg[:], in_=zer[:], pattern=[[-1, 128]], compare_op=ALU.is_gt, fill=NEG,
                                           base=0, channel_multiplier=1), reads=[zer], writes=[wneg])
    k.op("pool", lambda e: e.affine_select(out=triS[:], in_=ones[:], pattern=[[-1, 128]], compare_op=ALU.is_gt, fill=0.0,
                                           base=0, channel_multiplier=1), reads=[ones], writes=[triS])
    b4 = lambda ap: ap.unsqueeze(1).to_broadcast([ap.shape[0], 4, ap.shape[1]])
    zb = k.sb("a_zb", [128, 512], BF16)
    k.op("pool", lambda e: e.memset(zb[:], 0.0), writes=[zb])

    def zinit(bank, ncols):
        mm(k, bank, bank[:, 0:ncols], zb, zb[:, 0:128], zb, zb[:, 0:ncols], True, False)

    PT = [k.sb("a_PT%d" % i, [128, 512], BF16) for i in range(4)]
    oc = k.sb("a_oc", [128, 4, 65], F32)
    os_ = k.sb("a_os", [128, 4, 65], F32)
    ow = k.sb("a_ow", [128, 4, 65], F32)
    rd = k.sb("a_rd", [128, 16], F32)
    dn = k.sb("a_dn", [128, 4, 3], F32)
    psl = k.sb("a_psl", [128, 64], F32)
    sc = k.sb("a_sc", [128, 64], F32)
    sc2 = k.sb("a_sc2", [128, 64], F32)
    v8 = k.sb("a_v8", [128, 16], F32)
    nsel = k.sb("a_nsel", [128, 64], BF16)
    nselT = k.sb("a_nselT", [64, 128], BF16)
    m1t = [k.sb("a_m1t%d" % i, [128, 64], F32) for i in range(2)]
    a1t = [k.sb("a_a1t%d" % i, [128, 64], F32) for i in range(2)]
    mg = k.sb("a_mg", [128, 4, 64], F32)
    mg2 = k.sb("a_mg2", [128, 4, 64], F32)
    otile = [k.sb("a_ot%d" % i, [128, 512], BF16) for i in range(2)]
    Esb2 = [k.sb("a_Esb%d" % i, [128, 512], F32) for i in range(2)]
    SP2 = [k.sb("a_SP%d" % i, [128, 512], F32) for i in range(2)]
    SPb2 = [k.sb("a_SPb%d" % i, [128, 512], BF16) for i in range(2)]
    T12 = [k.sb("a_T1%d" % i, [128, 512], F32) for i in range(2)]
    Wb = [k.sb("a_W%d" % i, [128, 512], BF16) for i in range(2)]
    npt = [0]

    def exp_pt(ST):
        p = PT[npt[0] % 4]
        npt[0] += 1
        k.op("act", lambda e: e.activation(out=p[:], in_=ST[:], func=AF.Exp), reads=[ST], writes=[p])
        return p

    toks = toks_dbg
    for t in range(n_qt):
        qs = slice(t * 128, (t + 1) * 128)
        ot = otile[t % 2]
        ncmp = 1 if t < 16 else 2
        k.dma("sp", m1t[t % 2][:], d["m1"][t], reads=[d["m1"]], writes=[m1t[t % 2]])
        k.dma("sp", a1t[t % 2][:], d["a1"][t], reads=[d["a1"]], writes=[a1t[t % 2]])
        pts = []
        for c in range(ncmp):
            ST = psum[c]
            full = 16 * (128 * c + 127) + 31 <= 128 * t
            mm(k, ST, ST[:], kcmpT, kcmpT[0:68, c * 128:(c + 1) * 128], nqT, nqT[0:68, :, qs], True, full)
            if not full:
                mm(k, ST, ST[:], idb, idb[:], cmask, cmask[:, c:c + 1, qs].to_broadcast([128, 4, 128]), False, True)
            pts.append(exp_pt(ST))
        outA, outB = psum[4], psum[5]
        zinit(outA, 260)
        zinit(outB, 256)
        first = False
        for h in range(4):
            for c in range(ncmp):
                p = pts[c]
                k.op("pe", lambda e, h=h, c=c, p=p, first=first: e.matmul(
                    outA[:, h * 65:(h + 1) * 65], lhsT=p[:, h * 128:(h + 1) * 128], rhs=vcmpA[:, c, 0:65],
                    start=first, stop=(h == 3 and c == ncmp - 1), skip_group_check=True), reads=[p, vcmpA], writes=[outA])
                first = False
        first = False
        for h in range(4):
            for c in range(ncmp):
                p = pts[c]
                k.op("pe", lambda e, h=h, c=c, p=p, first=first: e.matmul(
                    outB[:, h * 64:(h + 1) * 64], lhsT=p[:, h * 128:(h + 1) * 128], rhs=vcmpA[:, c, 65:129],
                    start=first, stop=(h == 3 and c == ncmp - 1), skip_group_check=True), reads=[p, vcmpA], writes=[outB])
                first = False
        k.op("act", lambda e: e.activation(out=oc[:], in_=outA[:, 0:260].rearrange("p (a b) -> p a b", a=4), func=AF.Copy),
             reads=[outA], writes=[oc])
        k.op("dve", lambda e: e.tensor_scalar(out=rd[:, 0:4], in0=oc[:, :, 64], scalar1=1e-30, scalar2=None, op0=ALU.max),
             reads=[oc], writes=[rd])
        k.op("dve", lambda e: e.reciprocal(out=rd[:, 4:8], in_=rd[:, 0:4]), reads=[rd], writes=[rd])
        k.op("dve", lambda e: e.tensor_scalar(out=psl[:], in0=outB[:, 0:64], scalar1=rd[:, 4:5], scalar2=None, op0=ALU.mult),
             reads=[outB, rd], writes=[psl])
        for h in range(1, 4):
            k.op("dve", lambda e, h=h: e.scalar_tensor_tensor(out=psl[:], in0=outB[:, h * 64:(h + 1) * 64], scalar=rd[:, 4 + h:5 + h],
                                                              in1=psl[:], op0=ALU.mult, op1=ALU.add), reads=[outB, rd, psl], writes=[psl])
        k.op("dve", lambda e, t=t: e.tensor_tensor(out=sc[:], in0=psl[:], in1=m1t[t % 2][:], op=ALU.mult),
             reads=[psl, m1t[t % 2]], writes=[sc])
        k.op("dve", lambda e, t=t: e.tensor_tensor(out=sc[:], in0=sc[:], in1=a1t[t % 2][:], op=ALU.add),
             reads=[sc, a1t[t % 2]], writes=[sc])
        k.op("dve", lambda e: e.max(out=v8[:, 0:8], in_=sc[:]), reads=[sc], writes=[v8])
        k.op("dve", lambda e: e.match_replace(out=sc2[:], in_to_replace=v8[:, 0:8], in_values=sc[:], imm_value=-3e6),
             reads=[sc, v8], writes=[sc2])
        k.op("dve", lambda e: e.max(out=v8[:, 8:16], in_=sc2[:]), reads=[sc2], writes=[v8])
        k.op("dve", lambda e: e.tensor_scalar(out=sc2[:], in0=sc[:], scalar1=v8[:, 15:16], scalar2=1.0, op0=ALU.is_ge,
                                              op1=ALU.subtract), reads=[sc, v8], writes=[sc2])
        k.op("dve", lambda e: e.tensor_scalar(out=nsel[:], in0=sc2[:], scalar1=-NEG, scalar2=None, op0=ALU.mult),
             reads=[sc2], writes=[nsel])
        if dbg is not None and "nsel" in dbg:
            toks.append(k.dma("sp", dbg["nsel"][qs, :], nsel[:], reads=[nsel], writes=[dbg["nsel"]]))
        pb, pbv = psum[2], pbf[2]
        tr(k, pb, pbv[0:64, 0:128], nsel, nsel[:], idb, idb[:])
        k.op("act", lambda e, pbv=pbv: e.activation(out=nselT[:], in_=pbv[0:64, 0:128], func=AF.Copy), reads=[pb], writes=[nselT])
        outS, outW = psum[6], psum[7]
        zinit(outS, 260)
        zinit(outW, 260)
        s0 = max(0, t - 4)
        work = [("S", s) for s in range(t + 1)] + [("W", s) for s in range(s0, t + 1)]

        def score_stage(i):
            kind, s = work[i]
            ST = psum[i % 4]
            ks_ = slice(s * 128, (s + 1) * 128)
            if kind == "S":
                mm(k, ST, ST[:], ksT, ksT[0:68, ks_], nqT, nqT[0:68, :, qs], True, False)
                mm(k, ST, ST[:], Et, Et[:, s, :], nselT, b4(nselT[:]), False, s != t)
                if s == t:
                    mm(k, ST, ST[:], idb, idb[:], cneg, b4(cneg[:]), False, True)
            else:
                lo, hi = (s == t - 4), (s == t)
                mm(k, ST, ST[:], kwT, kwT[0:68, ks_], nqT, nqT[0:68, :, qs], True, not (lo or hi))
                if lo:
                    mm(k, ST, ST[:], idb, idb[:], wneg, b4(wneg[:]), False, True)
                if hi:
                    mm(k, ST, ST[:], idb, idb[:], cneg, b4(cneg[:]), False, True)
            return exp_pt(ST)

        def pv_stage(i, p):
            kind, s = work[i]
            outX, vA = (outS, vsA) if kind == "S" else (outW, vwA)
            for h in range(4):
                k.op("pe", lambda e, h=h, s=s, p=p, outX=outX, vA=vA: e.matmul(
                    outX[:, h * 65:(h + 1) * 65], lhsT=p[:, h * 128:(h + 1) * 128], rhs=vA[:, s, :],
                    start=False, stop=(s == t and h == 3), skip_group_check=True), reads=[p, vA], writes=[outX])

        pend = score_stage(0)
        for i in range(len(work)):
            nxt = score_stage(i + 1) if i + 1 < len(work) else None
            pv_stage(i, pend)
            pend = nxt
        k.op("act", lambda e: e.activation(out=os_[:], in_=outS[:, 0:260].rearrange("p (a b) -> p a b", a=4), func=AF.Copy),
             reads=[outS], writes=[os_])
        k.op("act", lambda e: e.activation(out=ow[:], in_=outW[:, 0:260].rearrange("p (a b) -> p a b", a=4), func=AF.Copy),
             reads=[outW], writes=[ow])
        for bi, src in enumerate((oc, os_, ow)):
            k.op("dve", lambda e, bi=bi, src=src: e.tensor_scalar(out=dn[:, :, bi], in0=src[:, :, 64], scalar1=1e-30, scalar2=None,
                                                                  op0=ALU.max), reads=[src], writes=[dn])
        k.op("dve", lambda e: e.reciprocal(out=dn[:], in_=dn[:]), reads=[dn], writes=[dn])
        k.op("dve", lambda e, t=t: e.tensor_tensor(out=dn[:], in0=dn[:], in1=gts[:, t, :].rearrange("p (a b) -> p a b", a=4),
                                                   op=ALU.mult), reads=[dn, gts], writes=[dn])
        bc64 = lambda ap: ap.to_broadcast([128, 4, 64])
        k.op("dve", lambda e: e.tensor_tensor(out=mg[:], in0=oc[:, :, 0:64], in1=bc64(dn[:, :, 0:1]), op=ALU.mult),
             reads=[oc, dn], writes=[mg])
        k.op("pool", lambda e: e.tensor_tensor(out=mg2[:], in0=os_[:, :, 0:64], in1=bc64(dn[:, :, 1:2]), op=ALU.mult),
             reads=[os_, dn], writes=[mg2])
        k.op("dve", lambda e: e.tensor_tensor(out=mg[:], in0=mg[:], in1=mg2[:], op=ALU.add), reads=[mg, mg2], writes=[mg])
        k.op("pool", lambda e: e.tensor_tensor(out=mg2[:], in0=ow[:, :, 0:64], in1=bc64(dn[:, :, 2:3]), op=ALU.mult),
             reads=[ow, dn], writes=[mg2])
        k.op("dve", lambda e, ot=ot: e.tensor_tensor(out=ot[:, 256:512].rearrange("p (a b) -> p a b", a=4), in0=mg[:], in1=mg2[:],
                                                     op=ALU.add), reads=[mg, mg2], writes=[ot])
        hord = [0, 2, 1, 3]
        outSB = psum[6]
        CS = psum[7]
        zinit(outSB, 256)
        if t > 0:
            zinit(CS, 512)
        e3 = lambda ap: ap.rearrange("p (a b) -> p a b", a=2)

        def sb_A(s):
            pi = (t - s) % 2
            X0, X1 = psum[2 * pi], psum[2 * pi + 1]
            Xv = pall[:, 2 * pi * 512:(2 * pi + 2) * 512].rearrange("p (a b) -> p a b", a=2)[:, :, 0:256]
            E_, SP_, SPb_ = Esb2[pi], SP2[pi], SPb2[pi]
            ks_ = slice(s * 128, (s + 1) * 128)
            for hp, X in ((0, X0), (1, X1)):
                if s == t:
                    mm(k, X, X[:, 0:256], idb, idb[:], sneg, sneg[:].unsqueeze(1).to_broadcast([128, 2, 128]), True, False)
                for ci in range(2):
                    mm(k, X, X[:, ci * 128:(ci + 1) * 128], sbkT, sbkT[hp * 64:(hp + 1) * 64, ci, ks_],
                       sbqT, sbqT[hp * 64:(hp + 1) * 64, ci, qs], s != t, True)
            k.op("act", lambda e: e.activation(out=e3(E_[:]), in_=Xv, func=AF.Exp), reads=[X0, X1], writes=[E_])
            k.op("act", lambda e: e.activation(out=SPb_[:], in_=E_[:], func=AF.Ln, bias=1.0, scale=1.0), reads=[E_], writes=[SPb_])
            k.op("act", lambda e: e.activation(out=SP_[:], in_=E_[:], func=AF.Ln, bias=1.0, scale=1.0), reads=[E_], writes=[SP_])
            acc = psum[4 + pi]
            mm(k, acc, acc[:], triS, triS[:], SPb_, SPb_[:], True, True)

        def sb_B(s):
            pi = (t - s) % 2
            X0, X1 = psum[2 * pi], psum[2 * pi + 1]
            Xv = pall[:, 2 * pi * 512:(2 * pi + 2) * 512].rearrange("p (a b) -> p a b", a=2)[:, :, 0:256]
            SP_, SPb_, T1_, W, acc = SP2[pi], SPb2[pi], T12[pi], Wb[pi], psum[4 + pi]
            k.op("dve", lambda e: e.tensor_tensor(out=e3(T1_[:]), in0=Xv, in1=e3(SP_[:]), op=ALU.subtract),
                 reads=[X0, X1, SP_], writes=[T1_])
            if s != t:
                k.op("dve", lambda e: e.tensor_tensor(out=T1_[:], in0=T1_[:], in1=CS[:], op=ALU.subtract),
                     reads=[T1_, CS], writes=[T1_])
            if s > 0:
                k.op("pe", lambda e: e.matmul(CS[:], lhsT=onesb[:], rhs=SPb_[:], start=False, stop=(s == 1),
                                              skip_group_check=True), reads=[onesb, SPb_], writes=[CS])
            k.op("dve", lambda e: e.tensor_tensor(out=T1_[:], in0=T1_[:], in1=acc[:], op=ALU.subtract),
                 reads=[T1_, acc], writes=[T1_])
            k.op("act", lambda e: e.activation(out=W[:], in_=T1_[:], func=AF.Exp), reads=[T1_], writes=[W])
            for h in range(4):
                pos = hord.index(h)
                k.op("pe", lambda e, h=h, pos=pos: e.matmul(
                    outSB[:, h * 64:(h + 1) * 64], lhsT=W[:, pos * 128:(pos + 1) * 128], rhs=sbv[:, s, h * 64:(h + 1) * 64],
                    start=False, stop=(s == 0 and h == 3), skip_group_check=True), reads=[W, sbv], writes=[outSB])

        sb_A(t)
        for s in range(t, -1, -1):
            if s > 0:
                sb_A(s - 1)
            sb_B(s)
        k.op("act", lambda e, ot=ot: e.activation(out=ot[:, 0:256], in_=outSB[:, 0:256], func=AF.Copy), reads=[outSB], writes=[ot])
        toks.append(k.dma("sp", o_d[qs, :], ot[:], reads=[ot], writes=[o_d]))
    k.release(m0)
    return toks


def make_psum_all(k):
    pall = k.stack.enter_context(k.nc.psum_tensor("psall", [128, 4096], F32))
    psum = [Buf(pall[:, i * 512:(i + 1) * 512], "ps%d" % i) for i in range(8)]
    pbf = [p[:].bitcast(BF16) for p in psum]
    return psum, pbf, pall


def build_a(n_qt=32, debug=False):
    nc = bass.Bass("TRN2", target_bir_lowering=False)
    with contextlib.ExitStack() as stack:
        k = KB(nc, stack)
        psum, pbf, pall = make_psum_all(k)
        d = a_dram(k)
        o_d = k.dram("o", [SEQ, 512], BF16, "ExternalOutput")
        ident = make_ident(k, "id", BF16)
        dbg = None
        if debug:
            dbg = {"nsel": k.dram("dbg_nsel", [SEQ, 64], BF16, "ExternalOutput"),
                   "sbqT": k.dram("dbg_sbqT", [128, 2, SEQ], BF16, "ExternalOutput"),
                   "sbkT": k.dram("dbg_sbkT", [128, 2, SEQ], BF16, "ExternalOutput"),
                   "sbv": k.dram("dbg_sbv", [128, 32, 256], BF16, "ExternalOutput"),
                   "nqT": k.dram("dbg_nqT", [68, 4, SEQ], BF16, "ExternalOutput"),
                   "ksT": k.dram("dbg_ksT", [68, SEQ], BF16, "ExternalOutput"),
                   "kwT": k.dram("dbg_kwT", [68, SEQ], BF16, "ExternalOutput"),
                   "kcT": k.dram("dbg_kcT", [64, SEQ], BF16, "ExternalOutput"),
                   "vsA": k.dram("dbg_vsA", [128, 32, 65], BF16, "ExternalOutput"),
                   "gts": k.dram("dbg_gts", [128, 32, 12], F32, "ExternalOutput"),
                   "kcmpT": k.dram("dbg_kcmpT", [68, 256], BF16, "ExternalOutput"),
                   "vcmpA": k.dram("dbg_vcmpA", [128, 2, 129], BF16, "ExternalOutput")}
        toks = emit_a(k, d, psum, pbf, pall, o_d, ident, "", n_qt, dbg)
        k.emit(final_waits=toks)
        print("A arena peak words", k.apeak, "instr", {e: len(v) for e, v in k.prog.items()})
    return nc


CORES = list(range(8))


def _run(nc, in_maps):
    return run_bass_kernel_spmd(nc, in_maps, core_ids=CORES).results


def kernel_unfused(**inputs):
    P = {k_: np.ascontiguousarray(np.asarray(v, dtype=np.float32)) for k_, v in inputs.items()}
    ncA = build_a(32)
    resA = _run(ncA, [a_host_inputs(c // 2, c % 2, P) for c in CORES])
    o_full = []
    for b in range(4):
        o0, o1 = np.asarray(resA[2 * b]["o"]), np.asarray(resA[2 * b + 1]["o"])
        o_full.append(np.concatenate([o0[:, :256], o1[:, :256], o0[:, 256:], o1[:, 256:]], axis=1))
    del resA
    ncB = build_bd(1024)
    shared = bd_host_shared(0, P, "")
    maps = []
    for c in CORES:
        b, hh = c // 2, c % 2
        m = dict(shared)
        m.update(bd_host_inputs(0, b, P, P["attn_w_out"][0], ""))
        m["oin"] = np.ascontiguousarray(o_full[b][hh * 2048:(hh + 1) * 2048])
        m["hres"] = np.ascontiguousarray(P["x"][b, hh * 2048:(hh + 1) * 2048])
        maps.append(m)
    resB = _run(ncB, maps)
    h0 = [np.concatenate([np.asarray(resB[2 * b]["out"]), np.asarray(resB[2 * b + 1]["out"])], axis=0) for b in range(4)]
    del resB, maps, shared
    ncC = build_c(8)
    maps = []
    for c in CORES:
        b, hh = c // 2, c % 2
        m = c_host_inputs(b, hh, P)
        m["c_hin"] = h0[b]
        maps.append(m)
    resC = _run(ncC, maps)
    yn_full = [np.concatenate([np.asarray(resC[2 * b]["yn"]), np.asarray(resC[2 * b + 1]["yn"])], axis=1) for b in range(4)]
    del resC
    ncD = build_bd(2048)
    shared = bd_host_shared(1, P, "")
    maps = []
    for c in CORES:
        b, hh = c // 2, c % 2
        m = dict(shared)
        m.update(bd_host_inputs(1, b, P, P["ssm_w_out"][0], ""))
        m["oin"] = np.ascontiguousarray(yn_full[b][hh * 2048:(hh + 1) * 2048])
        m["hres"] = np.ascontiguousarray(h0[b][hh * 2048:(hh + 1) * 2048])
        maps.append(m)
    resD = _run(ncD, maps)
    out = np.stack([np.concatenate([np.asarray(resD[2 * b]["out"]), np.asarray(resD[2 * b + 1]["out"])], axis=0)
                    for b in range(4)])
    return out.astype(np.float32)


def build_fused():
    nc = bass.Bass("TRN2", target_bir_lowering=False)
    with contextlib.ExitStack() as stack:
        k = KB(nc, stack)
        psum, pbf, pall = make_psum_all(k)
        dA = [a_dram(k, "_0"), a_dram(k, "_1")]
        dB = [bd_dram(k, 1024, "_l0", 32, False), bd_dram(k, 2048, "_l1", 32, False)]
        dC = [c_dram(k, "_0", False), c_dram(k, "_1", False)]
        out_d = k.dram("out", [NTOK, 1024], F32, "ExternalOutput")
        ridx_d = k.dram("ridx", [128, NT], mybir.dt.uint32, "ExternalInput")
        o_scr = [k.dram("o_scr%d" % i, [SEQ, 512], BF16, "Internal") for i in range(2)]
        yn_scr = [k.dram("yn_scr%d" % i, [SEQ, 1024], BF16, "Internal") for i in range(2)]
        h0_scr = k.dram("h0_scr", [SEQ, 1024], F32, "Internal")
        hpre_scr = k.dram("hpre_scr", [NTOK, 1024], F32, "Internal")
        scr = {"u": k.dram("u_scr", [NTOK, 1024], BF16, "Internal"),
               "y": k.dram("y_scr", [32 * CAP2, 1024], BF16, "Internal")}
        ident = make_ident(k, "id", BF16)
        for hh in range(2):
            emit_a(k, dA[hh], psum, pbf, pall, o_scr[hh], ident, "_%d" % hh, 32, None)
            k.new_epoch()
        for th in range(2):
            r0 = th * NTOK
            parts = [(slice(0, 256), o_scr[0], slice(0, 256), r0), (slice(256, 512), o_scr[1], slice(0, 256), r0),
                     (slice(512, 768), o_scr[0], slice(256, 512), r0), (slice(768, 1024), o_scr[1], slice(256, 512), r0)]
            emit_bd_sp2(k, 1024, dB[0], psum, pbf, h0_scr, hpre_scr, ident, scr, "_l0", 32, None, parts, (dA[0]["xin"], r0), r0)
            k.new_epoch()
        for hh in range(2):
            dC[hh]["hin"] = h0_scr
            emit_c(k, dC[hh], psum, pbf, yn_scr[hh], ident, "_%d" % hh, 8, None)
            k.new_epoch()
        parts = [(slice(0, 1024), yn_scr[0], slice(0, 1024), 0), (slice(1024, 2048), yn_scr[1], slice(0, 1024), 0)]
        toks = emit_bd_sp2(k, 2048, dB[1], psum, pbf, out_d, hpre_scr, ident, scr, "_l1", 32, None, parts, (h0_scr, 0), 0, ridx_d)
        k.emit(final_waits=toks)
        print("FUSED arena peak words", k.apeak, "instr", {e: len(v) for e, v in k.prog.items()})
    return nc


def fused_host_inputs(b, P):
    m = {}
    for hh in range(2):
        for kk, v in a_host_inputs(b, hh, P).items():
            m[kk + "_%d" % hh] = v
        for kk, v in c_host_inputs(b, hh, P).items():
            m[kk + "_%d" % hh] = v
    m.update(bd_host_inputs(0, b, P, P["attn_w_out"][0], "_l0"))
    m.update(bd_host_inputs(1, b, P, P["ssm_w_out"][0], "_l1"))
    return m


def kernel(**inputs):
    P = {k_: np.ascontiguousarray(np.asarray(v, dtype=np.float32)) for k_, v in inputs.items()}
    nc = build_fused()
    shared = {}
    shared.update(bd_host_shared(0, P, "_l0"))
    shared.update(bd_host_shared(1, P, "_l1"))
    per_b = []
    for b in range(4):
        m = dict(shared)
        m.update(fused_host_inputs(b, P))
        per_b.append(m)
    maps = []
    for c in CORES:
        m = dict(per_b[c % 4])
        th = c // 4
        m["ridx"] = np.ascontiguousarray((th * NTOK + np.arange(NTOK).reshape(NT, 128).T).astype(np.uint32))
        maps.append(m)
    res = run_bass_kernel_spmd(nc, maps, core_ids=CORES).results
    return np.stack([np.concatenate([np.asarray(res[b]["out"]), np.asarray(res[b + 4]["out"])], axis=0)
                     for b in range(4)]).astype(np.float32)


CAP = 384
NST = CAP // 128


def emit_bd_sparse(k, F, d, psum, pbf, out_d, hpre_d, ident, tag="", n_exp=32, dbg=None, oin_parts=None, hres_src=None,
                   out_row0=0):
    FC = F // 128
    if oin_parts is None:
        oin_parts = [(slice(0, F), d["oin"], slice(0, F), 0)]
    if hres_src is None:
        hres_src = (d["hres"], 0)
    pg, pu, pd, pm = psum[0:2], psum[2:4], psum[4:6], psum[6:8]
    pgb, pmb = pbf[0:2], pbf[6:8]
    idf, idb = ident
    m0 = k.mark()
    utok = [k.sb("utok%d%s" % (t, tag), [128, 1024], BF16) for t in range(NT)]
    gw = k.sb("gw" + tag, [128, NT, 32], F32)
    maskf = k.sb("maskf" + tag, [128, NT, 32], F32)
    maskb = k.sb("maskb" + tag, [128, NT, 32], BF16)
    pos_all = k.sb("pos" + tag, [128, NT, 32], F32)
    m_g2 = k.sb("m_g2" + tag, [128, 1024], F32)
    bgu = k.sb("bgu" + tag, [128, 32, 2, 8], F32)
    bgu1 = k.sb("bgu1" + tag, [128, 32, 8], F32)
    epsb = k.sb("epsb" + tag, [128, 1], F32)
    k.op("pool", lambda e: e.memset(epsb[:], EPS), writes=[epsb])
    k.dma("sp", bgu[:], d["bgu"][:], reads=[d["bgu"]], writes=[bgu])
    k.op("pool", lambda e: e.tensor_scalar(out=bgu1[:], in0=bgu[:, :, 1, :], scalar1=1.0, scalar2=None, op0=ALU.add),
         reads=[bgu], writes=[bgu1])

    m1 = k.mark()
    m_g1 = k.sb("m_g1" + tag, [128, 1024], F32)
    m_sh2 = k.sb("m_sh2" + tag, [128, 1024], F32)
    m_gm = k.sb("m_gm" + tag, [128, 1024], F32)
    gain_bc = k.sb("gainbc" + tag, [128, 1024], F32)
    stage = k.sb("adast" + tag, [128, 8, 1024], F32)
    woutb = k.sb("woutb" + tag, [128, FC, 1024], BF16)
    rwb = k.sb("rwb" + tag, [128, 8, 32], BF16)
    rb_bc = k.sb("rbbc" + tag, [128, 32], F32)
    o_tok = [k.sb("otok%d%s" % (i, tag), [128, F], BF16) for i in range(2)]
    oT = [k.sb("oT%d%s" % (i, tag), [128, FC, 128], BF16) for i in range(2)]
    hres_t = [k.sb("hrt%d%s" % (i, tag), [128, 1024], F32) for i in range(2)]
    tmp = [k.sb("tmp%d%s" % (i, tag), [128, 1024], F32) for i in range(2)]
    uTt = [k.sb("uTt%d%s" % (i, tag), [128, 8, 128], BF16) for i in range(2)]
    st = [k.sb("st%d%s" % (i, tag), [128, 64], F32) for i in range(2)]
    lg = [k.sb("lg%d%s" % (i, tag), [128, 4, 32], F32) for i in range(2)]
    triU = k.sb("triU" + tag, [128, 128], BF16)
    onesb = k.sb("onesb" + tag, [128, 128], BF16)
    onesf = k.sb("onesf" + tag, [128, 128], F32)
    k.op("pool", lambda e: e.memset(onesf[:], 1.0), writes=[onesf])
    k.op("pool", lambda e: e.tensor_copy(out=onesb[:], in_=onesf[:]), reads=[onesf], writes=[onesb])
    k.op("pool", lambda e: e.affine_select(out=triU[:], in_=onesf[:], pattern=[[1, 128]], compare_op=ALU.is_gt, fill=0.0,
                                           base=0, channel_multiplier=-1), reads=[onesf], writes=[triU])

    k.dma("pool", woutb[:], d["wout"][:], reads=[d["wout"]], writes=[woutb])
    k.dma("pool", rwb[:], d["rw"][:], reads=[d["rw"]], writes=[rwb])
    k.dma("sp", rb_bc[:], d["rb"][:].partition_broadcast(128), reads=[d["rb"]], writes=[rb_bc])
    k.dma("sp", gain_bc[:], d["gain"][:].partition_broadcast(128), reads=[d["gain"]], writes=[gain_bc])
    adaln(k, d["ccol"], d["adaw"], d["adab"], 4, [m_g1, m_sh2, m_gm, m_g2], [stage, stage], pm, tag)
    k.op("dve", lambda e: e.scalar_tensor_tensor(out=m_gm[:], in0=m_gm[:], scalar=1.0, in1=gain_bc[:],
                                                 op0=ALU.add, op1=ALU.mult), reads=[m_gm, gain_bc], writes=[m_gm])

    for t in range(NT):
        ot, oTt, hr, tp, ut, s_, l_, uT_ = o_tok[t % 2], oT[t % 2], hres_t[t % 2], tmp[t % 2], utok[t], st[t % 2], lg[t % 2], uTt[t % 2]
        rows = slice(t * 128, (t + 1) * 128)
        for (dcs, sbuf_, scs, r0) in oin_parts:
            k.dma("sp", ot[:, dcs], sbuf_[r0 + t * 128:r0 + (t + 1) * 128, scs], reads=[sbuf_], writes=[ot])
        k.dma("sp", hr[:], hres_src[0][hres_src[1] + t * 128:hres_src[1] + (t + 1) * 128, :], reads=[hres_src[0]], writes=[hr])
        for g in range(FC // 8):
            pb, pbv = pg[g % 2], pgb[g % 2]
            for c in range(8):
                fc = g * 8 + c
                tr(k, pb, pbv[:, c * 128:(c + 1) * 128], ot, ot[:, fc * 128:(fc + 1) * 128], idb, idb[:])
            k.op("act", lambda e, g=g, pbv=pbv, oTt=oTt: e.activation(
                out=oTt[:, g * 8:(g + 1) * 8, :], in_=pbv.rearrange("p (a b) -> p a b", a=8), func=AF.Copy),
                reads=[pb], writes=[oTt])
        for half in range(2):
            hs = slice(half * 512, (half + 1) * 512)
            for fc in range(FC):
                mm(k, pd[half], pd[half][:], oTt, oTt[:, fc, :], woutb, woutb[:, fc, hs], fc == 0, fc == FC - 1)
            k.op("dve", lambda e, half=half, hs=hs, tp=tp: e.tensor_tensor(
                out=tp[:, hs], in0=pd[half][:], in1=m_g1[:, hs], op=ALU.mult), reads=[pd[half], m_g1], writes=[tp])
        k.op("pool", lambda e, hr=hr, tp=tp: e.tensor_tensor(out=hr[:], in0=tp[:], in1=hr[:], op=ALU.add),
             reads=[tp, hr], writes=[hr])
        k.dma("sp", hpre_d[rows, :], hr[:], reads=[hr], writes=[hpre_d])
        k.op("act", lambda e, hr=hr, tp=tp, s_=s_: e.activation(out=tp[:], in_=hr[:], func=AF.Square,
                                                                 accum_out=s_[:, 0:1]), reads=[hr], writes=[tp, s_])
        k.op("act", lambda e, s_=s_: e.activation(out=s_[:, 1:2], in_=s_[:, 0:1], func=AF.Sqrt, bias=epsb[:],
                                                   scale=1.0 / 1024.0), reads=[s_, epsb], writes=[s_])
        k.op("dve", lambda e, s_=s_: e.reciprocal(out=s_[:, 2:3], in_=s_[:, 1:2]), reads=[s_], writes=[s_])
        k.op("dve", lambda e, hr=hr, tp=tp, s_=s_: e.scalar_tensor_tensor(
            out=tp[:], in0=hr[:], scalar=s_[:, 2:3], in1=m_gm[:], op0=ALU.mult, op1=ALU.mult),
            reads=[hr, s_, m_gm], writes=[tp])
        k.op("pool", lambda e, tp=tp, ut=ut: e.tensor_tensor(out=ut[:], in0=tp[:], in1=m_sh2[:], op=ALU.add),
             reads=[tp, m_sh2], writes=[ut])
        for kc in range(8):
            tr(k, pm[0], pmb[0][:, kc * 128:(kc + 1) * 128], ut, ut[:, kc * 128:(kc + 1) * 128], idb, idb[:])
        k.op("act", lambda e, uT_=uT_: e.activation(out=uT_[:], in_=pmb[0].rearrange("p (a b) -> p a b", a=8), func=AF.Copy),
             reads=[pm[0]], writes=[uT_])
        for kc in range(8):
            mm(k, pm[1], pm[1][:, 0:32], uT_, uT_[:, kc, :], rwb, rwb[:, kc, :], kc == 0, kc == 7)
        k.op("dve", lambda e, l_=l_: e.tensor_tensor(out=l_[:, 0, :], in0=pm[1][:, 0:32], in1=rb_bc[:], op=ALU.add),
             reads=[pm[1], rb_bc], writes=[l_])
        k.op("dve", lambda e, l_=l_, s_=s_: e.max(out=s_[:, 8:16], in_=l_[:, 0, :]), reads=[l_], writes=[s_])
        k.op("dve", lambda e, l_=l_, s_=s_, t=t: e.tensor_scalar(out=maskf[:, t, :], in0=l_[:, 0, :], scalar1=s_[:, 11:12],
                                                                 scalar2=None, op0=ALU.is_ge), reads=[l_, s_], writes=[maskf])
        k.op("pool", lambda e, t=t: e.tensor_copy(out=maskb[:, t, :], in_=maskf[:, t, :]), reads=[maskf], writes=[maskb])
        k.op("dve", lambda e, s_=s_: e.tensor_scalar(out=s_[:, 16:17], in0=s_[:, 8:9], scalar1=-1.0, scalar2=None,
                                                     op0=ALU.mult), reads=[s_], writes=[s_])
        k.op("act", lambda e, l_=l_, s_=s_: e.activation(out=l_[:, 2, :], in_=l_[:, 0, :], func=AF.Exp,
                                                          bias=s_[:, 16:17], scale=1.0), reads=[l_, s_], writes=[l_])
        k.op("dve", lambda e, l_=l_, s_=s_, t=t: e.scalar_tensor_tensor(
            out=l_[:, 3, :], in0=l_[:, 2, :], scalar=1.0, in1=maskf[:, t, :], op0=ALU.mult, op1=ALU.mult,
            accum_out=s_[:, 17:18]), reads=[l_, maskf], writes=[l_, s_])
        k.op("dve", lambda e, s_=s_: e.reciprocal(out=s_[:, 18:19], in_=s_[:, 17:18]), reads=[s_], writes=[s_])
        k.op("dve", lambda e, l_=l_, s_=s_, t=t: e.tensor_scalar(out=gw[:, t, :], in0=l_[:, 3, :], scalar1=s_[:, 18:19],
                                                                 scalar2=None, op0=ALU.mult), reads=[l_, s_], writes=[gw])
    for t in range(NT):
        pb = pm[t % 2]
        mm(k, pb, pb[:, 0:32], triU, triU[:], maskb, maskb[:, t, :], True, t == 0)
        for t2_ in range(t):
            mm(k, pb, pb[:, 0:32], onesb, onesb[:], maskb, maskb[:, t2_, :], False, t2_ == t - 1)
        k.op("act", lambda e, t=t, pb=pb: e.activation(out=pos_all[:, t, :], in_=pb[:, 0:32], func=AF.Copy),
             reads=[pb], writes=[pos_all])

    toks = []
    if dbg is not None:
        toks.append(k.dma("sp", dbg["gw"][:], gw[:], reads=[gw], writes=[dbg["gw"]]))
        toks.append(k.dma("sp", dbg["pos"][:], pos_all[:], reads=[pos_all], writes=[dbg["pos"]]))
    k.release(m1)
    acc = [k.sb("acc%d%s" % (t, tag), [128, 1024], F32) for t in range(NT)]
    for t in range(NT):
        k.op("pool", lambda e, t=t: e.memset(acc[t][:], 0.0), writes=[acc[t]])
    m2 = k.mark()
    iota_i = k.sb("iota_i" + tag, [128, CAP], mybir.dt.int32)
    iota_f = k.sb("iota_f" + tag, [128, CAP], F32)
    k.op("pool", lambda e: e.iota(iota_i[:], pattern=[[1, CAP]], base=0, channel_multiplier=0), writes=[iota_i])
    k.op("pool", lambda e: e.tensor_copy(out=iota_f[:], in_=iota_i[:]), reads=[iota_i], writes=[iota_f])
    Pm = [k.sb("Pm%d%s" % (t, tag), [128, CAP], BF16) for t in range(NT)]
    PmW = [k.sb("PmW%d%s" % (i, tag), [128, CAP], BF16) for i in range(2)]
    PmT = [k.sb("PmT%d%s" % (s_, tag), [128, NTOK], BF16) for s_ in range(NST)]
    XgT = k.sb("XgT" + tag, [128, 8, CAP], BF16)
    actT = [k.sb("actT%d%s" % (j, tag), [128, CAP], BF16) for j in range(8)]
    Y = [k.sb("Y%d%s" % (s_, tag), [128, 1024], BF16) for s_ in range(NST)]
    bdb = [k.sb("bdb%d%s" % (i, tag), [128, 1024], F32) for i in range(2)]
    NR = 3
    wring = [k.sb("wgur%d%s" % (i, tag), [128, 8, 2, 128], BF16) for i in range(NR)]
    ND = 2
    dring = [k.sb("wdr%d%s" % (i, tag), [128, 8, 512], BF16) for i in range(ND)]
    g_sb = k.sb("g_sb" + tag, [128, CAP], F32)
    s_sb = k.sb("s_sb" + tag, [128, CAP], F32)
    t1 = k.sb("t1" + tag, [128, CAP], F32)
    t2 = k.sb("t2" + tag, [128, CAP], F32)
    m_sb = k.sb("m_sb" + tag, [128, CAP], F32)

    units = [(e, j) for e in range(n_exp) for j in range(8)]
    dunits = [(e, h) for e in range(n_exp) for h in range(2)]

    def load_unit(i):
        if i < len(units):
            e, j = units[i]
            k.dma("pool", wring[i % NR][:], d["wgu"][e, j], reads=[d["wgu"]], writes=[wring[i % NR]])

    def load_dunit(i):
        if i < len(dunits):
            e, h = dunits[i]
            k.dma("pool", dring[i % ND][:], d["wd"][e, :, :, h * 512:(h + 1) * 512], reads=[d["wd"]],
                  writes=[dring[i % ND]])

    def build_P(e_):
        for t in range(NT):
            k.op("dve", lambda e, t=t, e_=e_: e.tensor_scalar(
                out=Pm[t][:], in0=iota_f[:], scalar1=pos_all[:, t, e_:e_ + 1], scalar2=maskf[:, t, e_:e_ + 1],
                op0=ALU.is_equal, op1=ALU.mult), reads=[iota_f, pos_all, maskf], writes=[Pm[t]])

    def build_PT(e_):
        for t in range(NT):
            pw = PmW[t % 2]
            k.op("pool", lambda e, t=t, e_=e_, pw=pw: e.tensor_scalar(
                out=pw[:], in0=Pm[t][:], scalar1=gw[:, t, e_:e_ + 1], scalar2=None, op0=ALU.mult),
                reads=[Pm[t], gw], writes=[pw])
            pb, pbv = psum[6 + t % 2], pbf[6 + t % 2]
            for s_ in range(NST):
                tr(k, pb, pbv[:, s_ * 128:(s_ + 1) * 128], pw, pw[:, s_ * 128:(s_ + 1) * 128], idb, idb[:])
            for s_ in range(NST):
                k.op("act", lambda e, t=t, s_=s_, pbv=pbv: e.activation(
                    out=PmT[s_][:, t * 128:(t + 1) * 128], in_=pbv[:, s_ * 128:(s_ + 1) * 128], func=AF.Copy),
                    reads=[pb], writes=[PmT[s_]])

    for i in range(NR - 1):
        load_unit(i)
    for i in range(ND - 1):
        load_dunit(i)
    build_P(0)
    cnt = 0
    for e_ in range(n_exp):
        bdt = bdb[e_ % 2]
        k.dma("sp", bdt[:], d["bd"][e_].partition_broadcast(128), reads=[d["bd"]], writes=[bdt])
        for grp in range(2):
            for kk in range(4):
                kc = grp * 4 + kk
                pb = psum[kk]
                for t in range(NT):
                    mm(k, pb, pb[:, 0:CAP], utok[t], utok[t][:, kc * 128:(kc + 1) * 128], Pm[t], Pm[t][:], t == 0, t == NT - 1)
            for kk in range(4):
                kc = grp * 4 + kk
                pb = psum[kk]
                k.op("act", lambda e, kc=kc, pb=pb: e.activation(out=XgT[:, kc, :], in_=pb[:, 0:CAP], func=AF.Copy),
                     reads=[pb], writes=[XgT])
        build_PT(e_)
        for j in range(8):
            ui = e_ * 8 + j
            load_unit(ui + NR - 1)
            w = wring[ui % NR]
            x = cnt % 2
            cnt += 1
            pgx, pux = psum[4 + 2 * x], psum[5 + 2 * x]
            for kc in range(8):
                mm(k, pgx, pgx[:, 0:CAP], w, w[:, kc, 0, :], XgT, XgT[:, kc, :], kc == 0, kc == 7)
            for kc in range(8):
                mm(k, pux, pux[:, 0:CAP], w, w[:, kc, 1, :], XgT, XgT[:, kc, :], kc == 0, kc == 7)
            k.op("dve", lambda e, pgx=pgx, e_=e_, j=j: e.tensor_scalar(
                out=g_sb[:], in0=pgx[:, 0:CAP], scalar1=bgu[:, e_, 0, j:j + 1], scalar2=SWIGLU_LIMIT,
                op0=ALU.add, op1=ALU.min), reads=[pgx, bgu], writes=[g_sb])
            k.op("act", lambda e: e.activation(out=s_sb[:], in_=g_sb[:], func=AF.Sigmoid, scale=SWIGLU_ALPHA),
                 reads=[g_sb], writes=[s_sb])
            k.op("act", lambda e, pux=pux, e_=e_, j=j: e.activation(
                out=t1[:], in_=pux[:, 0:CAP], func=AF.Identity, bias=bgu1[:, e_, j:j + 1], scale=1.0),
                reads=[pux, bgu1], writes=[t1])
            k.op("pool", lambda e: e.tensor_scalar(out=t2[:], in0=t1[:], scalar1=SWIGLU_LIMIT + 1.0,
                                                   scalar2=1.0 - SWIGLU_LIMIT, op0=ALU.min, op1=ALU.max),
                 reads=[t1], writes=[t2])
            k.op("dve", lambda e: e.tensor_tensor(out=m_sb[:], in0=g_sb[:], in1=s_sb[:], op=ALU.mult),
                 reads=[g_sb, s_sb], writes=[m_sb])
            k.op("pool", lambda e, j=j: e.tensor_tensor(out=actT[j][:], in0=m_sb[:], in1=t2[:], op=ALU.mult),
                 reads=[m_sb, t2], writes=[actT[j]])
        for half in range(2):
            di = e_ * 2 + half
            load_dunit(di + ND - 1)
            wdv = dring[di % ND]
            hs = slice(half * 512, (half + 1) * 512)
            for s_ in range(NST):
                pb = psum[s_ % 2]
                for fc in range(8):
                    mm(k, pb, pb[:], actT[fc], actT[fc][:, s_ * 128:(s_ + 1) * 128], wdv, wdv[:, fc, :], fc == 0, fc == 7)
                k.op("dve", lambda e, s_=s_, hs=hs, pb=pb, bdt=bdt: e.tensor_tensor(
                    out=Y[s_][:, hs], in0=pb[:], in1=bdt[:, hs], op=ALU.add), reads=[pb, bdt], writes=[Y[s_]])
        if e_ + 1 < n_exp:
            build_P(e_ + 1)
        for t in range(NT):
            for half in range(2):
                hs = slice(half * 512, (half + 1) * 512)
                pb = psum[2 + (2 * t + half) % 2]
                for s_ in range(NST):
                    mm(k, pb, pb[:], PmT[s_], PmT[s_][:, t * 128:(t + 1) * 128], Y[s_], Y[s_][:, hs], s_ == 0, s_ == NST - 1)
                k.op("dve", lambda e, t=t, hs=hs, pb=pb: e.tensor_tensor(
                    out=acc[t][:, hs], in0=pb[:], in1=acc[t][:, hs], op=ALU.add), reads=[pb, acc[t]], writes=[acc[t]])

    k.release(m2)
    hp = [k.sb("hp%d%s" % (i, tag), [128, 1024], F32) for i in range(2)]
    for t in range(NT):
        rows = slice(t * 128, (t + 1) * 128)
        h_ = hp[t % 2]
        k.dma("sp", h_[:], hpre_d[rows, :], reads=[hpre_d], writes=[h_])
        k.op("dve", lambda e, t=t: e.tensor_tensor(out=acc[t][:], in0=acc[t][:], in1=m_g2[:], op=ALU.mult),
             reads=[acc[t], m_g2], writes=[acc[t]])
        k.op("pool", lambda e, t=t, h_=h_: e.tensor_tensor(out=h_[:], in0=acc[t][:], in1=h_[:], op=ALU.add),
             reads=[acc[t], h_], writes=[h_])
        toks.append(k.dma("sp", out_d[out_row0 + t * 128:out_row0 + (t + 1) * 128, :], h_[:], reads=[h_], writes=[out_d]))
    k.release(m0)
    return toks


def build_bd_sparse(F, n_exp=32, debug=False):
    nc = bass.Bass("TRN2", target_bir_lowering=False)
    with contextlib.ExitStack() as stack:
        k = KB(nc, stack)
        psum, pbf, pall = make_psum_all(k)
        d = bd_dram(k, F, "", n_exp)
        out_d = k.dram("out", [NTOK, 1024], F32, "ExternalOutput")
        hpre_d = k.dram("hpre_scr", [NTOK, 1024], F32, "ExternalOutput" if debug else "Internal")
        ident = make_ident(k, "id", BF16)
        dbg = None
        if debug:
            dbg = {"gw": k.dram("dbg_gw", [128, NT, 32], F32, "ExternalOutput"),
                   "pos": k.dram("dbg_pos", [128, NT, 32], F32, "ExternalOutput")}
        toks = emit_bd_sparse(k, F, d, psum, pbf, out_d, hpre_d, ident, "", n_exp, dbg)
        k.emit(final_waits=toks)
        print("BDS arena peak words", k.apeak, "instr", {e: len(v) for e, v in k.prog.items()})
    return nc


CAP2 = 1024
NST2 = CAP2 // 128
U32 = mybir.dt.uint32
I32 = mybir.dt.int32


def emit_bd_sp2(k, F, d, psum, pbf, out_d, hpre_d, ident, scr, tag="", n_exp=32, dbg=None, oin_parts=None, hres_src=None,
                out_row0=0, ridx_d=None):
    FC = F // 128
    if oin_parts is None:
        oin_parts = [(slice(0, F), d["oin"], slice(0, F), 0)]
    if hres_src is None:
        hres_src = (d["hres"], 0)
    pg, pu, pd, pm = psum[0:2], psum[2:4], psum[4:6], psum[6:8]
    pgb, pmb = pbf[0:2], pbf[6:8]
    idf, idb = ident
    u_scr, y_scr = scr["u"], scr["y"]
    m0 = k.mark()
    gw = k.sb("gw" + tag, [128, NT, 32], F32)
    maskf = k.sb("maskf" + tag, [128, NT, 32], F32)
    maskb = k.sb("maskb" + tag, [128, NT, 32], BF16)
    pos_all = k.sb("pos" + tag, [128, NT, 32], F32)
    lgs = k.sb("lgs" + tag, [128, NT, 32], F32)
    tv = k.sb("tv" + tag, [128, NT, 8], F32)
    rt = k.sb("rt" + tag, [128, NT, 16], F32)
    rt_u = k.sb("rtu" + tag, [128, NT, 4], U32)
    m_g2 = k.sb("m_g2" + tag, [128, 1024], F32)
    bgu = k.sb("bgu" + tag, [128, 32, 2, 8], F32)
    bgu1 = k.sb("bgu1" + tag, [128, 32, 8], F32)
    epsb = k.sb("epsb" + tag, [128, 1], F32)
    k.op("pool", lambda e: e.memset(epsb[:], EPS), writes=[epsb])
    ridx = None
    if ridx_d is not None:
        ridx = k.sb("ridx" + tag, [128, NT], U32)
        k.dma("sp", ridx[:], ridx_d[:], reads=[ridx_d], writes=[ridx])
    k.dma("sp", bgu[:], d["bgu"][:], reads=[d["bgu"]], writes=[bgu])
    k.op("pool", lambda e: e.tensor_scalar(out=bgu1[:], in0=bgu[:, :, 1, :], scalar1=1.0, scalar2=None, op0=ALU.add),
         reads=[bgu], writes=[bgu1])

    m1 = k.mark()
    m_g1 = k.sb("m_g1" + tag, [128, 1024], F32)
    m_sh2 = k.sb("m_sh2" + tag, [128, 1024], F32)
    m_gm = k.sb("m_gm" + tag, [128, 1024], F32)
    gain_bc = k.sb("gainbc" + tag, [128, 1024], F32)
    stage = k.sb("adast" + tag, [128, 8, 1024], F32)
    woutb = k.sb("woutb" + tag, [128, FC, 1024], BF16)
    rwb = k.sb("rwb" + tag, [128, 8, 32], BF16)
    rb_bc = k.sb("rbbc" + tag, [128, 32], F32)
    o_tok = [k.sb("otok%d%s" % (i, tag), [128, F], BF16) for i in range(2)]
    oT = [k.sb("oT%d%s" % (i, tag), [128, FC, 128], BF16) for i in range(2)]
    hres_t = [k.sb("hrt%d%s" % (i, tag), [128, 1024], F32) for i in range(2)]
    tmp = [k.sb("tmp%d%s" % (i, tag), [128, 1024], F32) for i in range(2)]
    utk = [k.sb("utk%d%s" % (i, tag), [128, 1024], BF16) for i in range(2)]
    uTt = [k.sb("uTt%d%s" % (i, tag), [128, 8, 128], BF16) for i in range(2)]
    st = [k.sb("st%d%s" % (i, tag), [128, 64], F32) for i in range(2)]
    lg = [k.sb("lg%d%s" % (i, tag), [128, 4, 32], F32) for i in range(2)]
    triU = k.sb("triU" + tag, [128, 128], BF16)
    onesb = k.sb("onesb" + tag, [128, 128], BF16)
    onesf = k.sb("onesf" + tag, [128, 128], F32)
    ecap_i = k.sb("ecapi" + tag, [128, 32], I32)
    ecap = k.sb("ecap" + tag, [128, 32], F32)
    k.op("pool", lambda e: e.memset(onesf[:], 1.0), writes=[onesf])
    k.op("pool", lambda e: e.tensor_copy(out=onesb[:], in_=onesf[:]), reads=[onesf], writes=[onesb])
    k.op("pool", lambda e: e.affine_select(out=triU[:], in_=onesf[:], pattern=[[1, 128]], compare_op=ALU.is_gt, fill=0.0,
                                           base=0, channel_multiplier=-1), reads=[onesf], writes=[triU])
    k.op("pool", lambda e: e.iota(ecap_i[:], pattern=[[CAP2, 32]], base=0, channel_multiplier=0), writes=[ecap_i])
    k.op("pool", lambda e: e.tensor_copy(out=ecap[:], in_=ecap_i[:]), reads=[ecap_i], writes=[ecap])

    k.dma("pool", woutb[:], d["wout"][:], reads=[d["wout"]], writes=[woutb])
    k.dma("pool", rwb[:], d["rw"][:], reads=[d["rw"]], writes=[rwb])
    k.dma("sp", rb_bc[:], d["rb"][:].partition_broadcast(128), reads=[d["rb"]], writes=[rb_bc])
    k.dma("sp", gain_bc[:], d["gain"][:].partition_broadcast(128), reads=[d["gain"]], writes=[gain_bc])
    adaln(k, d["ccol"], d["adaw"], d["adab"], 4, [m_g1, m_sh2, m_gm, m_g2], [stage, stage], pm, tag)
    k.op("dve", lambda e: e.scalar_tensor_tensor(out=m_gm[:], in0=m_gm[:], scalar=1.0, in1=gain_bc[:],
                                                 op0=ALU.add, op1=ALU.mult), reads=[m_gm, gain_bc], writes=[m_gm])

    for t in range(NT):
        ot, oTt, hr, tp, ut, s_, l_, uT_ = o_tok[t % 2], oT[t % 2], hres_t[t % 2], tmp[t % 2], utk[t % 2], st[t % 2], lg[t % 2], uTt[t % 2]
        rows = slice(t * 128, (t + 1) * 128)
        if ridx is None:
            for (dcs, sbuf_, scs, r0) in oin_parts:
                k.dma("sp", ot[:, dcs], sbuf_[r0 + t * 128:r0 + (t + 1) * 128, scs], reads=[sbuf_], writes=[ot])
            k.dma("sp", hr[:], hres_src[0][hres_src[1] + t * 128:hres_src[1] + (t + 1) * 128, :], reads=[hres_src[0]], writes=[hr])
        else:
            for (dcs, sbuf_, scs, r0) in oin_parts:
                k.cc("pool", lambda e, ot=ot, dcs=dcs, sbuf_=sbuf_, t=t: e.indirect_dma_start(
                    out=ot[:, dcs], out_offset=None, in_=sbuf_[:, :],
                    in_offset=bass.IndirectOffsetOnAxis(ap=ridx[:, t:t + 1], axis=0)), reads=[sbuf_, ridx], writes=[ot])
            k.cc("pool", lambda e, hr=hr, t=t: e.indirect_dma_start(
                out=hr[:], out_offset=None, in_=hres_src[0][:, :],
                in_offset=bass.IndirectOffsetOnAxis(ap=ridx[:, t:t + 1], axis=0)), reads=[hres_src[0], ridx], writes=[hr])
        for g in range(FC // 8):
            pb, pbv = pg[g % 2], pgb[g % 2]
            for c in range(8):
                fc = g * 8 + c
                tr(k, pb, pbv[:, c * 128:(c + 1) * 128], ot, ot[:, fc * 128:(fc + 1) * 128], idb, idb[:])
            k.op("act", lambda e, g=g, pbv=pbv, oTt=oTt: e.activation(
                out=oTt[:, g * 8:(g + 1) * 8, :], in_=pbv.rearrange("p (a b) -> p a b", a=8), func=AF.Copy),
                reads=[pb], writes=[oTt])
        for half in range(2):
            hs = slice(half * 512, (half + 1) * 512)
            for fc in range(FC):
                mm(k, pd[half], pd[half][:], oTt, oTt[:, fc, :], woutb, woutb[:, fc, hs], fc == 0, fc == FC - 1)
            k.op("dve", lambda e, half=half, hs=hs, tp=tp: e.tensor_tensor(
                out=tp[:, hs], in0=pd[half][:], in1=m_g1[:, hs], op=ALU.mult), reads=[pd[half], m_g1], writes=[tp])
        k.op("pool", lambda e, hr=hr, tp=tp: e.tensor_tensor(out=hr[:], in0=tp[:], in1=hr[:], op=ALU.add),
             reads=[tp, hr], writes=[hr])
        k.dma("sp", hpre_d[rows, :], hr[:], reads=[hr], writes=[hpre_d])
        k.op("act", lambda e, hr=hr, tp=tp, s_=s_: e.activation(out=tp[:], in_=hr[:], func=AF.Square,
                                                                 accum_out=s_[:, 0:1]), reads=[hr], writes=[tp, s_])
        k.op("act", lambda e, s_=s_: e.activation(out=s_[:, 1:2], in_=s_[:, 0:1], func=AF.Sqrt, bias=epsb[:],
                                                   scale=1.0 / 1024.0), reads=[s_, epsb], writes=[s_])
        k.op("dve", lambda e, s_=s_: e.reciprocal(out=s_[:, 2:3], in_=s_[:, 1:2]), reads=[s_], writes=[s_])
        k.op("dve", lambda e, hr=hr, tp=tp, s_=s_: e.scalar_tensor_tensor(
            out=tp[:], in0=hr[:], scalar=s_[:, 2:3], in1=m_gm[:], op0=ALU.mult, op1=ALU.mult),
            reads=[hr, s_, m_gm], writes=[tp])
        k.op("pool", lambda e, tp=tp, ut=ut: e.tensor_tensor(out=ut[:], in0=tp[:], in1=m_sh2[:], op=ALU.add),
             reads=[tp, m_sh2], writes=[ut])
        k.dma("sp", u_scr[rows, :], ut[:], reads=[ut], writes=[u_scr])
        for kc in range(8):
            tr(k, pm[0], pmb[0][:, kc * 128:(kc + 1) * 128], ut, ut[:, kc * 128:(kc + 1) * 128], idb, idb[:])
        k.op("act", lambda e, uT_=uT_: e.activation(out=uT_[:], in_=pmb[0].rearrange("p (a b) -> p a b", a=8), func=AF.Copy),
             reads=[pm[0]], writes=[uT_])
        for kc in range(8):
            mm(k, pm[1], pm[1][:, 0:32], uT_, uT_[:, kc, :], rwb, rwb[:, kc, :], kc == 0, kc == 7)
        k.op("dve", lambda e, t=t: e.tensor_tensor(out=lgs[:, t, :], in0=pm[1][:, 0:32], in1=rb_bc[:], op=ALU.add),
             reads=[pm[1], rb_bc], writes=[lgs])
        k.op("dve", lambda e, t=t: e.max(out=tv[:, t, :], in_=lgs[:, t, :]), reads=[lgs], writes=[tv])
        k.op("dve", lambda e, t=t: e.tensor_scalar(out=maskf[:, t, :], in0=lgs[:, t, :], scalar1=tv[:, t, 3:4],
                                                   scalar2=None, op0=ALU.is_ge), reads=[lgs, tv], writes=[maskf])
        k.op("pool", lambda e, t=t: e.tensor_copy(out=maskb[:, t, :], in_=maskf[:, t, :]), reads=[maskf], writes=[maskb])
        k.op("dve", lambda e, s_=s_, t=t: e.tensor_scalar(out=s_[:, 16:17], in0=tv[:, t, 0:1], scalar1=-1.0, scalar2=None,
                                                          op0=ALU.mult), reads=[tv], writes=[s_])
        k.op("act", lambda e, l_=l_, s_=s_, t=t: e.activation(out=l_[:, 2, :], in_=lgs[:, t, :], func=AF.Exp,
                                                               bias=s_[:, 16:17], scale=1.0), reads=[lgs, s_], writes=[l_])
        k.op("dve", lambda e, l_=l_, s_=s_, t=t: e.scalar_tensor_tensor(
            out=l_[:, 3, :], in0=l_[:, 2, :], scalar=1.0, in1=maskf[:, t, :], op0=ALU.mult, op1=ALU.mult,
            accum_out=s_[:, 17:18]), reads=[l_, maskf], writes=[l_, s_])
        k.op("dve", lambda e, s_=s_: e.reciprocal(out=s_[:, 18:19], in_=s_[:, 17:18]), reads=[s_], writes=[s_])
        k.op("dve", lambda e, l_=l_, s_=s_, t=t: e.tensor_scalar(out=gw[:, t, :], in0=l_[:, 3, :], scalar1=s_[:, 18:19],
                                                                 scalar2=None, op0=ALU.mult), reads=[l_, s_], writes=[gw])
    for t in range(NT):
        pb = pm[t % 2]
        mm(k, pb, pb[:, 0:32], triU, triU[:], maskb, maskb[:, t, :], True, t == 0)
        for t2_ in range(t):
            mm(k, pb, pb[:, 0:32], onesb, onesb[:], maskb, maskb[:, t2_, :], False, t2_ == t - 1)
        k.op("act", lambda e, t=t, pb=pb: e.activation(out=pos_all[:, t, :], in_=pb[:, 0:32], func=AF.Copy),
             reads=[pb], writes=[pos_all])
    ohs = lg[0]
    for t in range(NT):
        for kk in range(4):
            k.op("dve", lambda e, t=t, kk=kk: e.tensor_scalar(out=ohs[:, 0, :], in0=lgs[:, t, :], scalar1=tv[:, t, kk:kk + 1],
                                                              scalar2=None, op0=ALU.is_equal), reads=[lgs, tv], writes=[ohs])
            for col, src in ((0, pos_all[:, t, :]), (4, ecap[:]), (8, gw[:, t, :])):
                k.op("dve", lambda e, t=t, kk=kk, col=col, src=src: e.scalar_tensor_tensor(
                    out=ohs[:, 1, :], in0=ohs[:, 0, :], scalar=1.0, in1=src, op0=ALU.mult, op1=ALU.mult,
                    accum_out=rt[:, t, col + kk:col + kk + 1]), reads=[ohs, pos_all, ecap, gw], writes=[ohs, rt])
    k.op("dve", lambda e: e.tensor_scalar(out=rt[:, :, 12:16], in0=rt[:, :, 0:4], scalar1=float(CAP2) - 0.5, scalar2=None,
                                          op0=ALU.is_lt), reads=[rt], writes=[rt])
    k.op("dve", lambda e: e.tensor_tensor(out=rt[:, :, 8:12], in0=rt[:, :, 8:12], in1=rt[:, :, 12:16], op=ALU.mult),
         reads=[rt], writes=[rt])
    k.op("dve", lambda e: e.tensor_scalar(out=rt[:, :, 12:16], in0=rt[:, :, 0:4], scalar1=float(CAP2 - 1), scalar2=None,
                                          op0=ALU.min), reads=[rt], writes=[rt])
    k.op("dve", lambda e: e.tensor_tensor(out=rt[:, :, 12:16], in0=rt[:, :, 12:16], in1=rt[:, :, 4:8], op=ALU.add),
         reads=[rt], writes=[rt])
    k.op("dve", lambda e: e.tensor_copy(out=rt_u[:], in_=rt[:, :, 12:16]), reads=[rt], writes=[rt_u])

    toks = []
    if dbg is not None:
        toks.append(k.dma("sp", dbg["gw"][:], gw[:], reads=[gw], writes=[dbg["gw"]]))
        toks.append(k.dma("sp", dbg["pos"][:], pos_all[:], reads=[pos_all], writes=[dbg["pos"]]))
        toks.append(k.dma("sp", dbg["rt"][:], rt[:], reads=[rt], writes=[dbg["rt"]]))
    k.release(m1)
    m2 = k.mark()
    iota_i = k.sb("iota_i" + tag, [128, CAP2], I32)
    iota_f = k.sb("iota_f" + tag, [128, CAP2], F32)
    tokc_i = k.sb("tokci" + tag, [128, NT, 2], I32)
    tokc = k.sb("tokc" + tag, [128, NT, 2], BF16)
    k.op("pool", lambda e: e.iota(iota_i[:], pattern=[[1, CAP2]], base=0, channel_multiplier=0), writes=[iota_i])
    k.op("pool", lambda e: e.tensor_copy(out=iota_f[:], in_=iota_i[:]), reads=[iota_i], writes=[iota_f])
    k.op("pool", lambda e: e.iota(tokc_i[:, :, 0], pattern=[[0, NT]], base=0, channel_multiplier=1), writes=[tokc_i])
    k.op("pool", lambda e: e.iota(tokc_i[:, :, 1], pattern=[[1, NT]], base=0, channel_multiplier=0), writes=[tokc_i])
    k.op("pool", lambda e: e.tensor_copy(out=tokc[:], in_=tokc_i[:]), reads=[tokc_i], writes=[tokc])
    Pm = [k.sb("Pm%d%s" % (t, tag), [128, CAP2], BF16) for t in range(NT)]
    sidx_f = k.sb("sidxf" + tag, [128, NST2, 2], F32)
    sidx = [k.sb("sidx%d%s" % (i, tag), [128, NST2], U32) for i in range(2)]
    sidx_t = k.sb("sidxt" + tag, [128, NST2], F32)
    Xg = [k.sb("Xg%d%s" % (i, tag), [128, 1024], BF16) for i in range(2)]
    XgT = k.sb("XgT" + tag, [128, 8, CAP2], BF16)
    actT = [k.sb("actT%d%s" % (j, tag), [128, CAP2], BF16) for j in range(8)]
    Ysb = k.sb("Ysb" + tag, [128, NST2, 1024], BF16)
    bdb = [k.sb("bdb%d%s" % (i, tag), [128, 1024], F32) for i in range(2)]
    NR = 3
    wring = [k.sb("wgur%d%s" % (i, tag), [128, 8, 2, 128], BF16) for i in range(NR)]
    ND = 2
    dring = [k.sb("wdr%d%s" % (i, tag), [128, 8, 512], BF16) for i in range(ND)]
    g_sb = k.sb("g_sb" + tag, [128, 512], F32)
    s_sb = k.sb("s_sb" + tag, [128, 512], F32)
    t1 = k.sb("t1" + tag, [128, 512], F32)
    t2 = k.sb("t2" + tag, [128, 512], F32)
    m_sb = k.sb("m_sb" + tag, [128, 512], F32)

    units = [(e, j) for e in range(n_exp) for j in range(8)]
    dunits = [(e, h) for e in range(n_exp) for h in range(2)]

    def load_unit(i):
        if i < len(units):
            e, j = units[i]
            k.dma("pool", wring[i % NR][:], d["wgu"][e, j], reads=[d["wgu"]], writes=[wring[i % NR]])

    def load_dunit(i):
        if i < len(dunits):
            e, h = dunits[i]
            k.dma("pool", dring[i % ND][:], d["wd"][e, :, :, h * 512:(h + 1) * 512], reads=[d["wd"]],
                  writes=[dring[i % ND]])

    def build_P(e_):
        for t in range(NT):
            k.op("dve", lambda e, t=t, e_=e_: e.tensor_scalar(
                out=Pm[t][:], in0=iota_f[:], scalar1=pos_all[:, t, e_:e_ + 1], scalar2=maskf[:, t, e_:e_ + 1],
                op0=ALU.is_equal, op1=ALU.mult), reads=[iota_f, pos_all, maskf], writes=[Pm[t]])

    for i in range(NR - 1):
        load_unit(i)
    for i in range(ND - 1):
        load_dunit(i)
    build_P(0)
    cnt = 0
    for e_ in range(n_exp):
        bdt = bdb[e_ % 2]
        si = sidx[e_ % 2]
        k.dma("sp", bdt[:], d["bd"][e_].partition_broadcast(128), reads=[d["bd"]], writes=[bdt])
        pb = psum[7]
        for s_ in range(NST2):
            for t in range(NT):
                mm(k, pb, pb[:, 2 * s_:2 * s_ + 2], Pm[t], Pm[t][:, s_ * 128:(s_ + 1) * 128], tokc, tokc[:, t, :], t == 0, t == NT - 1)
        k.op("act", lambda e, pb=pb: e.activation(out=sidx_f[:], in_=pb[:, 0:2 * NST2].rearrange("p (a b) -> p a b", a=NST2),
                                                  func=AF.Copy), reads=[pb], writes=[sidx_f])
        k.op("dve", lambda e: e.scalar_tensor_tensor(out=sidx_t[:], in0=sidx_f[:, :, 1], scalar=128.0, in1=sidx_f[:, :, 0],
                                                     op0=ALU.mult, op1=ALU.add), reads=[sidx_f], writes=[sidx_t])
        k.op("dve", lambda e, si=si: e.tensor_copy(out=si[:], in_=sidx_t[:]), reads=[sidx_t], writes=[si])
        for s_ in range(NST2):
            xg = Xg[s_ % 2]
            k.cc("pool", lambda e, xg=xg, si=si, s_=s_: e.indirect_dma_start(
                out=xg[:], out_offset=None, in_=u_scr[:, :],
                in_offset=bass.IndirectOffsetOnAxis(ap=si[:, s_:s_ + 1], axis=0)), reads=[u_scr, si], writes=[xg])
            pbt, pbv = psum[6], pbf[6]
            for kc in range(8):
                tr(k, pbt, pbv[:, kc * 128:(kc + 1) * 128], xg, xg[:, kc * 128:(kc + 1) * 128], idb, idb[:])
            k.op("act", lambda e, s_=s_, pbv=pbv: e.activation(
                out=XgT[:, :, s_ * 128:(s_ + 1) * 128], in_=pbv.rearrange("p (a b) -> p a b", a=8), func=AF.Copy),
                reads=[pbt], writes=[XgT])
        if e_ + 1 < n_exp:
            build_P(e_ + 1)
        for j in range(8):
            ui = e_ * 8 + j
            load_unit(ui + NR - 1)
            w = wring[ui % NR]
            for T in range(CAP2 // 512):
                x = cnt % 2
                cnt += 1
                ts_ = slice(T * 512, (T + 1) * 512)
                for kc in range(8):
                    mm(k, pg[x], pg[x][:], w, w[:, kc, 0, :], XgT, XgT[:, kc, ts_], kc == 0, kc == 7)
                for kc in range(8):
                    mm(k, pu[x], pu[x][:], w, w[:, kc, 1, :], XgT, XgT[:, kc, ts_], kc == 0, kc == 7)
                k.op("dve", lambda e, x=x, e_=e_, j=j: e.tensor_scalar(
                    out=g_sb[:], in0=pg[x][:], scalar1=bgu[:, e_, 0, j:j + 1], scalar2=SWIGLU_LIMIT,
                    op0=ALU.add, op1=ALU.min), reads=[pg[x], bgu], writes=[g_sb])
                k.op("act", lambda e: e.activation(out=s_sb[:], in_=g_sb[:], func=AF.Sigmoid, scale=SWIGLU_ALPHA),
                     reads=[g_sb], writes=[s_sb])
                k.op("act", lambda e, x=x, e_=e_, j=j: e.activation(
                    out=t1[:], in_=pu[x][:], func=AF.Identity, bias=bgu1[:, e_, j:j + 1], scale=1.0),
                    reads=[pu[x], bgu1], writes=[t1])
                k.op("pool", lambda e: e.tensor_scalar(out=t2[:], in0=t1[:], scalar1=SWIGLU_LIMIT + 1.0,
                                                       scalar2=1.0 - SWIGLU_LIMIT, op0=ALU.min, op1=ALU.max),
                     reads=[t1], writes=[t2])
                k.op("dve", lambda e: e.tensor_tensor(out=m_sb[:], in0=g_sb[:], in1=s_sb[:], op=ALU.mult),
                     reads=[g_sb, s_sb], writes=[m_sb])
                k.op("pool", lambda e, j=j, ts_=ts_: e.tensor_tensor(out=actT[j][:, ts_], in0=m_sb[:], in1=t2[:], op=ALU.mult),
                     reads=[m_sb, t2], writes=[actT[j]])
        for half in range(2):
            di = e_ * 2 + half
            load_dunit(di + ND - 1)
            wdv = dring[di % ND]
            hs = slice(half * 512, (half + 1) * 512)
            for s_ in range(NST2):
                pb = pd[s_ % 2]
                for fc in range(8):
                    mm(k, pb, pb[:], actT[fc], actT[fc][:, s_ * 128:(s_ + 1) * 128], wdv, wdv[:, fc, :], fc == 0, fc == 7)
                k.op("dve", lambda e, s_=s_, hs=hs, pb=pb, bdt=bdt: e.tensor_tensor(
                    out=Ysb[:, s_, hs], in0=pb[:], in1=bdt[:, hs], op=ALU.add), reads=[pb, bdt], writes=[Ysb])
        k.dma("sp", y_scr[e_ * CAP2:(e_ + 1) * CAP2, :].rearrange("(a p) c -> p a c", p=128), Ysb[:], reads=[Ysb], writes=[y_scr])

    k.release(m2)
    hp = [k.sb("hp%d%s" % (i, tag), [128, 1024], F32) for i in range(2)]
    yk = [k.sb("yk%d%s" % (i, tag), [128, 1024], BF16) for i in range(4)]
    ac = [k.sb("ac%d%s" % (i, tag), [128, 1024], F32) for i in range(2)]
    for t in range(NT):
        rows = slice(t * 128, (t + 1) * 128)
        h_, a_ = hp[t % 2], ac[t % 2]
        k.dma("sp", h_[:], hpre_d[rows, :], reads=[hpre_d], writes=[h_])
        for kk in range(4):
            k.cc("pool", lambda e, kk=kk, t=t: e.indirect_dma_start(
                out=yk[kk][:], out_offset=None, in_=y_scr[:, :],
                in_offset=bass.IndirectOffsetOnAxis(ap=rt_u[:, t, kk:kk + 1], axis=0)), reads=[y_scr, rt_u], writes=[yk[kk]])
        k.op("dve", lambda e, t=t, a_=a_: e.tensor_scalar(out=a_[:], in0=yk[0][:], scalar1=rt[:, t, 8:9], scalar2=None,
                                                          op0=ALU.mult), reads=[yk[0], rt], writes=[a_])
        for kk in range(1, 4):
            k.op("dve", lambda e, t=t, kk=kk, a_=a_: e.scalar_tensor_tensor(
                out=a_[:], in0=yk[kk][:], scalar=rt[:, t, 8 + kk:9 + kk], in1=a_[:], op0=ALU.mult, op1=ALU.add),
                reads=[yk[kk], rt, a_], writes=[a_])
        k.op("pool", lambda e, a_=a_: e.tensor_tensor(out=a_[:], in0=a_[:], in1=m_g2[:], op=ALU.mult),
             reads=[a_, m_g2], writes=[a_])
        k.op("pool", lambda e, a_=a_, h_=h_: e.tensor_tensor(out=h_[:], in0=a_[:], in1=h_[:], op=ALU.add),
             reads=[a_, h_], writes=[h_])
        toks.append(k.dma("sp", out_d[out_row0 + t * 128:out_row0 + (t + 1) * 128, :], h_[:], reads=[h_], writes=[out_d]))
    k.release(m0)
    return toks


def build_bd_sp2(F, n_exp=32, debug=False):
    nc = bass.Bass("TRN2", target_bir_lowering=False)
    with contextlib.ExitStack() as stack:
        k = KB(nc, stack)
        psum, pbf, pall = make_psum_all(k)
        d = bd_dram(k, F, "", n_exp)
        out_d = k.dram("out", [NTOK, 1024], F32, "ExternalOutput")
        hpre_d = k.dram("hpre_scr", [NTOK, 1024], F32, "ExternalOutput" if debug else "Internal")
        scr = {"u": k.dram("u_scr", [NTOK, 1024], BF16, "Internal"),
               "y": k.dram("y_scr", [32 * CAP2, 1024], BF16, "ExternalOutput" if debug else "Internal")}
        ident = make_ident(k, "id", BF16)
        dbg = None
        if debug:
            dbg = {"gw": k.dram("dbg_gw", [128, NT, 32], F32, "ExternalOutput"),
                   "pos": k.dram("dbg_pos", [128, NT, 32], F32, "ExternalOutput"),
                   "rt": k.dram("dbg_rt", [128, NT, 16], F32, "ExternalOutput")}
        toks = emit_bd_sp2(k, F, d, psum, pbf, out_d, hpre_d, ident, scr, "", n_exp, dbg)
        k.emit(final_waits=toks)
        print("BDS2 arena peak words", k.apeak, "instr", {e: len(v) for e, v in k.prog.items()})
    return nc
```
